# Optimizing a Trainium2 kernel written in Bass

```python
import math
import jax
import jax.numpy as jnp
from jax import lax
import numpy as np

D_MODEL = 2048
BATCH = 4
SEQ = 2048
DEPTH = 2

GRID_W = 64
CTX_LEN = 256
EPS = 1e-6
ROPE_THETA = 10000.0
HEAD_DIM = 128

SSD_HEADS = 32
SSD_HEAD_DIM = 64
SSD_INNER = SSD_HEADS * SSD_HEAD_DIM
SSD_GROUPS = 4
SSD_STATE = 128
SSD_GN = SSD_GROUPS * SSD_STATE
SSD_XBC = SSD_INNER + 2 * SSD_GN
SSD_CONV = 5
SSD_CHUNK = 128
SSD_PROJ = SSD_INNER + SSD_XBC + 2 * SSD_HEADS

NA_HEADS = 16
NA_HEAD_DIM = HEAD_DIM
NA_WIDTH = NA_HEADS * NA_HEAD_DIM
NA_WIN_ROWS = 8
NA_WIN_COLS = 16

EVEN_IN = SSD_PROJ + 3 * NA_WIDTH
EVEN_MIX = SSD_INNER + NA_WIDTH

GQA_HEADS = 16
GQA_KV_HEADS = 4
GQA_GROUP = GQA_HEADS // GQA_KV_HEADS
GQA_Q = GQA_HEADS * HEAD_DIM
GQA_KV = GQA_KV_HEADS * HEAD_DIM
DIFF_HEADS = 8
DIFF_QK = DIFF_HEADS * 2 * HEAD_DIM
DIFF_V_DIM = 2 * HEAD_DIM
DIFF_V = DIFF_HEADS * DIFF_V_DIM

ODD_Q = GQA_Q + DIFF_QK
ODD_IN = ODD_Q + 2 * GQA_KV + DIFF_QK + DIFF_V
ODD_MIX = GQA_Q + DIFF_V
Q_BLOCK = 128

MOE_GROUPS = 8
MOE_EXPERTS_PER_GROUP = 8
MOE_EXPERTS = MOE_GROUPS * MOE_EXPERTS_PER_GROUP
MOE_HIDDEN = 512
MOE_TOPK = 2
MOE_BLOCK = 128

kernel_name = 'hybrid_ssd_natten_gqa_diffattn_hmoe'


def rms_norm(x, w):
    xf = x.astype(jnp.float32)
    y = xf * lax.rsqrt(jnp.mean(xf * xf, axis=-1, keepdims=True) + EPS)
    return (y * w.astype(jnp.float32)).astype(x.dtype)


def axial_rope(n_tokens):
    t = jnp.arange(n_tokens, dtype=jnp.int32)
    row = (t // GRID_W).astype(jnp.float32)
    col = (t % GRID_W).astype(jnp.float32)
    axis_dim = HEAD_DIM // 2
    inv = 1.0 / (ROPE_THETA ** (jnp.arange(0, axis_dim, 2, dtype=jnp.float32) / axis_dim))
    ang = jnp.concatenate([row[:, None] * inv[None], col[:, None] * inv[None]], axis=-1)
    return jnp.cos(ang), jnp.sin(ang)


def apply_rope(x, cos, sin):
    xf = x.astype(jnp.float32).reshape(*x.shape[:-1], -1, 2)
    x0, x1 = xf[..., 0], xf[..., 1]
    c, s = cos[:, None, :], sin[:, None, :]
    out = jnp.stack([x0 * c - x1 * s, x0 * s + x1 * c], axis=-1)
    return out.reshape(x.shape).astype(x.dtype)


def softmax_attend(q, k, v):
    s = jnp.einsum('bqhgd,bkhd->bhgqk', q, k).astype(jnp.float32) * (q.shape[-1] ** -0.5)
    p = jax.nn.softmax(s, axis=-1).astype(v.dtype)
    o = jnp.einsum('bhgqk,bkhd->bqhgd', p, v)
    return o.reshape(*q.shape[:2], -1)


def diff_attend(q, k, v, lam, subln_w, lambda_init):
    s = jnp.einsum('bqhcd,bkhcd->bhcqk', q, k).astype(jnp.float32) * (q.shape[-1] ** -0.5)
    p = jax.nn.softmax(s, axis=-1)
    w = p[:, :, 0] - lam * p[:, :, 1]
    o = jnp.einsum('bhqk,bkhe->bqhe', w, v.astype(jnp.float32))
    o = rms_norm(o, subln_w) * (1.0 - lambda_init)
    return o.reshape(*q.shape[:2], -1).astype(v.dtype)


def centred_depthwise_conv(x, w, b):
    ch = x.shape[-1]
    y = lax.conv_general_dilated(x, w[:, None, :], window_strides=(1,),
                                 padding=[(SSD_CONV // 2, SSD_CONV // 2)],
                                 dimension_numbers=('NWC', 'WIO', 'NWC'),
                                 feature_group_count=ch)
    return y + b


def segsum(a):
    n = a.shape[-1]
    cs = jnp.cumsum(a, axis=-1)
    diff = cs[..., :, None] - cs[..., None, :]
    return jnp.where(jnp.tril(jnp.ones((n, n), dtype=bool)), diff, -jnp.inf)


def ssd_scan(xs, dt, a_head, bm, cm, init, need_y):
    f32 = jnp.float32
    bsz, L, H, P = xs.shape
    G, N = bm.shape[-2:]
    E = H // G
    nc = L // SSD_CHUNK
    X = (xs.astype(f32) * dt[..., None]).reshape(bsz, nc, SSD_CHUNK, G, E, P)
    a = (dt * a_head).reshape(bsz, nc, SSD_CHUNK, G, E).transpose(0, 3, 4, 1, 2)
    Bc = bm.astype(f32).reshape(bsz, nc, SSD_CHUNK, G, N)
    Cc = cm.astype(f32).reshape(bsz, nc, SSD_CHUNK, G, N)
    a_cs = jnp.cumsum(a, axis=-1)
    decay_to_end = jnp.exp(a_cs[..., -1:] - a_cs)
    chunk_states = jnp.einsum('bclgn,bgecl,bclgep->bcgepn', Bc, decay_to_end, X)
    states = jnp.concatenate([init.reshape(bsz, 1, G, E, P, N), chunk_states], axis=1)
    chunk_decay = jnp.exp(segsum(jnp.pad(a_cs[..., -1], ((0, 0), (0, 0), (0, 0), (1, 0)))))
    states = jnp.einsum('bgezc,bcgepn->bzgepn', chunk_decay, states)
    final = states[:, -1].reshape(bsz, H, P, N)
    if not need_y:
        return None, final
    cb = jnp.einsum('bclgn,bcsgn->bgcls', Cc, Bc)
    y_diag = jnp.einsum('bgcls,bgecls,bcsgep->bclgep', cb, jnp.exp(segsum(a)), X)
    y_off = jnp.einsum('bclgn,bcgepn,bgecl->bclgep', Cc, states[:, :-1], jnp.exp(a_cs))
    return (y_diag + y_off).reshape(bsz, L, H, P), final


def ssd_branch(p_lat, p_ctx, conv_w, conv_b, dt_bias, a_log, d_skip, norm_w, need_ctx):
    f32 = jnp.float32
    A = -jnp.exp(a_log.astype(f32))

    def prep(p):
        bsz, L, _ = p.shape
        z = p[..., :SSD_INNER]
        xbc = jax.nn.silu(centred_depthwise_conv(p[..., SSD_INNER:SSD_INNER + SSD_XBC], conv_w, conv_b))
        dt = jax.nn.softplus(p[..., SSD_INNER + SSD_XBC:].astype(f32).reshape(bsz, L, 2, SSD_HEADS)
                             + dt_bias.astype(f32))
        xs = xbc[..., :SSD_INNER].reshape(bsz, L, SSD_HEADS, SSD_HEAD_DIM)
        bm = xbc[..., SSD_INNER:SSD_INNER + SSD_GN].reshape(bsz, L, SSD_GROUPS, SSD_STATE)
        cm = xbc[..., SSD_INNER + SSD_GN:].reshape(bsz, L, SSD_GROUPS, SSD_STATE)
        return z, xs, dt, bm, cm

    def flip(t):
        return jnp.flip(t, axis=1)

    def bidir(xs, dt, bm, cm, init_f, init_b, need_y):
        y_f, fin_f = ssd_scan(xs, dt[:, :, 0], A[0], bm, cm, init_f, need_y)
        y_b, fin_b = ssd_scan(flip(xs), flip(dt[:, :, 1]), A[1], flip(bm), flip(cm), init_b, need_y)
        y = (y_f + flip(y_b) + d_skip.astype(f32)[:, None] * xs.astype(f32)) if need_y else None
        return y, fin_f, fin_b

    def gate_out(y, z):
        bsz, L = z.shape[:2]
        gsz = SSD_INNER // SSD_GROUPS
        g = y.reshape(bsz, L, SSD_GROUPS, gsz) * jax.nn.silu(z.astype(f32)).reshape(bsz, L, SSD_GROUPS, gsz)
        return rms_norm(g, norm_w.reshape(SSD_GROUPS, gsz)).reshape(bsz, L, SSD_INNER).astype(z.dtype)

    zc, xc, dtc, bc, cc = prep(p_ctx)
    zero = jnp.zeros((xc.shape[0], SSD_HEADS, SSD_HEAD_DIM, SSD_STATE), f32)
    y_c, st_f, st_b = bidir(xc, dtc, bc, cc, zero, zero, need_ctx)
    zl, xl, dtl, bl, cl = prep(p_lat)
    y_l, _, _ = bidir(xl, dtl, bl, cl, st_f, st_b, True)
    return gate_out(y_l, zl), (gate_out(y_c, zc) if need_ctx else None)


def na_branch(p_lat, p_ctx, q_norm_w, k_norm_w, rpb, need_ctx):
    f32 = jnp.float32
    B, S, _ = p_lat.shape
    rows = S // GRID_W
    wr = min(NA_WIN_ROWS, rows)
    scale = NA_HEAD_DIM ** -0.5

    def qkv(p):
        shp = (B, p.shape[1], NA_HEADS, NA_HEAD_DIM)
        q = rms_norm(p[..., :NA_WIDTH].reshape(shp), q_norm_w)
        k = rms_norm(p[..., NA_WIDTH:2 * NA_WIDTH].reshape(shp), k_norm_w)
        v = p[..., 2 * NA_WIDTH:].reshape(shp)
        return q, k, v

    ql, kl, vl = qkv(p_lat)
    qc, kc, vc = qkv(p_ctx)
    qg = ql.reshape(B, rows, GRID_W, NA_HEADS, NA_HEAD_DIM)
    kg = kl.reshape(B, rows, GRID_W, NA_HEADS, NA_HEAD_DIM)
    vg = vl.reshape(B, rows, GRID_W, NA_HEADS, NA_HEAD_DIM)
    qcol = jnp.arange(GRID_W)
    col_start = jnp.clip(qcol - NA_WIN_COLS // 2, 0, GRID_W - NA_WIN_COLS)
    col_mask = (qcol[None, :] >= col_start[:, None]) & (qcol[None, :] < col_start[:, None] + NA_WIN_COLS)
    dcol_idx = jnp.clip(qcol[None, :] - qcol[:, None] + NA_WIN_COLS - 1, 0, 2 * NA_WIN_COLS - 2)
    rpb_cols = rpb[:, :, dcol_idx]
    n_win = wr * GRID_W

    def row_block(r):
        r0 = jnp.clip(r - wr // 2, 0, rows - wr)
        q_r = lax.dynamic_index_in_dim(qg, r, axis=1, keepdims=False)
        k_w = lax.dynamic_slice_in_dim(kg, r0, wr, axis=1)
        v_w = lax.dynamic_slice_in_dim(vg, r0, wr, axis=1)
        bias = jnp.take(rpb_cols, r0 + jnp.arange(wr) - r + NA_WIN_ROWS - 1, axis=1)
        s_win = (jnp.einsum('bqhd,bikhd->bhqik', q_r, k_w).astype(f32) * scale
                 + bias.transpose(0, 2, 1, 3).astype(f32))
        s_win = jnp.where(col_mask[:, None, :], s_win, -jnp.inf)
        s_ctx = jnp.einsum('bqhd,bthd->bhqt', q_r, kc).astype(f32) * scale
        p = jax.nn.softmax(jnp.concatenate([s_win.reshape(B, NA_HEADS, GRID_W, n_win), s_ctx], axis=-1),
                           axis=-1).astype(vl.dtype)
        o = (jnp.einsum('bhqik,bikhd->bqhd', p[..., :n_win].reshape(B, NA_HEADS, GRID_W, wr, GRID_W), v_w)
             + jnp.einsum('bhqt,bthd->bqhd', p[..., n_win:], vc))
        return o

    o = lax.map(row_block, jnp.arange(rows))
    o_lat = o.swapaxes(0, 1).reshape(B, S, NA_WIDTH)
    o_ctx = softmax_attend(qc[:, :, :, None], kc, vc) if need_ctx else None
    return o_lat, o_ctx


def ssd_na_layer(a_lat, a_ctx, in_w, conv_w, conv_b, dt_bias, a_log, d_skip, ssd_norm_w,
                 na_q_norm, na_k_norm, na_rpb, out_w, need_ctx):
    p_lat = a_lat @ in_w
    p_ctx = a_ctx @ in_w
    s_lat, s_ctx = ssd_branch(p_lat[..., :SSD_PROJ], p_ctx[..., :SSD_PROJ], conv_w, conv_b,
                              dt_bias, a_log, d_skip, ssd_norm_w, need_ctx)
    n_lat, n_ctx = na_branch(p_lat[..., SSD_PROJ:], p_ctx[..., SSD_PROJ:], na_q_norm, na_k_norm,
                             na_rpb, need_ctx)
    m_lat = jnp.concatenate([s_lat, n_lat.astype(s_lat.dtype)], axis=-1) @ out_w
    m_ctx = (jnp.concatenate([s_ctx, n_ctx.astype(s_ctx.dtype)], axis=-1) @ out_w) if need_ctx else None
    return m_lat, m_ctx


def gqa_diff_layer(a_lat, a_ctx, in_w, gq_norm, gk_norm, dq_norm, dk_norm, lam_vecs, subln_w, out_w,
                   lambda_init, cos, sin, need_ctx):
    B, S, _ = a_lat.shape
    T = a_ctx.shape[1]
    lv = lam_vecs.astype(jnp.float32)
    lam = jnp.exp(jnp.dot(lv[0], lv[1])) - jnp.exp(jnp.dot(lv[2], lv[3])) + lambda_init

    def queries(pq):
        L = pq.shape[1]
        gq = rms_norm(pq[..., :GQA_Q].reshape(B, L, GQA_HEADS, HEAD_DIM), gq_norm)
        dq = rms_norm(pq[..., GQA_Q:ODD_Q].reshape(B, L, 2 * DIFF_HEADS, HEAD_DIM), dq_norm)
        return gq, dq

    def keys_values(pkv):
        L = pkv.shape[1]
        o1, o2 = GQA_KV, 2 * GQA_KV
        o3 = o2 + DIFF_QK
        gk = rms_norm(pkv[..., :o1].reshape(B, L, GQA_KV_HEADS, HEAD_DIM), gk_norm)
        gv = pkv[..., o1:o2].reshape(B, L, GQA_KV_HEADS, HEAD_DIM)
        dk = rms_norm(pkv[..., o2:o3].reshape(B, L, 2 * DIFF_HEADS, HEAD_DIM), dk_norm)
        dv = pkv[..., o3:].reshape(B, L, DIFF_HEADS, DIFF_V_DIM)
        return gk, gv, dk, dv

    p_lat = a_lat @ in_w
    gq, dq = queries(p_lat)
    gk, gv, dk, dv = keys_values(p_lat[..., ODD_Q:])
    gq, dq, gk, dk = [apply_rope(t, cos, sin) for t in (gq, dq, gk, dk)]
    cgk, cgv, cdk, cdv = keys_values(a_ctx @ in_w[:, ODD_Q:])
    gk_all = jnp.concatenate([cgk, gk], axis=1)
    gv_all = jnp.concatenate([cgv, gv], axis=1)
    dk_all = jnp.concatenate([cdk, dk], axis=1).reshape(B, T + S, DIFF_HEADS, 2, HEAD_DIM)
    dv_all = jnp.concatenate([cdv, dv], axis=1)
    nb = S // Q_BLOCK
    gq_b = gq.reshape(B, nb, Q_BLOCK, GQA_KV_HEADS, GQA_GROUP, HEAD_DIM).swapaxes(0, 1)
    dq_b = dq.reshape(B, nb, Q_BLOCK, DIFF_HEADS, 2, HEAD_DIM).swapaxes(0, 1)

    def query_block(qs):
        gqb, dqb = qs
        return (softmax_attend(gqb, gk_all, gv_all),
                diff_attend(dqb, dk_all, dv_all, lam, subln_w, lambda_init))

    og, od = lax.map(query_block, (gq_b, dq_b))
    mix = jnp.concatenate([og.swapaxes(0, 1).reshape(B, S, GQA_Q),
                           od.swapaxes(0, 1).reshape(B, S, DIFF_V)], axis=-1)
    m_lat = mix @ out_w
    m_ctx = None
    if need_ctx:
        cgq, cdq = queries(a_ctx @ in_w[:, :ODD_Q])
        c_mix = jnp.concatenate([
            softmax_attend(cgq.reshape(B, T, GQA_KV_HEADS, GQA_GROUP, HEAD_DIM), cgk, cgv),
            diff_attend(cdq.reshape(B, T, DIFF_HEADS, 2, HEAD_DIM),
                        cdk.reshape(B, T, DIFF_HEADS, 2, HEAD_DIM), cdv, lam, subln_w, lambda_init)], axis=-1)
        m_ctx = c_mix @ out_w
    return m_lat, m_ctx


def routed_experts(t, e_idx, gate, w1, w3, w2):
    N, D = t.shape
    E = w1.shape[0]
    A = N * MOE_TOPK
    flat_e = e_idx.reshape(-1)
    flat_tok = jnp.repeat(jnp.arange(N, dtype=jnp.int32), MOE_TOPK)
    flat_gate = gate.reshape(-1)
    order = jnp.argsort(flat_e)
    se = flat_e[order]
    counts = jnp.bincount(flat_e, length=E)
    padded = (counts + MOE_BLOCK - 1) // MOE_BLOCK * MOE_BLOCK
    pad_end = jnp.cumsum(padded)
    pad_start = pad_end - padded
    raw_start = jnp.cumsum(counts) - counts
    dest = pad_start[se] + jnp.arange(A) - raw_start[se]
    n_blocks = -(-A // MOE_BLOCK) + E
    slot_tok = jnp.full((n_blocks * MOE_BLOCK,), N, jnp.int32).at[dest].set(flat_tok[order])
    block_e = jnp.minimum(jnp.searchsorted(pad_end, jnp.arange(n_blocks) * MOE_BLOCK, side='right'), E - 1)
    t_pad = jnp.concatenate([t, jnp.zeros((1, D), t.dtype)], axis=0)
    xb = t_pad[slot_tok].reshape(n_blocks, MOE_BLOCK, D)

    def expert_block(args):
        xblk, e = args
        h = jax.nn.silu(xblk @ w1[e]) * (xblk @ w3[e])
        return h @ w2[e]

    yb = lax.map(expert_block, (xb, block_e)).reshape(n_blocks * MOE_BLOCK, D)
    y_assign = yb[dest].astype(jnp.float32) * flat_gate[order][:, None]
    return jax.ops.segment_sum(y_assign, flat_tok[order], num_segments=N).astype(t.dtype)


def hier_moe(t, group_w, expert_w, w1, w3, w2):
    N = t.shape[0]
    g_logits = (t @ group_w).astype(jnp.float32)
    g_prob = jax.nn.softmax(g_logits, axis=-1)
    _, g_idx = lax.top_k(g_logits, 1)
    g_gate = jnp.take_along_axis(g_prob, g_idx, axis=-1)
    e_logits = (t @ expert_w).astype(jnp.float32).reshape(N, MOE_GROUPS, MOE_EXPERTS_PER_GROUP)
    e_in_g = jnp.take_along_axis(e_logits, g_idx[:, :, None], axis=1)[:, 0]
    top_v, top_i = lax.top_k(e_in_g, MOE_TOPK)
    gate = jax.nn.softmax(top_v, axis=-1) * g_gate
    e_idx = g_idx * MOE_EXPERTS_PER_GROUP + top_i
    return routed_experts(t, e_idx, gate, w1, w3, w2)


def setup_inputs(seed: int = 0) -> dict:
    key = jax.random.key(seed)
    ks = jax.random.split(key, 40)
    f32 = jnp.float32
    D = D_MODEL
    n_even = (DEPTH + 1) // 2
    n_odd = DEPTH // 2

    def nrm(k, shape, scale):
        return jax.random.normal(k, shape, f32) * scale

    dt0 = jnp.exp(jax.random.uniform(ks[10], (n_even, 2, SSD_HEADS), f32, math.log(1e-3), math.log(1e-1)))
    return {
        'x': nrm(ks[0], (BATCH, SEQ, D), 1.0),
        'c': nrm(ks[1], (BATCH, D), 1.0),
        'ctx': nrm(ks[2], (BATCH, CTX_LEN, D), 1.0),
        'c_ctx': nrm(ks[3], (D,), 1.0),
        'ada_w': nrm(ks[4], (DEPTH, D, 6 * D), 0.5 * D ** -0.5),
        'ada_b': nrm(ks[5], (DEPTH, 6 * D), 0.02),
        'norm_w': 1.0 + nrm(ks[6], (DEPTH, 2, D), 0.02),
        'ev_in_w': nrm(ks[7], (n_even, D, EVEN_IN), D ** -0.5),
        'ev_conv_w': nrm(ks[8], (n_even, SSD_CONV, SSD_XBC), SSD_CONV ** -0.5),
        'ev_conv_b': nrm(ks[9], (n_even, SSD_XBC), 0.02),
        'ev_dt_bias': dt0 + jnp.log(-jnp.expm1(-dt0)),
        'ev_a_log': jnp.log(jax.random.uniform(ks[11], (n_even, 2, SSD_HEADS), f32, 1.0, 16.0)),
        'ev_d_skip': 1.0 + nrm(ks[12], (n_even, SSD_HEADS), 0.02),
        'ev_ssd_norm_w': 1.0 + nrm(ks[13], (n_even, SSD_INNER), 0.02),
        'ev_na_q_norm': 1.0 + nrm(ks[14], (n_even, NA_HEAD_DIM), 0.02),
        'ev_na_k_norm': 1.0 + nrm(ks[15], (n_even, NA_HEAD_DIM), 0.02),
        'ev_na_rpb': nrm(ks[16], (n_even, NA_HEADS, 2 * NA_WIN_ROWS - 1, 2 * NA_WIN_COLS - 1), 0.1),
        'ev_out_w': nrm(ks[17], (n_even, EVEN_MIX, D), EVEN_MIX ** -0.5),
        'od_in_w': nrm(ks[18], (n_odd, D, ODD_IN), D ** -0.5),
        'od_gqa_q_norm': 1.0 + nrm(ks[19], (n_odd, HEAD_DIM), 0.02),
        'od_gqa_k_norm': 1.0 + nrm(ks[20], (n_odd, HEAD_DIM), 0.02),
        'od_diff_q_norm': 1.0 + nrm(ks[21], (n_odd, HEAD_DIM), 0.02),
        'od_diff_k_norm': 1.0 + nrm(ks[22], (n_odd, HEAD_DIM), 0.02),
        'od_lambda': nrm(ks[23], (n_odd, 4, HEAD_DIM), 0.1),
        'od_diff_subln': 1.0 + nrm(ks[24], (n_odd, DIFF_V_DIM), 0.02),
        'od_out_w': nrm(ks[25], (n_odd, ODD_MIX, D), ODD_MIX ** -0.5),
        'moe_group_w': nrm(ks[26], (DEPTH, D, MOE_GROUPS), D ** -0.5),
        'moe_expert_w': nrm(ks[27], (DEPTH, D, MOE_EXPERTS), D ** -0.5),
        'moe_w1': nrm(ks[28], (DEPTH, MOE_EXPERTS, D, MOE_HIDDEN), D ** -0.5),
        'moe_w3': nrm(ks[29], (DEPTH, MOE_EXPERTS, D, MOE_HIDDEN), D ** -0.5),
        'moe_w2': nrm(ks[30], (DEPTH, MOE_EXPERTS, MOE_HIDDEN, D), MOE_HIDDEN ** -0.5),
    }


def reference(x, c, ctx, c_ctx, ada_w, ada_b, norm_w,
              ev_in_w, ev_conv_w, ev_conv_b, ev_dt_bias, ev_a_log, ev_d_skip, ev_ssd_norm_w,
              ev_na_q_norm, ev_na_k_norm, ev_na_rpb, ev_out_w,
              od_in_w, od_gqa_q_norm, od_gqa_k_norm, od_diff_q_norm, od_diff_k_norm,
              od_lambda, od_diff_subln, od_out_w,
              moe_group_w, moe_expert_w, moe_w1, moe_w3, moe_w2):
    B, S, D = x.shape
    T = ctx.shape[1]
    cos, sin = axial_rope(S)
    h_lat, h_ctx = x, ctx
    for l in range(DEPTH):
        last = l == DEPTH - 1
        i = l // 2
        mod_lat = jax.nn.silu(c) @ ada_w[l] + ada_b[l]
        mod_ctx = jax.nn.silu(c_ctx) @ ada_w[l] + ada_b[l]
        sh1, sc1, g1, sh2, sc2, g2 = jnp.split(mod_lat[:, None, :], 6, axis=-1)
        csh1, csc1, cg1, csh2, csc2, cg2 = jnp.split(mod_ctx, 6)
        a_lat = rms_norm(h_lat, norm_w[l, 0]) * (1 + sc1) + sh1
        a_ctx = rms_norm(h_ctx, norm_w[l, 0]) * (1 + csc1) + csh1
        if l % 2 == 0:
            m_lat, m_ctx = ssd_na_layer(a_lat, a_ctx, ev_in_w[i], ev_conv_w[i], ev_conv_b[i], ev_dt_bias[i],
                                        ev_a_log[i], ev_d_skip[i], ev_ssd_norm_w[i], ev_na_q_norm[i],
                                        ev_na_k_norm[i], ev_na_rpb[i], ev_out_w[i], not last)
        else:
            lambda_init = 0.8 - 0.6 * math.exp(-0.3 * l)
            m_lat, m_ctx = gqa_diff_layer(a_lat, a_ctx, od_in_w[i], od_gqa_q_norm[i], od_gqa_k_norm[i],
                                          od_diff_q_norm[i], od_diff_k_norm[i], od_lambda[i],
                                          od_diff_subln[i], od_out_w[i], lambda_init, cos, sin, not last)
        h_lat = h_lat + g1 * m_lat.astype(h_lat.dtype)
        f_lat = rms_norm(h_lat, norm_w[l, 1]) * (1 + sc2) + sh2
        if last:
            y = hier_moe(f_lat.reshape(B * S, D), moe_group_w[l], moe_expert_w[l],
                         moe_w1[l], moe_w3[l], moe_w2[l])
            h_lat = h_lat + g2 * y.reshape(B, S, D)
        else:
            h_ctx = h_ctx + cg1 * m_ctx.astype(h_ctx.dtype)
            f_ctx = rms_norm(h_ctx, norm_w[l, 1]) * (1 + csc2) + csh2
            y = hier_moe(jnp.concatenate([f_lat.reshape(B * S, D), f_ctx.reshape(B * T, D)], axis=0),
                         moe_group_w[l], moe_expert_w[l], moe_w1[l], moe_w3[l], moe_w2[l])
            h_lat = h_lat + g2 * y[:B * S].reshape(B, S, D)
            h_ctx = h_ctx + cg2 * y[B * S:].reshape(B, T, D)
    return h_lat
```

```python
import numpy as np
import ml_dtypes
import concourse.bass as bass
import concourse.mybir as mybir
from concourse.bass_utils import run_bass_kernel_spmd

F32 = mybir.dt.float32
BF16 = mybir.dt.bfloat16
I32 = mybir.dt.int32
U32 = mybir.dt.uint32
AF = mybir.ActivationFunctionType
ALU = mybir.AluOpType
AX = mybir.AxisListType


class Ev:
    __slots__ = ("src", "count", "clock")

    def __init__(self, src, count, clock):
        self.src = src
        self.count = count
        self.clock = clock


class Src:
    def __init__(self, name, sem, mult):
        self.name = name
        self.sem = sem
        self.mult = mult
        self.n = 0


class Buf:
    __slots__ = ("name", "w", "r")

    def __init__(self, name=""):
        self.name = name
        self.w = None
        self.r = {}


class Eng:
    def __init__(self, fw, raw, name):
        self.fw = fw
        self.raw = raw
        self.name = name
        self.src = Src(name, fw._sem("s_" + name), 1)
        self.clock = {}
        self.slots = []
        self.slot_i = 0

    def wait(self, ev):
        if ev is None:
            return
        if self.name == "pe" and ev.src.name.startswith("pe"):
            return
        if self.clock.get(ev.src.name, 0) >= ev.count:
            return
        self.raw.wait_ge(ev.src.sem, ev.count * ev.src.mult)
        self.fw.n_waits += 1
        for k, v in ev.clock.items():
            if self.clock.get(k, 0) < v:
                self.clock[k] = v
        self.clock[ev.src.name] = ev.count


class FW:
    def __init__(self, nc, stack, n_dma_slots=12):
        self.nc = nc
        self.stack = stack
        self.sem_stack = stack
        self.n_waits = 0
        self.n_ops = 0
        self.pe = Eng(self, nc.tensor, "pe")
        self.dve = Eng(self, nc.vector, "dve")
        self.act = Eng(self, nc.scalar, "act")
        self.pool = Eng(self, nc.gpsimd, "pool")
        self.sp = Eng(self, nc.sync, "sp")
        for e in (self.sp, self.pool, self.act):
            for i in range(n_dma_slots):
                nm = "d_%s%d" % (e.name, i)
                e.slots.append(Src(nm, self._sem(nm), 16))
        self.uid = 0
        self.psum = []
        for i in range(8):
            t = stack.enter_context(nc.psum_tensor("ps%d" % i, [128, 512], F32))
            self.psum.append((t, Buf("ps%d" % i)))
        self.ps_i = 0
        self.ps_pool = list(range(8))

    def _sem(self, name):
        return self.sem_stack.enter_context(self.nc.semaphore(name))

    def sb(self, shape, dtype, name=None):
        self.uid += 1
        name = "sb%d_%s" % (self.uid, name or "t")
        t = self.stack.enter_context(self.nc.sbuf_tensor(name, list(shape), dtype))
        return t

    def next_psum(self):
        self.ps_i = (self.ps_i + 1) % len(self.ps_pool)
        return self.psum[self.ps_pool[self.ps_i]]

    def _deps(self, eng, reads, writes):
        for b in reads:
            eng.wait(b.w)
        for b in writes:
            eng.wait(b.w)
            for ev in list(b.r.values()):
                eng.wait(ev)

    def _mark(self, ev, reads, writes):
        for b in reads:
            b.r[ev.src.name] = ev
        for b in writes:
            b.w = ev
            b.r = {}

    def op(self, eng, fn, reads=(), writes=()):
        self._deps(eng, reads, writes)
        ins = fn(eng.raw)
        if eng.src.n >= 30000:
            eng.gen = getattr(eng, "gen", 0) + 1
            nm = "%s_g%d" % (eng.name, eng.gen)
            eng.src = Src(nm, self._sem("s_" + nm), 1)
        eng.src.n += 1
        ins.then_inc(eng.src.sem, 1)
        clock = dict(eng.clock)
        ev = Ev(eng.src, eng.src.n, clock)
        self._mark(ev, reads, writes)
        self.n_ops += 1
        return ev

    def dma(self, eng, out, in_, reads=(), writes=(), fn=None):
        slot = eng.slots[eng.slot_i]
        eng.slot_i = (eng.slot_i + 1) % len(eng.slots)
        if slot.n > 0:
            eng.wait(Ev(slot, slot.n, {}))
        self._deps(eng, reads, writes)
        if fn is None:
            ins = eng.raw.dma_start(out=out, in_=in_)
        else:
            ins = fn(eng.raw)
        ins.then_inc(slot.sem, 16)
        slot.n += 1
        ev = Ev(slot, slot.n, dict(eng.clock))
        self._mark(ev, reads, writes)
        self.n_ops += 1
        return ev

    def bound_reg(self, val):
        if not hasattr(self, "_bregs"):
            self._bregs = {}
        if val not in self._bregs:
            self._bregs[val] = self.nc.gpsimd.to_reg(val)
        return self._bregs[val]

    def engines(self):
        return (self.pe, self.dve, self.act, self.pool, self.sp)

    def barrier(self):
        evs = []
        for e in self.engines():
            if e.src.n > 0:
                evs.append(Ev(e.src, e.src.n, dict(e.clock)))
            for s in e.slots:
                if s.n > 0:
                    evs.append(Ev(s, s.n, {}))
        for e in self.engines():
            for ev in evs:
                e.wait(ev)

    def finish(self, bufs):
        for b in bufs:
            self.sp.wait(b.w)

from contextlib import ExitStack

D = 2048
EPS = 1e-6


def bview(ap_row, n=128):
    return ap_row.partition_broadcast(n)


def emit_consts(fw, ident_d):
    ident = fw.sb([128, 128], F32, "ident"); b_ident = Buf("ident")
    fw.dma(fw.sp, ident[:], ident_d, writes=[b_ident])
    return ident, b_ident


def emit_mods(fw, cvec, ada_w, ada_b, ncols, modrows, d_mod, ident, b_ident):
    pe, dve, act, pool, sp = fw.pe, fw.dve, fw.act, fw.pool, fw.sp
    outer = fw.stack
    with ExitStack() as st:
        fw.stack = st
        cv = fw.sb([2, D], F32, "cv"); b_cv = Buf()
        fw.dma(sp, cv[:], cvec, writes=[b_cv])
        siluT = fw.sb([128, 16, 2], BF16, "siluT"); b_sT = Buf()
        pt, pb = fw.next_psum()
        for kc in range(16):
            fw.op(pe, lambda e, kc=kc: e.transpose(pt[:, 2 * kc:2 * kc + 2], cv[0:2, kc * 128:(kc + 1) * 128], ident[0:2, 0:2]),
                  reads=[b_cv, b_ident], writes=[pb])
        fw.op(act, lambda e: e.activation(siluT[:, :, :].rearrange("p a b -> p (a b)"), pt[:, 0:32], AF.Silu), reads=[pb], writes=[b_sT])
        adab = fw.sb([2, ncols], F32, "adab"); b_adab = Buf()
        fw.dma(sp, adab[:], bview(ada_b, 2), writes=[b_adab])
        wts = [fw.sb([128, 16, 512], BF16, "adaw%d" % i) for i in range(2)]
        b_wts = [Buf() for _ in range(2)]
        modsb = fw.sb([2, ncols], F32, "modsb"); b_modsb = Buf()
        for j in range(ncols // 512):
            wt, bw = wts[j % 2], b_wts[j % 2]
            fw.dma(pool, wt[:], ada_w[:, j * 512:(j + 1) * 512].rearrange("(kc p) c -> p kc c", p=128), writes=[bw])
            pt, pb = fw.next_psum()
            for kc in range(16):
                fw.op(pe, lambda e, kc=kc, wt=wt, pt=pt: e.matmul(pt[0:2, :], siluT[:, kc, :], wt[:, kc, :], start=(kc == 0), stop=(kc == 15)),
                      reads=[b_sT, bw], writes=[pb])
            fw.op(dve, lambda e, pt=pt, j=j: e.tensor_tensor(modsb[:, j * 512:(j + 1) * 512], pt[0:2, :], adab[:, j * 512:(j + 1) * 512], ALU.add),
                  reads=[pb, b_adab], writes=[b_modsb])
        fw.dma(sp, modrows, modsb[:, :], reads=[b_modsb], writes=[d_mod])
        fw.barrier()
    fw.stack = outer


def emit_aT(fw, h_all, n_tiles, n_ctx_tiles, norm_w1, modrows, d_mod, aT, b_aT, ident, b_ident, row_of=None, h_bufs=()):
    pe, dve, act, pool, sp = fw.pe, fw.dve, fw.act, fw.pool, fw.sp
    outer = fw.stack
    with ExitStack() as st:
        fw.stack = st
        nw = fw.sb([128, D], F32, "nw"); b_nw = Buf()
        fw.dma(sp, nw[:], bview(norm_w1), writes=[b_nw])
        nsc = []; sh = []; b_nsc = []; b_sh = []
        for v in range(2):
            s = fw.sb([128, D], F32, "sh1_%d" % v); bs = Buf()
            fw.dma(sp, s[:], bview(modrows[v:v + 1, 0:2048]), reads=[d_mod], writes=[bs])
            sh.append(s); b_sh.append(bs)
            c = fw.sb([128, D], F32, "nsc1_%d" % v); bc = Buf()
            fw.dma(sp, c[:], bview(modrows[v:v + 1, 2048:4096]), reads=[d_mod], writes=[bc])
            fw.op(dve, lambda e, c=c: e.scalar_tensor_tensor(c[:, :], c[:, :], 1.0, nw[:, :], ALU.add, ALU.mult), reads=[b_nw], writes=[bc])
            nsc.append(c); b_nsc.append(bc)
        hts = [fw.sb([128, D], F32, "ht%d" % i) for i in range(2)]; b_hts = [Buf() for _ in range(2)]
        ats = [fw.sb([128, D], F32, "at%d" % i) for i in range(2)]; b_ats = [Buf() for _ in range(2)]
        sq = fw.sb([128, D], F32, "sq"); b_sq = Buf()
        ssqs = [fw.sb([128, 1], F32, "ssq%d" % i) for i in range(2)]; b_ssqs = [Buf() for _ in range(2)]
        for t in range(n_tiles):
            v = 1 if t < n_ctx_tiles else 0
            ht, bh = hts[t % 2], b_hts[t % 2]
            at, ba = ats[t % 2], b_ats[t % 2]
            ssq, b_ssq = ssqs[t % 2], b_ssqs[t % 2]
            r0 = t * 128 if row_of is None else row_of(t)
            fw.dma(sp, ht[:], h_all[r0:r0 + 128, :], reads=list(h_bufs), writes=[bh])
            fw.op(act, lambda e, ht=ht, ssq=ssq: e.activation(sq[:, :], ht[:, :], AF.Square, accum_out=ssq[:, :]), reads=[bh], writes=[b_sq, b_ssq])
            fw.op(act, lambda e, ssq=ssq: e.activation(ssq[:, :], ssq[:, :], AF.Sqrt, bias=EPS, scale=1.0 / D), writes=[b_ssq])
            fw.op(dve, lambda e, ssq=ssq: e.reciprocal(ssq[:, :], ssq[:, :]), writes=[b_ssq])
            fw.op(dve, lambda e, ht=ht, at=at, ssq=ssq, v=v: e.scalar_tensor_tensor(at[:, :], ht[:, :], ssq[:, 0:1], nsc[v][:, :], ALU.mult, ALU.mult),
                  reads=[bh, b_ssq, b_nsc[v]], writes=[ba])
            fw.op(pool, lambda e, at=at, v=v: e.tensor_tensor(at[:, :], at[:, :], sh[v][:, :], ALU.add), reads=[b_sh[v]], writes=[ba])
            for q in range(4):
                pt, pb = fw.next_psum()
                for i in range(4):
                    kc = q * 4 + i
                    fw.op(pe, lambda e, pt=pt, i=i, kc=kc, at=at: e.transpose(pt[:, i * 128:(i + 1) * 128], at[:, kc * 128:(kc + 1) * 128], ident[:, :]),
                          reads=[ba, b_ident], writes=[pb])
                dst = aT[:, q * 4:(q + 1) * 4, t * 128:(t + 1) * 128]
                src = pt[:, :].rearrange("p (a b) -> p a b", a=4)
                if q % 2 == 0:
                    fw.op(act, lambda e, dst=dst, src=src: e.copy(dst, src), reads=[pb], writes=[b_aT[t]])
                else:
                    fw.op(dve, lambda e, dst=dst, src=src: e.tensor_copy(dst, src), reads=[pb], writes=[b_aT[t]])
        fw.barrier()
    fw.stack = outer

from contextlib import ExitStack

D = 2048
CAP = 128
NE = 64
EPS = 1e-6


def bview(ap_row, n=128):
    return ap_row.partition_broadcast(n)


def build_phaseB(n_tiles, has_ctx, ctx=None):
    fused = ctx is not None
    nc = ctx["nc"] if fused else bass.Bass("TRN2", target_bir_lowering=False)
    pre = ctx["pre"] if fused else ""
    T = n_tiles * 128
    I = lambda name, shape, dt=F32: nc.dram_tensor(pre + name, shape, dt, kind="ExternalInput").ap()
    h_src = ctx.get("h_src") if fused else None
    h_in = I("h_in", [T, D]) if h_src is None else h_src[0]
    h_bufs = [] if h_src is None else h_src[1]
    mix_in = None if fused else I("mix_in", [T, 4096])
    gidx_d = I("gidx", [128, n_tiles, 2], I32) if fused else None
    cvec = I("cvec", [2, D])
    ada_w = I("ada_w", [D, 8192])
    ada_b = I("ada_b", [1, 8192])
    norm_w2 = I("norm_w2", [1, D])
    out_w = I("out_w", [4096, D])
    gwew = I("gwew", [D, 72])
    w1 = I("w1", [NE, D, 512])
    w3 = I("w3", [NE, D, 512])
    w2 = I("w2", [NE, 512, D])
    ident_d = I("ident", [128, 128])
    trilt_d = I("trilt", [128, 128])
    iota_d = I("iota64", [1, 64])
    out_fn = ctx.get("out_fn") if fused else None
    h_out = nc.dram_tensor("h_out", [T, D], F32, kind="ExternalOutput").ap() if out_fn is None else None
    modrows = nc.dram_tensor(pre + "modrows", [2, 8192], F32).ap()
    m_all = nc.dram_tensor(pre + "m_all", [T, D], F32).ap()
    h1_all = nc.dram_tensor(pre + "h1_all", [T, D], F32).ap()
    xdisp = nc.dram_tensor(pre + "xdisp", [NE * CAP, D], F32).ap()
    ybuf = nc.dram_tensor(pre + "ybuf", [NE * CAP, D], F32).ap()
    d_mod = Buf("modrows"); d_m = [Buf() for _ in range(n_tiles)]; d_h1 = [Buf() for _ in range(n_tiles)]
    d_x = Buf("xdisp"); d_y = [Buf() for _ in range(NE)]; d_out = [Buf() for _ in range(n_tiles)]
    d_xz = [Buf() for _ in range(NE * CAP // 512)]

    with ExitStack() as st0:
        if fused:
            fw = ctx["fw"]; fw.stack = st0; fw.ps_pool = list(range(8))
        else:
            fw = FW(nc, st0)
        pe, dve, act, pool, sp = fw.pe, fw.dve, fw.act, fw.pool, fw.sp
        ident = fw.sb([128, 128], F32, "ident"); b_ident = Buf()
        fw.dma(sp, ident[:], ident_d, writes=[b_ident])
        trilt = fw.sb([128, 128], BF16, "trilt"); b_tri = Buf()
        fw.dma(pool, trilt[:], trilt_d, writes=[b_tri])
        ones_bf = fw.sb([128, 128], BF16, "ones_bf"); b_ones = Buf()
        fw.op(pool, lambda e: e.memset(ones_bf[:, :], 1.0), writes=[b_ones])
        iota = fw.sb([128, 64], F32, "iota"); b_iota = Buf()
        fw.dma(sp, iota[:], bview(iota_d), writes=[b_iota])
        zcol = fw.sb([128, 1], F32, "zcol"); b_z = Buf()
        fw.op(pool, lambda e: e.memset(zcol[:, :], 0.0), writes=[b_z])
        dest_all = fw.sb([128, n_tiles, 2], I32, "dest_all"); b_dest = [Buf() for _ in range(n_tiles)]
        gate_all = fw.sb([128, n_tiles, 2], F32, "gate_all"); b_gate = [Buf() for _ in range(n_tiles)]
        with ExitStack() as st:
            fw.stack = st
            zt = fw.sb([128, 8192], F32, "zt"); b_zt = Buf()
            fw.op(pool, lambda e: e.memset(zt[:, :], 0.0), writes=[b_zt])
            xv = xdisp.rearrange("(a p r) d -> a p (r d)", p=128, r=4)
            for a in range(NE * CAP // 512):
                fw.dma(sp, xv[a], zt[:, :], reads=[b_zt], writes=[d_xz[a]])
            cv = fw.sb([2, D], F32, "cv"); b_cv = Buf()
            fw.dma(sp, cv[:], cvec, writes=[b_cv])
            siluT = fw.sb([128, 16, 2], BF16, "siluT"); b_sT = Buf()
            pt, pb = fw.next_psum()
            for kc in range(16):
                fw.op(pe, lambda e, kc=kc: e.transpose(pt[:, 2 * kc:2 * kc + 2], cv[0:2, kc * 128:(kc + 1) * 128], ident[0:2, 0:2]),
                      reads=[b_cv, b_ident], writes=[pb])
            fw.op(act, lambda e: e.activation(siluT[:, :, :].rearrange("p a b -> p (a b)"), pt[:, 0:32], AF.Silu), reads=[pb], writes=[b_sT])
            adab = fw.sb([2, 8192], F32, "adab"); b_adab = Buf()
            fw.dma(sp, adab[:], bview(ada_b, 2), writes=[b_adab])
            wts = [fw.sb([128, 16, 512], BF16, "adaw%d" % i) for i in range(2)]
            b_wts = [Buf() for _ in range(2)]
            modsb = fw.sb([2, 8192], F32, "modsb"); b_modsb = Buf()
            for j in range(16):
                wt, bw = wts[j % 2], b_wts[j % 2]
                fw.dma(pool, wt[:], ada_w[:, j * 512:(j + 1) * 512].rearrange("(kc p) c -> p kc c", p=128), writes=[bw])
                pt, pb = fw.next_psum()
                for kc in range(16):
                    fw.op(pe, lambda e, kc=kc, wt=wt, pt=pt: e.matmul(pt[0:2, :], siluT[:, kc, :], wt[:, kc, :], start=(kc == 0), stop=(kc == 15)),
                          reads=[b_sT, bw], writes=[pb])
                fw.op(dve, lambda e, pt=pt, j=j: e.tensor_tensor(modsb[:, j * 512:(j + 1) * 512], pt[0:2, :], adab[:, j * 512:(j + 1) * 512], ALU.add),
                      reads=[pb, b_adab], writes=[b_modsb])
            fw.dma(sp, modrows, modsb[:, :], reads=[b_modsb], writes=[d_mod])
            fw.barrier()
        with ExitStack() as st:
            fw.stack = st
            mixT = fw.sb([128, n_tiles, 32, 128], BF16, "mixT"); b_mixT = [Buf() for _ in range(n_tiles)]
            mts = [fw.sb([128, 4096], F32, "mixt%d" % i) for i in range(2)]; b_mts = [Buf() for _ in range(2)]
            if fused:
                gsrc, gbufs, _ = ctx["mix_src"]
                gix = fw.sb([128, n_tiles, 2], I32, "gix"); b_gix = Buf()
                fw.dma(sp, gix[:], gidx_d, writes=[b_gix])
                m16 = [fw.sb([128, 2048], BF16, "m16_%d" % i) for i in range(4)]; b_m16 = [Buf() for _ in range(4)]
            for t in range(n_tiles):
                mt, bm = mts[t % 2], b_mts[t % 2]
                if not fused:
                    fw.dma(sp, mt[:], mix_in[t * 128:(t + 1) * 128, :], writes=[bm])
                else:
                    for k in range(2):
                        mm, bmm = m16[(2 * t + k) % 4], b_m16[(2 * t + k) % 4]
                        fw.dma(pool, None, None, reads=gbufs + [b_gix], writes=[bmm],
                               fn=lambda e, mm=mm, t=t, k=k: e.indirect_dma_start(
                                   out=mm[:, :], out_offset=None, in_=gsrc[:, :],
                                   in_offset=bass.IndirectOffsetOnAxis(ap=gix[:, t, k:k + 1], axis=0),
                                   bounds_check=fw.bound_reg(gsrc.shape[0] - 1), oob_is_err=False))
                        if k == 0:
                            fw.op(act, lambda e, mm=mm, mt=mt: e.copy(mt[:, 0:2048], mm[:, :]), reads=[bmm], writes=[bm])
                        else:
                            fw.op(dve, lambda e, mm=mm, mt=mt: e.tensor_copy(mt[:, 2048:4096], mm[:, :]), reads=[bmm], writes=[bm])
                for q in range(8):
                    pt, pb = fw.next_psum()
                    for i in range(4):
                        kc = q * 4 + i
                        fw.op(pe, lambda e, pt=pt, i=i, kc=kc, mt=mt: e.transpose(pt[:, i * 128:(i + 1) * 128], mt[:, kc * 128:(kc + 1) * 128], ident[:, :]),
                              reads=[bm, b_ident], writes=[pb])
                    eng = act if q % 2 == 0 else dve
                    if eng is act:
                        fw.op(act, lambda e, pt=pt, t=t, q=q: e.copy(mixT[:, t, q * 4:(q + 1) * 4, :].rearrange("p a b -> p (a b)"), pt[:, :]),
                              reads=[pb], writes=[b_mixT[t]])
                    else:
                        fw.op(dve, lambda e, pt=pt, t=t, q=q: e.tensor_copy(mixT[:, t, q * 4:(q + 1) * 4, :].rearrange("p a b -> p (a b)"), pt[:, :]),
                              reads=[pb], writes=[b_mixT[t]])
            ows = [fw.sb([128, 32, 256], BF16, "ow%d" % i) for i in range(2)]; b_ows = [Buf() for _ in range(2)]
            mbs = [fw.sb([128, 256], F32, "mb%d" % i) for i in range(3)]; b_mbs = [Buf() for _ in range(3)]
            cnt = 0
            for j in range(8):
                ow, bo = ows[j % 2], b_ows[j % 2]
                fw.dma(pool, ow[:], out_w[:, j * 256:(j + 1) * 256].rearrange("(kc p) c -> p kc c", p=128), writes=[bo])
                for t in range(n_tiles):
                    pt, pb = fw.next_psum()
                    for kc in range(32):
                        fw.op(pe, lambda e, pt=pt, kc=kc, t=t, ow=ow: e.matmul(pt[:, 0:256], mixT[:, t, kc, :], ow[:, kc, :], start=(kc == 0), stop=(kc == 31)),
                              reads=[b_mixT[t], bo], writes=[pb])
                    mb, bmb = mbs[cnt % 3], b_mbs[cnt % 3]; cnt += 1
                    if cnt % 2:
                        fw.op(act, lambda e, pt=pt, mb=mb: e.copy(mb[:, :], pt[:, 0:256]), reads=[pb], writes=[bmb])
                    else:
                        fw.op(dve, lambda e, pt=pt, mb=mb: e.tensor_copy(mb[:, :], pt[:, 0:256]), reads=[pb], writes=[bmb])
                    fw.dma(sp, m_all[t * 128:(t + 1) * 128, j * 256:(j + 1) * 256], mb[:, :], reads=[bmb], writes=[d_m[t]])
            fw.barrier()
        with ExitStack() as st:
            fw.stack = st
            nvar = 2 if has_ctx else 1
            g1 = []; nsc = []; sh2 = []
            b_g1 = []; b_nsc = []; b_sh2 = []
            nw = fw.sb([128, D], F32, "nw"); b_nw = Buf()
            fw.dma(sp, nw[:], bview(norm_w2), writes=[b_nw])
            for v in range(nvar):
                a = fw.sb([128, D], F32, "g1_%d" % v); ba = Buf()
                fw.dma(sp, a[:], bview(modrows[v:v + 1, 0:2048]), reads=[d_mod], writes=[ba])
                g1.append(a); b_g1.append(ba)
                s = fw.sb([128, D], F32, "sh2_%d" % v); bs = Buf()
                fw.dma(sp, s[:], bview(modrows[v:v + 1, 2048:4096]), reads=[d_mod], writes=[bs])
                sh2.append(s); b_sh2.append(bs)
                c = fw.sb([128, D], F32, "nsc_%d" % v); bc = Buf()
                fw.dma(sp, c[:], bview(modrows[v:v + 1, 4096:6144]), reads=[d_mod], writes=[bc])
                fw.op(dve, lambda e, c=c: e.scalar_tensor_tensor(c[:, :], c[:, :], 1.0, nw[:, :], ALU.add, ALU.mult), reads=[b_nw], writes=[bc])
                nsc.append(c); b_nsc.append(bc)
            gw = fw.sb([128, 16, 72], F32, "gw"); b_gw = Buf()
            fw.dma(sp, gw[:], gwew.rearrange("(kc p) c -> p kc c", p=128), writes=[b_gw])
            acum = fw.sb([128, 64], F32, "acum"); b_acum = Buf()
            fw.op(pool, lambda e: e.memset(acum[:, :], 0.0), writes=[b_acum])
            acum_bf = fw.sb([128, 64], BF16, "acum_bf"); b_acbf = Buf()
            fw.op(pool, lambda e: e.memset(acum_bf[:, :], 0.0), writes=[b_acbf])
            NB = 2
            hts = [fw.sb([128, D], F32, "ht%d" % i) for i in range(NB)]; b_hts = [Buf() for _ in range(NB)]
            mts = [fw.sb([128, D], F32, "mt%d" % i) for i in range(NB)]; b_mts = [Buf() for _ in range(NB)]
            fts = [fw.sb([128, D], F32, "ft%d" % i) for i in range(NB)]; b_fts = [Buf() for _ in range(NB)]
            fTs = [fw.sb([128, 16, 128], F32, "fT%d" % i) for i in range(NB)]; b_fTs = [Buf() for _ in range(NB)]
            sq = fw.sb([128, D], F32, "sq"); b_sq = Buf()
            sm = {}
            def S(name, shape, dt=F32):
                if name not in sm:
                    sm[name] = (fw.sb(shape, dt, "r_" + name), Buf(name))
                return sm[name]
            for t in range(n_tiles):
                v = 1 if (has_ctx and t == 0) else 0
                ht, bh = hts[t % NB], b_hts[t % NB]
                mt, bm = mts[t % NB], b_mts[t % NB]
                ft, bf = fts[t % NB], b_fts[t % NB]
                fT, bfT = fTs[t % NB], b_fTs[t % NB]
                fw.dma(sp, ht[:], h_in[t * 128:(t + 1) * 128, :], reads=h_bufs, writes=[bh])
                fw.dma(sp, mt[:], m_all[t * 128:(t + 1) * 128, :], reads=[d_m[t]], writes=[bm])
                fw.op(pool, lambda e, mt=mt, v=v: e.tensor_tensor(mt[:, :], mt[:, :], g1[v][:, :], ALU.mult), reads=[b_g1[v]], writes=[bm])
                fw.op(dve, lambda e, mt=mt, ht=ht: e.tensor_tensor(ht[:, :], mt[:, :], ht[:, :], ALU.add), reads=[bm], writes=[bh])
                fw.dma(sp, h1_all[t * 128:(t + 1) * 128, :], ht[:, :], reads=[bh], writes=[d_h1[t]])
                ssq, b_ssq = S("ssq", [128, 1])
                fw.op(act, lambda e, ht=ht: e.activation(sq[:, :], ht[:, :], AF.Square, accum_out=ssq[:, :]), reads=[bh], writes=[b_sq, b_ssq])
                rstd, b_rstd = S("rstd", [128, 1])
                fw.op(act, lambda e: e.activation(rstd[:, :], ssq[:, :], AF.Sqrt, bias=EPS, scale=1.0 / D), reads=[b_ssq], writes=[b_rstd])
                fw.op(dve, lambda e: e.reciprocal(rstd[:, :], rstd[:, :]), writes=[b_rstd])
                fw.op(dve, lambda e, ht=ht, ft=ft, v=v: e.scalar_tensor_tensor(ft[:, :], ht[:, :], rstd[:, 0:1], nsc[v][:, :], ALU.mult, ALU.mult),
                      reads=[bh, b_rstd, b_nsc[v]], writes=[bf])
                fw.op(pool, lambda e, ft=ft, v=v: e.tensor_tensor(ft[:, :], ft[:, :], sh2[v][:, :], ALU.add), reads=[b_sh2[v]], writes=[bf])
                for q in range(4):
                    pt, pb = fw.next_psum()
                    for i in range(4):
                        kc = q * 4 + i
                        fw.op(pe, lambda e, pt=pt, i=i, kc=kc, ft=ft: e.transpose(pt[:, i * 128:(i + 1) * 128], ft[:, kc * 128:(kc + 1) * 128], ident[:, :]),
                              reads=[bf, b_ident], writes=[pb])
                    if q % 2 == 0:
                        fw.op(act, lambda e, pt=pt, q=q, fT=fT: e.copy(fT[:, q * 4:(q + 1) * 4, :].rearrange("p a b -> p (a b)"), pt[:, :]), reads=[pb], writes=[bfT])
                    else:
                        fw.op(dve, lambda e, pt=pt, q=q, fT=fT: e.tensor_copy(fT[:, q * 4:(q + 1) * 4, :].rearrange("p a b -> p (a b)"), pt[:, :]), reads=[pb], writes=[bfT])
                pl, pbl = fw.next_psum()
                for kc in range(16):
                    fw.op(pe, lambda e, kc=kc, fT=fT, pl=pl: e.matmul(pl[:, 0:72], fT[:, kc, :], gw[:, kc, :], start=(kc == 0), stop=(kc == 15)),
                          reads=[bfT, b_gw], writes=[pbl])
                lg, b_lg = S("lg", [128, 72])
                fw.op(dve, lambda e, pl=pl: e.tensor_copy(lg[:, :], pl[:, 0:72]), reads=[pbl], writes=[b_lg])
                g8, b_g8 = S("g8", [128, 8])
                fw.op(dve, lambda e: e.max(g8[:, :], lg[:, 0:8]), reads=[b_lg], writes=[b_g8])
                ngm, b_ngm = S("ngm", [128, 1])
                fw.op(dve, lambda e: e.tensor_scalar(ngm[:, :], g8[:, 0:1], -1.0, None, ALU.mult), reads=[b_g8], writes=[b_ngm])
                gex, b_gex = S("gex", [128, 8]); gsum, b_gsum = S("gsum", [128, 1])
                fw.op(act, lambda e: e.activation(gex[:, :], lg[:, 0:8], AF.Exp, bias=ngm[:, 0:1], scale=1.0, accum_out=gsum[:, :]),
                      reads=[b_lg, b_ngm], writes=[b_gex, b_gsum])
                ggate, b_gg = S("ggate", [128, 1])
                fw.op(dve, lambda e: e.reciprocal(ggate[:, :], gsum[:, :]), reads=[b_gsum], writes=[b_gg])
                pen, b_pen = S("pen", [128, 8])
                fw.op(dve, lambda e: e.tensor_scalar(pen[:, :], lg[:, 0:8], g8[:, 0:1], zcol[:, 0:1], ALU.is_equal, ALU.add), reads=[b_lg, b_g8, b_z], writes=[b_pen])
                fw.op(dve, lambda e: e.tensor_scalar(pen[:, :], pen[:, :], -1.0, 1e9, ALU.add, ALU.mult), writes=[b_pen])
                lem, b_lem = S("lem", [128, 64])
                fw.op(dve, lambda e: e.tensor_tensor(lem[:, :].rearrange("p (g e) -> p g e", g=8), lg[:, 8:72].rearrange("p (g e) -> p g e", g=8),
                                                     pen[:, :].unsqueeze(2).to_broadcast([128, 8, 8]), ALU.add), reads=[b_lg, b_pen], writes=[b_lem])
                t8, b_t8 = S("t8", [128, 8]); i8, b_i8 = S("i8", [128, 8], U32)
                fw.op(dve, lambda e: e.max(t8[:, :], lem[:, :]), reads=[b_lem], writes=[b_t8])
                fw.op(dve, lambda e: e.max_index(i8[:, :], t8[:, :], lem[:, :]), reads=[b_lem, b_t8], writes=[b_i8])
                ef, b_ef = S("ef", [128, 2])
                fw.op(dve, lambda e: e.tensor_copy(ef[:, :], i8[:, 0:2]), reads=[b_i8], writes=[b_ef])
                dd, b_dd = S("dd", [128, 1])
                fw.op(dve, lambda e: e.tensor_tensor(dd[:, :], t8[:, 1:2], t8[:, 0:1], ALU.subtract), reads=[b_t8], writes=[b_dd])
                ex, b_ex = S("ex", [128, 1])
                fw.op(act, lambda e: e.activation(ex[:, :], dd[:, :], AF.Exp), reads=[b_dd], writes=[b_ex])
                den, b_den = S("den", [128, 1])
                fw.op(dve, lambda e: e.tensor_scalar(den[:, :], ex[:, :], 1.0, None, ALU.add), reads=[b_ex], writes=[b_den])
                fw.op(dve, lambda e: e.reciprocal(den[:, :], den[:, :]), writes=[b_den])
                fw.op(dve, lambda e, t=t: e.tensor_tensor(gate_all[:, t, 0:1], den[:, :], ggate[:, :], ALU.mult), reads=[b_den, b_gg], writes=[b_gate[t]])
                fw.op(dve, lambda e, t=t: e.tensor_tensor(gate_all[:, t, 1:2], gate_all[:, t, 0:1], ex[:, :], ALU.mult), reads=[b_ex], writes=[b_gate[t]])
                A0, b_A0 = S("A0", [128, 64]); A1, b_A1 = S("A1", [128, 64]); A, b_A = S("A", [128, 64]); Abf, b_Abf = S("Abf", [128, 64], BF16)
                fw.op(dve, lambda e: e.tensor_scalar(A0[:, :], iota[:, :], ef[:, 0:1], None, ALU.is_equal), reads=[b_iota, b_ef], writes=[b_A0])
                fw.op(dve, lambda e: e.tensor_scalar(A1[:, :], iota[:, :], ef[:, 1:2], None, ALU.is_equal), reads=[b_iota, b_ef], writes=[b_A1])
                fw.op(dve, lambda e: e.tensor_tensor(A[:, :], A0[:, :], A1[:, :], ALU.add), reads=[b_A0, b_A1], writes=[b_A])
                fw.op(dve, lambda e: e.tensor_copy(Abf[:, :], A[:, :]), reads=[b_A], writes=[b_Abf])
                pr, pbr = fw.next_psum()
                fw.op(pe, lambda e, pr=pr: e.matmul(pr[:, 0:64], trilt[:, :], Abf[:, :], start=True, stop=False), reads=[b_tri, b_Abf], writes=[pbr])
                fw.op(pe, lambda e, pr=pr: e.matmul(pr[:, 0:64], ones_bf[:, :], acum_bf[:, :], start=False, stop=True), reads=[b_ones, b_acbf], writes=[pbr])
                rk, b_rk = S("rk", [128, 2]); tmp, b_tmp = S("tmp", [128, 64])
                for k, (Ak, bAk) in enumerate(((A0, b_A0), (A1, b_A1))):
                    fw.op(dve, lambda e, Ak=Ak, pr=pr: e.tensor_tensor(tmp[:, :], Ak[:, :], pr[:, 0:64], ALU.mult), reads=[bAk, pbr], writes=[b_tmp])
                    fw.op(dve, lambda e, k=k: e.reduce_sum(rk[:, k:k + 1], tmp[:, :], axis=AX.X), reads=[b_tmp], writes=[b_rk])
                fw.op(dve, lambda e: e.tensor_tensor(acum[:, :], acum[:, :], A[:, :], ALU.add), reads=[b_A], writes=[b_acum])
                fw.op(dve, lambda e: e.tensor_copy(acum_bf[:, :], acum[:, :]), reads=[b_acum], writes=[b_acbf])
                df, b_df = S("df", [128, 2]); ov, b_ov = S("ov", [128, 2])
                fw.op(dve, lambda e: e.tensor_scalar(ov[:, :], rk[:, :], float(CAP), 1e6, ALU.is_ge, ALU.mult), reads=[b_rk], writes=[b_ov])
                fw.op(dve, lambda e: e.scalar_tensor_tensor(df[:, :], ef[:, :], float(CAP), rk[:, :], ALU.mult, ALU.add), reads=[b_ef, b_rk], writes=[b_df])
                fw.op(dve, lambda e: e.tensor_tensor(df[:, :], df[:, :], ov[:, :], ALU.add), reads=[b_ov], writes=[b_df])
                fw.op(dve, lambda e, t=t: e.tensor_copy(dest_all[:, t, :], df[:, :]), reads=[b_df], writes=[b_dest[t]])
                for k in range(2):
                    fw.dma(pool, None, None, reads=[bf, b_dest[t]] + d_xz, writes=[d_x],
                           fn=lambda e, t=t, k=k, ft=ft: e.indirect_dma_start(
                               out=xdisp[:, :], out_offset=bass.IndirectOffsetOnAxis(ap=dest_all[:, t, k:k + 1], axis=0),
                               in_=ft[:, :], in_offset=None, bounds_check=fw.bound_reg(NE * CAP - 1), oob_is_err=False))
            fw.barrier()
        with ExitStack() as st:
            fw.stack = st
            NW = 2
            w1s = [fw.sb([128, 16, 512], BF16, "w1s%d" % i) for i in range(NW)]; b_w1s = [Buf() for _ in range(NW)]
            w3s = [fw.sb([128, 16, 512], BF16, "w3s%d" % i) for i in range(NW)]; b_w3s = [Buf() for _ in range(NW)]
            w2s = [fw.sb([128, 4, D], BF16, "w2s%d" % i) for i in range(NW)]; b_w2s = [Buf() for _ in range(NW)]
            xes = [fw.sb([128, D], F32, "xe%d" % i) for i in range(2)]; b_xes = [Buf() for _ in range(2)]
            xTs = [fw.sb([128, 16, 128], BF16, "xT%d" % i) for i in range(2)]; b_xTs = [Buf() for _ in range(2)]
            sil = fw.sb([128, 512], F32, "sil"); b_sil = Buf()
            hact = fw.sb([128, 512], F32, "hact"); b_hact = Buf()
            hT = fw.sb([128, 4, 128], BF16, "hT"); b_hT = Buf()
            yes = [fw.sb([128, D], F32, "ye%d" % i) for i in range(2)]; b_yes = [Buf() for _ in range(2)]
            for ex_ in range(NE):
                i2 = ex_ % 2
                w1t, bw1 = w1s[ex_ % NW], b_w1s[ex_ % NW]
                w3t, bw3 = w3s[ex_ % NW], b_w3s[ex_ % NW]
                w2t, bw2 = w2s[ex_ % NW], b_w2s[ex_ % NW]
                fw.dma(pool, w1t[:], w1[ex_].rearrange("(kc p) h -> p kc h", p=128), writes=[bw1])
                fw.dma(pool, w3t[:], w3[ex_].rearrange("(kc p) h -> p kc h", p=128), writes=[bw3])
                fw.dma(pool, w2t[:], w2[ex_].rearrange("(hc p) d -> p hc d", p=128), writes=[bw2])
                xe, bxe = xes[i2], b_xes[i2]
                xT, bxT = xTs[i2], b_xTs[i2]
                fw.dma(sp, xe[:], xdisp[ex_ * CAP:(ex_ + 1) * CAP, :], reads=[d_x, d_xz[ex_ // 4]], writes=[bxe])
                for q in range(4):
                    pt, pb = fw.next_psum()
                    for i in range(4):
                        kc = q * 4 + i
                        fw.op(pe, lambda e, pt=pt, i=i, kc=kc, xe=xe: e.transpose(pt[:, i * 128:(i + 1) * 128], xe[:, kc * 128:(kc + 1) * 128], ident[:, :]),
                              reads=[bxe, b_ident], writes=[pb])
                    if q % 2 == 0:
                        fw.op(act, lambda e, pt=pt, q=q, xT=xT: e.copy(xT[:, q * 4:(q + 1) * 4, :].rearrange("p a b -> p (a b)"), pt[:, :]), reads=[pb], writes=[bxT])
                    else:
                        fw.op(dve, lambda e, pt=pt, q=q, xT=xT: e.tensor_copy(xT[:, q * 4:(q + 1) * 4, :].rearrange("p a b -> p (a b)"), pt[:, :]), reads=[pb], writes=[bxT])
                p1, pb1 = fw.next_psum()
                for kc in range(16):
                    fw.op(pe, lambda e, kc=kc, p1=p1, xT=xT, w1t=w1t: e.matmul(p1[:, :], xT[:, kc, :], w1t[:, kc, :], start=(kc == 0), stop=(kc == 15)),
                          reads=[bxT, bw1], writes=[pb1])
                p3, pb3 = fw.next_psum()
                for kc in range(16):
                    fw.op(pe, lambda e, kc=kc, p3=p3, xT=xT, w3t=w3t: e.matmul(p3[:, :], xT[:, kc, :], w3t[:, kc, :], start=(kc == 0), stop=(kc == 15)),
                          reads=[bxT, bw3], writes=[pb3])
                fw.op(act, lambda e, p1=p1: e.activation(sil[:, :], p1[:, :], AF.Silu), reads=[pb1], writes=[b_sil])
                fw.op(dve, lambda e, p3=p3: e.tensor_tensor(hact[:, :], sil[:, :], p3[:, :], ALU.mult), reads=[b_sil, pb3], writes=[b_hact])
                pt, pb = fw.next_psum()
                for hc in range(4):
                    fw.op(pe, lambda e, pt=pt, hc=hc: e.transpose(pt[:, hc * 128:(hc + 1) * 128], hact[:, hc * 128:(hc + 1) * 128], ident[:, :]),
                          reads=[b_hact, b_ident], writes=[pb])
                fw.op(act, lambda e, pt=pt: e.copy(hT[:, :, :].rearrange("p a b -> p (a b)"), pt[:, :]), reads=[pb], writes=[b_hT])
                ye, bye = yes[i2], b_yes[i2]
                for db in range(4):
                    py, pby = fw.next_psum()
                    for hc in range(4):
                        fw.op(pe, lambda e, py=py, hc=hc, db=db, w2t=w2t: e.matmul(py[:, :], hT[:, hc, :], w2t[:, hc, db * 512:(db + 1) * 512], start=(hc == 0), stop=(hc == 3)),
                              reads=[b_hT, bw2], writes=[pby])
                    if db % 2 == 0:
                        fw.op(dve, lambda e, py=py, db=db, ye=ye: e.tensor_copy(ye[:, db * 512:(db + 1) * 512], py[:, :]), reads=[pby], writes=[bye])
                    else:
                        fw.op(act, lambda e, py=py, db=db, ye=ye: e.copy(ye[:, db * 512:(db + 1) * 512], py[:, :]), reads=[pby], writes=[bye])
                fw.dma(sp, ybuf[ex_ * CAP:(ex_ + 1) * CAP, :], ye[:, :], reads=[bye], writes=[d_y[ex_]])
            fw.barrier()
        with ExitStack() as st:
            fw.stack = st
            nvar = 2 if has_ctx else 1
            g2 = []; b_g2 = []
            for v in range(nvar):
                a = fw.sb([128, D], F32, "g2_%d" % v); ba = Buf()
                fw.dma(sp, a[:], bview(modrows[v:v + 1, 6144:8192]), reads=[d_mod], writes=[ba])
                g2.append(a); b_g2.append(ba)
            r0s = [fw.sb([128, D], F32, "r0_%d" % i) for i in range(2)]; b_r0s = [Buf() for _ in range(2)]
            r1s = [fw.sb([128, D], F32, "r1_%d" % i) for i in range(2)]; b_r1s = [Buf() for _ in range(2)]
            h1s = [fw.sb([128, D], F32, "h1_%d" % i) for i in range(2)]; b_h1s = [Buf() for _ in range(2)]
            for t in range(n_tiles):
                v = 1 if (has_ctx and t == 0) else 0
                r0, br0 = r0s[t % 2], b_r0s[t % 2]
                r1, br1 = r1s[t % 2], b_r1s[t % 2]
                h1, bh1 = h1s[t % 2], b_h1s[t % 2]
                fw.dma(sp, h1[:], h1_all[t * 128:(t + 1) * 128, :], reads=[d_h1[t]], writes=[bh1])
                for k, (r, br) in enumerate(((r0, br0), (r1, br1))):
                    fw.op(pool, lambda e, r=r: e.memset(r[:, :], 0.0), writes=[br])
                    fw.dma(pool, None, None, reads=d_y + [b_dest[t]], writes=[br],
                           fn=lambda e, r=r, t=t, k=k: e.indirect_dma_start(
                               out=r[:, :], out_offset=None, in_=ybuf[:, :],
                               in_offset=bass.IndirectOffsetOnAxis(ap=dest_all[:, t, k:k + 1], axis=0),
                               bounds_check=fw.bound_reg(NE * CAP - 1), oob_is_err=False))
                fw.op(dve, lambda e, r0=r0, t=t: e.tensor_scalar(r0[:, :], r0[:, :], gate_all[:, t, 0:1], None, ALU.mult), reads=[b_gate[t]], writes=[br0])
                fw.op(dve, lambda e, r0=r0, r1=r1, t=t: e.scalar_tensor_tensor(r0[:, :], r1[:, :], gate_all[:, t, 1:2], r0[:, :], ALU.mult, ALU.add),
                      reads=[br1, b_gate[t]], writes=[br0])
                fw.op(pool, lambda e, r0=r0, v=v: e.tensor_tensor(r0[:, :], r0[:, :], g2[v][:, :], ALU.mult), reads=[b_g2[v]], writes=[br0])
                fw.op(dve, lambda e, r0=r0, h1=h1: e.tensor_tensor(h1[:, :], h1[:, :], r0[:, :], ALU.add), reads=[br0], writes=[bh1])
                if out_fn is None:
                    fw.dma(sp, h_out[t * 128:(t + 1) * 128, :], h1[:, :], reads=[bh1], writes=[d_out[t]])
                else:
                    d_out[t] = out_fn(fw, t, h1, bh1)
            if not fused:
                fw.finish(d_out)
            else:
                fw.barrier()
        print("phaseB ops", fw.n_ops, "waits", fw.n_waits)
    if fused:
        return [b for x in d_out for b in (x if isinstance(x, list) else [x])]
    return nc

from contextlib import ExitStack

NT = 18
NTOK = NT * 128
Z0, X0, B0, C0, DT0, Q0, K0, V0, NCOL = 0, 1024, 2048, 2304, 2560, 2592, 3616, 4640, 5664
TB = [(0, 256)] + [(256 + i * 512, 512) for i in range(4)]


def build_phaseA0(stop_after=None, dbg=False, ctx=None):
    fused = ctx is not None
    nc = ctx["nc"] if fused else bass.Bass("TRN2", target_bir_lowering=False)
    pre = ctx["pre"] if fused else ""
    out_fn = ctx["out_fn"] if fused else None
    I = lambda name, shape, dt=F32: nc.dram_tensor(pre + name, shape, dt, kind="ExternalInput").ap()
    h_all = I("h_all", [NTOK, D])
    cvec = I("cvec", [2, D])
    ada_w = I("ada_w", [D, 4096]); ada_b = I("ada_b", [1, 4096])
    norm_w1 = I("norm_w1", [1, D])
    w_in = I("w_in", [D, NCOL])
    convw = I("convw", [1536, 5]); convb = I("convb", [128, 12])
    dt_bias = I("dt_bias", [1, 32]); a_log = I("a_log", [1, 32]); d_skip = I("d_skip", [1, 16]); ssd_nw = I("ssd_nw", [1, 1024])
    qnw = I("qnw", [128, 1]); knw = I("knw", [128, 1])
    biasT = I("biasT", [8, 25, 128, 128])
    ident_d = I("ident", [128, 128]); ule_d = I("ule", [128, 128]); uge_d = I("uge", [128, 128])
    mix = None if fused else nc.dram_tensor("mix_part", [NTOK, 2048], F32, kind="ExternalOutput").ap()
    modrows = nc.dram_tensor(pre + "modrows", [2, 4096], F32).ap()
    Hb_d = nc.dram_tensor(pre + "Hb_d", [NT, 128, 512], BF16).ap()
    d_mod = Buf("modrows"); d_mix = []

    with ExitStack() as st0:
        if fused:
            fw = ctx["fw"]; fw.stack = st0; fw.ps_pool = list(range(8))
        else:
            fw = FW(nc, st0)
        pe, dve, act, pool, sp = fw.pe, fw.dve, fw.act, fw.pool, fw.sp
        ident, b_ident = emit_consts(fw, ident_d)
        ule = fw.sb([128, 128], F32, "ule"); b_ule = Buf(); fw.dma(sp, ule[:], ule_d, writes=[b_ule])
        uge = fw.sb([128, 128], F32, "uge"); b_uge = Buf(); fw.dma(sp, uge[:], uge_d, writes=[b_uge])
        ones = fw.sb([128, 128], F32, "ones"); b_ones = Buf(); fw.op(pool, lambda e: e.memset(ones[:, :], 1.0), writes=[b_ones])
        identb = fw.sb([128, 128], BF16, "identb"); b_identb = Buf(); fw.dma(pool, identb[:], ident_d, writes=[b_identb])
        zcol = fw.sb([128, 1], F32, "zcol"); b_z = Buf(); fw.op(pool, lambda e: e.memset(zcol[:, :], 0.0), writes=[b_z])
        aT = fw.sb([128, 16, NTOK], BF16, "aT"); b_aT = [Buf() for _ in range(NT)]
        emit_mods(fw, cvec, ada_w, ada_b, 4096, modrows, d_mod, ident, b_ident)
        emit_aT(fw, h_all, NT, 2, norm_w1, modrows, d_mod, aT, b_aT, ident, b_ident)

        def tiles_of(s, n):
            return list(range(s // 128, (s + n) // 128))

        with ExitStack() as st:
            fw.stack = st
            wdt = fw.sb([128, 16, 32], BF16, "wdt"); b_wdt = Buf()
            fw.dma(pool, wdt[:], w_in[:, DT0:DT0 + 32].rearrange("(kc p) c -> p kc c", p=128), writes=[b_wdt])
            dtb = fw.sb([128, 32], F32, "dtb"); b_dtb = Buf(); fw.dma(sp, dtb[:], bview(dt_bias), writes=[b_dtb])
            Aneg = fw.sb([128, 32], F32, "Aneg"); b_A = Buf(); fw.dma(sp, Aneg[:], bview(a_log), writes=[b_A])
            fw.op(act, lambda e: e.activation(Aneg[:, :], Aneg[:, :], AF.Exp), writes=[b_A])
            fw.op(dve, lambda e: e.tensor_scalar(Aneg[:, :], Aneg[:, :], -1.0, None, ALU.mult), writes=[b_A])
            dsk = fw.sb([128, 16], F32, "dsk"); b_dsk = Buf(); fw.dma(sp, dsk[:], bview(d_skip), writes=[b_dsk])
            snw = fw.sb([128, 1024], F32, "snw"); b_snw = Buf(); fw.dma(sp, snw[:], bview(ssd_nw), writes=[b_snw])
            cw = fw.sb([128, 12, 5], F32, "cw"); b_cw = Buf(); fw.dma(sp, cw[:], convw.rearrange("(ct p) k -> p ct k", p=128), writes=[b_cw])
            cb = fw.sb([128, 12], F32, "cb"); b_cb = Buf(); fw.dma(sp, cb[:], convb, writes=[b_cb])
            def T3(name):
                return fw.sb([128, NT, 32], F32, name), [Buf() for _ in range(NT)]
            dt_all, b_dt = T3("dt_all"); a_all, b_a = T3("a_all"); negcs, b_ncs = T3("negcs")
            dfs, b_dfs = T3("dfs"); dte, b_dte = T3("dte"); etot, b_etot = T3("etot")
            tmpA = fw.sb([128, 32], F32, "tmpA"); b_tA = Buf(); tmpB = fw.sb([128, 32], F32, "tmpB"); b_tB = Buf()
            tmpC = fw.sb([128, 32], F32, "tmpC"); b_tC = Buf()
            for t in range(NT):
                pt, pb = fw.next_psum()
                for kc in range(16):
                    fw.op(pe, lambda e, kc=kc, pt=pt, t=t: e.matmul(pt[:, 0:32], aT[:, kc, t * 128:(t + 1) * 128], wdt[:, kc, :], start=(kc == 0), stop=(kc == 15)),
                          reads=[b_aT[t], b_wdt], writes=[pb])
                fw.op(dve, lambda e, pt=pt: e.tensor_tensor(tmpA[:, :], pt[:, 0:32], dtb[:, :], ALU.add), reads=[pb, b_dtb], writes=[b_tA])
                fw.op(act, lambda e: e.activation(tmpB[:, :], tmpA[:, :], AF.Abs), reads=[b_tA], writes=[b_tB])
                fw.op(act, lambda e: e.activation(tmpB[:, :], tmpB[:, :], AF.Exp, scale=-1.0), writes=[b_tB])
                fw.op(act, lambda e: e.activation(tmpB[:, :], tmpB[:, :], AF.Ln, bias=1.0, scale=1.0), writes=[b_tB])
                fw.op(dve, lambda e: e.tensor_scalar(tmpA[:, :], tmpA[:, :], 0.0, None, ALU.max), writes=[b_tA])
                fw.op(dve, lambda e, t=t: e.tensor_tensor(dt_all[:, t, :], tmpA[:, :], tmpB[:, :], ALU.add), reads=[b_tA, b_tB], writes=[b_dt[t]])
                fw.op(dve, lambda e, t=t: e.tensor_tensor(a_all[:, t, :], dt_all[:, t, :], Aneg[:, :], ALU.mult), reads=[b_dt[t], b_A], writes=[b_a[t]])
                pc_, pbc = fw.next_psum()
                fw.op(pe, lambda e, pc_=pc_, t=t: e.matmul(pc_[:, 0:16], ule[:, :], a_all[:, t, 0:16], start=True, stop=True), reads=[b_ule, b_a[t]], writes=[pbc])
                fw.op(pe, lambda e, pc_=pc_, t=t: e.matmul(pc_[:, 16:32], uge[:, :], a_all[:, t, 16:32], start=True, stop=True), reads=[b_uge, b_a[t]], writes=[pbc])
                fw.op(pe, lambda e, pc_=pc_, t=t: e.matmul(pc_[:, 32:64], ones[:, :], a_all[:, t, :], start=True, stop=True), reads=[b_ones, b_a[t]], writes=[pbc])
                fw.op(dve, lambda e, pc_=pc_, t=t: e.tensor_scalar(negcs[:, t, :], pc_[:, 0:32], -1.0, None, ALU.mult), reads=[pbc], writes=[b_ncs[t]])
                fw.op(act, lambda e, pc_=pc_, t=t: e.activation(dfs[:, t, :], pc_[:, 0:32], AF.Exp), reads=[pbc], writes=[b_dfs[t]])
                fw.op(act, lambda e, pc_=pc_, t=t: e.activation(etot[:, t, :], pc_[:, 32:64], AF.Exp), reads=[pbc], writes=[b_etot[t]])
                fw.op(dve, lambda e, pc_=pc_, t=t: e.tensor_tensor(tmpC[:, :], pc_[:, 32:64], negcs[:, t, :], ALU.add), reads=[pbc, b_ncs[t]], writes=[b_tC])
                fw.op(act, lambda e, t=t: e.activation(dte[:, t, :], tmpC[:, :], AF.Exp), reads=[b_tC], writes=[b_dte[t]])

            xs = fw.sb([128, NT, 512], BF16, "xs"); b_xs = [Buf() for _ in range(NT)]
            Btok = fw.sb([128, NT, 128], BF16, "Btok"); b_Btok = [Buf() for _ in range(NT)]
            BT = fw.sb([128, NTOK], BF16, "BT"); b_BT = Buf()
            CT = fw.sb([128, NTOK], BF16, "CT"); b_CT = Buf()
            d_Hb = [Buf() for _ in range(NT)]
            Hbs = [fw.sb([128, 512], BF16, "Hbs%d" % i) for i in range(2)]; b_Hbs = [Buf() for _ in range(2)]
            pcs = [fw.sb([128, 2312], F32, "pc%d" % i) for i in range(1)]; b_pcs = [Buf() for _ in range(1)]
            for i in range(1):
                fw.op(pool, lambda e, i=i: e.memset(pcs[i][:, :], 0.0), writes=[b_pcs[i]])
            acc = fw.sb([128, 2308], F32, "acc"); b_acc = Buf()
            wcs = [fw.sb([128, 16, 128], BF16, "wc%d" % i) for i in range(2)]; b_wcs = [Buf() for _ in range(2)]
            wz = fw.sb([128, 16, 512], BF16, "wz"); b_wz = Buf()
            H = fw.sb([128, 512], F32, "H"); b_H = Buf()
            Hbf = fw.sb([128, 512], BF16, "Hbf"); b_Hbf = Buf()
            coef = fw.sb([128, 8], F32, "coef"); b_coef = Buf()
            Xe = fw.sb([128, 512], BF16, "Xe"); b_Xe = Buf()
            Xtf = fw.sb([128, 512], BF16, "Xtf"); b_Xtf = Buf()
            Xtb = fw.sb([128, 512], BF16, "Xtb"); b_Xtb = Buf()
            CBf = fw.sb([128, 128], F32, "CBf"); b_CBf = Buf()
            CBb = fw.sb([128, 128], F32, "CBb"); b_CBb = Buf()
            Es = [fw.sb([128, 128], F32, "E%d" % i) for i in range(3)]; b_Es = [Buf() for _ in range(3)]
            Ls = [fw.sb([128, 128], F32, "L%d" % i) for i in range(3)]; b_Ls = [Buf() for _ in range(3)]
            Ms = [fw.sb([128, 128], BF16, "M%d" % i) for i in range(3)]; b_Ms = [Buf() for _ in range(3)]
            t1 = fw.sb([128, 512], F32, "t1"); b_t1 = Buf()
            t2 = fw.sb([128, 512], F32, "t2"); b_t2 = Buf()
            sz = fw.sb([128, 512], F32, "sz"); b_sz = Buf()
            gy = fw.sb([128, 512], F32, "gy"); b_gy = Buf()
            ssq = fw.sb([128, 1], F32, "ssq_s"); b_ssq = Buf()
            outs = [fw.sb([128, 512], F32, "o%d" % i) for i in range(2)]; b_outs = [Buf() for _ in range(2)]
            wcnt = 0
            lm = 0

            def bc8(ap):
                return ap.unsqueeze(2).to_broadcast([128, 8, 64])

            def v3(ap):
                return ap.rearrange("p (h d) -> p h d", h=8)

            for g in range(1 if dbg else 2):
                hf = g * 8
                hb = 16 + g * 8
                fw.dma(pool, wz[:], w_in[:, Z0 + g * 512:Z0 + (g + 1) * 512].rearrange("(kc p) c -> p kc c", p=128), writes=[b_wz])
                for ci in range(6):
                    if ci < 4:
                        col0 = X0 + g * 512 + ci * 128; ct = g * 4 + ci
                    elif ci == 4:
                        col0 = B0 + g * 128; ct = 8 + g
                    else:
                        col0 = C0 + g * 128; ct = 10 + g
                    wc, bwc = wcs[wcnt % 2], b_wcs[wcnt % 2]
                    pc, bpc = pcs[0], b_pcs[0]
                    wcnt += 1
                    fw.dma(pool, wc[:], w_in[:, col0:col0 + 128].rearrange("(kc p) c -> p kc c", p=128), writes=[bwc])
                    for (s0, n) in TB:
                        pt, pb = fw.next_psum()
                        for kc in range(16):
                            fw.op(pe, lambda e, kc=kc, pt=pt, wc=wc, s0=s0, n=n: e.matmul(pt[:, 0:n], wc[:, kc, :], aT[:, kc, s0:s0 + n], start=(kc == 0), stop=(kc == 15)),
                                  reads=[bwc] + [b_aT[t] for t in tiles_of(s0, n)], writes=[pb])
                        off = 2 if s0 == 0 else 6
                        fw.op(act, lambda e, pt=pt, pc=pc, s0=s0, n=n, off=off: e.copy(pc[:, s0 + off:s0 + off + n], pt[:, 0:n]), reads=[pb], writes=[bpc])
                    fw.op(dve, lambda e, pc=pc, ct=ct: e.tensor_scalar(acc[:, :], pc[:, 0:2308], cw[:, ct, 0:1], cb[:, ct:ct + 1], ALU.mult, ALU.add),
                          reads=[bpc, b_cw, b_cb], writes=[b_acc])
                    for k in range(1, 5):
                        eng = dve
                        fw.op(eng, lambda e, pc=pc, ct=ct, k=k: e.scalar_tensor_tensor(acc[:, :], pc[:, k:k + 2308], cw[:, ct, k:k + 1], acc[:, :], ALU.mult, ALU.add),
                              reads=[bpc, b_cw], writes=[b_acc])
                    if ci == 5:
                        fw.op(act, lambda e: e.activation(CT[:, 0:256], acc[:, 0:256], AF.Silu), reads=[b_acc], writes=[b_CT])
                        fw.op(act, lambda e: e.activation(CT[:, 256:NTOK], acc[:, 260:2308], AF.Silu), reads=[b_acc], writes=[b_CT])
                        continue
                    fw.op(act, lambda e: e.activation(acc[:, 0:256], acc[:, 0:256], AF.Silu), writes=[b_acc])
                    fw.op(act, lambda e: e.activation(acc[:, 260:2308], acc[:, 260:2308], AF.Silu), writes=[b_acc])
                    if ci == 4:
                        fw.op(pool, lambda e: e.tensor_copy(BT[:, 0:256], acc[:, 0:256]), reads=[b_acc], writes=[b_BT])
                        fw.op(pool, lambda e: e.tensor_copy(BT[:, 256:NTOK], acc[:, 260:2308]), reads=[b_acc], writes=[b_BT])
                    for q in range(5):
                        ts = list(range(q * 4, min(NT, q * 4 + 4)))
                        pt, pb = fw.next_psum()
                        for i, t in enumerate(ts):
                            a0 = t * 128 if t < 2 else 260 + (t - 2) * 128
                            fw.op(pe, lambda e, pt=pt, i=i, a0=a0: e.transpose(pt[:, i * 128:(i + 1) * 128], acc[:, a0:a0 + 128], ident[:, :]),
                                  reads=[b_acc, b_ident], writes=[pb])
                        n = len(ts)
                        src = pt[:, 0:n * 128].rearrange("p (a b) -> p a b", a=n)
                        if ci < 4:
                            dstv = xs[:, ts[0]:ts[0] + n, ci * 128:(ci + 1) * 128]; bl = [b_xs[t] for t in ts]
                        else:
                            dstv = Btok[:, ts[0]:ts[0] + n, :]; bl = [b_Btok[t] for t in ts]
                        if q % 2 == 0:
                            fw.op(dve, lambda e, dstv=dstv, src=src: e.tensor_copy(dstv, src), reads=[pb], writes=bl)
                        else:
                            fw.op(act, lambda e, dstv=dstv, src=src: e.copy(dstv, src), reads=[pb], writes=bl)

                fw.op(pool, lambda e: e.memset(H[:, :], 0.0), writes=[b_H])
                border = [1, 0] + list(range(17, 1, -1))
                for bi_, c in enumerate(border):
                    hs_, bhs_ = Hbs[bi_ % 2], b_Hbs[bi_ % 2]
                    fw.op(act, lambda e, hs_=hs_: e.copy(hs_[:, :], H[:, :]), reads=[b_H], writes=[bhs_])
                    fw.dma(sp, Hb_d[c], hs_[:, :], reads=[bhs_], writes=[d_Hb[c]])
                    fw.op(dve, lambda e, c=c: e.tensor_tensor(coef[:, :], dt_all[:, c, hb:hb + 8], dte[:, c, hb:hb + 8], ALU.mult), reads=[b_dt[c], b_dte[c]], writes=[b_coef])
                    fw.op(dve, lambda e, c=c: e.tensor_tensor(v3(Xe[:, :]), v3(xs[:, c, :]), bc8(coef[:, :]), ALU.mult), reads=[b_xs[c], b_coef], writes=[b_Xe])
                    ps, pbs = fw.next_psum()
                    fw.op(pe, lambda e, ps=ps, c=c: e.matmul(ps[:, :], Btok[:, c, :], Xe[:, :], start=True, stop=True), reads=[b_Btok[c], b_Xe], writes=[pbs])
                    fw.op(pool, lambda e, c=c: e.tensor_tensor(v3(H[:, :]), v3(H[:, :]), bc8(etot[:, c, hb:hb + 8]), ALU.mult), reads=[b_etot[c]], writes=[b_H])
                    fw.op(dve, lambda e, ps=ps: e.tensor_tensor(H[:, :], H[:, :], ps[:, :], ALU.add), reads=[pbs], writes=[b_H])

                fw.op(pool, lambda e: e.memset(H[:, :], 0.0), writes=[b_H])
                for c in range(NT):
                    cs_ = slice(c * 128, (c + 1) * 128)
                    hs_, bhs_ = Hbs[c % 2], b_Hbs[c % 2]
                    fw.dma(sp, hs_[:, :], Hb_d[c], reads=[d_Hb[c]], writes=[bhs_])
                    fw.op(act, lambda e: e.copy(Hbf[:, :], H[:, :]), reads=[b_H], writes=[b_Hbf])
                    fw.op(dve, lambda e, c=c: e.tensor_tensor(coef[:, :], dt_all[:, c, hf:hf + 8], dte[:, c, hf:hf + 8], ALU.mult), reads=[b_dt[c], b_dte[c]], writes=[b_coef])
                    fw.op(dve, lambda e, c=c: e.tensor_tensor(v3(Xe[:, :]), v3(xs[:, c, :]), bc8(coef[:, :]), ALU.mult), reads=[b_xs[c], b_coef], writes=[b_Xe])
                    fw.op(pool, lambda e, c=c: e.tensor_tensor(v3(Xtf[:, :]), v3(xs[:, c, :]), bc8(dt_all[:, c, hf:hf + 8]), ALU.mult), reads=[b_xs[c], b_dt[c]], writes=[b_Xtf])
                    fw.op(pool, lambda e, c=c: e.tensor_tensor(v3(Xtb[:, :]), v3(xs[:, c, :]), bc8(dt_all[:, c, hb:hb + 8]), ALU.mult), reads=[b_xs[c], b_dt[c]], writes=[b_Xtb])
                    pyf, pbyf = fw.next_psum()
                    fw.op(pe, lambda e, pyf=pyf, cs_=cs_: e.matmul(pyf[:, :], CT[:, cs_], Hbf[:, :], start=True, stop=True), reads=[b_CT, b_Hbf], writes=[pbyf])
                    pyb, pbyb = fw.next_psum()
                    fw.op(pe, lambda e, pyb=pyb, cs_=cs_, hs_=hs_: e.matmul(pyb[:, :], CT[:, cs_], hs_[:, :], start=True, stop=True), reads=[b_CT, bhs_], writes=[pbyb])
                    fw.op(dve, lambda e, pyf=pyf, c=c: e.tensor_tensor(v3(t1[:, :]), v3(pyf[:, :]), bc8(dfs[:, c, hf:hf + 8]), ALU.mult), reads=[pbyf, b_dfs[c]], writes=[b_t1])
                    fw.op(dve, lambda e, pyb=pyb, c=c: e.tensor_tensor(v3(t2[:, :]), v3(pyb[:, :]), bc8(dfs[:, c, hb:hb + 8]), ALU.mult), reads=[pbyb, b_dfs[c]], writes=[b_t2])
                    fw.op(pool, lambda e: e.tensor_tensor(t1[:, :], t1[:, :], t2[:, :], ALU.add), reads=[b_t2], writes=[b_t1])
                    fw.op(pool, lambda e, c=c: e.tensor_tensor(v3(t2[:, :]), v3(xs[:, c, :]), bc8(dsk[:, g * 8:(g + 1) * 8]), ALU.mult), reads=[b_xs[c], b_dsk], writes=[b_t2])
                    fw.op(pool, lambda e: e.tensor_tensor(t1[:, :], t1[:, :], t2[:, :], ALU.add), reads=[b_t2], writes=[b_t1])
                    ps, pbs = fw.next_psum()
                    fw.op(pe, lambda e, ps=ps, c=c: e.matmul(ps[:, :], Btok[:, c, :], Xe[:, :], start=True, stop=True), reads=[b_Btok[c], b_Xe], writes=[pbs])
                    fw.op(pool, lambda e, c=c: e.tensor_tensor(v3(H[:, :]), v3(H[:, :]), bc8(etot[:, c, hf:hf + 8]), ALU.mult), reads=[b_etot[c], b_Hbf], writes=[b_H])
                    fw.op(dve, lambda e, ps=ps: e.tensor_tensor(H[:, :], H[:, :], ps[:, :], ALU.add), reads=[pbs], writes=[b_H])
                    pcb, pbcb = fw.next_psum()
                    fw.op(pe, lambda e, pcb=pcb, cs_=cs_: e.matmul(pcb[:, 0:128], BT[:, cs_], CT[:, cs_], start=True, stop=True), reads=[b_BT, b_CT], writes=[pbcb])
                    fw.op(dve, lambda e, pcb=pcb: e.tensor_tensor(CBf[:, :], pcb[:, 0:128], ule[:, :], ALU.mult), reads=[pbcb, b_ule], writes=[b_CBf])
                    fw.op(dve, lambda e, pcb=pcb: e.tensor_tensor(CBb[:, :], pcb[:, 0:128], uge[:, :], ALU.mult), reads=[pbcb, b_uge], writes=[b_CBb])
                    pyd, pbyd = fw.next_psum()
                    for hq in range(4):
                        pr, pbr = fw.next_psum()
                        combos = [(h, d_) for h in (2 * hq, 2 * hq + 1) for d_ in (0, 1)]
                        for i, (h, d_) in enumerate(combos):
                            col = (hf if d_ == 0 else hb) + h
                            U = ule if d_ == 0 else uge
                            bU = b_ule if d_ == 0 else b_uge
                            fw.op(pe, lambda e, pr=pr, i=i, c=c, col=col, U=U: e.matmul(pr[:, i * 128:(i + 1) * 128], a_all[:, c, col:col + 1].to_broadcast([128, 128]), U[:, :], start=True, stop=True),
                                  reads=[b_a[c], bU], writes=[pbr])
                        for i, (h, d_) in enumerate(combos):
                            col = (hf if d_ == 0 else hb) + h
                            E_, bE = Es[lm % 3], b_Es[lm % 3]; L_, bL = Ls[lm % 3], b_Ls[lm % 3]; M_, bM = Ms[lm % 3], b_Ms[lm % 3]; lm += 1
                            CB_, bCB = (CBf, b_CBf) if d_ == 0 else (CBb, b_CBb)
                            Xt_, bXt = (Xtf, b_Xtf) if d_ == 0 else (Xtb, b_Xtb)
                            fw.op(dve, lambda e, pr=pr, i=i, c=c, col=col, E_=E_: e.tensor_scalar(E_[:, :], pr[:, i * 128:(i + 1) * 128], negcs[:, c, col:col + 1], zcol[:, 0:1], ALU.add, ALU.min),
                                  reads=[pbr, b_ncs[c], b_z], writes=[bE])
                            fw.op(act, lambda e, E_=E_, L_=L_: e.activation(L_[:, :], E_[:, :], AF.Exp), reads=[bE], writes=[bL])
                            fw.op(pool, lambda e, L_=L_, M_=M_, CB_=CB_: e.tensor_tensor(M_[:, :], L_[:, :], CB_[:, :], ALU.mult), reads=[bL, bCB], writes=[bM])
                            fw.op(pe, lambda e, pyd=pyd, h=h, M_=M_, Xt_=Xt_, d_=d_: e.matmul(pyd[:, h * 64:(h + 1) * 64], M_[:, :], Xt_[:, h * 64:(h + 1) * 64], start=(d_ == 0), stop=(d_ == 1)),
                                  reads=[bM, bXt], writes=[pbyd])
                    if dbg and c == NT - 1:
                        dd = {}
                        for nm, pp, pbb in (("pyd", pyd, pbyd),):
                            tt = fw.sb([128, 512], F32, "dbg_" + nm); bb = Buf()
                            fw.op(dve, lambda e, tt=tt, pp=pp: e.tensor_copy(tt[:, :], pp[:, :]), reads=[pbb], writes=[bb])
                            dd[nm] = (tt, bb)
                        fw.dbgd = dd
                    fw.op(dve, lambda e, pyd=pyd: e.tensor_tensor(t1[:, :], t1[:, :], pyd[:, :], ALU.add), reads=[pbyd], writes=[b_t1])
                    pz, pbz = fw.next_psum()
                    for kc in range(16):
                        fw.op(pe, lambda e, kc=kc, pz=pz, cs_=cs_: e.matmul(pz[:, :], aT[:, kc, cs_], wz[:, kc, :], start=(kc == 0), stop=(kc == 15)),
                              reads=[b_aT[c], b_wz], writes=[pbz])
                    fw.op(act, lambda e, pz=pz: e.activation(sz[:, :], pz[:, :], AF.Silu), reads=[pbz], writes=[b_sz])
                    fw.op(dve, lambda e: e.tensor_tensor(gy[:, :], t1[:, :], sz[:, :], ALU.mult), reads=[b_t1, b_sz], writes=[b_gy])
                    fw.op(act, lambda e: e.activation(t2[:, :], gy[:, :], AF.Square, accum_out=ssq[:, :]), reads=[b_gy], writes=[b_t2, b_ssq])
                    fw.op(act, lambda e: e.activation(ssq[:, :], ssq[:, :], AF.Sqrt, bias=EPS, scale=1.0 / 512), writes=[b_ssq])
                    fw.op(dve, lambda e: e.reciprocal(ssq[:, :], ssq[:, :]), writes=[b_ssq])
                    o_, bo = outs[c % 2], b_outs[c % 2]
                    fw.op(dve, lambda e, o_=o_: e.scalar_tensor_tensor(o_[:, :], gy[:, :], ssq[:, 0:1], snw[:, g * 512:(g + 1) * 512], ALU.mult, ALU.mult),
                          reads=[b_gy, b_ssq, b_snw], writes=[bo])
                    if fused:
                        d_mix.extend(out_fn(fw, c * 128, g * 512, 512, o_, bo))
                    else:
                        db = Buf(); d_mix.append(db)
                        fw.dma(sp, mix[c * 128:(c + 1) * 128, g * 512:(g + 1) * 512], o_[:, :], reads=[bo], writes=[db])
            if dbg:
                def dump(name, ap, shape, dt, bufs):
                    o = nc.dram_tensor("dbg_" + name, shape, dt, kind="ExternalOutput").ap()
                    db = Buf(); d_mix.append(db)
                    fw.dma(sp, o, ap, reads=bufs, writes=[db])
                dump("aT0", aT[:, 0, :], [128, NTOK], BF16, b_aT)
                dump("dt", dt_all[:, :, :], [128, NT, 32], F32, b_dt)
                dump("negcs", negcs[:, :, :], [128, NT, 32], F32, b_ncs)
                dump("dfs", dfs[:, :, :], [128, NT, 32], F32, b_dfs)
                dump("dte", dte[:, :, :], [128, NT, 32], F32, b_dte)
                dump("etot", etot[:, :, :], [128, NT, 32], F32, b_etot)
                dump("xs", xs[:, :, :], [128, NT, 512], BF16, b_xs)
                dump("Btok", Btok[:, :, :], [128, NT, 128], BF16, b_Btok)
                dump("BT", BT[:, :], [128, NTOK], BF16, [b_BT])
                dump("CT", CT[:, :], [128, NTOK], BF16, [b_CT])
                dump("H", H[:, :], [128, 512], F32, [b_H])
                dump("t1", t1[:, :], [128, 512], F32, [b_t1])
                dump("gy", gy[:, :], [128, 512], F32, [b_gy])
                for nm, (tt, bb) in fw.dbgd.items():
                    dump(nm, tt[:, :], [128, 512], F32, [bb])
                dump("CBf", CBf[:, :], [128, 128], F32, [b_CBf])
                dump("CBb", CBb[:, :], [128, 128], F32, [b_CBb])
                dump("Elast", Es[(lm - 1) % 3][:, :], [128, 128], F32, [b_Es[(lm - 1) % 3]])
                dump("Llast", Ls[(lm - 1) % 3][:, :], [128, 128], F32, [b_Ls[(lm - 1) % 3]])
                dump("Mlast", Ms[(lm - 1) % 3][:, :], [128, 128], BF16, [b_Ms[(lm - 1) % 3]])
                dump("Xtf", Xtf[:, :], [128, 512], BF16, [b_Xtf])
                dump("Xtb", Xtb[:, :], [128, 512], BF16, [b_Xtb])
            fw.barrier()

        if stop_after != "ssd":
          with ExitStack() as st:
            fw.stack = st
            V1 = fw.sb([128, NT, 8, 129], BF16, "V1"); b_V1 = [Buf() for _ in range(NT)]
            fw.op(pool, lambda e: e.memset(V1[:, :, :, :].rearrange("p a b c -> p (a b c)"), 1.0), writes=b_V1)
            with ExitStack() as st2:
                fw.stack = st2
                wv = fw.sb([128, 16, 1024], BF16, "wv"); b_wv = Buf()
                fw.dma(pool, wv[:], w_in[:, V0:V0 + 1024].rearrange("(kc p) c -> p kc c", p=128), writes=[b_wv])
                for t in range(NT):
                    for hh_ in range(2):
                        pt, pb = fw.next_psum()
                        for kc in range(16):
                            fw.op(pe, lambda e, kc=kc, pt=pt, t=t, hh_=hh_: e.matmul(pt[:, :], aT[:, kc, t * 128:(t + 1) * 128], wv[:, kc, hh_ * 512:(hh_ + 1) * 512], start=(kc == 0), stop=(kc == 15)),
                                  reads=[b_aT[t], b_wv], writes=[pb])
                        dstv = V1[:, t, hh_ * 4:(hh_ + 1) * 4, 0:128]
                        src = pt[:, :].rearrange("p (a b) -> p a b", a=4)
                        if hh_ == 0:
                            fw.op(act, lambda e, dstv=dstv, src=src: e.copy(dstv, src), reads=[pb], writes=[b_V1[t]])
                        else:
                            fw.op(dve, lambda e, dstv=dstv, src=src: e.tensor_copy(dstv, src), reads=[pb], writes=[b_V1[t]])
                fw.barrier()
            fw.stack = st
            qw = fw.sb([128, 1], F32, "qw"); b_qw = Buf(); fw.dma(sp, qw[:], qnw, writes=[b_qw])
            fw.op(dve, lambda e: e.tensor_scalar(qw[:, :], qw[:, :], float(128 ** -0.5), None, ALU.mult), writes=[b_qw])
            kw = fw.sb([128, 1], F32, "kw"); b_kw = Buf(); fw.dma(sp, kw[:], knw, writes=[b_kw])
            qTs = [fw.sb([128, NTOK], BF16, "qT%d" % i) for i in range(2)]; b_qTs = [Buf() for _ in range(2)]
            kTs = [fw.sb([128, NTOK], BF16, "kT%d" % i) for i in range(2)]; b_kTs = [Buf() for _ in range(2)]
            wqs = [fw.sb([128, 16, 128], BF16, "wq%d" % i) for i in range(2)]; b_wqs = [Buf() for _ in range(2)]
            wks = [fw.sb([128, 16, 128], BF16, "wk%d" % i) for i in range(2)]; b_wks = [Buf() for _ in range(2)]
            bts = [fw.sb([128, 25, 128], BF16, "bt%d" % i) for i in range(2)]; b_bts = [Buf() for _ in range(2)]
            sqs = [fw.sb([128, 512], F32, "sq%d" % i) for i in range(2)]; b_sqs = [Buf() for _ in range(2)]
            rs = [fw.sb([128, 512], F32, "rs%d" % i) for i in range(2)]; b_rs = [Buf() for _ in range(2)]
            PTs = [fw.sb([128, 7, 128], BF16, "PT%d" % i) for i in range(3)]; b_PTs = [Buf() for _ in range(3)]
            rec = [fw.sb([128, 1], F32, "rec%d" % i) for i in range(2)]; b_rec = [Buf() for _ in range(2)]
            ons = [fw.sb([128, 128], F32, "on%d" % i) for i in range(3)]; b_ons = [Buf() for _ in range(3)]
            cnt = 0; pcnt = 0
            for hd in range(8):
                i2 = hd % 2
                qT, bqT = qTs[i2], b_qTs[i2]; kT, bkT = kTs[i2], b_kTs[i2]
                wq, bwq = wqs[i2], b_wqs[i2]; wk, bwk = wks[i2], b_wks[i2]
                bt, bbt = bts[i2], b_bts[i2]
                fw.dma(pool, wq[:], w_in[:, Q0 + hd * 128:Q0 + (hd + 1) * 128].rearrange("(kc p) c -> p kc c", p=128), writes=[bwq])
                fw.dma(pool, wk[:], w_in[:, K0 + hd * 128:K0 + (hd + 1) * 128].rearrange("(kc p) c -> p kc c", p=128), writes=[bwk])
                fw.dma(pool, bt[:], biasT[hd].rearrange("c k q -> k c q"), writes=[bbt])
                for (wt_, bwt_, dstT, bdst, nwc, bnw) in ((wq, bwq, qT, bqT, qw, b_qw), (wk, bwk, kT, bkT, kw, b_kw)):
                    for (s0, n) in TB:
                        pt, pb = fw.next_psum()
                        for kc in range(16):
                            fw.op(pe, lambda e, kc=kc, pt=pt, wt_=wt_, s0=s0, n=n: e.matmul(pt[:, 0:n], wt_[:, kc, :], aT[:, kc, s0:s0 + n], start=(kc == 0), stop=(kc == 15)),
                                  reads=[bwt_] + [b_aT[t] for t in tiles_of(s0, n)], writes=[pb])
                        sq_, bsq = sqs[cnt % 2], b_sqs[cnt % 2]; r_, br = rs[cnt % 2], b_rs[cnt % 2]; cnt += 1
                        fw.op(act, lambda e, pt=pt, sq_=sq_, n=n: e.activation(sq_[:, 0:n], pt[:, 0:n], AF.Square), reads=[pb], writes=[bsq])
                        p2, pb2 = fw.next_psum()
                        fw.op(pe, lambda e, p2=p2, sq_=sq_, n=n: e.matmul(p2[:, 0:n], ones[:, :], sq_[:, 0:n], start=True, stop=True), reads=[b_ones, bsq], writes=[pb2])
                        fw.op(act, lambda e, p2=p2, r_=r_, n=n: e.activation(r_[:, 0:n], p2[:, 0:n], AF.Sqrt, bias=EPS, scale=1.0 / 128), reads=[pb2], writes=[br])
                        fw.op(dve, lambda e, r_=r_, n=n: e.reciprocal(r_[:, 0:n], r_[:, 0:n]), writes=[br])
                        fw.op(dve, lambda e, pt=pt, r_=r_, n=n: e.tensor_tensor(r_[:, 0:n], pt[:, 0:n], r_[:, 0:n], ALU.mult), reads=[pb], writes=[br])
                        fw.op(pool, lambda e, r_=r_, n=n, s0=s0, dstT=dstT, nwc=nwc: e.tensor_scalar(dstT[:, s0:s0 + n], r_[:, 0:n], nwc[:, 0:1], None, ALU.mult),
                              reads=[br, bnw], writes=[bdst])
                for tq in range(NT):
                    if tq < 2:
                        keys = [(0, None), (1, None)]
                    else:
                        j = tq - 2
                        a = min(max(2 * j - 4, 0), 22)
                        cls = 0 if j == 0 else 1 if j == 1 else 3 if j == 14 else 4 if j == 15 else 2
                        keys = [(2 + a // 2 + i, cls * 5 + i) for i in range(5)] + [(0, None), (1, None)]
                    PT, bPT = PTs[pcnt % 3], b_PTs[pcnt % 3]
                    on, bon = ons[pcnt % 3], b_ons[pcnt % 3]
                    rc, brc = rec[pcnt % 2], b_rec[pcnt % 2]; pcnt += 1
                    nk = len(keys)
                    banks = []
                    for b0 in range(0, nk, 4):
                        pS, pbS = fw.next_psum()
                        grp = keys[b0:b0 + 4]
                        for i, (kt, bi) in enumerate(grp):
                            fw.op(pe, lambda e, pS=pS, i=i, kt=kt, tq=tq, bi=bi: e.matmul(pS[:, i * 128:(i + 1) * 128], kT[:, kt * 128:(kt + 1) * 128], qT[:, tq * 128:(tq + 1) * 128], start=True, stop=(bi is None)),
                                  reads=[bkT, bqT], writes=[pbS])
                            if bi is not None:
                                fw.op(pe, lambda e, pS=pS, i=i, bi=bi: e.matmul(pS[:, i * 128:(i + 1) * 128], identb[:, :], bt[:, bi, :], start=False, stop=True),
                                      reads=[b_identb, bbt], writes=[pbS])
                        ng = len(grp)
                        fw.op(act, lambda e, pS=pS, PT=PT, b0=b0, ng=ng: e.activation(PT[:, b0:b0 + ng, :].rearrange("p a b -> p (a b)"), pS[:, 0:ng * 128], AF.Exp),
                              reads=[pbS], writes=[bPT])
                    pO, pbO = fw.next_psum()
                    for i, (kt, bi) in enumerate(keys):
                        fw.op(pe, lambda e, pO=pO, i=i, kt=kt, PT=PT: e.matmul(pO[:, 0:129], PT[:, i, :], V1[:, kt, hd, :], start=(i == 0), stop=(i == nk - 1)),
                              reads=[bPT, b_V1[kt]], writes=[pbO])
                    fw.op(dve, lambda e, pO=pO, rc=rc: e.reciprocal(rc[:, :], pO[:, 128:129]), reads=[pbO], writes=[brc])
                    fw.op(dve, lambda e, pO=pO, rc=rc, on=on: e.tensor_scalar(on[:, :], pO[:, 0:128], rc[:, 0:1], None, ALU.mult), reads=[pbO, brc], writes=[bon])
                    if fused:
                        d_mix.extend(out_fn(fw, tq * 128, 1024 + hd * 128, 128, on, bon))
                    else:
                        db = Buf(); d_mix.append(db)
                        fw.dma(sp, mix[tq * 128:(tq + 1) * 128, 1024 + hd * 128:1024 + (hd + 1) * 128], on[:, :], reads=[bon], writes=[db])
            fw.barrier()
        if not fused:
            fw.finish(d_mix)
        print("phaseA0 ops", fw.n_ops, "waits", fw.n_waits)
    return d_mix if fused else nc

import math
from contextlib import ExitStack

NT = 18
NTOK = NT * 128
NLAT = 2048
GQ0, DQ0, GK0, DK0, GV0, DV0, NCOL1 = 0, 1024, 2048, 2304, 3328, 3584, 4608
TB = [(0, 256)] + [(256 + i * 512, 512) for i in range(4)]
LAMBDA_INIT = 0.8 - 0.6 * math.exp(-0.3 * 1)


def build_phaseA1(ctx=None):
    fused = ctx is not None
    nc = ctx["nc"] if fused else bass.Bass("TRN2", target_bir_lowering=False)
    pre = ctx["pre"] if fused else ""
    out_fn = ctx["out_fn"] if fused else None
    I = lambda name, shape, dt=F32: nc.dram_tensor(pre + name, shape, dt, kind="ExternalInput").ap()
    h_src = ctx.get("h_src") if fused else None
    h_all = I("h_all", [NTOK, D]) if h_src is None else h_src[0]
    h_bufs = () if h_src is None else h_src[1]
    row_of = None if h_src is None else h_src[2]
    cvec = I("cvec", [2, D])
    ada_w = I("ada_w", [D, 4096]); ada_b = I("ada_b", [1, 4096])
    norm_w1 = I("norm_w1", [1, D])
    w_in = I("w_in", [D, NCOL1])
    nws = I("nws", [128, 4])
    lamv = I("lamv", [1, 512])
    subln = I("subln", [1, 256])
    cosT_d = I("cosT", [128, NLAT]); sinT_d = I("sinT", [128, NLAT]); rmT_d = I("rmT", [128, 128])
    ident_d = I("ident", [128, 128])
    mix = None if fused else nc.dram_tensor("mix_part", [NLAT, 2048], F32, kind="ExternalOutput").ap()
    modrows = nc.dram_tensor(pre + "modrows", [2, 4096], F32).ap()
    d_mod = Buf("modrows"); d_mix = []

    with ExitStack() as st0:
        if fused:
            fw = ctx["fw"]; fw.stack = st0; fw.ps_pool = list(range(8))
        else:
            fw = FW(nc, st0)
        pe, dve, act, pool, sp = fw.pe, fw.dve, fw.act, fw.pool, fw.sp
        ident, b_ident = emit_consts(fw, ident_d)
        ones = fw.sb([128, 128], F32, "ones"); b_ones = Buf(); fw.op(pool, lambda e: e.memset(ones[:, :], 1.0), writes=[b_ones])
        aT = fw.sb([128, 16, NTOK], BF16, "aT"); b_aT = [Buf() for _ in range(NT)]
        emit_mods(fw, cvec, ada_w, ada_b, 4096, modrows, d_mod, ident, b_ident)
        emit_aT(fw, h_all, NT, 2, norm_w1, modrows, d_mod, aT, b_aT, ident, b_ident, row_of=row_of, h_bufs=h_bufs)

        def tiles_of(s, n):
            return list(range(s // 128, (s + n) // 128))

        with ExitStack() as st:
            fw.stack = st
            V1g = fw.sb([128, NT, 2, 129], BF16, "V1g"); V1d = fw.sb([128, NT, 4, 257], BF16, "V1d"); b_V = [Buf() for _ in range(NT)]
            fw.op(pool, lambda e: e.memset(V1g[:, :, :, :].rearrange("p a b c -> p (a b c)"), 1.0), writes=b_V)
            fw.op(pool, lambda e: e.memset(V1d[:, :, :, :].rearrange("p a b c -> p (a b c)"), 1.0), writes=b_V)
            with ExitStack() as st2:
                fw.stack = st2
                wv = fw.sb([128, 16, 1280], BF16, "wv"); b_wv = Buf()
                fw.dma(pool, wv[:], w_in[:, GV0:GV0 + 1280].rearrange("(kc p) c -> p kc c", p=128), writes=[b_wv])
                for t in range(NT):
                    for part in range(3):
                        c0 = part * 512; n = 512 if part < 2 else 256
                        pt, pb = fw.next_psum()
                        for kc in range(16):
                            fw.op(pe, lambda e, kc=kc, pt=pt, t=t, c0=c0, n=n: e.matmul(pt[:, 0:n], aT[:, kc, t * 128:(t + 1) * 128], wv[:, kc, c0:c0 + n], start=(kc == 0), stop=(kc == 15)),
                                  reads=[b_aT[t], b_wv], writes=[pb])
                        if part == 0:
                            fw.op(act, lambda e, pt=pt, t=t: e.copy(V1g[:, t, :, 0:128], pt[:, 0:256].rearrange("p (a b) -> p a b", a=2)), reads=[pb], writes=[b_V[t]])
                            fw.op(dve, lambda e, pt=pt, t=t: e.tensor_copy(V1d[:, t, 0, 0:256], pt[:, 256:512]), reads=[pb], writes=[b_V[t]])
                        elif part == 1:
                            fw.op(act, lambda e, pt=pt, t=t: e.copy(V1d[:, t, 1:3, 0:256], pt[:, 0:512].rearrange("p (a b) -> p a b", a=2)), reads=[pb], writes=[b_V[t]])
                        else:
                            fw.op(dve, lambda e, pt=pt, t=t: e.tensor_copy(V1d[:, t, 3, 0:256], pt[:, 0:256]), reads=[pb], writes=[b_V[t]])
                fw.barrier()
            fw.stack = st
            fw.ps_pool = [0, 1, 2, 3]
            cosT = fw.sb([128, NLAT], F32, "cosT"); b_cos = Buf(); fw.dma(sp, cosT[:], cosT_d, writes=[b_cos])
            sinT = fw.sb([128, NLAT], F32, "sinT"); b_sin = Buf(); fw.dma(sp, sinT[:], sinT_d, writes=[b_sin])
            rmT = fw.sb([128, 128], F32, "rmT"); b_rm = Buf(); fw.dma(sp, rmT[:], rmT_d, writes=[b_rm])
            nw4 = fw.sb([128, 4], F32, "nw4"); b_nw4 = Buf(); fw.dma(sp, nw4[:], nws, writes=[b_nw4])
            nwq = fw.sb([128, 4], F32, "nwq"); b_nwq = Buf()
            fw.op(dve, lambda e: e.tensor_scalar(nwq[:, :], nw4[:, :], float(128 ** -0.5), None, ALU.mult), reads=[b_nw4], writes=[b_nwq])
            sub = fw.sb([128, 256], F32, "sub"); b_sub = Buf(); fw.dma(sp, sub[:], bview(subln), writes=[b_sub])
            fw.op(dve, lambda e: e.tensor_scalar(sub[:, :], sub[:, :], float(1.0 - LAMBDA_INIT), None, ALU.mult), writes=[b_sub])
            lv = fw.sb([128, 512], F32, "lv"); b_lv = Buf(); fw.dma(sp, lv[:], bview(lamv), writes=[b_lv])
            lt = fw.sb([128, 256], F32, "lt"); b_lt = Buf()
            fw.op(dve, lambda e: e.tensor_tensor(lt[:, :].rearrange("p (a b) -> p a b", a=2), lv[:, :].rearrange("p (a c b) -> p a c b", a=2, c=2)[:, :, 0, :],
                                                 lv[:, :].rearrange("p (a c b) -> p a c b", a=2, c=2)[:, :, 1, :], ALU.mult), reads=[b_lv], writes=[b_lt])
            ld = fw.sb([128, 2], F32, "ld"); b_ld = Buf()
            fw.op(dve, lambda e: e.reduce_sum(ld[:, :], lt[:, :].rearrange("p (a b) -> p a b", a=2), axis=AX.X), reads=[b_lt], writes=[b_ld])
            fw.op(act, lambda e: e.activation(ld[:, :], ld[:, :], AF.Exp), writes=[b_ld])
            nlam = fw.sb([128, 1], F32, "nlam"); b_nlam = Buf()
            fw.op(dve, lambda e: e.tensor_tensor(nlam[:, :], ld[:, 1:2], ld[:, 0:1], ALU.subtract), reads=[b_ld], writes=[b_nlam])
            fw.op(dve, lambda e: e.tensor_scalar(nlam[:, :], nlam[:, :], float(-LAMBDA_INIT), None, ALU.add), writes=[b_nlam])

            sqs = [fw.sb([128, 512], F32, "sq%d" % i) for i in range(2)]; b_sqs = [Buf() for _ in range(2)]
            rs = [fw.sb([128, 512], F32, "rs%d" % i) for i in range(2)]; b_rs = [Buf() for _ in range(2)]
            xws = [fw.sb([128, 512], F32, "xw%d" % i) for i in range(2)]; b_xws = [Buf() for _ in range(2)]
            us = [fw.sb([128, 512], F32, "u%d" % i) for i in range(2)]; b_us = [Buf() for _ in range(2)]
            wus = [fw.sb([128, 16, 128], BF16, "wu%d" % i) for i in range(3)]; b_wus = [Buf() for _ in range(3)]
            st_ = {"cnt": 0, "w": 0}

            def qk_unit(col0, nwcol, b_nwcol, dstT, bdst, with_ctx):
                wu, bwu = wus[st_["w"] % 3], b_wus[st_["w"] % 3]; st_["w"] += 1
                fw.dma(pool, wu[:], w_in[:, col0:col0 + 128].rearrange("(kc p) c -> p kc c", p=128), writes=[bwu])
                for (s0, n) in (TB if with_ctx else TB[1:]):
                    i2 = st_["cnt"] % 2; st_["cnt"] += 1
                    sq_, bsq = sqs[i2], b_sqs[i2]; r_, br = rs[i2], b_rs[i2]; xw, bxw = xws[i2], b_xws[i2]; u_, bu = us[i2], b_us[i2]
                    pt, pb = fw.next_psum()
                    for kc in range(16):
                        fw.op(pe, lambda e, kc=kc, pt=pt, s0=s0, n=n: e.matmul(pt[:, 0:n], wu[:, kc, :], aT[:, kc, s0:s0 + n], start=(kc == 0), stop=(kc == 15)),
                              reads=[bwu] + [b_aT[t] for t in tiles_of(s0, n)], writes=[pb])
                    fw.op(act, lambda e, pt=pt, n=n: e.activation(sq_[:, 0:n], pt[:, 0:n], AF.Square), reads=[pb], writes=[bsq])
                    p2, pb2 = fw.next_psum()
                    fw.op(pe, lambda e, p2=p2, n=n: e.matmul(p2[:, 0:n], ones[:, :], sq_[:, 0:n], start=True, stop=True), reads=[b_ones, bsq], writes=[pb2])
                    fw.op(act, lambda e, p2=p2, n=n: e.activation(r_[:, 0:n], p2[:, 0:n], AF.Sqrt, bias=EPS, scale=1.0 / 128), reads=[pb2], writes=[br])
                    fw.op(dve, lambda e, n=n: e.reciprocal(r_[:, 0:n], r_[:, 0:n]), writes=[br])
                    fw.op(dve, lambda e, pt=pt, n=n: e.tensor_tensor(r_[:, 0:n], pt[:, 0:n], r_[:, 0:n], ALU.mult), reads=[pb], writes=[br])
                    d0 = s0 if with_ctx else s0 - 256
                    if s0 == 0:
                        fw.op(pool, lambda e, n=n, d0=d0: e.tensor_scalar(dstT[:, d0:d0 + n], r_[:, 0:n], nwcol, None, ALU.mult), reads=[br, b_nwcol], writes=[bdst])
                        continue
                    l0 = s0 - 256
                    fw.op(pool, lambda e, n=n: e.tensor_scalar(xw[:, 0:n], r_[:, 0:n], nwcol, None, ALU.mult), reads=[br, b_nwcol], writes=[bxw])
                    p3, pb3 = fw.next_psum()
                    fw.op(pe, lambda e, p3=p3, n=n: e.matmul(p3[:, 0:n], rmT[:, :], xw[:, 0:n], start=True, stop=True), reads=[b_rm, bxw], writes=[pb3])
                    fw.op(dve, lambda e, p3=p3, n=n, l0=l0: e.tensor_tensor(u_[:, 0:n], p3[:, 0:n], sinT[:, l0:l0 + n], ALU.mult), reads=[pb3, b_sin], writes=[bu])
                    fw.op(pool, lambda e, n=n, l0=l0: e.tensor_tensor(xw[:, 0:n], xw[:, 0:n], cosT[:, l0:l0 + n], ALU.mult), reads=[b_cos], writes=[bxw])
                    fw.op(pool, lambda e, n=n, d0=d0: e.tensor_tensor(dstT[:, d0:d0 + n], xw[:, 0:n], u_[:, 0:n], ALU.add), reads=[bxw, bu], writes=[bdst])

            kTs = [fw.sb([128, NTOK], BF16, "kT%d" % i) for i in range(2)]; b_kTs = [Buf() for _ in range(2)]
            qTs = [fw.sb([128, NLAT], BF16, "qT%d" % i) for i in range(2)]; b_qTs = [Buf() for _ in range(2)]
            PTs = [fw.sb([128, 512], BF16, "PT%d" % i) for i in range(3)]; b_PTs = [Buf() for _ in range(3)]
            ogs = [fw.sb([128, 128], F32, "og%d" % i) for i in range(3)]; b_ogs = [Buf() for _ in range(3)]
            rcs = [fw.sb([128, 1], F32, "rc%d" % i) for i in range(3)]; b_rcs = [Buf() for _ in range(3)]
            o0n = fw.sb([128, 4, 256], F32, "o0n"); b_o0n = [Buf() for _ in range(4)]
            ods = [fw.sb([128, 256], F32, "od%d" % i) for i in range(2)]; b_ods = [Buf() for _ in range(2)]
            o1s = [fw.sb([128, 256], F32, "o1_%d" % i) for i in range(2)]; b_o1s = [Buf() for _ in range(2)]
            sq2 = fw.sb([128, 256], F32, "sq2"); b_sq2 = Buf()
            ss2 = [fw.sb([128, 1], F32, "ss2_%d" % i) for i in range(2)]; b_ss2 = [Buf() for _ in range(2)]
            cn = {"pt": 0, "o": 0, "k": 0, "q": 0, "d": 0}

            def attend(qT, bqT, kT, bkT, vfn, vw, banks, qb, finish):
                stride = 512
                per_bank = 1
                for kt in range(NT):
                    pS, pbS = fw.next_psum()
                    fw.op(pe, lambda e, pS=pS, kt=kt: e.matmul(pS[:, :], kT[:, kt * 128:(kt + 1) * 128], qT[:, qb * 512:(qb + 1) * 512], start=True, stop=True),
                          reads=[bkT, bqT], writes=[pbS])
                    PT, bPT = PTs[cn["pt"] % 3], b_PTs[cn["pt"] % 3]; cn["pt"] += 1
                    fw.op(act, lambda e, pS=pS, PT=PT: e.activation(PT[:, :], pS[:, :], AF.Exp), reads=[pbS], writes=[bPT])
                    for qs in range(4):
                        bank = banks[qs // per_bank]; off = (qs % per_bank) * stride
                        pO, pbO = fw.psum[bank]
                        fw.op(pe, lambda e, pO=pO, off=off, qs=qs, PT=PT, kt=kt: e.matmul(pO[:, off:off + vw], PT[:, qs * 128:(qs + 1) * 128], vfn(kt), start=(kt == 0), stop=(kt == NT - 1)),
                              reads=[bPT, b_V[kt]], writes=[pbO])
                for qs in range(4):
                    bank = banks[qs // per_bank]; off = (qs % per_bank) * stride
                    pO, pbO = fw.psum[bank]
                    finish(qs, pO, pbO, off)

            for kv in range(2):
                kT, bkT = kTs[cn["k"] % 2], b_kTs[cn["k"] % 2]; cn["k"] += 1
                qk_unit(GK0 + kv * 128, nw4[:, 1:2], b_nw4, kT, bkT, True)
                for hq in range(4):
                    hd = kv * 4 + hq
                    qT, bqT = qTs[cn["q"] % 2], b_qTs[cn["q"] % 2]; cn["q"] += 1
                    qk_unit(GQ0 + hd * 128, nwq[:, 0:1], b_nwq, qT, bqT, False)
                    for qb in range(4):
                        def fin(qs, pO, pbO, off, hd=hd, qb=qb):
                            i3 = cn["o"] % 3; cn["o"] += 1
                            og, bog = ogs[i3], b_ogs[i3]; rc, brc = rcs[i3], b_rcs[i3]
                            fw.op(dve, lambda e: e.reciprocal(rc[:, :], pO[:, off + 128:off + 129]), reads=[pbO], writes=[brc])
                            fw.op(dve, lambda e: e.tensor_scalar(og[:, :], pO[:, off:off + 128], rc[:, 0:1], None, ALU.mult), reads=[pbO, brc], writes=[bog])
                            r0 = qb * 512 + qs * 128
                            if fused:
                                d_mix.extend(out_fn(fw, r0, hd * 128, 128, og, bog))
                            else:
                                db = Buf(); d_mix.append(db)
                                fw.dma(sp, mix[r0:r0 + 128, hd * 128:(hd + 1) * 128], og[:, :], reads=[bog], writes=[db])
                        attend(qT, bqT, kT, bkT, lambda kt, kv=kv: V1g[:, kt, kv, :], 129, [4, 5, 6, 7], qb, fin)
            for h in range(4):
                kq = []
                for c in range(2):
                    kT, bkT = kTs[cn["k"] % 2], b_kTs[cn["k"] % 2]; cn["k"] += 1
                    qk_unit(DK0 + (h * 2 + c) * 128, nw4[:, 3:4], b_nw4, kT, bkT, True)
                    qT, bqT = qTs[cn["q"] % 2], b_qTs[cn["q"] % 2]; cn["q"] += 1
                    qk_unit(DQ0 + (h * 2 + c) * 128, nwq[:, 2:3], b_nwq, qT, bqT, False)
                    kq.append((kT, bkT, qT, bqT))
                for qb in range(4):
                    for c in range(2):
                        kT, bkT, qT, bqT = kq[c]
                        if c == 0:
                            def fin(qs, pO, pbO, off):
                                i3 = cn["o"] % 3; cn["o"] += 1
                                rc, brc = rcs[i3], b_rcs[i3]
                                fw.op(dve, lambda e: e.reciprocal(rc[:, :], pO[:, off + 256:off + 257]), reads=[pbO], writes=[brc])
                                fw.op(dve, lambda e: e.tensor_scalar(o0n[:, qs, :], pO[:, off:off + 256], rc[:, 0:1], None, ALU.mult), reads=[pbO, brc], writes=[b_o0n[qs]])
                        else:
                            def fin(qs, pO, pbO, off, h=h, qb=qb):
                                i3 = cn["o"] % 3; cn["o"] += 1
                                i2 = cn["d"] % 2; cn["d"] += 1
                                rc, brc = rcs[i3], b_rcs[i3]
                                o1, bo1 = o1s[i2], b_o1s[i2]; od, bod = ods[i2], b_ods[i2]; s2, bs2 = ss2[i2], b_ss2[i2]
                                fw.op(dve, lambda e: e.reciprocal(rc[:, :], pO[:, off + 256:off + 257]), reads=[pbO], writes=[brc])
                                fw.op(dve, lambda e: e.tensor_scalar(rc[:, :], rc[:, :], nlam[:, 0:1], None, ALU.mult), reads=[b_nlam], writes=[brc])
                                fw.op(dve, lambda e: e.scalar_tensor_tensor(o1[:, :], pO[:, off:off + 256], rc[:, 0:1], o0n[:, qs, :], ALU.mult, ALU.add),
                                      reads=[pbO, brc, b_o0n[qs]], writes=[bo1])
                                fw.op(act, lambda e: e.activation(sq2[:, :], o1[:, :], AF.Square, accum_out=s2[:, :]), reads=[bo1], writes=[b_sq2, bs2])
                                fw.op(act, lambda e: e.activation(s2[:, :], s2[:, :], AF.Sqrt, bias=EPS, scale=1.0 / 256), writes=[bs2])
                                fw.op(dve, lambda e: e.reciprocal(s2[:, :], s2[:, :]), writes=[bs2])
                                fw.op(dve, lambda e: e.scalar_tensor_tensor(od[:, :], o1[:, :], s2[:, 0:1], sub[:, :], ALU.mult, ALU.mult), reads=[bo1, bs2, b_sub], writes=[bod])
                                r0 = qb * 512 + qs * 128
                                if fused:
                                    d_mix.extend(out_fn(fw, r0, 1024 + h * 256, 256, od, bod))
                                else:
                                    db = Buf(); d_mix.append(db)
                                    fw.dma(sp, mix[r0:r0 + 128, 1024 + h * 256:1024 + (h + 1) * 256], od[:, :], reads=[bod], writes=[db])
                        attend(qT, bqT, kT, bkT, lambda kt, h=h: V1d[:, kt, h, :], 257, [4, 5, 6, 7], qb, fin)
            fw.barrier()
        if not fused:
            fw.finish(d_mix)
        fw.ps_pool = list(range(8))
        print("phaseA1 ops", fw.n_ops, "waits", fw.n_waits)
    return d_mix if fused else nc

from contextlib import ExitStack

PAIRS = [[0, 1], [2, 3], [4, 5], [6, 7]]
CC_BYTES = 4 * 1024 * 1024


def build_fused():
    nc = bass.Bass("TRN2", target_bir_lowering=False)
    msk_d = nc.dram_tensor("msk", [128, 2], F32, kind="ExternalInput").ap()
    x1s = nc.dram_tensor("x1s", [2 * 2304, 2048], BF16).ap(); x1d = nc.dram_tensor("x1d", [2 * 2304, 2048], BF16).ap()
    x2s = nc.dram_tensor("x2s", [2 * 1152, 2048], F32).ap(); x2d = nc.dram_tensor("x2d", [2 * 1152, 2048], F32).ap()
    x3s = nc.dram_tensor("x3s", [2 * 2048, 2048], BF16).ap(); x3d = nc.dram_tensor("x3d", [2 * 2048, 2048], BF16).ap()
    h1own = nc.dram_tensor("h1own", [1024, 2048], F32).ap()
    with ExitStack() as st0:
        fw = FW(nc, st0)
        cc = Src("cc", fw._sem("cc"), 1)
        msk = fw.sb([128, 2], F32, "msk"); b_msk = Buf()
        fw.dma(fw.sp, msk[:], msk_d, writes=[b_msk])

        def staging(shape, dt, n):
            stk = fw.stack
            key = "_stg_%s_%d" % (str(dt), shape[1])
            if not hasattr(stk, key):
                setattr(stk, key, {"t": [fw.sb(shape, dt, "stg%d" % i) for i in range(n)], "b": [Buf() for _ in range(n)], "i": 0})
            d = getattr(stk, key)
            k = d["i"] % n; d["i"] += 1
            return d["t"][k], d["b"][k]

        def mk_mix_out(dst, R):
            def out_fn(fw_, r0, c0, n, tile, btile):
                res = []
                for s in range(2):
                    u, bu = staging([128, 512], BF16, 4)
                    fw.op(fw.pool, lambda e: e.tensor_scalar(u[:, 0:n], tile[:, :], msk[:, s:s + 1], None, ALU.mult), reads=[btile, b_msk], writes=[bu])
                    db = Buf(); res.append(db)
                    fw.dma(fw.sp, dst[s * R + r0:s * R + r0 + 128, c0:c0 + n], u[:, 0:n], reads=[bu], writes=[db])
                return res
            return out_fn

        b_h1own = []

        def b0_out(fw_, t, h1, bh1):
            res = []
            for s in range(2):
                u, bu = staging([128, 2048], F32, 4)
                fw.op(fw.pool, lambda e: e.tensor_scalar(u[:, :], h1[:, :], msk[:, s:s + 1], None, ALU.mult), reads=[bh1, b_msk], writes=[bu])
                db = Buf(); res.append(db)
                fw.dma(fw.sp, x2s[s * 1152 + t * 128:s * 1152 + (t + 1) * 128, :], u[:, :], reads=[bu], writes=[db])
            if t >= 1:
                db = Buf(); b_h1own.append(db); res.append(db)
                fw.dma(fw.sp, h1own[(t - 1) * 128:t * 128, :], h1[:, :], reads=[bh1], writes=[db])
            return res

        def exchange(src, dst, writers):
            pool = fw.pool
            for b in writers:
                pool.wait(b.w)
            nrows = src.shape[0]
            elt = 2 if src.dtype == BF16 else 4
            rows_per = CC_BYTES // (src.shape[1] * elt)
            for r0 in range(0, nrows, rows_per):
                r1 = min(nrows, r0 + rows_per)
                ins = pool.raw.collective_compute("AllReduce", ALU.add, replica_groups=PAIRS,
                                                  ins=[src[r0:r1, :].opt()], outs=[dst[r0:r1, :].opt()])
                ins.then_inc(cc.sem)
                cc.n += 1
            eb = Buf()
            eb.w = Ev(cc, cc.n, dict(pool.clock))
            return eb

        w1_ = build_phaseA0(ctx=dict(nc=nc, fw=fw, pre="a0_", out_fn=mk_mix_out(x1s, 2304)))
        e1 = exchange(x1s, x1d, w1_)
        w2_ = build_phaseB(9, True, ctx=dict(nc=nc, fw=fw, pre="b0_", mix_src=(x1d, [e1], 2304), out_fn=b0_out))
        e2 = exchange(x2s, x2d, w2_)

        def row_of(t):
            return 0 if t == 0 else 1152 if t == 1 else 128 + (t - 2) * 128 if t < 10 else 1280 + (t - 10) * 128

        w3_ = build_phaseA1(ctx=dict(nc=nc, fw=fw, pre="a1_", h_src=(x2d, [e2], row_of), out_fn=mk_mix_out(x3s, 2048)))
        e3 = exchange(x3s, x3d, w3_)
        outs = build_phaseB(8, False, ctx=dict(nc=nc, fw=fw, pre="b1_", h_src=(h1own, list(b_h1own)), mix_src=(x3d, [e3], 2048), out_fn=None))
        fw.finish(outs)
        print("fused ops", fw.n_ops, "waits", fw.n_waits)
    return nc

import numpy as np

PERM = np.concatenate([np.arange(0, 1024), np.arange(2048, 3072), np.arange(1024, 2048), np.arange(3072, 4096)])
_CONST = {}

def consts():
    if "ident" not in _CONST:
        s = np.arange(128)
        _CONST["ident"] = np.eye(128, dtype=np.float32)
        _CONST["trilt"] = (s[:, None] < s[None, :]).astype(np.float32)
        _CONST["ule"] = (s[:, None] <= s[None, :]).astype(np.float32)
        _CONST["uge"] = (s[:, None] >= s[None, :]).astype(np.float32)
        _CONST["iota64"] = np.arange(64, dtype=np.float32).reshape(1, 64)
    return _CONST

def f32c(a):
    return np.ascontiguousarray(a, dtype=np.float32)

def na_bias_tables(rpb, heads):
    classes = [(0, 0), (1, 0), (2, 0), (14, 22), (15, 22)]
    out = np.full((len(heads), 25, 128, 128), -30000.0, np.float32)
    k = np.arange(128); q = np.arange(128)
    for ci, (j, a) in enumerate(classes):
        r = 2 * j + q // 64; c = q % 64
        r0 = np.clip(r - 4, 0, 24); cs = np.clip(c - 8, 0, 48)
        for i in range(5):
            kr = a + 2 * i + k // 64; kc = k % 64
            vis = ((kr[:, None] >= r0[None, :]) & (kr[:, None] < r0[None, :] + 8) &
                   (kc[:, None] >= cs[None, :]) & (kc[:, None] < cs[None, :] + 16))
            ri = np.clip(kr[:, None] - r[None, :] + 7, 0, 14)
            cj = np.clip(kc[:, None] - c[None, :] + 15, 0, 30)
            for hi, h in enumerate(heads):
                vals = rpb[h][ri, cj]
                out[hi, ci * 5 + i] = np.where(vis, vals, np.float32(-30000.0))
    return out

def pack_A0(P, b, hh, h_all):
    in_w = P["ev_in_w"][0]
    gs = [2 * hh, 2 * hh + 1]
    heads16 = np.arange(16 * hh, 16 * hh + 16)
    nah = np.arange(8 * hh, 8 * hh + 8)
    cols = np.concatenate(
        [np.arange(g * 512, (g + 1) * 512) for g in gs] +
        [2048 + np.arange(g * 512, (g + 1) * 512) for g in gs] +
        [4096 + np.arange(g * 128, (g + 1) * 128) for g in gs] +
        [4608 + np.arange(g * 128, (g + 1) * 128) for g in gs] +
        [5120 + heads16, 5152 + heads16] +
        [5184 + np.arange(h * 128, (h + 1) * 128) for h in nah] +
        [5184 + 2048 + np.arange(h * 128, (h + 1) * 128) for h in nah] +
        [5184 + 4096 + np.arange(h * 128, (h + 1) * 128) for h in nah])
    chans = np.concatenate([np.arange(g * 512, (g + 1) * 512) for g in gs] +
                           [2048 + np.arange(g * 128, (g + 1) * 128) for g in gs] +
                           [2560 + np.arange(g * 128, (g + 1) * 128) for g in gs])
    d = {k: consts()[k] for k in ("ident", "ule", "uge")}
    d.update({
        "h_all": h_all, "cvec": np.stack([P["c"][b], P["c_ctx"]], 0),
        "ada_w": P["ada_w"][0][:, 0:4096], "ada_b": P["ada_b"][0][None, 0:4096],
        "norm_w1": P["norm_w"][0, 0][None, :], "w_in": in_w[:, cols],
        "convw": P["ev_conv_w"][0][:, chans].T, "convb": P["ev_conv_b"][0][chans].reshape(12, 128).T,
        "dt_bias": np.concatenate([P["ev_dt_bias"][0][0, heads16], P["ev_dt_bias"][0][1, heads16]])[None, :],
        "a_log": np.concatenate([P["ev_a_log"][0][0, heads16], P["ev_a_log"][0][1, heads16]])[None, :],
        "d_skip": P["ev_d_skip"][0][heads16][None, :],
        "ssd_nw": np.concatenate([P["ev_ssd_norm_w"][0][g * 512:(g + 1) * 512] for g in gs])[None, :],
        "qnw": P["ev_na_q_norm"][0][:, None], "knw": P["ev_na_k_norm"][0][:, None],
        "biasT": na_bias_tables(P["ev_na_rpb"][0], list(nah)),
    })
    return {k: f32c(v) for k, v in d.items()}

def rope_tables():
    if "cosT" not in _CONST:
        t = np.arange(2048)
        row = (t // 64).astype(np.float32); col = (t % 64).astype(np.float32)
        inv = (1.0 / (np.float32(10000.0) ** (np.arange(0, 64, 2, dtype=np.float32) / np.float32(64)))).astype(np.float32)
        ang = np.concatenate([row[:, None] * inv[None], col[:, None] * inv[None]], -1).astype(np.float32)
        c = np.cos(ang).astype(np.float32); s = np.sin(ang).astype(np.float32)
        _CONST["cosT"] = np.ascontiguousarray(np.repeat(c, 2, axis=1).T)
        _CONST["sinT"] = np.ascontiguousarray(np.repeat(s, 2, axis=1).T)
        rm = np.zeros((128, 128), np.float32)
        for i in range(64):
            rm[2 * i + 1, 2 * i] = -1.0
            rm[2 * i, 2 * i + 1] = 1.0
        _CONST["rmT"] = rm
    return _CONST["cosT"], _CONST["sinT"], _CONST["rmT"]

def pack_A1(P, b, hh, h_all):
    in_w = P["od_in_w"][0]
    cols = np.concatenate(
        [np.arange((8 * hh + i) * 128, (8 * hh + i + 1) * 128) for i in range(8)] +
        [2048 + np.arange((8 * hh + i) * 128, (8 * hh + i + 1) * 128) for i in range(8)] +
        [4096 + np.arange((2 * hh + i) * 128, (2 * hh + i + 1) * 128) for i in range(2)] +
        [5120 + np.arange((8 * hh + i) * 128, (8 * hh + i + 1) * 128) for i in range(8)] +
        [4608 + np.arange((2 * hh + i) * 128, (2 * hh + i + 1) * 128) for i in range(2)] +
        [7168 + np.arange((4 * hh + i) * 256, (4 * hh + i + 1) * 256) for i in range(4)])
    cosT, sinT, rmT = rope_tables()
    d = {
        "ident": consts()["ident"],
        "h_all": h_all, "cvec": np.stack([P["c"][b], P["c_ctx"]], 0),
        "ada_w": P["ada_w"][1][:, 0:4096], "ada_b": P["ada_b"][1][None, 0:4096],
        "norm_w1": P["norm_w"][1, 0][None, :], "w_in": in_w[:, cols],
        "nws": np.stack([P["od_gqa_q_norm"][0], P["od_gqa_k_norm"][0], P["od_diff_q_norm"][0], P["od_diff_k_norm"][0]], 1),
        "lamv": P["od_lambda"][0].reshape(1, 512), "subln": P["od_diff_subln"][0][None, :],
        "cosT": cosT, "sinT": sinT, "rmT": rmT,
    }
    return {k: f32c(v) for k, v in d.items()}


_PROG = {}


def _shared_B(P, layer):
    ow = P["ev_out_w"][0] if layer == 0 else P["od_out_w"][0]
    c = consts()
    sh = {"ident": c["ident"], "trilt": c["trilt"], "iota64": c["iota64"],
          "ada_w": P["ada_w"][layer][:, 4096:], "ada_b": P["ada_b"][layer][None, 4096:],
          "norm_w2": P["norm_w"][layer, 1][None, :], "out_w": ow[PERM],
          "gwew": np.concatenate([P["moe_group_w"][layer], P["moe_expert_w"][layer]], 1),
          "w1": P["moe_w1"][layer], "w3": P["moe_w3"][layer], "w2": P["moe_w2"][layer]}
    return {k: f32c(v) for k, v in sh.items()}


def kernel(**inputs):
    P = {k: np.asarray(v) for k, v in inputs.items()}
    B = 4
    cores = [(b, hh) for b in range(B) for hh in range(2)]
    if "nc" not in _PROG:
        _PROG["nc"] = build_fused()
    shB = [_shared_B(P, 0), _shared_B(P, 1)]
    a0 = {}; a1 = {}
    maps = []
    for (b, hh) in cores:
        h_all = np.concatenate([P["ctx"][b], P["x"][b]], 0)
        if hh not in a0:
            a0[hh] = pack_A0(P, b, hh, h_all)
            a1[hh] = pack_A1(P, b, hh, h_all)
            a1[hh].pop("h_all")
        cvec = f32c(np.stack([P["c"][b], P["c_ctx"]], 0))
        d = {}
        for k, v in a0[hh].items():
            d["a0_" + k] = v
        d["a0_h_all"] = f32c(h_all); d["a0_cvec"] = cvec
        for k, v in a1[hh].items():
            d["a1_" + k] = v
        d["a1_cvec"] = cvec
        for l, pre in ((0, "b0_"), (1, "b1_")):
            for k, v in shB[l].items():
                d[pre + k] = v
            d[pre + "cvec"] = cvec
        rows0 = np.concatenate([np.arange(hh * 128, (hh + 1) * 128), 256 + np.arange(hh * 1024, (hh + 1) * 1024)])
        d["b0_h_in"] = f32c(h_all[rows0])
        g0 = rows0.reshape(9, 128).T
        d["b0_gidx"] = np.ascontiguousarray(np.stack([g0, 2304 + g0], -1).astype(np.int32))
        g1 = (hh * 1024 + np.arange(1024)).reshape(8, 128).T
        d["b1_gidx"] = np.ascontiguousarray(np.stack([g1, 2048 + g1], -1).astype(np.int32))
        m = np.zeros((128, 2), np.float32); m[:, hh] = 1.0
        d["msk"] = m
        maps.append(d)
    res = run_bass_kernel_spmd(_PROG["nc"], maps, core_ids=list(range(8)))
    out = np.zeros((B, 2048, 2048), np.float32)
    for i, (b, hh) in enumerate(cores):
        out[b, hh * 1024:(hh + 1) * 1024] = res.results[i]["h_out"]
    return out
```

```python
import numpy as np
import ml_dtypes
import concourse.bass as bass
import concourse.mybir as mybir
from concourse.bass_utils import run_bass_kernel_spmd

F32 = mybir.dt.float32
BF16 = mybir.dt.bfloat16
I32 = mybir.dt.int32
U32 = mybir.dt.uint32
AF = mybir.ActivationFunctionType
ALU = mybir.AluOpType
AX = mybir.AxisListType


class Ev:
    __slots__ = ("src", "count", "clock")

    def __init__(self, src, count, clock):
        self.src = src
        self.count = count
        self.clock = clock


class Src:
    def __init__(self, name, sem, mult):
        self.name = name
        self.sem = sem
        self.mult = mult
        self.n = 0


class Buf:
    __slots__ = ("name", "w", "r")

    def __init__(self, name=""):
        self.name = name
        self.w = None
        self.r = {}


class Eng:
    def __init__(self, fw, raw, name):
        self.fw = fw
        self.raw = raw
        self.name = name
        self.src = Src(name, fw._sem("s_" + name), 1)
        self.clock = {}
        self.slots = []
        self.slot_i = 0

    def wait(self, ev):
        if ev is None:
            return
        if self.name == "pe" and ev.src.name.startswith("pe"):
            return
        if self.clock.get(ev.src.name, 0) >= ev.count:
            return
        self.raw.wait_ge(ev.src.sem, ev.count * ev.src.mult)
        self.fw.n_waits += 1
        for k, v in ev.clock.items():
            if self.clock.get(k, 0) < v:
                self.clock[k] = v
        self.clock[ev.src.name] = ev.count


class FW:
    def __init__(self, nc, stack, n_dma_slots=12):
        self.nc = nc
        self.stack = stack
        self.sem_stack = stack
        self.n_waits = 0
        self.n_ops = 0
        self.pe = Eng(self, nc.tensor, "pe")
        self.dve = Eng(self, nc.vector, "dve")
        self.act = Eng(self, nc.scalar, "act")
        self.pool = Eng(self, nc.gpsimd, "pool")
        self.sp = Eng(self, nc.sync, "sp")
        for e in (self.sp, self.pool, self.act):
            for i in range(n_dma_slots):
                nm = "d_%s%d" % (e.name, i)
                e.slots.append(Src(nm, self._sem(nm), 16))
        self.uid = 0
        self.psum = []
        for i in range(8):
            t = stack.enter_context(nc.psum_tensor("ps%d" % i, [128, 512], F32))
            self.psum.append((t, Buf("ps%d" % i)))
        self.ps_i = 0
        self.ps_pool = list(range(8))

    def _sem(self, name):
        return self.sem_stack.enter_context(self.nc.semaphore(name))

    def sb(self, shape, dtype, name=None):
        self.uid += 1
        name = "sb%d_%s" % (self.uid, name or "t")
        t = self.stack.enter_context(self.nc.sbuf_tensor(name, list(shape), dtype))
        return t

    def next_psum(self):
        self.ps_i = (self.ps_i + 1) % len(self.ps_pool)
        return self.psum[self.ps_pool[self.ps_i]]

    def _deps(self, eng, reads, writes):
        for b in reads:
            eng.wait(b.w)
        for b in writes:
            eng.wait(b.w)
            for ev in list(b.r.values()):
                eng.wait(ev)

    def _mark(self, ev, reads, writes):
        for b in reads:
            b.r[ev.src.name] = ev
        for b in writes:
            b.w = ev
            b.r = {}

    def op(self, eng, fn, reads=(), writes=()):
        self._deps(eng, reads, writes)
        ins = fn(eng.raw)
        if eng.src.n >= 30000:
            eng.gen = getattr(eng, "gen", 0) + 1
            nm = "%s_g%d" % (eng.name, eng.gen)
            eng.src = Src(nm, self._sem("s_" + nm), 1)
        eng.src.n += 1
        ins.then_inc(eng.src.sem, 1)
        clock = dict(eng.clock)
        ev = Ev(eng.src, eng.src.n, clock)
        self._mark(ev, reads, writes)
        self.n_ops += 1
        return ev

    def dma(self, eng, out, in_, reads=(), writes=(), fn=None):
        slot = eng.slots[eng.slot_i]
        eng.slot_i = (eng.slot_i + 1) % len(eng.slots)
        if slot.n > 0:
            eng.wait(Ev(slot, slot.n, {}))
        self._deps(eng, reads, writes)
        if fn is None:
            ins = eng.raw.dma_start(out=out, in_=in_)
        else:
            ins = fn(eng.raw)
        ins.then_inc(slot.sem, 16)
        slot.n += 1
        ev = Ev(slot, slot.n, dict(eng.clock))
        self._mark(ev, reads, writes)
        self.n_ops += 1
        return ev

    def bound_reg(self, val):
        if not hasattr(self, "_bregs"):
            self._bregs = {}
        if val not in self._bregs:
            self._bregs[val] = self.nc.gpsimd.to_reg(val)
        return self._bregs[val]

    def engines(self):
        return (self.pe, self.dve, self.act, self.pool, self.sp)

    def barrier(self):
        evs = []
        for e in self.engines():
            if e.src.n > 0:
                evs.append(Ev(e.src, e.src.n, dict(e.clock)))
            for s in e.slots:
                if s.n > 0:
                    evs.append(Ev(s, s.n, {}))
        for e in self.engines():
            for ev in evs:
                e.wait(ev)

    def finish(self, bufs):
        for b in bufs:
            self.sp.wait(b.w)

from contextlib import ExitStack

D = 2048
EPS = 1e-6


def bview(ap_row, n=128):
    return ap_row.partition_broadcast(n)


def emit_consts(fw, ident_d):
    ident = fw.sb([128, 128], F32, "ident"); b_ident = Buf("ident")
    fw.dma(fw.sp, ident[:], ident_d, writes=[b_ident])
    return ident, b_ident


def emit_mods(fw, cvec, ada_w, ada_b, ncols, modrows, d_mod, ident, b_ident):
    pe, dve, act, pool, sp = fw.pe, fw.dve, fw.act, fw.pool, fw.sp
    outer = fw.stack
    with ExitStack() as st:
        fw.stack = st
        cv = fw.sb([2, D], F32, "cv"); b_cv = Buf()
        fw.dma(sp, cv[:], cvec, writes=[b_cv])
        siluT = fw.sb([128, 16, 2], BF16, "siluT"); b_sT = Buf()
        pt, pb = fw.next_psum()
        for kc in range(16):
            fw.op(pe, lambda e, kc=kc: e.transpose(pt[:, 2 * kc:2 * kc + 2], cv[0:2, kc * 128:(kc + 1) * 128], ident[0:2, 0:2]),
                  reads=[b_cv, b_ident], writes=[pb])
        fw.op(act, lambda e: e.activation(siluT[:, :, :].rearrange("p a b -> p (a b)"), pt[:, 0:32], AF.Silu), reads=[pb], writes=[b_sT])
        adab = fw.sb([2, ncols], F32, "adab"); b_adab = Buf()
        fw.dma(sp, adab[:], bview(ada_b, 2), writes=[b_adab])
        wts = [fw.sb([128, 16, 512], BF16, "adaw%d" % i) for i in range(2)]
        b_wts = [Buf() for _ in range(2)]
        modsb = fw.sb([2, ncols], F32, "modsb"); b_modsb = Buf()
        for j in range(ncols // 512):
            wt, bw = wts[j % 2], b_wts[j % 2]
            fw.dma(pool, wt[:], ada_w[:, j * 512:(j + 1) * 512].rearrange("(kc p) c -> p kc c", p=128), writes=[bw])
            pt, pb = fw.next_psum()
            for kc in range(16):
                fw.op(pe, lambda e, kc=kc, wt=wt, pt=pt: e.matmul(pt[0:2, :], siluT[:, kc, :], wt[:, kc, :], start=(kc == 0), stop=(kc == 15)),
                      reads=[b_sT, bw], writes=[pb])
            fw.op(dve, lambda e, pt=pt, j=j: e.tensor_tensor(modsb[:, j * 512:(j + 1) * 512], pt[0:2, :], adab[:, j * 512:(j + 1) * 512], ALU.add),
                  reads=[pb, b_adab], writes=[b_modsb])
        fw.dma(sp, modrows, modsb[:, :], reads=[b_modsb], writes=[d_mod])
        fw.barrier()
    fw.stack = outer


def emit_aT(fw, h_all, n_tiles, n_ctx_tiles, norm_w1, modrows, d_mod, aT, b_aT, ident, b_ident, row_of=None, h_bufs=()):
    pe, dve, act, pool, sp = fw.pe, fw.dve, fw.act, fw.pool, fw.sp
    outer = fw.stack
    with ExitStack() as st:
        fw.stack = st
        nw = fw.sb([128, D], F32, "nw"); b_nw = Buf()
        fw.dma(sp, nw[:], bview(norm_w1), writes=[b_nw])
        nsc = []; sh = []; b_nsc = []; b_sh = []
        for v in range(2):
            s = fw.sb([128, D], F32, "sh1_%d" % v); bs = Buf()
            fw.dma(sp, s[:], bview(modrows[v:v + 1, 0:2048]), reads=[d_mod], writes=[bs])
            sh.append(s); b_sh.append(bs)
            c = fw.sb([128, D], F32, "nsc1_%d" % v); bc = Buf()
            fw.dma(sp, c[:], bview(modrows[v:v + 1, 2048:4096]), reads=[d_mod], writes=[bc])
            fw.op(dve, lambda e, c=c: e.scalar_tensor_tensor(c[:, :], c[:, :], 1.0, nw[:, :], ALU.add, ALU.mult), reads=[b_nw], writes=[bc])
            nsc.append(c); b_nsc.append(bc)
        hts = [fw.sb([128, D], F32, "ht%d" % i) for i in range(2)]; b_hts = [Buf() for _ in range(2)]
        ats = [fw.sb([128, D], F32, "at%d" % i) for i in range(2)]; b_ats = [Buf() for _ in range(2)]
        sq = fw.sb([128, D], F32, "sq"); b_sq = Buf()
        ssqs = [fw.sb([128, 1], F32, "ssq%d" % i) for i in range(2)]; b_ssqs = [Buf() for _ in range(2)]
        for t in range(n_tiles):
            v = 1 if t < n_ctx_tiles else 0
            ht, bh = hts[t % 2], b_hts[t % 2]
            at, ba = ats[t % 2], b_ats[t % 2]
            ssq, b_ssq = ssqs[t % 2], b_ssqs[t % 2]
            r0 = t * 128 if row_of is None else row_of(t)
            fw.dma(sp, ht[:], h_all[r0:r0 + 128, :], reads=list(h_bufs), writes=[bh])
            fw.op(act, lambda e, ht=ht, ssq=ssq: e.activation(sq[:, :], ht[:, :], AF.Square, accum_out=ssq[:, :]), reads=[bh], writes=[b_sq, b_ssq])
            fw.op(act, lambda e, ssq=ssq: e.activation(ssq[:, :], ssq[:, :], AF.Sqrt, bias=EPS, scale=1.0 / D), writes=[b_ssq])
            fw.op(dve, lambda e, ssq=ssq: e.reciprocal(ssq[:, :], ssq[:, :]), writes=[b_ssq])
            fw.op(dve, lambda e, ht=ht, at=at, ssq=ssq, v=v: e.scalar_tensor_tensor(at[:, :], ht[:, :], ssq[:, 0:1], nsc[v][:, :], ALU.mult, ALU.mult),
                  reads=[bh, b_ssq, b_nsc[v]], writes=[ba])
            fw.op(pool, lambda e, at=at, v=v: e.tensor_tensor(at[:, :], at[:, :], sh[v][:, :], ALU.add), reads=[b_sh[v]], writes=[ba])
            for q in range(4):
                pt, pb = fw.next_psum()
                for i in range(4):
                    kc = q * 4 + i
                    fw.op(pe, lambda e, pt=pt, i=i, kc=kc, at=at: e.transpose(pt[:, i * 128:(i + 1) * 128], at[:, kc * 128:(kc + 1) * 128], ident[:, :]),
                          reads=[ba, b_ident], writes=[pb])
                dst = aT[:, q * 4:(q + 1) * 4, t * 128:(t + 1) * 128]
                src = pt[:, :].rearrange("p (a b) -> p a b", a=4)
                if q % 2 == 0:
                    fw.op(act, lambda e, dst=dst, src=src: e.copy(dst, src), reads=[pb], writes=[b_aT[t]])
                else:
                    fw.op(dve, lambda e, dst=dst, src=src: e.tensor_copy(dst, src), reads=[pb], writes=[b_aT[t]])
        fw.barrier()
    fw.stack = outer

from contextlib import ExitStack

D = 2048
CAP = 128
NE = 64
EPS = 1e-6


def bview(ap_row, n=128):
    return ap_row.partition_broadcast(n)


def build_phaseB(n_tiles, has_ctx, ctx=None):
    fused = ctx is not None
    nc = ctx["nc"] if fused else bass.Bass("TRN2", target_bir_lowering=False)
    pre = ctx["pre"] if fused else ""
    T = n_tiles * 128
    I = lambda name, shape, dt=F32: nc.dram_tensor(pre + name, shape, dt, kind="ExternalInput").ap()
    h_src = ctx.get("h_src") if fused else None
    h_in = I("h_in", [T, D]) if h_src is None else h_src[0]
    h_bufs = [] if h_src is None else h_src[1]
    mix_in = None if fused else I("mix_in", [T, 4096])
    gidx_d = I("gidx", [128, n_tiles, 2], I32) if fused else None
    cvec = I("cvec", [2, D])
    ada_w = I("ada_w", [D, 8192])
    ada_b = I("ada_b", [1, 8192])
    norm_w2 = I("norm_w2", [1, D])
    out_w = I("out_w", [4096, D])
    gwew = I("gwew", [D, 72])
    w1 = I("w1", [NE, D, 512])
    w3 = I("w3", [NE, D, 512])
    w2 = I("w2", [NE, 512, D])
    ident_d = I("ident", [128, 128])
    trilt_d = I("trilt", [128, 128])
    iota_d = I("iota64", [1, 64])
    out_fn = ctx.get("out_fn") if fused else None
    h_out = nc.dram_tensor("h_out", [T, D], F32, kind="ExternalOutput").ap() if out_fn is None else None
    modrows = nc.dram_tensor(pre + "modrows", [2, 8192], F32).ap()
    m_all = nc.dram_tensor(pre + "m_all", [T, D], F32).ap()
    h1_all = nc.dram_tensor(pre + "h1_all", [T, D], F32).ap()
    xdisp = nc.dram_tensor(pre + "xdisp", [NE * CAP, D], F32).ap()
    ybuf = nc.dram_tensor(pre + "ybuf", [NE * CAP, D], F32).ap()
    d_mod = Buf("modrows"); d_m = [Buf() for _ in range(n_tiles)]; d_h1 = [Buf() for _ in range(n_tiles)]
    d_x = Buf("xdisp"); d_y = [Buf() for _ in range(NE)]; d_out = [Buf() for _ in range(n_tiles)]
    d_xz = [Buf() for _ in range(NE * CAP // 512)]

    with ExitStack() as st0:
        if fused:
            fw = ctx["fw"]; fw.stack = st0; fw.ps_pool = list(range(8))
        else:
            fw = FW(nc, st0)
        pe, dve, act, pool, sp = fw.pe, fw.dve, fw.act, fw.pool, fw.sp
        ident = fw.sb([128, 128], F32, "ident"); b_ident = Buf()
        fw.dma(sp, ident[:], ident_d, writes=[b_ident])
        trilt = fw.sb([128, 128], BF16, "trilt"); b_tri = Buf()
        fw.dma(pool, trilt[:], trilt_d, writes=[b_tri])
        ones_bf = fw.sb([128, 128], BF16, "ones_bf"); b_ones = Buf()
        fw.op(pool, lambda e: e.memset(ones_bf[:, :], 1.0), writes=[b_ones])
        iota = fw.sb([128, 64], F32, "iota"); b_iota = Buf()
        fw.dma(sp, iota[:], bview(iota_d), writes=[b_iota])
        zcol = fw.sb([128, 1], F32, "zcol"); b_z = Buf()
        fw.op(pool, lambda e: e.memset(zcol[:, :], 0.0), writes=[b_z])
        dest_all = fw.sb([128, n_tiles, 2], I32, "dest_all"); b_dest = [Buf() for _ in range(n_tiles)]
        gate_all = fw.sb([128, n_tiles, 2], F32, "gate_all"); b_gate = [Buf() for _ in range(n_tiles)]
        with ExitStack() as st:
            fw.stack = st
            zt = fw.sb([128, 8192], F32, "zt"); b_zt = Buf()
            fw.op(pool, lambda e: e.memset(zt[:, :], 0.0), writes=[b_zt])
            xv = xdisp.rearrange("(a p r) d -> a p (r d)", p=128, r=4)
            for a in range(NE * CAP // 512):
                fw.dma(sp, xv[a], zt[:, :], reads=[b_zt], writes=[d_xz[a]])
            cv = fw.sb([2, D], F32, "cv"); b_cv = Buf()
            fw.dma(sp, cv[:], cvec, writes=[b_cv])
            siluT = fw.sb([128, 16, 2], BF16, "siluT"); b_sT = Buf()
            pt, pb = fw.next_psum()
            for kc in range(16):
                fw.op(pe, lambda e, kc=kc: e.transpose(pt[:, 2 * kc:2 * kc + 2], cv[0:2, kc * 128:(kc + 1) * 128], ident[0:2, 0:2]),
                      reads=[b_cv, b_ident], writes=[pb])
            fw.op(act, lambda e: e.activation(siluT[:, :, :].rearrange("p a b -> p (a b)"), pt[:, 0:32], AF.Silu), reads=[pb], writes=[b_sT])
            adab = fw.sb([2, 8192], F32, "adab"); b_adab = Buf()
            fw.dma(sp, adab[:], bview(ada_b, 2), writes=[b_adab])
            wts = [fw.sb([128, 16, 512], BF16, "adaw%d" % i) for i in range(2)]
            b_wts = [Buf() for _ in range(2)]
            modsb = fw.sb([2, 8192], F32, "modsb"); b_modsb = Buf()
            for j in range(16):
                wt, bw = wts[j % 2], b_wts[j % 2]
                fw.dma(pool, wt[:], ada_w[:, j * 512:(j + 1) * 512].rearrange("(kc p) c -> p kc c", p=128), writes=[bw])
                pt, pb = fw.next_psum()
                for kc in range(16):
                    fw.op(pe, lambda e, kc=kc, wt=wt, pt=pt: e.matmul(pt[0:2, :], siluT[:, kc, :], wt[:, kc, :], start=(kc == 0), stop=(kc == 15)),
                          reads=[b_sT, bw], writes=[pb])
                fw.op(dve, lambda e, pt=pt, j=j: e.tensor_tensor(modsb[:, j * 512:(j + 1) * 512], pt[0:2, :], adab[:, j * 512:(j + 1) * 512], ALU.add),
                      reads=[pb, b_adab], writes=[b_modsb])
            fw.dma(sp, modrows, modsb[:, :], reads=[b_modsb], writes=[d_mod])
            fw.barrier()
        with ExitStack() as st:
            fw.stack = st
            mixT = fw.sb([128, n_tiles, 32, 128], BF16, "mixT"); b_mixT = [Buf() for _ in range(n_tiles)]
            mts = [fw.sb([128, 4096], F32, "mixt%d" % i) for i in range(2)]; b_mts = [Buf() for _ in range(2)]
            if fused:
                gsrc, gbufs, _ = ctx["mix_src"]
                gix = fw.sb([128, n_tiles, 2], I32, "gix"); b_gix = Buf()
                fw.dma(sp, gix[:], gidx_d, writes=[b_gix])
                m16 = [fw.sb([128, 2048], BF16, "m16_%d" % i) for i in range(4)]; b_m16 = [Buf() for _ in range(4)]
            for t in range(n_tiles):
                mt, bm = mts[t % 2], b_mts[t % 2]
                if not fused:
                    fw.dma(sp, mt[:], mix_in[t * 128:(t + 1) * 128, :], writes=[bm])
                else:
                    for k in range(2):
                        mm, bmm = m16[(2 * t + k) % 4], b_m16[(2 * t + k) % 4]
                        fw.dma(pool, None, None, reads=gbufs + [b_gix], writes=[bmm],
                               fn=lambda e, mm=mm, t=t, k=k: e.indirect_dma_start(
                                   out=mm[:, :], out_offset=None, in_=gsrc[:, :],
                                   in_offset=bass.IndirectOffsetOnAxis(ap=gix[:, t, k:k + 1], axis=0),
                                   bounds_check=fw.bound_reg(gsrc.shape[0] - 1), oob_is_err=False))
                        if k == 0:
                            fw.op(act, lambda e, mm=mm, mt=mt: e.copy(mt[:, 0:2048], mm[:, :]), reads=[bmm], writes=[bm])
                        else:
                            fw.op(dve, lambda e, mm=mm, mt=mt: e.tensor_copy(mt[:, 2048:4096], mm[:, :]), reads=[bmm], writes=[bm])
                for q in range(8):
                    pt, pb = fw.next_psum()
                    for i in range(4):
                        kc = q * 4 + i
                        fw.op(pe, lambda e, pt=pt, i=i, kc=kc, mt=mt: e.transpose(pt[:, i * 128:(i + 1) * 128], mt[:, kc * 128:(kc + 1) * 128], ident[:, :]),
                              reads=[bm, b_ident], writes=[pb])
                    eng = act if q % 2 == 0 else dve
                    if eng is act:
                        fw.op(act, lambda e, pt=pt, t=t, q=q: e.copy(mixT[:, t, q * 4:(q + 1) * 4, :].rearrange("p a b -> p (a b)"), pt[:, :]),
                              reads=[pb], writes=[b_mixT[t]])
                    else:
                        fw.op(dve, lambda e, pt=pt, t=t, q=q: e.tensor_copy(mixT[:, t, q * 4:(q + 1) * 4, :].rearrange("p a b -> p (a b)"), pt[:, :]),
                              reads=[pb], writes=[b_mixT[t]])
            ows = [fw.sb([128, 32, 256], BF16, "ow%d" % i) for i in range(2)]; b_ows = [Buf() for _ in range(2)]
            mbs = [fw.sb([128, 256], F32, "mb%d" % i) for i in range(3)]; b_mbs = [Buf() for _ in range(3)]
            cnt = 0
            for j in range(8):
                ow, bo = ows[j % 2], b_ows[j % 2]
                fw.dma(pool, ow[:], out_w[:, j * 256:(j + 1) * 256].rearrange("(kc p) c -> p kc c", p=128), writes=[bo])
                for t in range(n_tiles):
                    pt, pb = fw.next_psum()
                    for kc in range(32):
                        fw.op(pe, lambda e, pt=pt, kc=kc, t=t, ow=ow: e.matmul(pt[:, 0:256], mixT[:, t, kc, :], ow[:, kc, :], start=(kc == 0), stop=(kc == 31)),
                              reads=[b_mixT[t], bo], writes=[pb])
                    mb, bmb = mbs[cnt % 3], b_mbs[cnt % 3]; cnt += 1
                    if cnt % 2:
                        fw.op(act, lambda e, pt=pt, mb=mb: e.copy(mb[:, :], pt[:, 0:256]), reads=[pb], writes=[bmb])
                    else:
                        fw.op(dve, lambda e, pt=pt, mb=mb: e.tensor_copy(mb[:, :], pt[:, 0:256]), reads=[pb], writes=[bmb])
                    fw.dma(sp, m_all[t * 128:(t + 1) * 128, j * 256:(j + 1) * 256], mb[:, :], reads=[bmb], writes=[d_m[t]])
            fw.barrier()
        with ExitStack() as st:
            fw.stack = st
            nvar = 2 if has_ctx else 1
            g1 = []; nsc = []; sh2 = []
            b_g1 = []; b_nsc = []; b_sh2 = []
            nw = fw.sb([128, D], F32, "nw"); b_nw = Buf()
            fw.dma(sp, nw[:], bview(norm_w2), writes=[b_nw])
            for v in range(nvar):
                a = fw.sb([128, D], F32, "g1_%d" % v); ba = Buf()
                fw.dma(sp, a[:], bview(modrows[v:v + 1, 0:2048]), reads=[d_mod], writes=[ba])
                g1.append(a); b_g1.append(ba)
                s = fw.sb([128, D], F32, "sh2_%d" % v); bs = Buf()
                fw.dma(sp, s[:], bview(modrows[v:v + 1, 2048:4096]), reads=[d_mod], writes=[bs])
                sh2.append(s); b_sh2.append(bs)
                c = fw.sb([128, D], F32, "nsc_%d" % v); bc = Buf()
                fw.dma(sp, c[:], bview(modrows[v:v + 1, 4096:6144]), reads=[d_mod], writes=[bc])
                fw.op(dve, lambda e, c=c: e.scalar_tensor_tensor(c[:, :], c[:, :], 1.0, nw[:, :], ALU.add, ALU.mult), reads=[b_nw], writes=[bc])
                nsc.append(c); b_nsc.append(bc)
            gw = fw.sb([128, 16, 72], F32, "gw"); b_gw = Buf()
            fw.dma(sp, gw[:], gwew.rearrange("(kc p) c -> p kc c", p=128), writes=[b_gw])
            acum = fw.sb([128, 64], F32, "acum"); b_acum = Buf()
            fw.op(pool, lambda e: e.memset(acum[:, :], 0.0), writes=[b_acum])
            acum_bf = fw.sb([128, 64], BF16, "acum_bf"); b_acbf = Buf()
            fw.op(pool, lambda e: e.memset(acum_bf[:, :], 0.0), writes=[b_acbf])
            NB = 2
            hts = [fw.sb([128, D], F32, "ht%d" % i) for i in range(NB)]; b_hts = [Buf() for _ in range(NB)]
            mts = [fw.sb([128, D], F32, "mt%d" % i) for i in range(NB)]; b_mts = [Buf() for _ in range(NB)]
            fts = [fw.sb([128, D], F32, "ft%d" % i) for i in range(NB)]; b_fts = [Buf() for _ in range(NB)]
            fTs = [fw.sb([128, 16, 128], F32, "fT%d" % i) for i in range(NB)]; b_fTs = [Buf() for _ in range(NB)]
            sq = fw.sb([128, D], F32, "sq"); b_sq = Buf()
            sm = {}
            def S(name, shape, dt=F32):
                if name not in sm:
                    sm[name] = (fw.sb(shape, dt, "r_" + name), Buf(name))
                return sm[name]
            for t in range(n_tiles):
                v = 1 if (has_ctx and t == 0) else 0
                ht, bh = hts[t % NB], b_hts[t % NB]
                mt, bm = mts[t % NB], b_mts[t % NB]
                ft, bf = fts[t % NB], b_fts[t % NB]
                fT, bfT = fTs[t % NB], b_fTs[t % NB]
                fw.dma(sp, ht[:], h_in[t * 128:(t + 1) * 128, :], reads=h_bufs, writes=[bh])
                fw.dma(sp, mt[:], m_all[t * 128:(t + 1) * 128, :], reads=[d_m[t]], writes=[bm])
                fw.op(pool, lambda e, mt=mt, v=v: e.tensor_tensor(mt[:, :], mt[:, :], g1[v][:, :], ALU.mult), reads=[b_g1[v]], writes=[bm])
                fw.op(dve, lambda e, mt=mt, ht=ht: e.tensor_tensor(ht[:, :], mt[:, :], ht[:, :], ALU.add), reads=[bm], writes=[bh])
                fw.dma(sp, h1_all[t * 128:(t + 1) * 128, :], ht[:, :], reads=[bh], writes=[d_h1[t]])
                ssq, b_ssq = S("ssq", [128, 1])
                fw.op(act, lambda e, ht=ht: e.activation(sq[:, :], ht[:, :], AF.Square, accum_out=ssq[:, :]), reads=[bh], writes=[b_sq, b_ssq])
                rstd, b_rstd = S("rstd", [128, 1])
                fw.op(act, lambda e: e.activation(rstd[:, :], ssq[:, :], AF.Sqrt, bias=EPS, scale=1.0 / D), reads=[b_ssq], writes=[b_rstd])
                fw.op(dve, lambda e: e.reciprocal(rstd[:, :], rstd[:, :]), writes=[b_rstd])
                fw.op(dve, lambda e, ht=ht, ft=ft, v=v: e.scalar_tensor_tensor(ft[:, :], ht[:, :], rstd[:, 0:1], nsc[v][:, :], ALU.mult, ALU.mult),
                      reads=[bh, b_rstd, b_nsc[v]], writes=[bf])
                fw.op(pool, lambda e, ft=ft, v=v: e.tensor_tensor(ft[:, :], ft[:, :], sh2[v][:, :], ALU.add), reads=[b_sh2[v]], writes=[bf])
                for q in range(4):
                    pt, pb = fw.next_psum()
                    for i in range(4):
                        kc = q * 4 + i
                        fw.op(pe, lambda e, pt=pt, i=i, kc=kc, ft=ft: e.transpose(pt[:, i * 128:(i + 1) * 128], ft[:, kc * 128:(kc + 1) * 128], ident[:, :]),
                              reads=[bf, b_ident], writes=[pb])
                    if q % 2 == 0:
                        fw.op(act, lambda e, pt=pt, q=q, fT=fT: e.copy(fT[:, q * 4:(q + 1) * 4, :].rearrange("p a b -> p (a b)"), pt[:, :]), reads=[pb], writes=[bfT])
                    else:
                        fw.op(dve, lambda e, pt=pt, q=q, fT=fT: e.tensor_copy(fT[:, q * 4:(q + 1) * 4, :].rearrange("p a b -> p (a b)"), pt[:, :]), reads=[pb], writes=[bfT])
                pl, pbl = fw.next_psum()
                for kc in range(16):
                    fw.op(pe, lambda e, kc=kc, fT=fT, pl=pl: e.matmul(pl[:, 0:72], fT[:, kc, :], gw[:, kc, :], start=(kc == 0), stop=(kc == 15)),
                          reads=[bfT, b_gw], writes=[pbl])
                lg, b_lg = S("lg", [128, 72])
                fw.op(dve, lambda e, pl=pl: e.tensor_copy(lg[:, :], pl[:, 0:72]), reads=[pbl], writes=[b_lg])
                g8, b_g8 = S("g8", [128, 8])
                fw.op(dve, lambda e: e.max(g8[:, :], lg[:, 0:8]), reads=[b_lg], writes=[b_g8])
                ngm, b_ngm = S("ngm", [128, 1])
                fw.op(dve, lambda e: e.tensor_scalar(ngm[:, :], g8[:, 0:1], -1.0, None, ALU.mult), reads=[b_g8], writes=[b_ngm])
                gex, b_gex = S("gex", [128, 8]); gsum, b_gsum = S("gsum", [128, 1])
                fw.op(act, lambda e: e.activation(gex[:, :], lg[:, 0:8], AF.Exp, bias=ngm[:, 0:1], scale=1.0, accum_out=gsum[:, :]),
                      reads=[b_lg, b_ngm], writes=[b_gex, b_gsum])
                ggate, b_gg = S("ggate", [128, 1])
                fw.op(dve, lambda e: e.reciprocal(ggate[:, :], gsum[:, :]), reads=[b_gsum], writes=[b_gg])
                pen, b_pen = S("pen", [128, 8])
                fw.op(dve, lambda e: e.tensor_scalar(pen[:, :], lg[:, 0:8], g8[:, 0:1], zcol[:, 0:1], ALU.is_equal, ALU.add), reads=[b_lg, b_g8, b_z], writes=[b_pen])
                fw.op(dve, lambda e: e.tensor_scalar(pen[:, :], pen[:, :], -1.0, 1e9, ALU.add, ALU.mult), writes=[b_pen])
                lem, b_lem = S("lem", [128, 64])
                fw.op(dve, lambda e: e.tensor_tensor(lem[:, :].rearrange("p (g e) -> p g e", g=8), lg[:, 8:72].rearrange("p (g e) -> p g e", g=8),
                                                     pen[:, :].unsqueeze(2).to_broadcast([128, 8, 8]), ALU.add), reads=[b_lg, b_pen], writes=[b_lem])
                t8, b_t8 = S("t8", [128, 8]); i8, b_i8 = S("i8", [128, 8], U32)
                fw.op(dve, lambda e: e.max(t8[:, :], lem[:, :]), reads=[b_lem], writes=[b_t8])
                fw.op(dve, lambda e: e.max_index(i8[:, :], t8[:, :], lem[:, :]), reads=[b_lem, b_t8], writes=[b_i8])
                ef, b_ef = S("ef", [128, 2])
                fw.op(dve, lambda e: e.tensor_copy(ef[:, :], i8[:, 0:2]), reads=[b_i8], writes=[b_ef])
                dd, b_dd = S("dd", [128, 1])
                fw.op(dve, lambda e: e.tensor_tensor(dd[:, :], t8[:, 1:2], t8[:, 0:1], ALU.subtract), reads=[b_t8], writes=[b_dd])
                ex, b_ex = S("ex", [128, 1])
                fw.op(act, lambda e: e.activation(ex[:, :], dd[:, :], AF.Exp), reads=[b_dd], writes=[b_ex])
                den, b_den = S("den", [128, 1])
                fw.op(dve, lambda e: e.tensor_scalar(den[:, :], ex[:, :], 1.0, None, ALU.add), reads=[b_ex], writes=[b_den])
                fw.op(dve, lambda e: e.reciprocal(den[:, :], den[:, :]), writes=[b_den])
                fw.op(dve, lambda e, t=t: e.tensor_tensor(gate_all[:, t, 0:1], den[:, :], ggate[:, :], ALU.mult), reads=[b_den, b_gg], writes=[b_gate[t]])
                fw.op(dve, lambda e, t=t: e.tensor_tensor(gate_all[:, t, 1:2], gate_all[:, t, 0:1], ex[:, :], ALU.mult), reads=[b_ex], writes=[b_gate[t]])
                A0, b_A0 = S("A0", [128, 64]); A1, b_A1 = S("A1", [128, 64]); A, b_A = S("A", [128, 64]); Abf, b_Abf = S("Abf", [128, 64], BF16)
                fw.op(dve, lambda e: e.tensor_scalar(A0[:, :], iota[:, :], ef[:, 0:1], None, ALU.is_equal), reads=[b_iota, b_ef], writes=[b_A0])
                fw.op(dve, lambda e: e.tensor_scalar(A1[:, :], iota[:, :], ef[:, 1:2], None, ALU.is_equal), reads=[b_iota, b_ef], writes=[b_A1])
                fw.op(dve, lambda e: e.tensor_tensor(A[:, :], A0[:, :], A1[:, :], ALU.add), reads=[b_A0, b_A1], writes=[b_A])
                fw.op(dve, lambda e: e.tensor_copy(Abf[:, :], A[:, :]), reads=[b_A], writes=[b_Abf])
                pr, pbr = fw.next_psum()
                fw.op(pe, lambda e, pr=pr: e.matmul(pr[:, 0:64], trilt[:, :], Abf[:, :], start=True, stop=False), reads=[b_tri, b_Abf], writes=[pbr])
                fw.op(pe, lambda e, pr=pr: e.matmul(pr[:, 0:64], ones_bf[:, :], acum_bf[:, :], start=False, stop=True), reads=[b_ones, b_acbf], writes=[pbr])
                rk, b_rk = S("rk", [128, 2]); tmp, b_tmp = S("tmp", [128, 64])
                for k, (Ak, bAk) in enumerate(((A0, b_A0), (A1, b_A1))):
                    fw.op(dve, lambda e, Ak=Ak, pr=pr: e.tensor_tensor(tmp[:, :], Ak[:, :], pr[:, 0:64], ALU.mult), reads=[bAk, pbr], writes=[b_tmp])
                    fw.op(dve, lambda e, k=k: e.reduce_sum(rk[:, k:k + 1], tmp[:, :], axis=AX.X), reads=[b_tmp], writes=[b_rk])
                fw.op(dve, lambda e: e.tensor_tensor(acum[:, :], acum[:, :], A[:, :], ALU.add), reads=[b_A], writes=[b_acum])
                fw.op(dve, lambda e: e.tensor_copy(acum_bf[:, :], acum[:, :]), reads=[b_acum], writes=[b_acbf])
                df, b_df = S("df", [128, 2]); ov, b_ov = S("ov", [128, 2])
                fw.op(dve, lambda e: e.tensor_scalar(ov[:, :], rk[:, :], float(CAP), 1e6, ALU.is_ge, ALU.mult), reads=[b_rk], writes=[b_ov])
                fw.op(dve, lambda e: e.scalar_tensor_tensor(df[:, :], ef[:, :], float(CAP), rk[:, :], ALU.mult, ALU.add), reads=[b_ef, b_rk], writes=[b_df])
                fw.op(dve, lambda e: e.tensor_tensor(df[:, :], df[:, :], ov[:, :], ALU.add), reads=[b_ov], writes=[b_df])
                fw.op(dve, lambda e, t=t: e.tensor_copy(dest_all[:, t, :], df[:, :]), reads=[b_df], writes=[b_dest[t]])
                for k in range(2):
                    fw.dma(pool, None, None, reads=[bf, b_dest[t]] + d_xz, writes=[d_x],
                           fn=lambda e, t=t, k=k, ft=ft: e.indirect_dma_start(
                               out=xdisp[:, :], out_offset=bass.IndirectOffsetOnAxis(ap=dest_all[:, t, k:k + 1], axis=0),
                               in_=ft[:, :], in_offset=None, bounds_check=fw.bound_reg(NE * CAP - 1), oob_is_err=False))
            fw.barrier()
        with ExitStack() as st:
            fw.stack = st
            NW = 2
            w1s = [fw.sb([128, 16, 512], BF16, "w1s%d" % i) for i in range(NW)]; b_w1s = [Buf() for _ in range(NW)]
            w3s = [fw.sb([128, 16, 512], BF16, "w3s%d" % i) for i in range(NW)]; b_w3s = [Buf() for _ in range(NW)]
            w2s = [fw.sb([128, 4, D], BF16, "w2s%d" % i) for i in range(NW)]; b_w2s = [Buf() for _ in range(NW)]
            xes = [fw.sb([128, D], F32, "xe%d" % i) for i in range(2)]; b_xes = [Buf() for _ in range(2)]
            xTs = [fw.sb([128, 16, 128], BF16, "xT%d" % i) for i in range(2)]; b_xTs = [Buf() for _ in range(2)]
            sil = fw.sb([128, 512], F32, "sil"); b_sil = Buf()
            hact = fw.sb([128, 512], F32, "hact"); b_hact = Buf()
            hT = fw.sb([128, 4, 128], BF16, "hT"); b_hT = Buf()
            yes = [fw.sb([128, D], F32, "ye%d" % i) for i in range(2)]; b_yes = [Buf() for _ in range(2)]
            for ex_ in range(NE):
                i2 = ex_ % 2
                w1t, bw1 = w1s[ex_ % NW], b_w1s[ex_ % NW]
                w3t, bw3 = w3s[ex_ % NW], b_w3s[ex_ % NW]
                w2t, bw2 = w2s[ex_ % NW], b_w2s[ex_ % NW]
                fw.dma(pool, w1t[:], w1[ex_].rearrange("(kc p) h -> p kc h", p=128), writes=[bw1])
                fw.dma(pool, w3t[:], w3[ex_].rearrange("(kc p) h -> p kc h", p=128), writes=[bw3])
                fw.dma(pool, w2t[:], w2[ex_].rearrange("(hc p) d -> p hc d", p=128), writes=[bw2])
                xe, bxe = xes[i2], b_xes[i2]
                xT, bxT = xTs[i2], b_xTs[i2]
                fw.dma(sp, xe[:], xdisp[ex_ * CAP:(ex_ + 1) * CAP, :], reads=[d_x, d_xz[ex_ // 4]], writes=[bxe])
                for q in range(4):
                    pt, pb = fw.next_psum()
                    for i in range(4):
                        kc = q * 4 + i
                        fw.op(pe, lambda e, pt=pt, i=i, kc=kc, xe=xe: e.transpose(pt[:, i * 128:(i + 1) * 128], xe[:, kc * 128:(kc + 1) * 128], ident[:, :]),
                              reads=[bxe, b_ident], writes=[pb])
                    if q % 2 == 0:
                        fw.op(act, lambda e, pt=pt, q=q, xT=xT: e.copy(xT[:, q * 4:(q + 1) * 4, :].rearrange("p a b -> p (a b)"), pt[:, :]), reads=[pb], writes=[bxT])
                    else:
                        fw.op(dve, lambda e, pt=pt, q=q, xT=xT: e.tensor_copy(xT[:, q * 4:(q + 1) * 4, :].rearrange("p a b -> p (a b)"), pt[:, :]), reads=[pb], writes=[bxT])
                p1, pb1 = fw.next_psum()
                for kc in range(16):
                    fw.op(pe, lambda e, kc=kc, p1=p1, xT=xT, w1t=w1t: e.matmul(p1[:, :], xT[:, kc, :], w1t[:, kc, :], start=(kc == 0), stop=(kc == 15)),
                          reads=[bxT, bw1], writes=[pb1])
                p3, pb3 = fw.next_psum()
                for kc in range(16):
                    fw.op(pe, lambda e, kc=kc, p3=p3, xT=xT, w3t=w3t: e.matmul(p3[:, :], xT[:, kc, :], w3t[:, kc, :], start=(kc == 0), stop=(kc == 15)),
                          reads=[bxT, bw3], writes=[pb3])
                fw.op(act, lambda e, p1=p1: e.activation(sil[:, :], p1[:, :], AF.Silu), reads=[pb1], writes=[b_sil])
                fw.op(dve, lambda e, p3=p3: e.tensor_tensor(hact[:, :], sil[:, :], p3[:, :], ALU.mult), reads=[b_sil, pb3], writes=[b_hact])
                pt, pb = fw.next_psum()
                for hc in range(4):
                    fw.op(pe, lambda e, pt=pt, hc=hc: e.transpose(pt[:, hc * 128:(hc + 1) * 128], hact[:, hc * 128:(hc + 1) * 128], ident[:, :]),
                          reads=[b_hact, b_ident], writes=[pb])
                fw.op(act, lambda e, pt=pt: e.copy(hT[:, :, :].rearrange("p a b -> p (a b)"), pt[:, :]), reads=[pb], writes=[b_hT])
                ye, bye = yes[i2], b_yes[i2]
                for db in range(4):
                    py, pby = fw.next_psum()
                    for hc in range(4):
                        fw.op(pe, lambda e, py=py, hc=hc, db=db, w2t=w2t: e.matmul(py[:, :], hT[:, hc, :], w2t[:, hc, db * 512:(db + 1) * 512], start=(hc == 0), stop=(hc == 3)),
                              reads=[b_hT, bw2], writes=[pby])
                    if db % 2 == 0:
                        fw.op(dve, lambda e, py=py, db=db, ye=ye: e.tensor_copy(ye[:, db * 512:(db + 1) * 512], py[:, :]), reads=[pby], writes=[bye])
                    else:
                        fw.op(act, lambda e, py=py, db=db, ye=ye: e.copy(ye[:, db * 512:(db + 1) * 512], py[:, :]), reads=[pby], writes=[bye])
                fw.dma(sp, ybuf[ex_ * CAP:(ex_ + 1) * CAP, :], ye[:, :], reads=[bye], writes=[d_y[ex_]])
            fw.barrier()
        with ExitStack() as st:
            fw.stack = st
            nvar = 2 if has_ctx else 1
            g2 = []; b_g2 = []
            for v in range(nvar):
                a = fw.sb([128, D], F32, "g2_%d" % v); ba = Buf()
                fw.dma(sp, a[:], bview(modrows[v:v + 1, 6144:8192]), reads=[d_mod], writes=[ba])
                g2.append(a); b_g2.append(ba)
            r0s = [fw.sb([128, D], F32, "r0_%d" % i) for i in range(2)]; b_r0s = [Buf() for _ in range(2)]
            r1s = [fw.sb([128, D], F32, "r1_%d" % i) for i in range(2)]; b_r1s = [Buf() for _ in range(2)]
            h1s = [fw.sb([128, D], F32, "h1_%d" % i) for i in range(2)]; b_h1s = [Buf() for _ in range(2)]
            for t in range(n_tiles):
                v = 1 if (has_ctx and t == 0) else 0
                r0, br0 = r0s[t % 2], b_r0s[t % 2]
                r1, br1 = r1s[t % 2], b_r1s[t % 2]
                h1, bh1 = h1s[t % 2], b_h1s[t % 2]
                fw.dma(sp, h1[:], h1_all[t * 128:(t + 1) * 128, :], reads=[d_h1[t]], writes=[bh1])
                for k, (r, br) in enumerate(((r0, br0), (r1, br1))):
                    fw.op(pool, lambda e, r=r: e.memset(r[:, :], 0.0), writes=[br])
                    fw.dma(pool, None, None, reads=d_y + [b_dest[t]], writes=[br],
                           fn=lambda e, r=r, t=t, k=k: e.indirect_dma_start(
                               out=r[:, :], out_offset=None, in_=ybuf[:, :],
                               in_offset=bass.IndirectOffsetOnAxis(ap=dest_all[:, t, k:k + 1], axis=0),
                               bounds_check=fw.bound_reg(NE * CAP - 1), oob_is_err=False))
                fw.op(dve, lambda e, r0=r0, t=t: e.tensor_scalar(r0[:, :], r0[:, :], gate_all[:, t, 0:1], None, ALU.mult), reads=[b_gate[t]], writes=[br0])
                fw.op(dve, lambda e, r0=r0, r1=r1, t=t: e.scalar_tensor_tensor(r0[:, :], r1[:, :], gate_all[:, t, 1:2], r0[:, :], ALU.mult, ALU.add),
                      reads=[br1, b_gate[t]], writes=[br0])
                fw.op(pool, lambda e, r0=r0, v=v: e.tensor_tensor(r0[:, :], r0[:, :], g2[v][:, :], ALU.mult), reads=[b_g2[v]], writes=[br0])
                fw.op(dve, lambda e, r0=r0, h1=h1: e.tensor_tensor(h1[:, :], h1[:, :], r0[:, :], ALU.add), reads=[br0], writes=[bh1])
                if out_fn is None:
                    fw.dma(sp, h_out[t * 128:(t + 1) * 128, :], h1[:, :], reads=[bh1], writes=[d_out[t]])
                else:
                    d_out[t] = out_fn(fw, t, h1, bh1)
            if not fused:
                fw.finish(d_out)
            else:
                fw.barrier()
        print("phaseB ops", fw.n_ops, "waits", fw.n_waits)
    if fused:
        return [b for x in d_out for b in (x if isinstance(x, list) else [x])]
    return nc

from contextlib import ExitStack

NT = 18
NTOK = NT * 128
Z0, X0, B0, C0, DT0, Q0, K0, V0, NCOL = 0, 1024, 2048, 2304, 2560, 2592, 3616, 4640, 5664
TB = [(0, 256)] + [(256 + i * 512, 512) for i in range(4)]


def build_phaseA0(stop_after=None, dbg=False, ctx=None):
    fused = ctx is not None
    nc = ctx["nc"] if fused else bass.Bass("TRN2", target_bir_lowering=False)
    pre = ctx["pre"] if fused else ""
    out_fn = ctx["out_fn"] if fused else None
    I = lambda name, shape, dt=F32: nc.dram_tensor(pre + name, shape, dt, kind="ExternalInput").ap()
    h_all = I("h_all", [NTOK, D])
    cvec = I("cvec", [2, D])
    ada_w = I("ada_w", [D, 4096]); ada_b = I("ada_b", [1, 4096])
    norm_w1 = I("norm_w1", [1, D])
    w_in = I("w_in", [D, NCOL])
    convw = I("convw", [1536, 5]); convb = I("convb", [128, 12])
    dt_bias = I("dt_bias", [1, 32]); a_log = I("a_log", [1, 32]); d_skip = I("d_skip", [1, 16]); ssd_nw = I("ssd_nw", [1, 1024])
    qnw = I("qnw", [128, 1]); knw = I("knw", [128, 1])
    biasT = I("biasT", [8, 25, 128, 128])
    ident_d = I("ident", [128, 128]); ule_d = I("ule", [128, 128]); uge_d = I("uge", [128, 128])
    mix = None if fused else nc.dram_tensor("mix_part", [NTOK, 2048], F32, kind="ExternalOutput").ap()
    modrows = nc.dram_tensor(pre + "modrows", [2, 4096], F32).ap()
    Hb_d = nc.dram_tensor(pre + "Hb_d", [NT, 128, 512], BF16).ap()
    d_mod = Buf("modrows"); d_mix = []

    with ExitStack() as st0:
        if fused:
            fw = ctx["fw"]; fw.stack = st0; fw.ps_pool = list(range(8))
        else:
            fw = FW(nc, st0)
        pe, dve, act, pool, sp = fw.pe, fw.dve, fw.act, fw.pool, fw.sp
        ident, b_ident = emit_consts(fw, ident_d)
        ule = fw.sb([128, 128], F32, "ule"); b_ule = Buf(); fw.dma(sp, ule[:], ule_d, writes=[b_ule])
        uge = fw.sb([128, 128], F32, "uge"); b_uge = Buf(); fw.dma(sp, uge[:], uge_d, writes=[b_uge])
        ones = fw.sb([128, 128], F32, "ones"); b_ones = Buf(); fw.op(pool, lambda e: e.memset(ones[:, :], 1.0), writes=[b_ones])
        identb = fw.sb([128, 128], BF16, "identb"); b_identb = Buf(); fw.dma(pool, identb[:], ident_d, writes=[b_identb])
        zcol = fw.sb([128, 1], F32, "zcol"); b_z = Buf(); fw.op(pool, lambda e: e.memset(zcol[:, :], 0.0), writes=[b_z])
        aT = fw.sb([128, 16, NTOK], BF16, "aT"); b_aT = [Buf() for _ in range(NT)]
        emit_mods(fw, cvec, ada_w, ada_b, 4096, modrows, d_mod, ident, b_ident)
        emit_aT(fw, h_all, NT, 2, norm_w1, modrows, d_mod, aT, b_aT, ident, b_ident)

        def tiles_of(s, n):
            return list(range(s // 128, (s + n) // 128))

        with ExitStack() as st:
            fw.stack = st
            wdt = fw.sb([128, 16, 32], BF16, "wdt"); b_wdt = Buf()
            fw.dma(pool, wdt[:], w_in[:, DT0:DT0 + 32].rearrange("(kc p) c -> p kc c", p=128), writes=[b_wdt])
            dtb = fw.sb([128, 32], F32, "dtb"); b_dtb = Buf(); fw.dma(sp, dtb[:], bview(dt_bias), writes=[b_dtb])
            Aneg = fw.sb([128, 32], F32, "Aneg"); b_A = Buf(); fw.dma(sp, Aneg[:], bview(a_log), writes=[b_A])
            fw.op(act, lambda e: e.activation(Aneg[:, :], Aneg[:, :], AF.Exp), writes=[b_A])
            fw.op(dve, lambda e: e.tensor_scalar(Aneg[:, :], Aneg[:, :], -1.0, None, ALU.mult), writes=[b_A])
            dsk = fw.sb([128, 16], F32, "dsk"); b_dsk = Buf(); fw.dma(sp, dsk[:], bview(d_skip), writes=[b_dsk])
            snw = fw.sb([128, 1024], F32, "snw"); b_snw = Buf(); fw.dma(sp, snw[:], bview(ssd_nw), writes=[b_snw])
            cw = fw.sb([128, 12, 5], F32, "cw"); b_cw = Buf(); fw.dma(sp, cw[:], convw.rearrange("(ct p) k -> p ct k", p=128), writes=[b_cw])
            cb = fw.sb([128, 12], F32, "cb"); b_cb = Buf(); fw.dma(sp, cb[:], convb, writes=[b_cb])
            def T3(name):
                return fw.sb([128, NT, 32], F32, name), [Buf() for _ in range(NT)]
            dt_all, b_dt = T3("dt_all"); a_all, b_a = T3("a_all"); negcs, b_ncs = T3("negcs")
            dfs, b_dfs = T3("dfs"); dte, b_dte = T3("dte"); etot, b_etot = T3("etot")
            tmpA = fw.sb([128, 32], F32, "tmpA"); b_tA = Buf(); tmpB = fw.sb([128, 32], F32, "tmpB"); b_tB = Buf()
            tmpC = fw.sb([128, 32], F32, "tmpC"); b_tC = Buf()
            for t in range(NT):
                pt, pb = fw.next_psum()
                for kc in range(16):
                    fw.op(pe, lambda e, kc=kc, pt=pt, t=t: e.matmul(pt[:, 0:32], aT[:, kc, t * 128:(t + 1) * 128], wdt[:, kc, :], start=(kc == 0), stop=(kc == 15)),
                          reads=[b_aT[t], b_wdt], writes=[pb])
                fw.op(dve, lambda e, pt=pt: e.tensor_tensor(tmpA[:, :], pt[:, 0:32], dtb[:, :], ALU.add), reads=[pb, b_dtb], writes=[b_tA])
                fw.op(act, lambda e: e.activation(tmpB[:, :], tmpA[:, :], AF.Abs), reads=[b_tA], writes=[b_tB])
                fw.op(act, lambda e: e.activation(tmpB[:, :], tmpB[:, :], AF.Exp, scale=-1.0), writes=[b_tB])
                fw.op(act, lambda e: e.activation(tmpB[:, :], tmpB[:, :], AF.Ln, bias=1.0, scale=1.0), writes=[b_tB])
                fw.op(dve, lambda e: e.tensor_scalar(tmpA[:, :], tmpA[:, :], 0.0, None, ALU.max), writes=[b_tA])
                fw.op(dve, lambda e, t=t: e.tensor_tensor(dt_all[:, t, :], tmpA[:, :], tmpB[:, :], ALU.add), reads=[b_tA, b_tB], writes=[b_dt[t]])
                fw.op(dve, lambda e, t=t: e.tensor_tensor(a_all[:, t, :], dt_all[:, t, :], Aneg[:, :], ALU.mult), reads=[b_dt[t], b_A], writes=[b_a[t]])
                pc_, pbc = fw.next_psum()
                fw.op(pe, lambda e, pc_=pc_, t=t: e.matmul(pc_[:, 0:16], ule[:, :], a_all[:, t, 0:16], start=True, stop=True), reads=[b_ule, b_a[t]], writes=[pbc])
                fw.op(pe, lambda e, pc_=pc_, t=t: e.matmul(pc_[:, 16:32], uge[:, :], a_all[:, t, 16:32], start=True, stop=True), reads=[b_uge, b_a[t]], writes=[pbc])
                fw.op(pe, lambda e, pc_=pc_, t=t: e.matmul(pc_[:, 32:64], ones[:, :], a_all[:, t, :], start=True, stop=True), reads=[b_ones, b_a[t]], writes=[pbc])
                fw.op(dve, lambda e, pc_=pc_, t=t: e.tensor_scalar(negcs[:, t, :], pc_[:, 0:32], -1.0, None, ALU.mult), reads=[pbc], writes=[b_ncs[t]])
                fw.op(act, lambda e, pc_=pc_, t=t: e.activation(dfs[:, t, :], pc_[:, 0:32], AF.Exp), reads=[pbc], writes=[b_dfs[t]])
                fw.op(act, lambda e, pc_=pc_, t=t: e.activation(etot[:, t, :], pc_[:, 32:64], AF.Exp), reads=[pbc], writes=[b_etot[t]])
                fw.op(dve, lambda e, pc_=pc_, t=t: e.tensor_tensor(tmpC[:, :], pc_[:, 32:64], negcs[:, t, :], ALU.add), reads=[pbc, b_ncs[t]], writes=[b_tC])
                fw.op(act, lambda e, t=t: e.activation(dte[:, t, :], tmpC[:, :], AF.Exp), reads=[b_tC], writes=[b_dte[t]])

            xs = fw.sb([128, NT, 512], BF16, "xs"); b_xs = [Buf() for _ in range(NT)]
            Btok = fw.sb([128, NT, 128], BF16, "Btok"); b_Btok = [Buf() for _ in range(NT)]
            BT = fw.sb([128, NTOK], BF16, "BT"); b_BT = Buf()
            CT = fw.sb([128, NTOK], BF16, "CT"); b_CT = Buf()
            d_Hb = [Buf() for _ in range(NT)]
            Hbs = [fw.sb([128, 512], BF16, "Hbs%d" % i) for i in range(2)]; b_Hbs = [Buf() for _ in range(2)]
            pcs = [fw.sb([128, 2312], F32, "pc%d" % i) for i in range(1)]; b_pcs = [Buf() for _ in range(1)]
            for i in range(1):
                fw.op(pool, lambda e, i=i: e.memset(pcs[i][:, :], 0.0), writes=[b_pcs[i]])
            acc = fw.sb([128, 2308], F32, "acc"); b_acc = Buf()
            wcs = [fw.sb([128, 16, 128], BF16, "wc%d" % i) for i in range(2)]; b_wcs = [Buf() for _ in range(2)]
            wz = fw.sb([128, 16, 512], BF16, "wz"); b_wz = Buf()
            H = fw.sb([128, 512], F32, "H"); b_H = Buf()
            Hbf = fw.sb([128, 512], BF16, "Hbf"); b_Hbf = Buf()
            coef = fw.sb([128, 8], F32, "coef"); b_coef = Buf()
            Xe = fw.sb([128, 512], BF16, "Xe"); b_Xe = Buf()
            Xtf = fw.sb([128, 512], BF16, "Xtf"); b_Xtf = Buf()
            Xtb = fw.sb([128, 512], BF16, "Xtb"); b_Xtb = Buf()
            CBf = fw.sb([128, 128], F32, "CBf"); b_CBf = Buf()
            CBb = fw.sb([128, 128], F32, "CBb"); b_CBb = Buf()
            Es = [fw.sb([128, 128], F32, "E%d" % i) for i in range(3)]; b_Es = [Buf() for _ in range(3)]
            Ls = [fw.sb([128, 128], F32, "L%d" % i) for i in range(3)]; b_Ls = [Buf() for _ in range(3)]
            Ms = [fw.sb([128, 128], BF16, "M%d" % i) for i in range(3)]; b_Ms = [Buf() for _ in range(3)]
            t1 = fw.sb([128, 512], F32, "t1"); b_t1 = Buf()
            t2 = fw.sb([128, 512], F32, "t2"); b_t2 = Buf()
            sz = fw.sb([128, 512], F32, "sz"); b_sz = Buf()
            gy = fw.sb([128, 512], F32, "gy"); b_gy = Buf()
            ssq = fw.sb([128, 1], F32, "ssq_s"); b_ssq = Buf()
            outs = [fw.sb([128, 512], F32, "o%d" % i) for i in range(2)]; b_outs = [Buf() for _ in range(2)]
            wcnt = 0
            lm = 0

            def bc8(ap):
                return ap.unsqueeze(2).to_broadcast([128, 8, 64])

            def v3(ap):
                return ap.rearrange("p (h d) -> p h d", h=8)

            for g in range(1 if dbg else 2):
                hf = g * 8
                hb = 16 + g * 8
                fw.dma(pool, wz[:], w_in[:, Z0 + g * 512:Z0 + (g + 1) * 512].rearrange("(kc p) c -> p kc c", p=128), writes=[b_wz])
                for ci in range(6):
                    if ci < 4:
                        col0 = X0 + g * 512 + ci * 128; ct = g * 4 + ci
                    elif ci == 4:
                        col0 = B0 + g * 128; ct = 8 + g
                    else:
                        col0 = C0 + g * 128; ct = 10 + g
                    wc, bwc = wcs[wcnt % 2], b_wcs[wcnt % 2]
                    pc, bpc = pcs[0], b_pcs[0]
                    wcnt += 1
                    fw.dma(pool, wc[:], w_in[:, col0:col0 + 128].rearrange("(kc p) c -> p kc c", p=128), writes=[bwc])
                    for (s0, n) in TB:
                        pt, pb = fw.next_psum()
                        for kc in range(16):
                            fw.op(pe, lambda e, kc=kc, pt=pt, wc=wc, s0=s0, n=n: e.matmul(pt[:, 0:n], wc[:, kc, :], aT[:, kc, s0:s0 + n], start=(kc == 0), stop=(kc == 15)),
                                  reads=[bwc] + [b_aT[t] for t in tiles_of(s0, n)], writes=[pb])
                        off = 2 if s0 == 0 else 6
                        fw.op(act, lambda e, pt=pt, pc=pc, s0=s0, n=n, off=off: e.copy(pc[:, s0 + off:s0 + off + n], pt[:, 0:n]), reads=[pb], writes=[bpc])
                    fw.op(dve, lambda e, pc=pc, ct=ct: e.tensor_scalar(acc[:, :], pc[:, 0:2308], cw[:, ct, 0:1], cb[:, ct:ct + 1], ALU.mult, ALU.add),
                          reads=[bpc, b_cw, b_cb], writes=[b_acc])
                    for k in range(1, 5):
                        eng = dve
                        fw.op(eng, lambda e, pc=pc, ct=ct, k=k: e.scalar_tensor_tensor(acc[:, :], pc[:, k:k + 2308], cw[:, ct, k:k + 1], acc[:, :], ALU.mult, ALU.add),
                              reads=[bpc, b_cw], writes=[b_acc])
                    if ci == 5:
                        fw.op(act, lambda e: e.activation(CT[:, 0:256], acc[:, 0:256], AF.Silu), reads=[b_acc], writes=[b_CT])
                        fw.op(act, lambda e: e.activation(CT[:, 256:NTOK], acc[:, 260:2308], AF.Silu), reads=[b_acc], writes=[b_CT])
                        continue
                    fw.op(act, lambda e: e.activation(acc[:, 0:256], acc[:, 0:256], AF.Silu), writes=[b_acc])
                    fw.op(act, lambda e: e.activation(acc[:, 260:2308], acc[:, 260:2308], AF.Silu), writes=[b_acc])
                    if ci == 4:
                        fw.op(pool, lambda e: e.tensor_copy(BT[:, 0:256], acc[:, 0:256]), reads=[b_acc], writes=[b_BT])
                        fw.op(pool, lambda e: e.tensor_copy(BT[:, 256:NTOK], acc[:, 260:2308]), reads=[b_acc], writes=[b_BT])
                    for q in range(5):
                        ts = list(range(q * 4, min(NT, q * 4 + 4)))
                        pt, pb = fw.next_psum()
                        for i, t in enumerate(ts):
                            a0 = t * 128 if t < 2 else 260 + (t - 2) * 128
                            fw.op(pe, lambda e, pt=pt, i=i, a0=a0: e.transpose(pt[:, i * 128:(i + 1) * 128], acc[:, a0:a0 + 128], ident[:, :]),
                                  reads=[b_acc, b_ident], writes=[pb])
                        n = len(ts)
                        src = pt[:, 0:n * 128].rearrange("p (a b) -> p a b", a=n)
                        if ci < 4:
                            dstv = xs[:, ts[0]:ts[0] + n, ci * 128:(ci + 1) * 128]; bl = [b_xs[t] for t in ts]
                        else:
                            dstv = Btok[:, ts[0]:ts[0] + n, :]; bl = [b_Btok[t] for t in ts]
                        if q % 2 == 0:
                            fw.op(dve, lambda e, dstv=dstv, src=src: e.tensor_copy(dstv, src), reads=[pb], writes=bl)
                        else:
                            fw.op(act, lambda e, dstv=dstv, src=src: e.copy(dstv, src), reads=[pb], writes=bl)

                fw.op(pool, lambda e: e.memset(H[:, :], 0.0), writes=[b_H])
                border = [1, 0] + list(range(17, 1, -1))
                for bi_, c in enumerate(border):
                    hs_, bhs_ = Hbs[bi_ % 2], b_Hbs[bi_ % 2]
                    fw.op(act, lambda e, hs_=hs_: e.copy(hs_[:, :], H[:, :]), reads=[b_H], writes=[bhs_])
                    fw.dma(sp, Hb_d[c], hs_[:, :], reads=[bhs_], writes=[d_Hb[c]])
                    fw.op(dve, lambda e, c=c: e.tensor_tensor(coef[:, :], dt_all[:, c, hb:hb + 8], dte[:, c, hb:hb + 8], ALU.mult), reads=[b_dt[c], b_dte[c]], writes=[b_coef])
                    fw.op(dve, lambda e, c=c: e.tensor_tensor(v3(Xe[:, :]), v3(xs[:, c, :]), bc8(coef[:, :]), ALU.mult), reads=[b_xs[c], b_coef], writes=[b_Xe])
                    ps, pbs = fw.next_psum()
                    fw.op(pe, lambda e, ps=ps, c=c: e.matmul(ps[:, :], Btok[:, c, :], Xe[:, :], start=True, stop=True), reads=[b_Btok[c], b_Xe], writes=[pbs])
                    fw.op(pool, lambda e, c=c: e.tensor_tensor(v3(H[:, :]), v3(H[:, :]), bc8(etot[:, c, hb:hb + 8]), ALU.mult), reads=[b_etot[c]], writes=[b_H])
                    fw.op(dve, lambda e, ps=ps: e.tensor_tensor(H[:, :], H[:, :], ps[:, :], ALU.add), reads=[pbs], writes=[b_H])

                fw.op(pool, lambda e: e.memset(H[:, :], 0.0), writes=[b_H])
                for c in range(NT):
                    cs_ = slice(c * 128, (c + 1) * 128)
                    hs_, bhs_ = Hbs[c % 2], b_Hbs[c % 2]
                    fw.dma(sp, hs_[:, :], Hb_d[c], reads=[d_Hb[c]], writes=[bhs_])
                    fw.op(act, lambda e: e.copy(Hbf[:, :], H[:, :]), reads=[b_H], writes=[b_Hbf])
                    fw.op(dve, lambda e, c=c: e.tensor_tensor(coef[:, :], dt_all[:, c, hf:hf + 8], dte[:, c, hf:hf + 8], ALU.mult), reads=[b_dt[c], b_dte[c]], writes=[b_coef])
                    fw.op(dve, lambda e, c=c: e.tensor_tensor(v3(Xe[:, :]), v3(xs[:, c, :]), bc8(coef[:, :]), ALU.mult), reads=[b_xs[c], b_coef], writes=[b_Xe])
                    fw.op(pool, lambda e, c=c: e.tensor_tensor(v3(Xtf[:, :]), v3(xs[:, c, :]), bc8(dt_all[:, c, hf:hf + 8]), ALU.mult), reads=[b_xs[c], b_dt[c]], writes=[b_Xtf])
                    fw.op(pool, lambda e, c=c: e.tensor_tensor(v3(Xtb[:, :]), v3(xs[:, c, :]), bc8(dt_all[:, c, hb:hb + 8]), ALU.mult), reads=[b_xs[c], b_dt[c]], writes=[b_Xtb])
                    pyf, pbyf = fw.next_psum()
                    fw.op(pe, lambda e, pyf=pyf, cs_=cs_: e.matmul(pyf[:, :], CT[:, cs_], Hbf[:, :], start=True, stop=True), reads=[b_CT, b_Hbf], writes=[pbyf])
                    pyb, pbyb = fw.next_psum()
                    fw.op(pe, lambda e, pyb=pyb, cs_=cs_, hs_=hs_: e.matmul(pyb[:, :], CT[:, cs_], hs_[:, :], start=True, stop=True), reads=[b_CT, bhs_], writes=[pbyb])
                    fw.op(dve, lambda e, pyf=pyf, c=c: e.tensor_tensor(v3(t1[:, :]), v3(pyf[:, :]), bc8(dfs[:, c, hf:hf + 8]), ALU.mult), reads=[pbyf, b_dfs[c]], writes=[b_t1])
                    fw.op(dve, lambda e, pyb=pyb, c=c: e.tensor_tensor(v3(t2[:, :]), v3(pyb[:, :]), bc8(dfs[:, c, hb:hb + 8]), ALU.mult), reads=[pbyb, b_dfs[c]], writes=[b_t2])
                    fw.op(pool, lambda e: e.tensor_tensor(t1[:, :], t1[:, :], t2[:, :], ALU.add), reads=[b_t2], writes=[b_t1])
                    fw.op(pool, lambda e, c=c: e.tensor_tensor(v3(t2[:, :]), v3(xs[:, c, :]), bc8(dsk[:, g * 8:(g + 1) * 8]), ALU.mult), reads=[b_xs[c], b_dsk], writes=[b_t2])
                    fw.op(pool, lambda e: e.tensor_tensor(t1[:, :], t1[:, :], t2[:, :], ALU.add), reads=[b_t2], writes=[b_t1])
                    ps, pbs = fw.next_psum()
                    fw.op(pe, lambda e, ps=ps, c=c: e.matmul(ps[:, :], Btok[:, c, :], Xe[:, :], start=True, stop=True), reads=[b_Btok[c], b_Xe], writes=[pbs])
                    fw.op(pool, lambda e, c=c: e.tensor_tensor(v3(H[:, :]), v3(H[:, :]), bc8(etot[:, c, hf:hf + 8]), ALU.mult), reads=[b_etot[c], b_Hbf], writes=[b_H])
                    fw.op(dve, lambda e, ps=ps: e.tensor_tensor(H[:, :], H[:, :], ps[:, :], ALU.add), reads=[pbs], writes=[b_H])
                    pcb, pbcb = fw.next_psum()
                    fw.op(pe, lambda e, pcb=pcb, cs_=cs_: e.matmul(pcb[:, 0:128], BT[:, cs_], CT[:, cs_], start=True, stop=True), reads=[b_BT, b_CT], writes=[pbcb])
                    fw.op(dve, lambda e, pcb=pcb: e.tensor_tensor(CBf[:, :], pcb[:, 0:128], ule[:, :], ALU.mult), reads=[pbcb, b_ule], writes=[b_CBf])
                    fw.op(dve, lambda e, pcb=pcb: e.tensor_tensor(CBb[:, :], pcb[:, 0:128], uge[:, :], ALU.mult), reads=[pbcb, b_uge], writes=[b_CBb])
                    pyd, pbyd = fw.next_psum()
                    for hq in range(4):
                        pr, pbr = fw.next_psum()
                        combos = [(h, d_) for h in (2 * hq, 2 * hq + 1) for d_ in (0, 1)]
                        for i, (h, d_) in enumerate(combos):
                            col = (hf if d_ == 0 else hb) + h
                            U = ule if d_ == 0 else uge
                            bU = b_ule if d_ == 0 else b_uge
                            fw.op(pe, lambda e, pr=pr, i=i, c=c, col=col, U=U: e.matmul(pr[:, i * 128:(i + 1) * 128], a_all[:, c, col:col + 1].to_broadcast([128, 128]), U[:, :], start=True, stop=True),
                                  reads=[b_a[c], bU], writes=[pbr])
                        for i, (h, d_) in enumerate(combos):
                            col = (hf if d_ == 0 else hb) + h
                            E_, bE = Es[lm % 3], b_Es[lm % 3]; L_, bL = Ls[lm % 3], b_Ls[lm % 3]; M_, bM = Ms[lm % 3], b_Ms[lm % 3]; lm += 1
                            CB_, bCB = (CBf, b_CBf) if d_ == 0 else (CBb, b_CBb)
                            Xt_, bXt = (Xtf, b_Xtf) if d_ == 0 else (Xtb, b_Xtb)
                            fw.op(dve, lambda e, pr=pr, i=i, c=c, col=col, E_=E_: e.tensor_scalar(E_[:, :], pr[:, i * 128:(i + 1) * 128], negcs[:, c, col:col + 1], zcol[:, 0:1], ALU.add, ALU.min),
                                  reads=[pbr, b_ncs[c], b_z], writes=[bE])
                            fw.op(act, lambda e, E_=E_, L_=L_: e.activation(L_[:, :], E_[:, :], AF.Exp), reads=[bE], writes=[bL])
                            fw.op(pool, lambda e, L_=L_, M_=M_, CB_=CB_: e.tensor_tensor(M_[:, :], L_[:, :], CB_[:, :], ALU.mult), reads=[bL, bCB], writes=[bM])
                            fw.op(pe, lambda e, pyd=pyd, h=h, M_=M_, Xt_=Xt_, d_=d_: e.matmul(pyd[:, h * 64:(h + 1) * 64], M_[:, :], Xt_[:, h * 64:(h + 1) * 64], start=(d_ == 0), stop=(d_ == 1)),
                                  reads=[bM, bXt], writes=[pbyd])
                    if dbg and c == NT - 1:
                        dd = {}
                        for nm, pp, pbb in (("pyd", pyd, pbyd),):
                            tt = fw.sb([128, 512], F32, "dbg_" + nm); bb = Buf()
                            fw.op(dve, lambda e, tt=tt, pp=pp: e.tensor_copy(tt[:, :], pp[:, :]), reads=[pbb], writes=[bb])
                            dd[nm] = (tt, bb)
                        fw.dbgd = dd
                    fw.op(dve, lambda e, pyd=pyd: e.tensor_tensor(t1[:, :], t1[:, :], pyd[:, :], ALU.add), reads=[pbyd], writes=[b_t1])
                    pz, pbz = fw.next_psum()
                    for kc in range(16):
                        fw.op(pe, lambda e, kc=kc, pz=pz, cs_=cs_: e.matmul(pz[:, :], aT[:, kc, cs_], wz[:, kc, :], start=(kc == 0), stop=(kc == 15)),
                              reads=[b_aT[c], b_wz], writes=[pbz])
                    fw.op(act, lambda e, pz=pz: e.activation(sz[:, :], pz[:, :], AF.Silu), reads=[pbz], writes=[b_sz])
                    fw.op(dve, lambda e: e.tensor_tensor(gy[:, :], t1[:, :], sz[:, :], ALU.mult), reads=[b_t1, b_sz], writes=[b_gy])
                    fw.op(act, lambda e: e.activation(t2[:, :], gy[:, :], AF.Square, accum_out=ssq[:, :]), reads=[b_gy], writes=[b_t2, b_ssq])
                    fw.op(act, lambda e: e.activation(ssq[:, :], ssq[:, :], AF.Sqrt, bias=EPS, scale=1.0 / 512), writes=[b_ssq])
                    fw.op(dve, lambda e: e.reciprocal(ssq[:, :], ssq[:, :]), writes=[b_ssq])
                    o_, bo = outs[c % 2], b_outs[c % 2]
                    fw.op(dve, lambda e, o_=o_: e.scalar_tensor_tensor(o_[:, :], gy[:, :], ssq[:, 0:1], snw[:, g * 512:(g + 1) * 512], ALU.mult, ALU.mult),
                          reads=[b_gy, b_ssq, b_snw], writes=[bo])
                    if fused:
                        d_mix.extend(out_fn(fw, c * 128, g * 512, 512, o_, bo))
                    else:
                        db = Buf(); d_mix.append(db)
                        fw.dma(sp, mix[c * 128:(c + 1) * 128, g * 512:(g + 1) * 512], o_[:, :], reads=[bo], writes=[db])
            if dbg:
                def dump(name, ap, shape, dt, bufs):
                    o = nc.dram_tensor("dbg_" + name, shape, dt, kind="ExternalOutput").ap()
                    db = Buf(); d_mix.append(db)
                    fw.dma(sp, o, ap, reads=bufs, writes=[db])
                dump("aT0", aT[:, 0, :], [128, NTOK], BF16, b_aT)
                dump("dt", dt_all[:, :, :], [128, NT, 32], F32, b_dt)
                dump("negcs", negcs[:, :, :], [128, NT, 32], F32, b_ncs)
                dump("dfs", dfs[:, :, :], [128, NT, 32], F32, b_dfs)
                dump("dte", dte[:, :, :], [128, NT, 32], F32, b_dte)
                dump("etot", etot[:, :, :], [128, NT, 32], F32, b_etot)
                dump("xs", xs[:, :, :], [128, NT, 512], BF16, b_xs)
                dump("Btok", Btok[:, :, :], [128, NT, 128], BF16, b_Btok)
                dump("BT", BT[:, :], [128, NTOK], BF16, [b_BT])
                dump("CT", CT[:, :], [128, NTOK], BF16, [b_CT])
                dump("H", H[:, :], [128, 512], F32, [b_H])
                dump("t1", t1[:, :], [128, 512], F32, [b_t1])
                dump("gy", gy[:, :], [128, 512], F32, [b_gy])
                for nm, (tt, bb) in fw.dbgd.items():
                    dump(nm, tt[:, :], [128, 512], F32, [bb])
                dump("CBf", CBf[:, :], [128, 128], F32, [b_CBf])
                dump("CBb", CBb[:, :], [128, 128], F32, [b_CBb])
                dump("Elast", Es[(lm - 1) % 3][:, :], [128, 128], F32, [b_Es[(lm - 1) % 3]])
                dump("Llast", Ls[(lm - 1) % 3][:, :], [128, 128], F32, [b_Ls[(lm - 1) % 3]])
                dump("Mlast", Ms[(lm - 1) % 3][:, :], [128, 128], BF16, [b_Ms[(lm - 1) % 3]])
                dump("Xtf", Xtf[:, :], [128, 512], BF16, [b_Xtf])
                dump("Xtb", Xtb[:, :], [128, 512], BF16, [b_Xtb])
            fw.barrier()

        if stop_after != "ssd":
          with ExitStack() as st:
            fw.stack = st
            V1 = fw.sb([128, NT, 8, 129], BF16, "V1"); b_V1 = [Buf() for _ in range(NT)]
            fw.op(pool, lambda e: e.memset(V1[:, :, :, :].rearrange("p a b c -> p (a b c)"), 1.0), writes=b_V1)
            with ExitStack() as st2:
                fw.stack = st2
                wv = fw.sb([128, 16, 1024], BF16, "wv"); b_wv = Buf()
                fw.dma(pool, wv[:], w_in[:, V0:V0 + 1024].rearrange("(kc p) c -> p kc c", p=128), writes=[b_wv])
                for t in range(NT):
                    for hh_ in range(2):
                        pt, pb = fw.next_psum()
                        for kc in range(16):
                            fw.op(pe, lambda e, kc=kc, pt=pt, t=t, hh_=hh_: e.matmul(pt[:, :], aT[:, kc, t * 128:(t + 1) * 128], wv[:, kc, hh_ * 512:(hh_ + 1) * 512], start=(kc == 0), stop=(kc == 15)),
                                  reads=[b_aT[t], b_wv], writes=[pb])
                        dstv = V1[:, t, hh_ * 4:(hh_ + 1) * 4, 0:128]
                        src = pt[:, :].rearrange("p (a b) -> p a b", a=4)
                        if hh_ == 0:
                            fw.op(act, lambda e, dstv=dstv, src=src: e.copy(dstv, src), reads=[pb], writes=[b_V1[t]])
                        else:
                            fw.op(dve, lambda e, dstv=dstv, src=src: e.tensor_copy(dstv, src), reads=[pb], writes=[b_V1[t]])
                fw.barrier()
            fw.stack = st
            qw = fw.sb([128, 1], F32, "qw"); b_qw = Buf(); fw.dma(sp, qw[:], qnw, writes=[b_qw])
            fw.op(dve, lambda e: e.tensor_scalar(qw[:, :], qw[:, :], float(128 ** -0.5), None, ALU.mult), writes=[b_qw])
            kw = fw.sb([128, 1], F32, "kw"); b_kw = Buf(); fw.dma(sp, kw[:], knw, writes=[b_kw])
            qTs = [fw.sb([128, NTOK], BF16, "qT%d" % i) for i in range(2)]; b_qTs = [Buf() for _ in range(2)]
            kTs = [fw.sb([128, NTOK], BF16, "kT%d" % i) for i in range(2)]; b_kTs = [Buf() for _ in range(2)]
            wqs = [fw.sb([128, 16, 128], BF16, "wq%d" % i) for i in range(2)]; b_wqs = [Buf() for _ in range(2)]
            wks = [fw.sb([128, 16, 128], BF16, "wk%d" % i) for i in range(2)]; b_wks = [Buf() for _ in range(2)]
            bts = [fw.sb([128, 25, 128], BF16, "bt%d" % i) for i in range(2)]; b_bts = [Buf() for _ in range(2)]
            sqs = [fw.sb([128, 512], F32, "sq%d" % i) for i in range(2)]; b_sqs = [Buf() for _ in range(2)]
            rs = [fw.sb([128, 512], F32, "rs%d" % i) for i in range(2)]; b_rs = [Buf() for _ in range(2)]
            PTs = [fw.sb([128, 7, 128], BF16, "PT%d" % i) for i in range(3)]; b_PTs = [Buf() for _ in range(3)]
            rec = [fw.sb([128, 1], F32, "rec%d" % i) for i in range(2)]; b_rec = [Buf() for _ in range(2)]
            ons = [fw.sb([128, 128], F32, "on%d" % i) for i in range(3)]; b_ons = [Buf() for _ in range(3)]
            cnt = 0; pcnt = 0
            for hd in range(8):
                i2 = hd % 2
                qT, bqT = qTs[i2], b_qTs[i2]; kT, bkT = kTs[i2], b_kTs[i2]
                wq, bwq = wqs[i2], b_wqs[i2]; wk, bwk = wks[i2], b_wks[i2]
                bt, bbt = bts[i2], b_bts[i2]
                fw.dma(pool, wq[:], w_in[:, Q0 + hd * 128:Q0 + (hd + 1) * 128].rearrange("(kc p) c -> p kc c", p=128), writes=[bwq])
                fw.dma(pool, wk[:], w_in[:, K0 + hd * 128:K0 + (hd + 1) * 128].rearrange("(kc p) c -> p kc c", p=128), writes=[bwk])
                fw.dma(pool, bt[:], biasT[hd].rearrange("c k q -> k c q"), writes=[bbt])
                blks = []
                for (wt_, bwt_, dstT, bdst, nwc, bnw) in ((wq, bwq, qT, bqT, qw, b_qw), (wk, bwk, kT, bkT, kw, b_kw)):
                    for (s0, n) in TB:
                        blk = {}

                        def s1(blk=blk, wt_=wt_, bwt_=bwt_, s0=s0, n=n):
                            nonlocal cnt
                            pt, pb = fw.next_psum(); blk["pt"] = (pt, pb); blk["i"] = cnt % 2; cnt += 1
                            for kc in range(16):
                                fw.op(pe, lambda e: e.matmul(pt[:, 0:n], wt_[:, kc, :], aT[:, kc, s0:s0 + n], start=(kc == 0), stop=(kc == 15)),
                                      reads=[bwt_] + [b_aT[t] for t in tiles_of(s0, n)], writes=[pb])
                            sq_, bsq = sqs[blk["i"]], b_sqs[blk["i"]]
                            fw.op(act, lambda e: e.activation(sq_[:, 0:n], pt[:, 0:n], AF.Square), reads=[pb], writes=[bsq])

                        def s2(blk=blk, dstT=dstT, bdst=bdst, nwc=nwc, bnw=bnw, s0=s0, n=n):
                            pt, pb = blk["pt"]; i_ = blk["i"]
                            sq_, bsq = sqs[i_], b_sqs[i_]; r_, br = rs[i_], b_rs[i_]
                            p2, pb2 = fw.next_psum()
                            fw.op(pe, lambda e: e.matmul(p2[:, 0:n], ones[:, :], sq_[:, 0:n], start=True, stop=True), reads=[b_ones, bsq], writes=[pb2])
                            fw.op(act, lambda e: e.activation(r_[:, 0:n], p2[:, 0:n], AF.Sqrt, bias=EPS, scale=1.0 / 128), reads=[pb2], writes=[br])
                            fw.op(dve, lambda e: e.reciprocal(r_[:, 0:n], r_[:, 0:n]), writes=[br])
                            fw.op(dve, lambda e: e.tensor_tensor(r_[:, 0:n], pt[:, 0:n], r_[:, 0:n], ALU.mult), reads=[pb], writes=[br])
                            fw.op(pool, lambda e: e.tensor_scalar(dstT[:, s0:s0 + n], r_[:, 0:n], nwc[:, 0:1], None, ALU.mult), reads=[br, bnw], writes=[bdst])
                        blks.append((s1, s2))
                nb = len(blks)
                blks[0][0](); blks[1][0]()
                for i in range(nb):
                    blks[i][1]()
                    if i + 2 < nb:
                        blks[i + 2][0]()

                def keys_of(tq):
                    if tq < 2:
                        return [(0, None), (1, None)]
                    j = tq - 2
                    a = min(max(2 * j - 4, 0), 22)
                    cls = 0 if j == 0 else 1 if j == 1 else 3 if j == 14 else 4 if j == 15 else 2
                    return [(2 + a // 2 + i, cls * 5 + i) for i in range(5)] + [(0, None), (1, None)]

                def att1(tq):
                    nonlocal pcnt
                    keys = keys_of(tq)
                    PT, bPT = PTs[pcnt % 3], b_PTs[pcnt % 3]
                    on, bon = ons[pcnt % 3], b_ons[pcnt % 3]
                    rc, brc = rec[pcnt % 2], b_rec[pcnt % 2]; pcnt += 1
                    nk = len(keys)
                    for b0 in range(0, nk, 4):
                        pS, pbS = fw.next_psum()
                        grp = keys[b0:b0 + 4]
                        for i, (kt, bi) in enumerate(grp):
                            fw.op(pe, lambda e: e.matmul(pS[:, i * 128:(i + 1) * 128], kT[:, kt * 128:(kt + 1) * 128], qT[:, tq * 128:(tq + 1) * 128], start=True, stop=(bi is None)),
                                  reads=[bkT, bqT], writes=[pbS])
                            if bi is not None:
                                fw.op(pe, lambda e: e.matmul(pS[:, i * 128:(i + 1) * 128], identb[:, :], bt[:, bi, :], start=False, stop=True),
                                      reads=[b_identb, bbt], writes=[pbS])
                        ng = len(grp)
                        fw.op(act, lambda e: e.activation(PT[:, b0:b0 + ng, :].rearrange("p a b -> p (a b)"), pS[:, 0:ng * 128], AF.Exp),
                              reads=[pbS], writes=[bPT])
                    return (tq, keys, PT, bPT, on, bon, rc, brc)

                def att2(st_):
                    tq, keys, PT, bPT, on, bon, rc, brc = st_
                    nk = len(keys)
                    pO, pbO = fw.next_psum()
                    for i, (kt, bi) in enumerate(keys):
                        fw.op(pe, lambda e: e.matmul(pO[:, 0:129], PT[:, i, :], V1[:, kt, hd, :], start=(i == 0), stop=(i == nk - 1)),
                              reads=[bPT, b_V1[kt]], writes=[pbO])
                    fw.op(dve, lambda e: e.reciprocal(rc[:, :], pO[:, 128:129]), reads=[pbO], writes=[brc])
                    fw.op(dve, lambda e: e.tensor_scalar(on[:, :], pO[:, 0:128], rc[:, 0:1], None, ALU.mult), reads=[pbO, brc], writes=[bon])
                    if fused:
                        d_mix.extend(out_fn(fw, tq * 128, 1024 + hd * 128, 128, on, bon))
                    else:
                        db = Buf(); d_mix.append(db)
                        fw.dma(sp, mix[tq * 128:(tq + 1) * 128, 1024 + hd * 128:1024 + (hd + 1) * 128], on[:, :], reads=[bon], writes=[db])

                cur = att1(0)
                for tq in range(NT):
                    nxt = att1(tq + 1) if tq + 1 < NT else None
                    att2(cur)
                    cur = nxt
            fw.barrier()
        if not fused:
            fw.finish(d_mix)
        print("phaseA0 ops", fw.n_ops, "waits", fw.n_waits)
    return d_mix if fused else nc

import math
from contextlib import ExitStack

NT = 18
NTOK = NT * 128
NLAT = 2048
GQ0, DQ0, GK0, DK0, GV0, DV0, NCOL1 = 0, 1024, 2048, 2304, 3328, 3584, 4608
TB = [(0, 256)] + [(256 + i * 512, 512) for i in range(4)]
LAMBDA_INIT = 0.8 - 0.6 * math.exp(-0.3 * 1)


def build_phaseA1(ctx=None):
    fused = ctx is not None
    nc = ctx["nc"] if fused else bass.Bass("TRN2", target_bir_lowering=False)
    pre = ctx["pre"] if fused else ""
    out_fn = ctx["out_fn"] if fused else None
    I = lambda name, shape, dt=F32: nc.dram_tensor(pre + name, shape, dt, kind="ExternalInput").ap()
    h_src = ctx.get("h_src") if fused else None
    h_all = I("h_all", [NTOK, D]) if h_src is None else h_src[0]
    h_bufs = () if h_src is None else h_src[1]
    row_of = None if h_src is None else h_src[2]
    cvec = I("cvec", [2, D])
    ada_w = I("ada_w", [D, 4096]); ada_b = I("ada_b", [1, 4096])
    norm_w1 = I("norm_w1", [1, D])
    w_in = I("w_in", [D, NCOL1])
    nws = I("nws", [128, 4])
    lamv = I("lamv", [1, 512])
    subln = I("subln", [1, 256])
    cosT_d = I("cosT", [128, NLAT]); sinT_d = I("sinT", [128, NLAT]); rmT_d = I("rmT", [128, 128])
    ident_d = I("ident", [128, 128])
    mix = None if fused else nc.dram_tensor("mix_part", [NLAT, 2048], F32, kind="ExternalOutput").ap()
    modrows = nc.dram_tensor(pre + "modrows", [2, 4096], F32).ap()
    d_mod = Buf("modrows"); d_mix = []

    with ExitStack() as st0:
        if fused:
            fw = ctx["fw"]; fw.stack = st0; fw.ps_pool = list(range(8))
        else:
            fw = FW(nc, st0)
        pe, dve, act, pool, sp = fw.pe, fw.dve, fw.act, fw.pool, fw.sp
        ident, b_ident = emit_consts(fw, ident_d)
        ones = fw.sb([128, 128], F32, "ones"); b_ones = Buf(); fw.op(pool, lambda e: e.memset(ones[:, :], 1.0), writes=[b_ones])
        aT = fw.sb([128, 16, NTOK], BF16, "aT"); b_aT = [Buf() for _ in range(NT)]
        emit_mods(fw, cvec, ada_w, ada_b, 4096, modrows, d_mod, ident, b_ident)
        emit_aT(fw, h_all, NT, 2, norm_w1, modrows, d_mod, aT, b_aT, ident, b_ident, row_of=row_of, h_bufs=h_bufs)

        def tiles_of(s, n):
            return list(range(s // 128, (s + n) // 128))

        with ExitStack() as st:
            fw.stack = st
            V1g = fw.sb([128, NT, 2, 129], BF16, "V1g"); V1d = fw.sb([128, NT, 4, 257], BF16, "V1d"); b_V = [Buf() for _ in range(NT)]
            fw.op(pool, lambda e: e.memset(V1g[:, :, :, :].rearrange("p a b c -> p (a b c)"), 1.0), writes=b_V)
            fw.op(pool, lambda e: e.memset(V1d[:, :, :, :].rearrange("p a b c -> p (a b c)"), 1.0), writes=b_V)
            with ExitStack() as st2:
                fw.stack = st2
                wv = fw.sb([128, 16, 1280], BF16, "wv"); b_wv = Buf()
                fw.dma(pool, wv[:], w_in[:, GV0:GV0 + 1280].rearrange("(kc p) c -> p kc c", p=128), writes=[b_wv])
                for t in range(NT):
                    for part in range(3):
                        c0 = part * 512; n = 512 if part < 2 else 256
                        pt, pb = fw.next_psum()
                        for kc in range(16):
                            fw.op(pe, lambda e, kc=kc, pt=pt, t=t, c0=c0, n=n: e.matmul(pt[:, 0:n], aT[:, kc, t * 128:(t + 1) * 128], wv[:, kc, c0:c0 + n], start=(kc == 0), stop=(kc == 15)),
                                  reads=[b_aT[t], b_wv], writes=[pb])
                        if part == 0:
                            fw.op(act, lambda e, pt=pt, t=t: e.copy(V1g[:, t, :, 0:128], pt[:, 0:256].rearrange("p (a b) -> p a b", a=2)), reads=[pb], writes=[b_V[t]])
                            fw.op(dve, lambda e, pt=pt, t=t: e.tensor_copy(V1d[:, t, 0, 0:256], pt[:, 256:512]), reads=[pb], writes=[b_V[t]])
                        elif part == 1:
                            fw.op(act, lambda e, pt=pt, t=t: e.copy(V1d[:, t, 1:3, 0:256], pt[:, 0:512].rearrange("p (a b) -> p a b", a=2)), reads=[pb], writes=[b_V[t]])
                        else:
                            fw.op(dve, lambda e, pt=pt, t=t: e.tensor_copy(V1d[:, t, 3, 0:256], pt[:, 0:256]), reads=[pb], writes=[b_V[t]])
                fw.barrier()
            fw.stack = st
            fw.ps_pool = [0, 1, 2, 3]
            cosT = fw.sb([128, NLAT], F32, "cosT"); b_cos = Buf(); fw.dma(sp, cosT[:], cosT_d, writes=[b_cos])
            sinT = fw.sb([128, NLAT], F32, "sinT"); b_sin = Buf(); fw.dma(sp, sinT[:], sinT_d, writes=[b_sin])
            rmT = fw.sb([128, 128], F32, "rmT"); b_rm = Buf(); fw.dma(sp, rmT[:], rmT_d, writes=[b_rm])
            nw4 = fw.sb([128, 4], F32, "nw4"); b_nw4 = Buf(); fw.dma(sp, nw4[:], nws, writes=[b_nw4])
            nwq = fw.sb([128, 4], F32, "nwq"); b_nwq = Buf()
            fw.op(dve, lambda e: e.tensor_scalar(nwq[:, :], nw4[:, :], float(128 ** -0.5), None, ALU.mult), reads=[b_nw4], writes=[b_nwq])
            sub = fw.sb([128, 256], F32, "sub"); b_sub = Buf(); fw.dma(sp, sub[:], bview(subln), writes=[b_sub])
            fw.op(dve, lambda e: e.tensor_scalar(sub[:, :], sub[:, :], float(1.0 - LAMBDA_INIT), None, ALU.mult), writes=[b_sub])
            lv = fw.sb([128, 512], F32, "lv"); b_lv = Buf(); fw.dma(sp, lv[:], bview(lamv), writes=[b_lv])
            lt = fw.sb([128, 256], F32, "lt"); b_lt = Buf()
            fw.op(dve, lambda e: e.tensor_tensor(lt[:, :].rearrange("p (a b) -> p a b", a=2), lv[:, :].rearrange("p (a c b) -> p a c b", a=2, c=2)[:, :, 0, :],
                                                 lv[:, :].rearrange("p (a c b) -> p a c b", a=2, c=2)[:, :, 1, :], ALU.mult), reads=[b_lv], writes=[b_lt])
            ld = fw.sb([128, 2], F32, "ld"); b_ld = Buf()
            fw.op(dve, lambda e: e.reduce_sum(ld[:, :], lt[:, :].rearrange("p (a b) -> p a b", a=2), axis=AX.X), reads=[b_lt], writes=[b_ld])
            fw.op(act, lambda e: e.activation(ld[:, :], ld[:, :], AF.Exp), writes=[b_ld])
            nlam = fw.sb([128, 1], F32, "nlam"); b_nlam = Buf()
            fw.op(dve, lambda e: e.tensor_tensor(nlam[:, :], ld[:, 1:2], ld[:, 0:1], ALU.subtract), reads=[b_ld], writes=[b_nlam])
            fw.op(dve, lambda e: e.tensor_scalar(nlam[:, :], nlam[:, :], float(-LAMBDA_INIT), None, ALU.add), writes=[b_nlam])

            sqs = [fw.sb([128, 512], F32, "sq%d" % i) for i in range(2)]; b_sqs = [Buf() for _ in range(2)]
            rs = [fw.sb([128, 512], F32, "rs%d" % i) for i in range(2)]; b_rs = [Buf() for _ in range(2)]
            xws = [fw.sb([128, 512], F32, "xw%d" % i) for i in range(2)]; b_xws = [Buf() for _ in range(2)]
            us = [fw.sb([128, 512], F32, "u%d" % i) for i in range(2)]; b_us = [Buf() for _ in range(2)]
            wus = [fw.sb([128, 16, 128], BF16, "wu%d" % i) for i in range(3)]; b_wus = [Buf() for _ in range(3)]
            st_ = {"cnt": 0, "w": 0}

            pend = []

            def qk_unit(col0, nwcol, b_nwcol, dstT, bdst, with_ctx):
                wu, bwu = wus[st_["w"] % 3], b_wus[st_["w"] % 3]; st_["w"] += 1
                first = [True]
                for (s0, n) in (TB if with_ctx else TB[1:]):
                    blk = {}

                    def s1(blk=blk, s0=s0, n=n, is_first=first[0]):
                        if is_first:
                            fw.dma(pool, wu[:], w_in[:, col0:col0 + 128].rearrange("(kc p) c -> p kc c", p=128), writes=[bwu])
                        i2 = st_["cnt"] % 2; st_["cnt"] += 1
                        blk["i2"] = i2
                        sq_, bsq = sqs[i2], b_sqs[i2]
                        pt, pb = fw.next_psum(); blk["pt"] = (pt, pb)
                        for kc in range(16):
                            fw.op(pe, lambda e: e.matmul(pt[:, 0:n], wu[:, kc, :], aT[:, kc, s0:s0 + n], start=(kc == 0), stop=(kc == 15)),
                                  reads=[bwu] + [b_aT[t] for t in tiles_of(s0, n)], writes=[pb])
                        fw.op(act, lambda e: e.activation(sq_[:, 0:n], pt[:, 0:n], AF.Square), reads=[pb], writes=[bsq])

                    def s2(blk=blk, s0=s0, n=n):
                        i2 = blk["i2"]; pt, pb = blk["pt"]
                        sq_, bsq = sqs[i2], b_sqs[i2]; r_, br = rs[i2], b_rs[i2]; xw, bxw = xws[i2], b_xws[i2]
                        p2, pb2 = fw.next_psum()
                        fw.op(pe, lambda e: e.matmul(p2[:, 0:n], ones[:, :], sq_[:, 0:n], start=True, stop=True), reads=[b_ones, bsq], writes=[pb2])
                        fw.op(act, lambda e: e.activation(r_[:, 0:n], p2[:, 0:n], AF.Sqrt, bias=EPS, scale=1.0 / 128), reads=[pb2], writes=[br])
                        fw.op(dve, lambda e: e.reciprocal(r_[:, 0:n], r_[:, 0:n]), writes=[br])
                        fw.op(dve, lambda e: e.tensor_tensor(r_[:, 0:n], pt[:, 0:n], r_[:, 0:n], ALU.mult), reads=[pb], writes=[br])
                        d0 = s0 if with_ctx else s0 - 256
                        if s0 == 0:
                            fw.op(pool, lambda e: e.tensor_scalar(dstT[:, d0:d0 + n], r_[:, 0:n], nwcol, None, ALU.mult), reads=[br, b_nwcol], writes=[bdst])
                        else:
                            fw.op(pool, lambda e: e.tensor_scalar(xw[:, 0:n], r_[:, 0:n], nwcol, None, ALU.mult), reads=[br, b_nwcol], writes=[bxw])

                    def s3(blk=blk, s0=s0, n=n):
                        if s0 == 0:
                            return
                        i2 = blk["i2"]
                        xw, bxw = xws[i2], b_xws[i2]; u_, bu = us[i2], b_us[i2]
                        d0 = s0 if with_ctx else s0 - 256
                        l0 = s0 - 256
                        p3, pb3 = fw.next_psum()
                        fw.op(pe, lambda e: e.matmul(p3[:, 0:n], rmT[:, :], xw[:, 0:n], start=True, stop=True), reads=[b_rm, bxw], writes=[pb3])
                        fw.op(dve, lambda e: e.tensor_tensor(u_[:, 0:n], p3[:, 0:n], sinT[:, l0:l0 + n], ALU.mult), reads=[pb3, b_sin], writes=[bu])
                        fw.op(pool, lambda e: e.tensor_tensor(xw[:, 0:n], xw[:, 0:n], cosT[:, l0:l0 + n], ALU.mult), reads=[b_cos], writes=[bxw])
                        fw.op(pool, lambda e: e.tensor_tensor(dstT[:, d0:d0 + n], xw[:, 0:n], u_[:, 0:n], ALU.add), reads=[bxw, bu], writes=[bdst])

                    pend.append((s1, s2, s3))
                    first[0] = False

            def flush_qk():
                fw.ps_pool = list(range(8))
                blks = list(pend); pend.clear()
                nb = len(blks)
                for i in range(min(2, nb)):
                    blks[i][0]()
                for i in range(nb):
                    blks[i][1]()
                    if i + 2 < nb:
                        blks[i + 2][0]()
                    blks[i][2]()
                fw.ps_pool = [0, 1, 2, 3]

            kTs = [fw.sb([128, NTOK], BF16, "kT%d" % i) for i in range(2)]; b_kTs = [Buf() for _ in range(2)]
            qTs = [fw.sb([128, NLAT], BF16, "qT%d" % i) for i in range(2)]; b_qTs = [Buf() for _ in range(2)]
            PTs = [fw.sb([128, 512], BF16, "PT%d" % i) for i in range(3)]; b_PTs = [Buf() for _ in range(3)]
            ogs = [fw.sb([128, 128], F32, "og%d" % i) for i in range(3)]; b_ogs = [Buf() for _ in range(3)]
            rcs = [fw.sb([128, 1], F32, "rc%d" % i) for i in range(3)]; b_rcs = [Buf() for _ in range(3)]
            o0n = fw.sb([128, 4, 256], F32, "o0n"); b_o0n = [Buf() for _ in range(4)]
            ods = [fw.sb([128, 256], F32, "od%d" % i) for i in range(2)]; b_ods = [Buf() for _ in range(2)]
            o1s = [fw.sb([128, 256], F32, "o1_%d" % i) for i in range(2)]; b_o1s = [Buf() for _ in range(2)]
            sq2 = fw.sb([128, 256], F32, "sq2"); b_sq2 = Buf()
            ss2 = [fw.sb([128, 1], F32, "ss2_%d" % i) for i in range(2)]; b_ss2 = [Buf() for _ in range(2)]
            cn = {"pt": 0, "o": 0, "k": 0, "q": 0, "d": 0}

            def attend(qT, bqT, kT, bkT, vfn, vw, banks, qb, finish):
                stride = 512
                per_bank = 1
                def score(kt):
                    pS, pbS = fw.next_psum()
                    fw.op(pe, lambda e: e.matmul(pS[:, :], kT[:, kt * 128:(kt + 1) * 128], qT[:, qb * 512:(qb + 1) * 512], start=True, stop=True),
                          reads=[bkT, bqT], writes=[pbS])
                    return pS, pbS
                cur = score(0)
                for kt in range(NT):
                    nxt = score(kt + 1) if kt + 1 < NT else None
                    pS, pbS = cur
                    PT, bPT = PTs[cn["pt"] % 3], b_PTs[cn["pt"] % 3]; cn["pt"] += 1
                    fw.op(act, lambda e: e.activation(PT[:, :], pS[:, :], AF.Exp), reads=[pbS], writes=[bPT])
                    for qs in range(4):
                        bank = banks[qs // per_bank]; off = (qs % per_bank) * stride
                        pO, pbO = fw.psum[bank]
                        fw.op(pe, lambda e: e.matmul(pO[:, off:off + vw], PT[:, qs * 128:(qs + 1) * 128], vfn(kt), start=(kt == 0), stop=(kt == NT - 1)),
                              reads=[bPT, b_V[kt]], writes=[pbO])
                    cur = nxt
                for qs in range(4):
                    bank = banks[qs // per_bank]; off = (qs % per_bank) * stride
                    pO, pbO = fw.psum[bank]
                    finish(qs, pO, pbO, off)

            for kv in range(2):
                kT, bkT = kTs[cn["k"] % 2], b_kTs[cn["k"] % 2]; cn["k"] += 1
                qk_unit(GK0 + kv * 128, nw4[:, 1:2], b_nw4, kT, bkT, True)
                for hq in range(4):
                    hd = kv * 4 + hq
                    qT, bqT = qTs[cn["q"] % 2], b_qTs[cn["q"] % 2]; cn["q"] += 1
                    qk_unit(GQ0 + hd * 128, nwq[:, 0:1], b_nwq, qT, bqT, False)
                    flush_qk()
                    for qb in range(4):
                        def fin(qs, pO, pbO, off, hd=hd, qb=qb):
                            i3 = cn["o"] % 3; cn["o"] += 1
                            og, bog = ogs[i3], b_ogs[i3]; rc, brc = rcs[i3], b_rcs[i3]
                            fw.op(dve, lambda e: e.reciprocal(rc[:, :], pO[:, off + 128:off + 129]), reads=[pbO], writes=[brc])
                            fw.op(dve, lambda e: e.tensor_scalar(og[:, :], pO[:, off:off + 128], rc[:, 0:1], None, ALU.mult), reads=[pbO, brc], writes=[bog])
                            r0 = qb * 512 + qs * 128
                            if fused:
                                d_mix.extend(out_fn(fw, r0, hd * 128, 128, og, bog))
                            else:
                                db = Buf(); d_mix.append(db)
                                fw.dma(sp, mix[r0:r0 + 128, hd * 128:(hd + 1) * 128], og[:, :], reads=[bog], writes=[db])
                        attend(qT, bqT, kT, bkT, lambda kt, kv=kv: V1g[:, kt, kv, :], 129, [4, 5, 6, 7], qb, fin)
            for h in range(4):
                kq = []
                for c in range(2):
                    kT, bkT = kTs[cn["k"] % 2], b_kTs[cn["k"] % 2]; cn["k"] += 1
                    qk_unit(DK0 + (h * 2 + c) * 128, nw4[:, 3:4], b_nw4, kT, bkT, True)
                    qT, bqT = qTs[cn["q"] % 2], b_qTs[cn["q"] % 2]; cn["q"] += 1
                    qk_unit(DQ0 + (h * 2 + c) * 128, nwq[:, 2:3], b_nwq, qT, bqT, False)
                    kq.append((kT, bkT, qT, bqT))
                flush_qk()
                for qb in range(4):
                    for c in range(2):
                        kT, bkT, qT, bqT = kq[c]
                        if c == 0:
                            def fin(qs, pO, pbO, off):
                                i3 = cn["o"] % 3; cn["o"] += 1
                                rc, brc = rcs[i3], b_rcs[i3]
                                fw.op(dve, lambda e: e.reciprocal(rc[:, :], pO[:, off + 256:off + 257]), reads=[pbO], writes=[brc])
                                fw.op(dve, lambda e: e.tensor_scalar(o0n[:, qs, :], pO[:, off:off + 256], rc[:, 0:1], None, ALU.mult), reads=[pbO, brc], writes=[b_o0n[qs]])
                        else:
                            def fin(qs, pO, pbO, off, h=h, qb=qb):
                                i3 = cn["o"] % 3; cn["o"] += 1
                                i2 = cn["d"] % 2; cn["d"] += 1
                                rc, brc = rcs[i3], b_rcs[i3]
                                o1, bo1 = o1s[i2], b_o1s[i2]; od, bod = ods[i2], b_ods[i2]; s2, bs2 = ss2[i2], b_ss2[i2]
                                fw.op(dve, lambda e: e.reciprocal(rc[:, :], pO[:, off + 256:off + 257]), reads=[pbO], writes=[brc])
                                fw.op(dve, lambda e: e.tensor_scalar(rc[:, :], rc[:, :], nlam[:, 0:1], None, ALU.mult), reads=[b_nlam], writes=[brc])
                                fw.op(dve, lambda e: e.scalar_tensor_tensor(o1[:, :], pO[:, off:off + 256], rc[:, 0:1], o0n[:, qs, :], ALU.mult, ALU.add),
                                      reads=[pbO, brc, b_o0n[qs]], writes=[bo1])
                                fw.op(act, lambda e: e.activation(sq2[:, :], o1[:, :], AF.Square, accum_out=s2[:, :]), reads=[bo1], writes=[b_sq2, bs2])
                                fw.op(act, lambda e: e.activation(s2[:, :], s2[:, :], AF.Sqrt, bias=EPS, scale=1.0 / 256), writes=[bs2])
                                fw.op(dve, lambda e: e.reciprocal(s2[:, :], s2[:, :]), writes=[bs2])
                                fw.op(dve, lambda e: e.scalar_tensor_tensor(od[:, :], o1[:, :], s2[:, 0:1], sub[:, :], ALU.mult, ALU.mult), reads=[bo1, bs2, b_sub], writes=[bod])
                                r0 = qb * 512 + qs * 128
                                if fused:
                                    d_mix.extend(out_fn(fw, r0, 1024 + h * 256, 256, od, bod))
                                else:
                                    db = Buf(); d_mix.append(db)
                                    fw.dma(sp, mix[r0:r0 + 128, 1024 + h * 256:1024 + (h + 1) * 256], od[:, :], reads=[bod], writes=[db])
                        attend(qT, bqT, kT, bkT, lambda kt, h=h: V1d[:, kt, h, :], 257, [4, 5, 6, 7], qb, fin)
            fw.barrier()
        if not fused:
            fw.finish(d_mix)
        fw.ps_pool = list(range(8))
        print("phaseA1 ops", fw.n_ops, "waits", fw.n_waits)
    return d_mix if fused else nc

from contextlib import ExitStack

PAIRS = [[0, 1], [2, 3], [4, 5], [6, 7]]
CC_BYTES = 4 * 1024 * 1024


def build_fused():
    nc = bass.Bass("TRN2", target_bir_lowering=False)
    msk_d = nc.dram_tensor("msk", [128, 2], F32, kind="ExternalInput").ap()
    x1s = nc.dram_tensor("x1s", [2 * 2304, 2048], BF16).ap(); x1d = nc.dram_tensor("x1d", [2 * 2304, 2048], BF16).ap()
    x2s = nc.dram_tensor("x2s", [2 * 1152, 2048], F32).ap(); x2d = nc.dram_tensor("x2d", [2 * 1152, 2048], F32).ap()
    x3s = nc.dram_tensor("x3s", [2 * 2048, 2048], BF16).ap(); x3d = nc.dram_tensor("x3d", [2 * 2048, 2048], BF16).ap()
    h1own = nc.dram_tensor("h1own", [1024, 2048], F32).ap()
    with ExitStack() as st0:
        fw = FW(nc, st0)
        cc = Src("cc", fw._sem("cc"), 1)
        msk = fw.sb([128, 2], F32, "msk"); b_msk = Buf()
        fw.dma(fw.sp, msk[:], msk_d, writes=[b_msk])

        def staging(shape, dt, n):
            stk = fw.stack
            key = "_stg_%s_%d" % (str(dt), shape[1])
            if not hasattr(stk, key):
                setattr(stk, key, {"t": [fw.sb(shape, dt, "stg%d" % i) for i in range(n)], "b": [Buf() for _ in range(n)], "i": 0})
            d = getattr(stk, key)
            k = d["i"] % n; d["i"] += 1
            return d["t"][k], d["b"][k]

        def mk_mix_out(dst, R):
            def out_fn(fw_, r0, c0, n, tile, btile):
                res = []
                for s in range(2):
                    u, bu = staging([128, 512], BF16, 4)
                    fw.op(fw.pool, lambda e: e.tensor_scalar(u[:, 0:n], tile[:, :], msk[:, s:s + 1], None, ALU.mult), reads=[btile, b_msk], writes=[bu])
                    db = Buf(); res.append(db)
                    fw.dma(fw.sp, dst[s * R + r0:s * R + r0 + 128, c0:c0 + n], u[:, 0:n], reads=[bu], writes=[db])
                return res
            return out_fn

        b_h1own = []

        def b0_out(fw_, t, h1, bh1):
            res = []
            for s in range(2):
                u, bu = staging([128, 2048], F32, 4)
                fw.op(fw.pool, lambda e: e.tensor_scalar(u[:, :], h1[:, :], msk[:, s:s + 1], None, ALU.mult), reads=[bh1, b_msk], writes=[bu])
                db = Buf(); res.append(db)
                fw.dma(fw.sp, x2s[s * 1152 + t * 128:s * 1152 + (t + 1) * 128, :], u[:, :], reads=[bu], writes=[db])
            if t >= 1:
                db = Buf(); b_h1own.append(db); res.append(db)
                fw.dma(fw.sp, h1own[(t - 1) * 128:t * 128, :], h1[:, :], reads=[bh1], writes=[db])
            return res

        def exchange(src, dst, writers):
            pool = fw.pool
            for b in writers:
                pool.wait(b.w)
            nrows = src.shape[0]
            elt = 2 if src.dtype == BF16 else 4
            rows_per = CC_BYTES // (src.shape[1] * elt)
            for r0 in range(0, nrows, rows_per):
                r1 = min(nrows, r0 + rows_per)
                ins = pool.raw.collective_compute("AllReduce", ALU.add, replica_groups=PAIRS,
                                                  ins=[src[r0:r1, :].opt()], outs=[dst[r0:r1, :].opt()])
                ins.then_inc(cc.sem)
                cc.n += 1
            eb = Buf()
            eb.w = Ev(cc, cc.n, dict(pool.clock))
            return eb

        w1_ = build_phaseA0(ctx=dict(nc=nc, fw=fw, pre="a0_", out_fn=mk_mix_out(x1s, 2304)))
        e1 = exchange(x1s, x1d, w1_)
        w2_ = build_phaseB(9, True, ctx=dict(nc=nc, fw=fw, pre="b0_", mix_src=(x1d, [e1], 2304), out_fn=b0_out))
        e2 = exchange(x2s, x2d, w2_)

        def row_of(t):
            return 0 if t == 0 else 1152 if t == 1 else 128 + (t - 2) * 128 if t < 10 else 1280 + (t - 10) * 128

        w3_ = build_phaseA1(ctx=dict(nc=nc, fw=fw, pre="a1_", h_src=(x2d, [e2], row_of), out_fn=mk_mix_out(x3s, 2048)))
        e3 = exchange(x3s, x3d, w3_)
        outs = build_phaseB(8, False, ctx=dict(nc=nc, fw=fw, pre="b1_", h_src=(h1own, list(b_h1own)), mix_src=(x3d, [e3], 2048), out_fn=None))
        fw.finish(outs)
        print("fused ops", fw.n_ops, "waits", fw.n_waits)
    return nc

import numpy as np

PERM = np.concatenate([np.arange(0, 1024), np.arange(2048, 3072), np.arange(1024, 2048), np.arange(3072, 4096)])
_CONST = {}

def consts():
    if "ident" not in _CONST:
        s = np.arange(128)
        _CONST["ident"] = np.eye(128, dtype=np.float32)
        _CONST["trilt"] = (s[:, None] < s[None, :]).astype(np.float32)
        _CONST["ule"] = (s[:, None] <= s[None, :]).astype(np.float32)
        _CONST["uge"] = (s[:, None] >= s[None, :]).astype(np.float32)
        _CONST["iota64"] = np.arange(64, dtype=np.float32).reshape(1, 64)
    return _CONST

def f32c(a):
    return np.ascontiguousarray(a, dtype=np.float32)

def na_bias_tables(rpb, heads):
    classes = [(0, 0), (1, 0), (2, 0), (14, 22), (15, 22)]
    out = np.full((len(heads), 25, 128, 128), -30000.0, np.float32)
    k = np.arange(128); q = np.arange(128)
    for ci, (j, a) in enumerate(classes):
        r = 2 * j + q // 64; c = q % 64
        r0 = np.clip(r - 4, 0, 24); cs = np.clip(c - 8, 0, 48)
        for i in range(5):
            kr = a + 2 * i + k // 64; kc = k % 64
            vis = ((kr[:, None] >= r0[None, :]) & (kr[:, None] < r0[None, :] + 8) &
                   (kc[:, None] >= cs[None, :]) & (kc[:, None] < cs[None, :] + 16))
            ri = np.clip(kr[:, None] - r[None, :] + 7, 0, 14)
            cj = np.clip(kc[:, None] - c[None, :] + 15, 0, 30)
            for hi, h in enumerate(heads):
                vals = rpb[h][ri, cj]
                out[hi, ci * 5 + i] = np.where(vis, vals, np.float32(-30000.0))
    return out

def pack_A0(P, b, hh, h_all):
    in_w = P["ev_in_w"][0]
    gs = [2 * hh, 2 * hh + 1]
    heads16 = np.arange(16 * hh, 16 * hh + 16)
    nah = np.arange(8 * hh, 8 * hh + 8)
    cols = np.concatenate(
        [np.arange(g * 512, (g + 1) * 512) for g in gs] +
        [2048 + np.arange(g * 512, (g + 1) * 512) for g in gs] +
        [4096 + np.arange(g * 128, (g + 1) * 128) for g in gs] +
        [4608 + np.arange(g * 128, (g + 1) * 128) for g in gs] +
        [5120 + heads16, 5152 + heads16] +
        [5184 + np.arange(h * 128, (h + 1) * 128) for h in nah] +
        [5184 + 2048 + np.arange(h * 128, (h + 1) * 128) for h in nah] +
        [5184 + 4096 + np.arange(h * 128, (h + 1) * 128) for h in nah])
    chans = np.concatenate([np.arange(g * 512, (g + 1) * 512) for g in gs] +
                           [2048 + np.arange(g * 128, (g + 1) * 128) for g in gs] +
                           [2560 + np.arange(g * 128, (g + 1) * 128) for g in gs])
    d = {k: consts()[k] for k in ("ident", "ule", "uge")}
    d.update({
        "h_all": h_all, "cvec": np.stack([P["c"][b], P["c_ctx"]], 0),
        "ada_w": P["ada_w"][0][:, 0:4096], "ada_b": P["ada_b"][0][None, 0:4096],
        "norm_w1": P["norm_w"][0, 0][None, :], "w_in": in_w[:, cols],
        "convw": P["ev_conv_w"][0][:, chans].T, "convb": P["ev_conv_b"][0][chans].reshape(12, 128).T,
        "dt_bias": np.concatenate([P["ev_dt_bias"][0][0, heads16], P["ev_dt_bias"][0][1, heads16]])[None, :],
        "a_log": np.concatenate([P["ev_a_log"][0][0, heads16], P["ev_a_log"][0][1, heads16]])[None, :],
        "d_skip": P["ev_d_skip"][0][heads16][None, :],
        "ssd_nw": np.concatenate([P["ev_ssd_norm_w"][0][g * 512:(g + 1) * 512] for g in gs])[None, :],
        "qnw": P["ev_na_q_norm"][0][:, None], "knw": P["ev_na_k_norm"][0][:, None],
        "biasT": na_bias_tables(P["ev_na_rpb"][0], list(nah)),
    })
    return {k: f32c(v) for k, v in d.items()}

def rope_tables():
    if "cosT" not in _CONST:
        t = np.arange(2048)
        row = (t // 64).astype(np.float32); col = (t % 64).astype(np.float32)
        inv = (1.0 / (np.float32(10000.0) ** (np.arange(0, 64, 2, dtype=np.float32) / np.float32(64)))).astype(np.float32)
        ang = np.concatenate([row[:, None] * inv[None], col[:, None] * inv[None]], -1).astype(np.float32)
        c = np.cos(ang).astype(np.float32); s = np.sin(ang).astype(np.float32)
        _CONST["cosT"] = np.ascontiguousarray(np.repeat(c, 2, axis=1).T)
        _CONST["sinT"] = np.ascontiguousarray(np.repeat(s, 2, axis=1).T)
        rm = np.zeros((128, 128), np.float32)
        for i in range(64):
            rm[2 * i + 1, 2 * i] = -1.0
            rm[2 * i, 2 * i + 1] = 1.0
        _CONST["rmT"] = rm
    return _CONST["cosT"], _CONST["sinT"], _CONST["rmT"]

def pack_A1(P, b, hh, h_all):
    in_w = P["od_in_w"][0]
    cols = np.concatenate(
        [np.arange((8 * hh + i) * 128, (8 * hh + i + 1) * 128) for i in range(8)] +
        [2048 + np.arange((8 * hh + i) * 128, (8 * hh + i + 1) * 128) for i in range(8)] +
        [4096 + np.arange((2 * hh + i) * 128, (2 * hh + i + 1) * 128) for i in range(2)] +
        [5120 + np.arange((8 * hh + i) * 128, (8 * hh + i + 1) * 128) for i in range(8)] +
        [4608 + np.arange((2 * hh + i) * 128, (2 * hh + i + 1) * 128) for i in range(2)] +
        [7168 + np.arange((4 * hh + i) * 256, (4 * hh + i + 1) * 256) for i in range(4)])
    cosT, sinT, rmT = rope_tables()
    d = {
        "ident": consts()["ident"],
        "h_all": h_all, "cvec": np.stack([P["c"][b], P["c_ctx"]], 0),
        "ada_w": P["ada_w"][1][:, 0:4096], "ada_b": P["ada_b"][1][None, 0:4096],
        "norm_w1": P["norm_w"][1, 0][None, :], "w_in": in_w[:, cols],
        "nws": np.stack([P["od_gqa_q_norm"][0], P["od_gqa_k_norm"][0], P["od_diff_q_norm"][0], P["od_diff_k_norm"][0]], 1),
        "lamv": P["od_lambda"][0].reshape(1, 512), "subln": P["od_diff_subln"][0][None, :],
        "cosT": cosT, "sinT": sinT, "rmT": rmT,
    }
    return {k: f32c(v) for k, v in d.items()}


_PROG = {}


def _shared_B(P, layer):
    ow = P["ev_out_w"][0] if layer == 0 else P["od_out_w"][0]
    c = consts()
    sh = {"ident": c["ident"], "trilt": c["trilt"], "iota64": c["iota64"],
          "ada_w": P["ada_w"][layer][:, 4096:], "ada_b": P["ada_b"][layer][None, 4096:],
          "norm_w2": P["norm_w"][layer, 1][None, :], "out_w": ow[PERM],
          "gwew": np.concatenate([P["moe_group_w"][layer], P["moe_expert_w"][layer]], 1),
          "w1": P["moe_w1"][layer], "w3": P["moe_w3"][layer], "w2": P["moe_w2"][layer]}
    return {k: f32c(v) for k, v in sh.items()}


def kernel(**inputs):
    P = {k: np.asarray(v) for k, v in inputs.items()}
    B = 4
    cores = [(b, hh) for b in range(B) for hh in range(2)]
    if "nc" not in _PROG:
        _PROG["nc"] = build_fused()
    shB = [_shared_B(P, 0), _shared_B(P, 1)]
    a0 = {}; a1 = {}
    maps = []
    for (b, hh) in cores:
        h_all = np.concatenate([P["ctx"][b], P["x"][b]], 0)
        if hh not in a0:
            a0[hh] = pack_A0(P, b, hh, h_all)
            a1[hh] = pack_A1(P, b, hh, h_all)
            a1[hh].pop("h_all")
        cvec = f32c(np.stack([P["c"][b], P["c_ctx"]], 0))
        d = {}
        for k, v in a0[hh].items():
            d["a0_" + k] = v
        d["a0_h_all"] = f32c(h_all); d["a0_cvec"] = cvec
        for k, v in a1[hh].items():
            d["a1_" + k] = v
        d["a1_cvec"] = cvec
        for l, pre in ((0, "b0_"), (1, "b1_")):
            for k, v in shB[l].items():
                d[pre + k] = v
            d[pre + "cvec"] = cvec
        rows0 = np.concatenate([np.arange(hh * 128, (hh + 1) * 128), 256 + np.arange(hh * 1024, (hh + 1) * 1024)])
        d["b0_h_in"] = f32c(h_all[rows0])
        g0 = rows0.reshape(9, 128).T
        d["b0_gidx"] = np.ascontiguousarray(np.stack([g0, 2304 + g0], -1).astype(np.int32))
        g1 = (hh * 1024 + np.arange(1024)).reshape(8, 128).T
        d["b1_gidx"] = np.ascontiguousarray(np.stack([g1, 2048 + g1], -1).astype(np.int32))
        m = np.zeros((128, 2), np.float32); m[:, hh] = 1.0
        d["msk"] = m
        maps.append(d)
    res = run_bass_kernel_spmd(_PROG["nc"], maps, core_ids=list(range(8)))
    out = np.zeros((B, 2048, 2048), np.float32)
    for i, (b, hh) in enumerate(cores):
        out[b, hh * 1024:(hh + 1) * 1024] = res.results[i]["h_out"]
    return out
```

```python
import numpy as np
import ml_dtypes
import concourse.bass as bass
import concourse.mybir as mybir
from concourse.bass_utils import run_bass_kernel_spmd

F32 = mybir.dt.float32
BF16 = mybir.dt.bfloat16
I32 = mybir.dt.int32
U32 = mybir.dt.uint32
AF = mybir.ActivationFunctionType
ALU = mybir.AluOpType
AX = mybir.AxisListType


class Ev:
    __slots__ = ("src", "count", "clock")

    def __init__(self, src, count, clock):
        self.src = src
        self.count = count
        self.clock = clock


class Src:
    def __init__(self, name, sem, mult):
        self.name = name
        self.sem = sem
        self.mult = mult
        self.n = 0


class Buf:
    __slots__ = ("name", "w", "r")

    def __init__(self, name=""):
        self.name = name
        self.w = None
        self.r = {}


class Eng:
    def __init__(self, fw, raw, name):
        self.fw = fw
        self.raw = raw
        self.name = name
        self.src = Src(name, fw._sem("s_" + name), 1)
        self.clock = {}
        self.slots = []
        self.slot_i = 0

    def wait(self, ev):
        if ev is None:
            return
        if self.name == "pe" and ev.src.name.startswith("pe"):
            return
        if self.clock.get(ev.src.name, 0) >= ev.count:
            return
        self.raw.wait_ge(ev.src.sem, ev.count * ev.src.mult)
        self.fw.n_waits += 1
        for k, v in ev.clock.items():
            if self.clock.get(k, 0) < v:
                self.clock[k] = v
        self.clock[ev.src.name] = ev.count


class FW:
    def __init__(self, nc, stack, n_dma_slots=12):
        self.nc = nc
        self.stack = stack
        self.sem_stack = stack
        self.n_waits = 0
        self.n_ops = 0
        self.pe = Eng(self, nc.tensor, "pe")
        self.dve = Eng(self, nc.vector, "dve")
        self.act = Eng(self, nc.scalar, "act")
        self.pool = Eng(self, nc.gpsimd, "pool")
        self.sp = Eng(self, nc.sync, "sp")
        for e in (self.sp, self.pool, self.act):
            for i in range(n_dma_slots):
                nm = "d_%s%d" % (e.name, i)
                e.slots.append(Src(nm, self._sem(nm), 16))
        self.uid = 0
        self.psum = []
        for i in range(8):
            t = stack.enter_context(nc.psum_tensor("ps%d" % i, [128, 512], F32))
            self.psum.append((t, Buf("ps%d" % i)))
        self.ps_i = 0
        self.ps_pool = list(range(8))

    def _sem(self, name):
        return self.sem_stack.enter_context(self.nc.semaphore(name))

    def sb(self, shape, dtype, name=None):
        self.uid += 1
        name = "sb%d_%s" % (self.uid, name or "t")
        t = self.stack.enter_context(self.nc.sbuf_tensor(name, list(shape), dtype))
        return t

    def next_psum(self):
        self.ps_i = (self.ps_i + 1) % len(self.ps_pool)
        return self.psum[self.ps_pool[self.ps_i]]

    def _deps(self, eng, reads, writes):
        for b in reads:
            eng.wait(b.w)
        for b in writes:
            eng.wait(b.w)
            for ev in list(b.r.values()):
                eng.wait(ev)

    def _mark(self, ev, reads, writes):
        for b in reads:
            b.r[ev.src.name] = ev
        for b in writes:
            b.w = ev
            b.r = {}

    def op(self, eng, fn, reads=(), writes=()):
        self._deps(eng, reads, writes)
        ins = fn(eng.raw)
        if eng.src.n >= 30000:
            eng.gen = getattr(eng, "gen", 0) + 1
            nm = "%s_g%d" % (eng.name, eng.gen)
            eng.src = Src(nm, self._sem("s_" + nm), 1)
        eng.src.n += 1
        ins.then_inc(eng.src.sem, 1)
        clock = dict(eng.clock)
        ev = Ev(eng.src, eng.src.n, clock)
        self._mark(ev, reads, writes)
        self.n_ops += 1
        return ev

    def dma(self, eng, out, in_, reads=(), writes=(), fn=None):
        slot = eng.slots[eng.slot_i]
        eng.slot_i = (eng.slot_i + 1) % len(eng.slots)
        if slot.n > 0:
            eng.wait(Ev(slot, slot.n, {}))
        self._deps(eng, reads, writes)
        if fn is None:
            ins = eng.raw.dma_start(out=out, in_=in_)
        else:
            ins = fn(eng.raw)
        ins.then_inc(slot.sem, 16)
        slot.n += 1
        ev = Ev(slot, slot.n, dict(eng.clock))
        self._mark(ev, reads, writes)
        self.n_ops += 1
        return ev

    def bound_reg(self, val):
        if not hasattr(self, "_bregs"):
            self._bregs = {}
        if val not in self._bregs:
            self._bregs[val] = self.nc.gpsimd.to_reg(val)
        return self._bregs[val]

    def engines(self):
        return (self.pe, self.dve, self.act, self.pool, self.sp)

    def barrier(self):
        evs = []
        for e in self.engines():
            if e.src.n > 0:
                evs.append(Ev(e.src, e.src.n, dict(e.clock)))
            for s in e.slots:
                if s.n > 0:
                    evs.append(Ev(s, s.n, {}))
        for e in self.engines():
            for ev in evs:
                e.wait(ev)

    def finish(self, bufs):
        for b in bufs:
            self.sp.wait(b.w)

from contextlib import ExitStack

D = 2048
EPS = 1e-6


def bview(ap_row, n=128):
    return ap_row.partition_broadcast(n)


def emit_consts(fw, ident_d):
    ident = fw.sb([128, 128], F32, "ident"); b_ident = Buf("ident")
    fw.dma(fw.sp, ident[:], ident_d, writes=[b_ident])
    return ident, b_ident


def emit_mods(fw, cvec, ada_w, ada_b, ncols, modrows, d_mod, ident, b_ident):
    pe, dve, act, pool, sp = fw.pe, fw.dve, fw.act, fw.pool, fw.sp
    outer = fw.stack
    with ExitStack() as st:
        fw.stack = st
        cv = fw.sb([2, D], F32, "cv"); b_cv = Buf()
        fw.dma(sp, cv[:], cvec, writes=[b_cv])
        siluT = fw.sb([128, 16, 2], BF16, "siluT"); b_sT = Buf()
        pt, pb = fw.next_psum()
        for kc in range(16):
            fw.op(pe, lambda e, kc=kc: e.transpose(pt[:, 2 * kc:2 * kc + 2], cv[0:2, kc * 128:(kc + 1) * 128], ident[0:2, 0:2]),
                  reads=[b_cv, b_ident], writes=[pb])
        fw.op(act, lambda e: e.activation(siluT[:, :, :].rearrange("p a b -> p (a b)"), pt[:, 0:32], AF.Silu), reads=[pb], writes=[b_sT])
        adab = fw.sb([2, ncols], F32, "adab"); b_adab = Buf()
        fw.dma(sp, adab[:], bview(ada_b, 2), writes=[b_adab])
        wts = [fw.sb([128, 16, 512], BF16, "adaw%d" % i) for i in range(2)]
        b_wts = [Buf() for _ in range(2)]
        modsb = fw.sb([2, ncols], F32, "modsb"); b_modsb = Buf()
        for j in range(ncols // 512):
            wt, bw = wts[j % 2], b_wts[j % 2]
            fw.dma(pool, wt[:], ada_w[:, j * 512:(j + 1) * 512].rearrange("(kc p) c -> p kc c", p=128), writes=[bw])
            pt, pb = fw.next_psum()
            for kc in range(16):
                fw.op(pe, lambda e, kc=kc, wt=wt, pt=pt: e.matmul(pt[0:2, :], siluT[:, kc, :], wt[:, kc, :], start=(kc == 0), stop=(kc == 15)),
                      reads=[b_sT, bw], writes=[pb])
            fw.op(dve, lambda e, pt=pt, j=j: e.tensor_tensor(modsb[:, j * 512:(j + 1) * 512], pt[0:2, :], adab[:, j * 512:(j + 1) * 512], ALU.add),
                  reads=[pb, b_adab], writes=[b_modsb])
        fw.dma(sp, modrows, modsb[:, :], reads=[b_modsb], writes=[d_mod])
        fw.barrier()
    fw.stack = outer


def emit_aT(fw, h_all, n_tiles, n_ctx_tiles, norm_w1, modrows, d_mod, aT, b_aT, ident, b_ident, row_of=None, h_bufs=()):
    pe, dve, act, pool, sp = fw.pe, fw.dve, fw.act, fw.pool, fw.sp
    outer = fw.stack
    with ExitStack() as st:
        fw.stack = st
        nw = fw.sb([128, D], F32, "nw"); b_nw = Buf()
        fw.dma(sp, nw[:], bview(norm_w1), writes=[b_nw])
        nsc = []; sh = []; b_nsc = []; b_sh = []
        for v in range(2):
            s = fw.sb([128, D], F32, "sh1_%d" % v); bs = Buf()
            fw.dma(sp, s[:], bview(modrows[v:v + 1, 0:2048]), reads=[d_mod], writes=[bs])
            sh.append(s); b_sh.append(bs)
            c = fw.sb([128, D], F32, "nsc1_%d" % v); bc = Buf()
            fw.dma(sp, c[:], bview(modrows[v:v + 1, 2048:4096]), reads=[d_mod], writes=[bc])
            fw.op(dve, lambda e, c=c: e.scalar_tensor_tensor(c[:, :], c[:, :], 1.0, nw[:, :], ALU.add, ALU.mult), reads=[b_nw], writes=[bc])
            nsc.append(c); b_nsc.append(bc)
        hts = [fw.sb([128, D], F32, "ht%d" % i) for i in range(2)]; b_hts = [Buf() for _ in range(2)]
        ats = [fw.sb([128, D], F32, "at%d" % i) for i in range(2)]; b_ats = [Buf() for _ in range(2)]
        sq = fw.sb([128, D], F32, "sq"); b_sq = Buf()
        ssqs = [fw.sb([128, 1], F32, "ssq%d" % i) for i in range(2)]; b_ssqs = [Buf() for _ in range(2)]
        for t in range(n_tiles):
            v = 1 if t < n_ctx_tiles else 0
            ht, bh = hts[t % 2], b_hts[t % 2]
            at, ba = ats[t % 2], b_ats[t % 2]
            ssq, b_ssq = ssqs[t % 2], b_ssqs[t % 2]
            r0 = t * 128 if row_of is None else row_of(t)
            fw.dma(sp, ht[:], h_all[r0:r0 + 128, :], reads=list(h_bufs), writes=[bh])
            fw.op(act, lambda e, ht=ht, ssq=ssq: e.activation(sq[:, :], ht[:, :], AF.Square, accum_out=ssq[:, :]), reads=[bh], writes=[b_sq, b_ssq])
            fw.op(act, lambda e, ssq=ssq: e.activation(ssq[:, :], ssq[:, :], AF.Sqrt, bias=EPS, scale=1.0 / D), writes=[b_ssq])
            fw.op(dve, lambda e, ssq=ssq: e.reciprocal(ssq[:, :], ssq[:, :]), writes=[b_ssq])
            fw.op(dve, lambda e, ht=ht, at=at, ssq=ssq, v=v: e.scalar_tensor_tensor(at[:, :], ht[:, :], ssq[:, 0:1], nsc[v][:, :], ALU.mult, ALU.mult),
                  reads=[bh, b_ssq, b_nsc[v]], writes=[ba])
            fw.op(pool, lambda e, at=at, v=v: e.tensor_tensor(at[:, :], at[:, :], sh[v][:, :], ALU.add), reads=[b_sh[v]], writes=[ba])
            for q in range(4):
                pt, pb = fw.next_psum()
                for i in range(4):
                    kc = q * 4 + i
                    fw.op(pe, lambda e, pt=pt, i=i, kc=kc, at=at: e.transpose(pt[:, i * 128:(i + 1) * 128], at[:, kc * 128:(kc + 1) * 128], ident[:, :]),
                          reads=[ba, b_ident], writes=[pb])
                dst = aT[:, q * 4:(q + 1) * 4, t * 128:(t + 1) * 128]
                src = pt[:, :].rearrange("p (a b) -> p a b", a=4)
                if q % 2 == 0:
                    fw.op(act, lambda e, dst=dst, src=src: e.copy(dst, src), reads=[pb], writes=[b_aT[t]])
                else:
                    fw.op(dve, lambda e, dst=dst, src=src: e.tensor_copy(dst, src), reads=[pb], writes=[b_aT[t]])
        fw.barrier()
    fw.stack = outer

from contextlib import ExitStack

D = 2048
CAP = 128
NE = 64
EPS = 1e-6


def bview(ap_row, n=128):
    return ap_row.partition_broadcast(n)


def build_phaseB(n_tiles, has_ctx, ctx=None):
    fused = ctx is not None
    nc = ctx["nc"] if fused else bass.Bass("TRN2", target_bir_lowering=False)
    pre = ctx["pre"] if fused else ""
    T = n_tiles * 128
    I = lambda name, shape, dt=F32: nc.dram_tensor(pre + name, shape, dt, kind="ExternalInput").ap()
    h_src = ctx.get("h_src") if fused else None
    h_in = I("h_in", [T, D]) if h_src is None else h_src[0]
    h_bufs = [] if h_src is None else h_src[1]
    mix_in = None if fused else I("mix_in", [T, 4096])
    gidx_d = I("gidx", [128, n_tiles, 2], I32) if fused else None
    cvec = I("cvec", [2, D])
    ada_w = I("ada_w", [D, 8192])
    ada_b = I("ada_b", [1, 8192])
    norm_w2 = I("norm_w2", [1, D])
    out_w = I("out_w", [4096, D])
    gwew = I("gwew", [D, 72])
    w1 = I("w1", [NE, D, 512])
    w3 = I("w3", [NE, D, 512])
    w2 = I("w2", [NE, 512, D])
    ident_d = I("ident", [128, 128])
    trilt_d = I("trilt", [128, 128])
    iota_d = I("iota64", [1, 64])
    out_fn = ctx.get("out_fn") if fused else None
    h_out = nc.dram_tensor("h_out", [T, D], F32, kind="ExternalOutput").ap() if out_fn is None else None
    modrows = nc.dram_tensor(pre + "modrows", [2, 8192], F32).ap()
    m_all = nc.dram_tensor(pre + "m_all", [T, D], F32).ap()
    h1_all = nc.dram_tensor(pre + "h1_all", [T, D], F32).ap()
    reuse_x = fused and ("xdisp" in ctx.get("shared", {}))
    if reuse_x:
        xdisp = ctx["shared"]["xdisp"]
    else:
        xdisp = nc.dram_tensor(pre + "xdisp", [NE * CAP, D], F32).ap()
        if fused and "shared" in ctx:
            ctx["shared"]["xdisp"] = xdisp
    ybuf = nc.dram_tensor(pre + "ybuf", [NE * CAP, D], BF16).ap()
    d_mod = Buf("modrows"); d_m = [Buf() for _ in range(n_tiles)]; d_h1 = [Buf() for _ in range(n_tiles)]
    d_x = Buf("xdisp"); d_y = [Buf() for _ in range(NE)]; d_out = [Buf() for _ in range(n_tiles)]
    d_xz = [] if reuse_x else [Buf() for _ in range(NE * CAP // 512)]

    with ExitStack() as st0:
        if fused:
            fw = ctx["fw"]; fw.stack = st0; fw.ps_pool = list(range(8))
        else:
            fw = FW(nc, st0)
        pe, dve, act, pool, sp = fw.pe, fw.dve, fw.act, fw.pool, fw.sp
        ident = fw.sb([128, 128], F32, "ident"); b_ident = Buf()
        fw.dma(sp, ident[:], ident_d, writes=[b_ident])
        trilt = fw.sb([128, 128], BF16, "trilt"); b_tri = Buf()
        fw.dma(pool, trilt[:], trilt_d, writes=[b_tri])
        ones_bf = fw.sb([128, 128], BF16, "ones_bf"); b_ones = Buf()
        fw.op(pool, lambda e: e.memset(ones_bf[:, :], 1.0), writes=[b_ones])
        iota = fw.sb([128, 64], F32, "iota"); b_iota = Buf()
        fw.dma(sp, iota[:], bview(iota_d), writes=[b_iota])
        zcol = fw.sb([128, 1], F32, "zcol"); b_z = Buf()
        fw.op(pool, lambda e: e.memset(zcol[:, :], 0.0), writes=[b_z])
        dest_all = fw.sb([128, n_tiles, 2], I32, "dest_all"); b_dest = [Buf() for _ in range(n_tiles)]
        gate_all = fw.sb([128, n_tiles, 2], F32, "gate_all"); b_gate = [Buf() for _ in range(n_tiles)]
        with ExitStack() as st:
            fw.stack = st
            if not reuse_x:
                zt = fw.sb([128, 8192], F32, "zt"); b_zt = Buf()
                fw.op(pool, lambda e: e.memset(zt[:, :], 0.0), writes=[b_zt])
                xv = xdisp.rearrange("(a p r) d -> a p (r d)", p=128, r=4)
                for a in range(NE * CAP // 512):
                    fw.dma(sp, xv[a], zt[:, :], reads=[b_zt], writes=[d_xz[a]])
            cv = fw.sb([2, D], F32, "cv"); b_cv = Buf()
            fw.dma(sp, cv[:], cvec, writes=[b_cv])
            siluT = fw.sb([128, 16, 2], BF16, "siluT"); b_sT = Buf()
            pt, pb = fw.next_psum()
            for kc in range(16):
                fw.op(pe, lambda e, kc=kc: e.transpose(pt[:, 2 * kc:2 * kc + 2], cv[0:2, kc * 128:(kc + 1) * 128], ident[0:2, 0:2]),
                      reads=[b_cv, b_ident], writes=[pb])
            fw.op(act, lambda e: e.activation(siluT[:, :, :].rearrange("p a b -> p (a b)"), pt[:, 0:32], AF.Silu), reads=[pb], writes=[b_sT])
            adab = fw.sb([2, 8192], F32, "adab"); b_adab = Buf()
            fw.dma(sp, adab[:], bview(ada_b, 2), writes=[b_adab])
            wts = [fw.sb([128, 16, 512], BF16, "adaw%d" % i) for i in range(2)]
            b_wts = [Buf() for _ in range(2)]
            modsb = fw.sb([2, 8192], F32, "modsb"); b_modsb = Buf()
            for j in range(16):
                wt, bw = wts[j % 2], b_wts[j % 2]
                fw.dma(pool, wt[:], ada_w[:, j * 512:(j + 1) * 512].rearrange("(kc p) c -> p kc c", p=128), writes=[bw])
                pt, pb = fw.next_psum()
                for kc in range(16):
                    fw.op(pe, lambda e, kc=kc, wt=wt, pt=pt: e.matmul(pt[0:2, :], siluT[:, kc, :], wt[:, kc, :], start=(kc == 0), stop=(kc == 15)),
                          reads=[b_sT, bw], writes=[pb])
                fw.op(dve, lambda e, pt=pt, j=j: e.tensor_tensor(modsb[:, j * 512:(j + 1) * 512], pt[0:2, :], adab[:, j * 512:(j + 1) * 512], ALU.add),
                      reads=[pb, b_adab], writes=[b_modsb])
            fw.dma(sp, modrows, modsb[:, :], reads=[b_modsb], writes=[d_mod])
            fw.barrier()
        with ExitStack() as st:
            fw.stack = st
            mixT = fw.sb([128, n_tiles, 32, 128], BF16, "mixT"); b_mixT = [Buf() for _ in range(n_tiles)]
            mts = [fw.sb([128, 4096], F32, "mixt%d" % i) for i in range(2)]; b_mts = [Buf() for _ in range(2)]
            if fused:
                parts = ctx["mix_src"]
                gix = fw.sb([128, n_tiles, 2], I32, "gix"); b_gix = Buf()
                fw.dma(sp, gix[:], gidx_d, writes=[b_gix])
                m16 = [fw.sb([128, 1024], BF16, "m16_%d" % i) for i in range(8)]; b_m16 = [Buf() for _ in range(8)]
                mcnt = 0
            for t in range(n_tiles):
                mt, bm = mts[t % 2], b_mts[t % 2]
                if not fused:
                    fw.dma(sp, mt[:], mix_in[t * 128:(t + 1) * 128, :], writes=[bm])
                else:
                    for k in range(2):
                        for a_, (gsrc, gbufs) in enumerate(parts):
                            mm, bmm = m16[mcnt % 8], b_m16[mcnt % 8]; mcnt += 1
                            fw.dma(pool, None, None, reads=gbufs + [b_gix], writes=[bmm],
                                   fn=lambda e: e.indirect_dma_start(
                                       out=mm[:, :], out_offset=None, in_=gsrc[:, :],
                                       in_offset=bass.IndirectOffsetOnAxis(ap=gix[:, t, k:k + 1], axis=0),
                                       bounds_check=fw.bound_reg(gsrc.shape[0] - 1), oob_is_err=False))
                            c0_ = k * 2048 + a_ * 1024
                            if a_ == 0:
                                fw.op(act, lambda e: e.copy(mt[:, c0_:c0_ + 1024], mm[:, :]), reads=[bmm], writes=[bm])
                            else:
                                fw.op(dve, lambda e: e.tensor_copy(mt[:, c0_:c0_ + 1024], mm[:, :]), reads=[bmm], writes=[bm])
                for q in range(8):
                    pt, pb = fw.next_psum()
                    for i in range(4):
                        kc = q * 4 + i
                        fw.op(pe, lambda e, pt=pt, i=i, kc=kc, mt=mt: e.transpose(pt[:, i * 128:(i + 1) * 128], mt[:, kc * 128:(kc + 1) * 128], ident[:, :]),
                              reads=[bm, b_ident], writes=[pb])
                    eng = act if q % 2 == 0 else dve
                    if eng is act:
                        fw.op(act, lambda e, pt=pt, t=t, q=q: e.copy(mixT[:, t, q * 4:(q + 1) * 4, :].rearrange("p a b -> p (a b)"), pt[:, :]),
                              reads=[pb], writes=[b_mixT[t]])
                    else:
                        fw.op(dve, lambda e, pt=pt, t=t, q=q: e.tensor_copy(mixT[:, t, q * 4:(q + 1) * 4, :].rearrange("p a b -> p (a b)"), pt[:, :]),
                              reads=[pb], writes=[b_mixT[t]])
            ows = [fw.sb([128, 32, 256], BF16, "ow%d" % i) for i in range(2)]; b_ows = [Buf() for _ in range(2)]
            mbs = [fw.sb([128, 256], F32, "mb%d" % i) for i in range(3)]; b_mbs = [Buf() for _ in range(3)]
            cnt = 0
            for j in range(8):
                ow, bo = ows[j % 2], b_ows[j % 2]
                fw.dma(pool, ow[:], out_w[:, j * 256:(j + 1) * 256].rearrange("(kc p) c -> p kc c", p=128), writes=[bo])
                for t in range(n_tiles):
                    pt, pb = fw.next_psum()
                    for kc in range(32):
                        fw.op(pe, lambda e, pt=pt, kc=kc, t=t, ow=ow: e.matmul(pt[:, 0:256], mixT[:, t, kc, :], ow[:, kc, :], start=(kc == 0), stop=(kc == 31)),
                              reads=[b_mixT[t], bo], writes=[pb])
                    mb, bmb = mbs[cnt % 3], b_mbs[cnt % 3]; cnt += 1
                    if cnt % 2:
                        fw.op(act, lambda e, pt=pt, mb=mb: e.copy(mb[:, :], pt[:, 0:256]), reads=[pb], writes=[bmb])
                    else:
                        fw.op(dve, lambda e, pt=pt, mb=mb: e.tensor_copy(mb[:, :], pt[:, 0:256]), reads=[pb], writes=[bmb])
                    fw.dma(sp, m_all[t * 128:(t + 1) * 128, j * 256:(j + 1) * 256], mb[:, :], reads=[bmb], writes=[d_m[t]])
            fw.barrier()
        with ExitStack() as st:
            fw.stack = st
            nvar = 2 if has_ctx else 1
            g1 = []; nsc = []; sh2 = []
            b_g1 = []; b_nsc = []; b_sh2 = []
            nw = fw.sb([128, D], F32, "nw"); b_nw = Buf()
            fw.dma(sp, nw[:], bview(norm_w2), writes=[b_nw])
            for v in range(nvar):
                a = fw.sb([128, D], F32, "g1_%d" % v); ba = Buf()
                fw.dma(sp, a[:], bview(modrows[v:v + 1, 0:2048]), reads=[d_mod], writes=[ba])
                g1.append(a); b_g1.append(ba)
                s = fw.sb([128, D], F32, "sh2_%d" % v); bs = Buf()
                fw.dma(sp, s[:], bview(modrows[v:v + 1, 2048:4096]), reads=[d_mod], writes=[bs])
                sh2.append(s); b_sh2.append(bs)
                c = fw.sb([128, D], F32, "nsc_%d" % v); bc = Buf()
                fw.dma(sp, c[:], bview(modrows[v:v + 1, 4096:6144]), reads=[d_mod], writes=[bc])
                fw.op(dve, lambda e, c=c: e.scalar_tensor_tensor(c[:, :], c[:, :], 1.0, nw[:, :], ALU.add, ALU.mult), reads=[b_nw], writes=[bc])
                nsc.append(c); b_nsc.append(bc)
            gw = fw.sb([128, 16, 72], F32, "gw"); b_gw = Buf()
            fw.dma(sp, gw[:], gwew.rearrange("(kc p) c -> p kc c", p=128), writes=[b_gw])
            acum = fw.sb([128, 64], F32, "acum"); b_acum = Buf()
            fw.op(pool, lambda e: e.memset(acum[:, :], 0.0), writes=[b_acum])
            acum_bf = fw.sb([128, 64], BF16, "acum_bf"); b_acbf = Buf()
            fw.op(pool, lambda e: e.memset(acum_bf[:, :], 0.0), writes=[b_acbf])
            NB = 2
            hts = [fw.sb([128, D], F32, "ht%d" % i) for i in range(NB)]; b_hts = [Buf() for _ in range(NB)]
            mts = [fw.sb([128, D], F32, "mt%d" % i) for i in range(NB)]; b_mts = [Buf() for _ in range(NB)]
            fts = [fw.sb([128, D], F32, "ft%d" % i) for i in range(NB)]; b_fts = [Buf() for _ in range(NB)]
            fTs = [fw.sb([128, 16, 128], F32, "fT%d" % i) for i in range(NB)]; b_fTs = [Buf() for _ in range(NB)]
            sq = fw.sb([128, D], F32, "sq"); b_sq = Buf()
            sm = {}
            def S(name, shape, dt=F32):
                if name not in sm:
                    sm[name] = (fw.sb(shape, dt, "r_" + name), Buf(name))
                return sm[name]
            for t in range(n_tiles):
                v = 1 if (has_ctx and t == 0) else 0
                ht, bh = hts[t % NB], b_hts[t % NB]
                mt, bm = mts[t % NB], b_mts[t % NB]
                ft, bf = fts[t % NB], b_fts[t % NB]
                fT, bfT = fTs[t % NB], b_fTs[t % NB]
                fw.dma(sp, ht[:], h_in[t * 128:(t + 1) * 128, :], reads=h_bufs, writes=[bh])
                fw.dma(sp, mt[:], m_all[t * 128:(t + 1) * 128, :], reads=[d_m[t]], writes=[bm])
                fw.op(pool, lambda e, mt=mt, v=v: e.tensor_tensor(mt[:, :], mt[:, :], g1[v][:, :], ALU.mult), reads=[b_g1[v]], writes=[bm])
                fw.op(dve, lambda e, mt=mt, ht=ht: e.tensor_tensor(ht[:, :], mt[:, :], ht[:, :], ALU.add), reads=[bm], writes=[bh])
                fw.dma(sp, h1_all[t * 128:(t + 1) * 128, :], ht[:, :], reads=[bh], writes=[d_h1[t]])
                ssq, b_ssq = S("ssq", [128, 1])
                fw.op(act, lambda e, ht=ht: e.activation(sq[:, :], ht[:, :], AF.Square, accum_out=ssq[:, :]), reads=[bh], writes=[b_sq, b_ssq])
                rstd, b_rstd = S("rstd", [128, 1])
                fw.op(act, lambda e: e.activation(rstd[:, :], ssq[:, :], AF.Sqrt, bias=EPS, scale=1.0 / D), reads=[b_ssq], writes=[b_rstd])
                fw.op(dve, lambda e: e.reciprocal(rstd[:, :], rstd[:, :]), writes=[b_rstd])
                fw.op(dve, lambda e, ht=ht, ft=ft, v=v: e.scalar_tensor_tensor(ft[:, :], ht[:, :], rstd[:, 0:1], nsc[v][:, :], ALU.mult, ALU.mult),
                      reads=[bh, b_rstd, b_nsc[v]], writes=[bf])
                fw.op(pool, lambda e, ft=ft, v=v: e.tensor_tensor(ft[:, :], ft[:, :], sh2[v][:, :], ALU.add), reads=[b_sh2[v]], writes=[bf])
                for q in range(4):
                    pt, pb = fw.next_psum()
                    for i in range(4):
                        kc = q * 4 + i
                        fw.op(pe, lambda e, pt=pt, i=i, kc=kc, ft=ft: e.transpose(pt[:, i * 128:(i + 1) * 128], ft[:, kc * 128:(kc + 1) * 128], ident[:, :]),
                              reads=[bf, b_ident], writes=[pb])
                    if q % 2 == 0:
                        fw.op(act, lambda e, pt=pt, q=q, fT=fT: e.copy(fT[:, q * 4:(q + 1) * 4, :].rearrange("p a b -> p (a b)"), pt[:, :]), reads=[pb], writes=[bfT])
                    else:
                        fw.op(dve, lambda e, pt=pt, q=q, fT=fT: e.tensor_copy(fT[:, q * 4:(q + 1) * 4, :].rearrange("p a b -> p (a b)"), pt[:, :]), reads=[pb], writes=[bfT])
                pl, pbl = fw.next_psum()
                for kc in range(16):
                    fw.op(pe, lambda e, kc=kc, fT=fT, pl=pl: e.matmul(pl[:, 0:72], fT[:, kc, :], gw[:, kc, :], start=(kc == 0), stop=(kc == 15)),
                          reads=[bfT, b_gw], writes=[pbl])
                lg, b_lg = S("lg", [128, 72])
                fw.op(dve, lambda e, pl=pl: e.tensor_copy(lg[:, :], pl[:, 0:72]), reads=[pbl], writes=[b_lg])
                g8, b_g8 = S("g8", [128, 8])
                fw.op(dve, lambda e: e.max(g8[:, :], lg[:, 0:8]), reads=[b_lg], writes=[b_g8])
                ngm, b_ngm = S("ngm", [128, 1])
                fw.op(dve, lambda e: e.tensor_scalar(ngm[:, :], g8[:, 0:1], -1.0, None, ALU.mult), reads=[b_g8], writes=[b_ngm])
                gex, b_gex = S("gex", [128, 8]); gsum, b_gsum = S("gsum", [128, 1])
                fw.op(act, lambda e: e.activation(gex[:, :], lg[:, 0:8], AF.Exp, bias=ngm[:, 0:1], scale=1.0, accum_out=gsum[:, :]),
                      reads=[b_lg, b_ngm], writes=[b_gex, b_gsum])
                ggate, b_gg = S("ggate", [128, 1])
                fw.op(dve, lambda e: e.reciprocal(ggate[:, :], gsum[:, :]), reads=[b_gsum], writes=[b_gg])
                pen, b_pen = S("pen", [128, 8])
                fw.op(dve, lambda e: e.tensor_scalar(pen[:, :], lg[:, 0:8], g8[:, 0:1], zcol[:, 0:1], ALU.is_equal, ALU.add), reads=[b_lg, b_g8, b_z], writes=[b_pen])
                fw.op(dve, lambda e: e.tensor_scalar(pen[:, :], pen[:, :], -1.0, 1e9, ALU.add, ALU.mult), writes=[b_pen])
                lem, b_lem = S("lem", [128, 64])
                fw.op(dve, lambda e: e.tensor_tensor(lem[:, :].rearrange("p (g e) -> p g e", g=8), lg[:, 8:72].rearrange("p (g e) -> p g e", g=8),
                                                     pen[:, :].unsqueeze(2).to_broadcast([128, 8, 8]), ALU.add), reads=[b_lg, b_pen], writes=[b_lem])
                t8, b_t8 = S("t8", [128, 8]); i8, b_i8 = S("i8", [128, 8], U32)
                fw.op(dve, lambda e: e.max(t8[:, :], lem[:, :]), reads=[b_lem], writes=[b_t8])
                fw.op(dve, lambda e: e.max_index(i8[:, :], t8[:, :], lem[:, :]), reads=[b_lem, b_t8], writes=[b_i8])
                ef, b_ef = S("ef", [128, 2])
                fw.op(dve, lambda e: e.tensor_copy(ef[:, :], i8[:, 0:2]), reads=[b_i8], writes=[b_ef])
                dd, b_dd = S("dd", [128, 1])
                fw.op(dve, lambda e: e.tensor_tensor(dd[:, :], t8[:, 1:2], t8[:, 0:1], ALU.subtract), reads=[b_t8], writes=[b_dd])
                ex, b_ex = S("ex", [128, 1])
                fw.op(act, lambda e: e.activation(ex[:, :], dd[:, :], AF.Exp), reads=[b_dd], writes=[b_ex])
                den, b_den = S("den", [128, 1])
                fw.op(dve, lambda e: e.tensor_scalar(den[:, :], ex[:, :], 1.0, None, ALU.add), reads=[b_ex], writes=[b_den])
                fw.op(dve, lambda e: e.reciprocal(den[:, :], den[:, :]), writes=[b_den])
                fw.op(dve, lambda e, t=t: e.tensor_tensor(gate_all[:, t, 0:1], den[:, :], ggate[:, :], ALU.mult), reads=[b_den, b_gg], writes=[b_gate[t]])
                fw.op(dve, lambda e, t=t: e.tensor_tensor(gate_all[:, t, 1:2], gate_all[:, t, 0:1], ex[:, :], ALU.mult), reads=[b_ex], writes=[b_gate[t]])
                A0, b_A0 = S("A0", [128, 64]); A1, b_A1 = S("A1", [128, 64]); A, b_A = S("A", [128, 64]); Abf, b_Abf = S("Abf", [128, 64], BF16)
                fw.op(dve, lambda e: e.tensor_scalar(A0[:, :], iota[:, :], ef[:, 0:1], None, ALU.is_equal), reads=[b_iota, b_ef], writes=[b_A0])
                fw.op(dve, lambda e: e.tensor_scalar(A1[:, :], iota[:, :], ef[:, 1:2], None, ALU.is_equal), reads=[b_iota, b_ef], writes=[b_A1])
                fw.op(dve, lambda e: e.tensor_tensor(A[:, :], A0[:, :], A1[:, :], ALU.add), reads=[b_A0, b_A1], writes=[b_A])
                fw.op(dve, lambda e: e.tensor_copy(Abf[:, :], A[:, :]), reads=[b_A], writes=[b_Abf])
                pr, pbr = fw.next_psum()
                fw.op(pe, lambda e, pr=pr: e.matmul(pr[:, 0:64], trilt[:, :], Abf[:, :], start=True, stop=False), reads=[b_tri, b_Abf], writes=[pbr])
                fw.op(pe, lambda e, pr=pr: e.matmul(pr[:, 0:64], ones_bf[:, :], acum_bf[:, :], start=False, stop=True), reads=[b_ones, b_acbf], writes=[pbr])
                rk, b_rk = S("rk", [128, 2]); tmp, b_tmp = S("tmp", [128, 64])
                for k, (Ak, bAk) in enumerate(((A0, b_A0), (A1, b_A1))):
                    fw.op(dve, lambda e, Ak=Ak, pr=pr: e.tensor_tensor(tmp[:, :], Ak[:, :], pr[:, 0:64], ALU.mult), reads=[bAk, pbr], writes=[b_tmp])
                    fw.op(dve, lambda e, k=k: e.reduce_sum(rk[:, k:k + 1], tmp[:, :], axis=AX.X), reads=[b_tmp], writes=[b_rk])
                fw.op(dve, lambda e: e.tensor_tensor(acum[:, :], acum[:, :], A[:, :], ALU.add), reads=[b_A], writes=[b_acum])
                fw.op(dve, lambda e: e.tensor_copy(acum_bf[:, :], acum[:, :]), reads=[b_acum], writes=[b_acbf])
                df, b_df = S("df", [128, 2]); ov, b_ov = S("ov", [128, 2])
                fw.op(dve, lambda e: e.tensor_scalar(ov[:, :], rk[:, :], float(CAP), 1e6, ALU.is_ge, ALU.mult), reads=[b_rk], writes=[b_ov])
                fw.op(dve, lambda e: e.scalar_tensor_tensor(df[:, :], ef[:, :], float(CAP), rk[:, :], ALU.mult, ALU.add), reads=[b_ef, b_rk], writes=[b_df])
                fw.op(dve, lambda e: e.tensor_tensor(df[:, :], df[:, :], ov[:, :], ALU.add), reads=[b_ov], writes=[b_df])
                fw.op(dve, lambda e, t=t: e.tensor_copy(dest_all[:, t, :], df[:, :]), reads=[b_df], writes=[b_dest[t]])
                for k in range(2):
                    fw.dma(pool, None, None, reads=[bf, b_dest[t]] + d_xz, writes=[d_x],
                           fn=lambda e, t=t, k=k, ft=ft: e.indirect_dma_start(
                               out=xdisp[:, :], out_offset=bass.IndirectOffsetOnAxis(ap=dest_all[:, t, k:k + 1], axis=0),
                               in_=ft[:, :], in_offset=None, bounds_check=fw.bound_reg(NE * CAP - 1), oob_is_err=False))
            fw.barrier()
        with ExitStack() as st:
            fw.stack = st
            NW = 2
            w1s = [fw.sb([128, 16, 512], BF16, "w1s%d" % i) for i in range(NW)]; b_w1s = [Buf() for _ in range(NW)]
            w3s = [fw.sb([128, 16, 512], BF16, "w3s%d" % i) for i in range(NW)]; b_w3s = [Buf() for _ in range(NW)]
            w2s = [fw.sb([128, 4, D], BF16, "w2s%d" % i) for i in range(NW)]; b_w2s = [Buf() for _ in range(NW)]
            xes = [fw.sb([128, D], F32, "xe%d" % i) for i in range(2)]; b_xes = [Buf() for _ in range(2)]
            xTs = [fw.sb([128, 16, 128], BF16, "xT%d" % i) for i in range(2)]; b_xTs = [Buf() for _ in range(2)]
            sil = fw.sb([128, 512], F32, "sil"); b_sil = Buf()
            hact = fw.sb([128, 512], F32, "hact"); b_hact = Buf()
            hT = fw.sb([128, 4, 128], BF16, "hT"); b_hT = Buf()
            yes = [fw.sb([128, D], BF16, "ye%d" % i) for i in range(2)]; b_yes = [Buf() for _ in range(2)]
            for ex_ in range(NE):
                i2 = ex_ % 2
                w1t, bw1 = w1s[ex_ % NW], b_w1s[ex_ % NW]
                w3t, bw3 = w3s[ex_ % NW], b_w3s[ex_ % NW]
                w2t, bw2 = w2s[ex_ % NW], b_w2s[ex_ % NW]
                fw.dma(pool, w1t[:], w1[ex_].rearrange("(kc p) h -> p kc h", p=128), writes=[bw1])
                fw.dma(pool, w3t[:], w3[ex_].rearrange("(kc p) h -> p kc h", p=128), writes=[bw3])
                fw.dma(pool, w2t[:], w2[ex_].rearrange("(hc p) d -> p hc d", p=128), writes=[bw2])
                xe, bxe = xes[i2], b_xes[i2]
                xT, bxT = xTs[i2], b_xTs[i2]
                fw.dma(sp, xe[:], xdisp[ex_ * CAP:(ex_ + 1) * CAP, :], reads=[d_x] + ([d_xz[ex_ // 4]] if d_xz else []), writes=[bxe])
                for q in range(4):
                    pt, pb = fw.next_psum()
                    for i in range(4):
                        kc = q * 4 + i
                        fw.op(pe, lambda e, pt=pt, i=i, kc=kc, xe=xe: e.transpose(pt[:, i * 128:(i + 1) * 128], xe[:, kc * 128:(kc + 1) * 128], ident[:, :]),
                              reads=[bxe, b_ident], writes=[pb])
                    if q % 2 == 0:
                        fw.op(act, lambda e, pt=pt, q=q, xT=xT: e.copy(xT[:, q * 4:(q + 1) * 4, :].rearrange("p a b -> p (a b)"), pt[:, :]), reads=[pb], writes=[bxT])
                    else:
                        fw.op(dve, lambda e, pt=pt, q=q, xT=xT: e.tensor_copy(xT[:, q * 4:(q + 1) * 4, :].rearrange("p a b -> p (a b)"), pt[:, :]), reads=[pb], writes=[bxT])
                p1, pb1 = fw.next_psum()
                for kc in range(16):
                    fw.op(pe, lambda e, kc=kc, p1=p1, xT=xT, w1t=w1t: e.matmul(p1[:, :], xT[:, kc, :], w1t[:, kc, :], start=(kc == 0), stop=(kc == 15)),
                          reads=[bxT, bw1], writes=[pb1])
                p3, pb3 = fw.next_psum()
                for kc in range(16):
                    fw.op(pe, lambda e, kc=kc, p3=p3, xT=xT, w3t=w3t: e.matmul(p3[:, :], xT[:, kc, :], w3t[:, kc, :], start=(kc == 0), stop=(kc == 15)),
                          reads=[bxT, bw3], writes=[pb3])
                fw.op(act, lambda e, p1=p1: e.activation(sil[:, :], p1[:, :], AF.Silu), reads=[pb1], writes=[b_sil])
                fw.op(dve, lambda e, p3=p3: e.tensor_tensor(hact[:, :], sil[:, :], p3[:, :], ALU.mult), reads=[b_sil, pb3], writes=[b_hact])
                pt, pb = fw.next_psum()
                for hc in range(4):
                    fw.op(pe, lambda e, pt=pt, hc=hc: e.transpose(pt[:, hc * 128:(hc + 1) * 128], hact[:, hc * 128:(hc + 1) * 128], ident[:, :]),
                          reads=[b_hact, b_ident], writes=[pb])
                fw.op(act, lambda e, pt=pt: e.copy(hT[:, :, :].rearrange("p a b -> p (a b)"), pt[:, :]), reads=[pb], writes=[b_hT])
                ye, bye = yes[i2], b_yes[i2]
                for db in range(4):
                    py, pby = fw.next_psum()
                    for hc in range(4):
                        fw.op(pe, lambda e, py=py, hc=hc, db=db, w2t=w2t: e.matmul(py[:, :], hT[:, hc, :], w2t[:, hc, db * 512:(db + 1) * 512], start=(hc == 0), stop=(hc == 3)),
                              reads=[b_hT, bw2], writes=[pby])
                    if db % 2 == 0:
                        fw.op(dve, lambda e, py=py, db=db, ye=ye: e.tensor_copy(ye[:, db * 512:(db + 1) * 512], py[:, :]), reads=[pby], writes=[bye])
                    else:
                        fw.op(act, lambda e, py=py, db=db, ye=ye: e.copy(ye[:, db * 512:(db + 1) * 512], py[:, :]), reads=[pby], writes=[bye])
                fw.dma(sp, ybuf[ex_ * CAP:(ex_ + 1) * CAP, :], ye[:, :], reads=[bye], writes=[d_y[ex_]])
            fw.barrier()
        with ExitStack() as st:
            fw.stack = st
            nvar = 2 if has_ctx else 1
            g2 = []; b_g2 = []
            for v in range(nvar):
                a = fw.sb([128, D], F32, "g2_%d" % v); ba = Buf()
                fw.dma(sp, a[:], bview(modrows[v:v + 1, 6144:8192]), reads=[d_mod], writes=[ba])
                g2.append(a); b_g2.append(ba)
            r0s = [fw.sb([128, D], BF16, "r0_%d" % i) for i in range(2)]; b_r0s = [Buf() for _ in range(2)]
            r1s = [fw.sb([128, D], BF16, "r1_%d" % i) for i in range(2)]; b_r1s = [Buf() for _ in range(2)]
            ycs = [fw.sb([128, D], F32, "yc_%d" % i) for i in range(2)]; b_ycs = [Buf() for _ in range(2)]
            h1s = [fw.sb([128, D], F32, "h1_%d" % i) for i in range(2)]; b_h1s = [Buf() for _ in range(2)]
            for t in range(n_tiles):
                v = 1 if (has_ctx and t == 0) else 0
                r0, br0 = r0s[t % 2], b_r0s[t % 2]
                r1, br1 = r1s[t % 2], b_r1s[t % 2]
                h1, bh1 = h1s[t % 2], b_h1s[t % 2]
                fw.dma(sp, h1[:], h1_all[t * 128:(t + 1) * 128, :], reads=[d_h1[t]], writes=[bh1])
                for k, (r, br) in enumerate(((r0, br0), (r1, br1))):
                    fw.op(pool, lambda e, r=r: e.memset(r[:, :], 0.0), writes=[br])
                    fw.dma(pool, None, None, reads=d_y + [b_dest[t]], writes=[br],
                           fn=lambda e, r=r, t=t, k=k: e.indirect_dma_start(
                               out=r[:, :], out_offset=None, in_=ybuf[:, :],
                               in_offset=bass.IndirectOffsetOnAxis(ap=dest_all[:, t, k:k + 1], axis=0),
                               bounds_check=fw.bound_reg(NE * CAP - 1), oob_is_err=False))
                yc, byc = ycs[t % 2], b_ycs[t % 2]
                fw.op(dve, lambda e: e.tensor_scalar(yc[:, :], r0[:, :], gate_all[:, t, 0:1], None, ALU.mult), reads=[br0, b_gate[t]], writes=[byc])
                fw.op(dve, lambda e: e.scalar_tensor_tensor(yc[:, :], r1[:, :], gate_all[:, t, 1:2], yc[:, :], ALU.mult, ALU.add),
                      reads=[br1, b_gate[t]], writes=[byc])
                fw.op(pool, lambda e: e.tensor_tensor(yc[:, :], yc[:, :], g2[v][:, :], ALU.mult), reads=[b_g2[v]], writes=[byc])
                fw.op(dve, lambda e: e.tensor_tensor(h1[:, :], h1[:, :], yc[:, :], ALU.add), reads=[byc], writes=[bh1])
                if out_fn is None:
                    fw.dma(sp, h_out[t * 128:(t + 1) * 128, :], h1[:, :], reads=[bh1], writes=[d_out[t]])
                else:
                    d_out[t] = out_fn(fw, t, h1, bh1)
            if not fused:
                fw.finish(d_out)
            else:
                fw.barrier()
        print("phaseB ops", fw.n_ops, "waits", fw.n_waits)
    if fused:
        return [b for x in d_out for b in (x if isinstance(x, list) else [x])]
    return nc

from contextlib import ExitStack

NT = 18
NTOK = NT * 128
Z0, X0, B0, C0, DT0, Q0, K0, V0, NCOL = 0, 1024, 2048, 2304, 2560, 2592, 3616, 4640, 5664
TB = [(0, 256)] + [(256 + i * 512, 512) for i in range(4)]


def build_phaseA0(stop_after=None, dbg=False, ctx=None):
    fused = ctx is not None
    nc = ctx["nc"] if fused else bass.Bass("TRN2", target_bir_lowering=False)
    pre = ctx["pre"] if fused else ""
    out_fn = ctx["out_fn"] if fused else None
    I = lambda name, shape, dt=F32: nc.dram_tensor(pre + name, shape, dt, kind="ExternalInput").ap()
    h_all = I("h_all", [NTOK, D])
    cvec = I("cvec", [2, D])
    ada_w = I("ada_w", [D, 4096]); ada_b = I("ada_b", [1, 4096])
    norm_w1 = I("norm_w1", [1, D])
    w_in = I("w_in", [D, NCOL])
    convw = I("convw", [1536, 5]); convb = I("convb", [128, 12])
    dt_bias = I("dt_bias", [1, 32]); a_log = I("a_log", [1, 32]); d_skip = I("d_skip", [1, 16]); ssd_nw = I("ssd_nw", [1, 1024])
    qnw = I("qnw", [128, 1]); knw = I("knw", [128, 1])
    biasT = I("biasT", [8, 25, 128, 128])
    ident_d = I("ident", [128, 128]); ule_d = I("ule", [128, 128]); uge_d = I("uge", [128, 128])
    mix = None if fused else nc.dram_tensor("mix_part", [NTOK, 2048], F32, kind="ExternalOutput").ap()
    modrows = nc.dram_tensor(pre + "modrows", [2, 4096], F32).ap()
    Hb_d = nc.dram_tensor(pre + "Hb_d", [NT, 128, 512], BF16).ap()
    d_mod = Buf("modrows"); d_mix = []

    with ExitStack() as st0:
        if fused:
            fw = ctx["fw"]; fw.stack = st0; fw.ps_pool = list(range(8))
        else:
            fw = FW(nc, st0)
        pe, dve, act, pool, sp = fw.pe, fw.dve, fw.act, fw.pool, fw.sp
        ident, b_ident = emit_consts(fw, ident_d)
        ule = fw.sb([128, 128], F32, "ule"); b_ule = Buf(); fw.dma(sp, ule[:], ule_d, writes=[b_ule])
        uge = fw.sb([128, 128], F32, "uge"); b_uge = Buf(); fw.dma(sp, uge[:], uge_d, writes=[b_uge])
        ones = fw.sb([128, 128], F32, "ones"); b_ones = Buf(); fw.op(pool, lambda e: e.memset(ones[:, :], 1.0), writes=[b_ones])
        identb = fw.sb([128, 128], BF16, "identb"); b_identb = Buf(); fw.dma(pool, identb[:], ident_d, writes=[b_identb])
        zcol = fw.sb([128, 1], F32, "zcol"); b_z = Buf(); fw.op(pool, lambda e: e.memset(zcol[:, :], 0.0), writes=[b_z])
        aT = fw.sb([128, 16, NTOK], BF16, "aT"); b_aT = [Buf() for _ in range(NT)]
        emit_mods(fw, cvec, ada_w, ada_b, 4096, modrows, d_mod, ident, b_ident)
        emit_aT(fw, h_all, NT, 2, norm_w1, modrows, d_mod, aT, b_aT, ident, b_ident)

        def tiles_of(s, n):
            return list(range(s // 128, (s + n) // 128))

        with ExitStack() as st:
            fw.stack = st
            wdt = fw.sb([128, 16, 32], BF16, "wdt"); b_wdt = Buf()
            fw.dma(pool, wdt[:], w_in[:, DT0:DT0 + 32].rearrange("(kc p) c -> p kc c", p=128), writes=[b_wdt])
            dtb = fw.sb([128, 32], F32, "dtb"); b_dtb = Buf(); fw.dma(sp, dtb[:], bview(dt_bias), writes=[b_dtb])
            Aneg = fw.sb([128, 32], F32, "Aneg"); b_A = Buf(); fw.dma(sp, Aneg[:], bview(a_log), writes=[b_A])
            fw.op(act, lambda e: e.activation(Aneg[:, :], Aneg[:, :], AF.Exp), writes=[b_A])
            fw.op(dve, lambda e: e.tensor_scalar(Aneg[:, :], Aneg[:, :], -1.0, None, ALU.mult), writes=[b_A])
            dsk = fw.sb([128, 16], F32, "dsk"); b_dsk = Buf(); fw.dma(sp, dsk[:], bview(d_skip), writes=[b_dsk])
            snw = fw.sb([128, 1024], F32, "snw"); b_snw = Buf(); fw.dma(sp, snw[:], bview(ssd_nw), writes=[b_snw])
            cw = fw.sb([128, 12, 5], F32, "cw"); b_cw = Buf(); fw.dma(sp, cw[:], convw.rearrange("(ct p) k -> p ct k", p=128), writes=[b_cw])
            cb = fw.sb([128, 12], F32, "cb"); b_cb = Buf(); fw.dma(sp, cb[:], convb, writes=[b_cb])
            def T3(name):
                return fw.sb([128, NT, 32], F32, name), [Buf() for _ in range(NT)]
            dt_all, b_dt = T3("dt_all"); a_all, b_a = T3("a_all"); negcs, b_ncs = T3("negcs")
            dfs, b_dfs = T3("dfs"); dte, b_dte = T3("dte"); etot, b_etot = T3("etot")
            tmpA = fw.sb([128, 32], F32, "tmpA"); b_tA = Buf(); tmpB = fw.sb([128, 32], F32, "tmpB"); b_tB = Buf()
            tmpC = fw.sb([128, 32], F32, "tmpC"); b_tC = Buf()
            for t in range(NT):
                pt, pb = fw.next_psum()
                for kc in range(16):
                    fw.op(pe, lambda e, kc=kc, pt=pt, t=t: e.matmul(pt[:, 0:32], aT[:, kc, t * 128:(t + 1) * 128], wdt[:, kc, :], start=(kc == 0), stop=(kc == 15)),
                          reads=[b_aT[t], b_wdt], writes=[pb])
                fw.op(dve, lambda e, pt=pt: e.tensor_tensor(tmpA[:, :], pt[:, 0:32], dtb[:, :], ALU.add), reads=[pb, b_dtb], writes=[b_tA])
                fw.op(act, lambda e: e.activation(tmpB[:, :], tmpA[:, :], AF.Abs), reads=[b_tA], writes=[b_tB])
                fw.op(act, lambda e: e.activation(tmpB[:, :], tmpB[:, :], AF.Exp, scale=-1.0), writes=[b_tB])
                fw.op(act, lambda e: e.activation(tmpB[:, :], tmpB[:, :], AF.Ln, bias=1.0, scale=1.0), writes=[b_tB])
                fw.op(dve, lambda e: e.tensor_scalar(tmpA[:, :], tmpA[:, :], 0.0, None, ALU.max), writes=[b_tA])
                fw.op(dve, lambda e, t=t: e.tensor_tensor(dt_all[:, t, :], tmpA[:, :], tmpB[:, :], ALU.add), reads=[b_tA, b_tB], writes=[b_dt[t]])
                fw.op(dve, lambda e, t=t: e.tensor_tensor(a_all[:, t, :], dt_all[:, t, :], Aneg[:, :], ALU.mult), reads=[b_dt[t], b_A], writes=[b_a[t]])
                pc_, pbc = fw.next_psum()
                fw.op(pe, lambda e, pc_=pc_, t=t: e.matmul(pc_[:, 0:16], ule[:, :], a_all[:, t, 0:16], start=True, stop=True), reads=[b_ule, b_a[t]], writes=[pbc])
                fw.op(pe, lambda e, pc_=pc_, t=t: e.matmul(pc_[:, 16:32], uge[:, :], a_all[:, t, 16:32], start=True, stop=True), reads=[b_uge, b_a[t]], writes=[pbc])
                fw.op(pe, lambda e, pc_=pc_, t=t: e.matmul(pc_[:, 32:64], ones[:, :], a_all[:, t, :], start=True, stop=True), reads=[b_ones, b_a[t]], writes=[pbc])
                fw.op(dve, lambda e, pc_=pc_, t=t: e.tensor_scalar(negcs[:, t, :], pc_[:, 0:32], -1.0, None, ALU.mult), reads=[pbc], writes=[b_ncs[t]])
                fw.op(act, lambda e, pc_=pc_, t=t: e.activation(dfs[:, t, :], pc_[:, 0:32], AF.Exp), reads=[pbc], writes=[b_dfs[t]])
                fw.op(act, lambda e, pc_=pc_, t=t: e.activation(etot[:, t, :], pc_[:, 32:64], AF.Exp), reads=[pbc], writes=[b_etot[t]])
                fw.op(dve, lambda e, pc_=pc_, t=t: e.tensor_tensor(tmpC[:, :], pc_[:, 32:64], negcs[:, t, :], ALU.add), reads=[pbc, b_ncs[t]], writes=[b_tC])
                fw.op(act, lambda e, t=t: e.activation(dte[:, t, :], tmpC[:, :], AF.Exp), reads=[b_tC], writes=[b_dte[t]])

            xs = fw.sb([128, NT, 512], BF16, "xs"); b_xs = [Buf() for _ in range(NT)]
            Btok = fw.sb([128, NT, 128], BF16, "Btok"); b_Btok = [Buf() for _ in range(NT)]
            BT = fw.sb([128, NTOK], BF16, "BT"); b_BT = Buf()
            CT = fw.sb([128, NTOK], BF16, "CT"); b_CT = Buf()
            d_Hb = [Buf() for _ in range(NT)]
            Hbs = [fw.sb([128, 512], BF16, "Hbs%d" % i) for i in range(2)]; b_Hbs = [Buf() for _ in range(2)]
            pcs = [fw.sb([128, 2312], F32, "pc%d" % i) for i in range(1)]; b_pcs = [Buf() for _ in range(1)]
            for i in range(1):
                fw.op(pool, lambda e, i=i: e.memset(pcs[i][:, :], 0.0), writes=[b_pcs[i]])
            acc = fw.sb([128, 2308], F32, "acc"); b_acc = Buf()
            wcs = [fw.sb([128, 16, 128], BF16, "wc%d" % i) for i in range(2)]; b_wcs = [Buf() for _ in range(2)]
            wz = fw.sb([128, 16, 512], BF16, "wz"); b_wz = Buf()
            H = fw.sb([128, 512], F32, "H"); b_H = Buf()
            Hbf = fw.sb([128, 512], BF16, "Hbf"); b_Hbf = Buf()
            coef = fw.sb([128, 8], F32, "coef"); b_coef = Buf()
            Xe = fw.sb([128, 512], BF16, "Xe"); b_Xe = Buf()
            Xtf = fw.sb([128, 512], BF16, "Xtf"); b_Xtf = Buf()
            Xtb = fw.sb([128, 512], BF16, "Xtb"); b_Xtb = Buf()
            CBf = fw.sb([128, 128], F32, "CBf"); b_CBf = Buf()
            CBb = fw.sb([128, 128], F32, "CBb"); b_CBb = Buf()
            Es = [fw.sb([128, 128], F32, "E%d" % i) for i in range(3)]; b_Es = [Buf() for _ in range(3)]
            Ls = [fw.sb([128, 128], F32, "L%d" % i) for i in range(3)]; b_Ls = [Buf() for _ in range(3)]
            Ms = [fw.sb([128, 128], BF16, "M%d" % i) for i in range(3)]; b_Ms = [Buf() for _ in range(3)]
            t1 = fw.sb([128, 512], F32, "t1"); b_t1 = Buf()
            t2 = fw.sb([128, 512], F32, "t2"); b_t2 = Buf()
            sz = fw.sb([128, 512], F32, "sz"); b_sz = Buf()
            gy = fw.sb([128, 512], F32, "gy"); b_gy = Buf()
            ssq = fw.sb([128, 1], F32, "ssq_s"); b_ssq = Buf()
            outs = [fw.sb([128, 512], F32, "o%d" % i) for i in range(2)]; b_outs = [Buf() for _ in range(2)]
            wcnt = 0
            lm = 0

            def bc8(ap):
                return ap.unsqueeze(2).to_broadcast([128, 8, 64])

            def v3(ap):
                return ap.rearrange("p (h d) -> p h d", h=8)

            for g in range(1 if dbg else 2):
                hf = g * 8
                hb = 16 + g * 8
                fw.dma(pool, wz[:], w_in[:, Z0 + g * 512:Z0 + (g + 1) * 512].rearrange("(kc p) c -> p kc c", p=128), writes=[b_wz])
                for ci in range(6):
                    if ci < 4:
                        col0 = X0 + g * 512 + ci * 128; ct = g * 4 + ci
                    elif ci == 4:
                        col0 = B0 + g * 128; ct = 8 + g
                    else:
                        col0 = C0 + g * 128; ct = 10 + g
                    wc, bwc = wcs[wcnt % 2], b_wcs[wcnt % 2]
                    pc, bpc = pcs[0], b_pcs[0]
                    wcnt += 1
                    fw.dma(pool, wc[:], w_in[:, col0:col0 + 128].rearrange("(kc p) c -> p kc c", p=128), writes=[bwc])
                    for (s0, n) in TB:
                        pt, pb = fw.next_psum()
                        for kc in range(16):
                            fw.op(pe, lambda e, kc=kc, pt=pt, wc=wc, s0=s0, n=n: e.matmul(pt[:, 0:n], wc[:, kc, :], aT[:, kc, s0:s0 + n], start=(kc == 0), stop=(kc == 15)),
                                  reads=[bwc] + [b_aT[t] for t in tiles_of(s0, n)], writes=[pb])
                        off = 2 if s0 == 0 else 6
                        fw.op(act, lambda e, pt=pt, pc=pc, s0=s0, n=n, off=off: e.copy(pc[:, s0 + off:s0 + off + n], pt[:, 0:n]), reads=[pb], writes=[bpc])
                    fw.op(dve, lambda e, pc=pc, ct=ct: e.tensor_scalar(acc[:, :], pc[:, 0:2308], cw[:, ct, 0:1], cb[:, ct:ct + 1], ALU.mult, ALU.add),
                          reads=[bpc, b_cw, b_cb], writes=[b_acc])
                    for k in range(1, 5):
                        eng = dve
                        fw.op(eng, lambda e, pc=pc, ct=ct, k=k: e.scalar_tensor_tensor(acc[:, :], pc[:, k:k + 2308], cw[:, ct, k:k + 1], acc[:, :], ALU.mult, ALU.add),
                              reads=[bpc, b_cw], writes=[b_acc])
                    if ci == 5:
                        fw.op(act, lambda e: e.activation(CT[:, 0:256], acc[:, 0:256], AF.Silu), reads=[b_acc], writes=[b_CT])
                        fw.op(act, lambda e: e.activation(CT[:, 256:NTOK], acc[:, 260:2308], AF.Silu), reads=[b_acc], writes=[b_CT])
                        continue
                    fw.op(act, lambda e: e.activation(acc[:, 0:256], acc[:, 0:256], AF.Silu), writes=[b_acc])
                    fw.op(act, lambda e: e.activation(acc[:, 260:2308], acc[:, 260:2308], AF.Silu), writes=[b_acc])
                    if ci == 4:
                        fw.op(pool, lambda e: e.tensor_copy(BT[:, 0:256], acc[:, 0:256]), reads=[b_acc], writes=[b_BT])
                        fw.op(pool, lambda e: e.tensor_copy(BT[:, 256:NTOK], acc[:, 260:2308]), reads=[b_acc], writes=[b_BT])
                    for q in range(5):
                        ts = list(range(q * 4, min(NT, q * 4 + 4)))
                        pt, pb = fw.next_psum()
                        for i, t in enumerate(ts):
                            a0 = t * 128 if t < 2 else 260 + (t - 2) * 128
                            fw.op(pe, lambda e, pt=pt, i=i, a0=a0: e.transpose(pt[:, i * 128:(i + 1) * 128], acc[:, a0:a0 + 128], ident[:, :]),
                                  reads=[b_acc, b_ident], writes=[pb])
                        n = len(ts)
                        src = pt[:, 0:n * 128].rearrange("p (a b) -> p a b", a=n)
                        if ci < 4:
                            dstv = xs[:, ts[0]:ts[0] + n, ci * 128:(ci + 1) * 128]; bl = [b_xs[t] for t in ts]
                        else:
                            dstv = Btok[:, ts[0]:ts[0] + n, :]; bl = [b_Btok[t] for t in ts]
                        if q % 2 == 0:
                            fw.op(dve, lambda e, dstv=dstv, src=src: e.tensor_copy(dstv, src), reads=[pb], writes=bl)
                        else:
                            fw.op(act, lambda e, dstv=dstv, src=src: e.copy(dstv, src), reads=[pb], writes=bl)

                fw.op(pool, lambda e: e.memset(H[:, :], 0.0), writes=[b_H])
                border = [1, 0] + list(range(17, 1, -1))
                for bi_, c in enumerate(border):
                    hs_, bhs_ = Hbs[bi_ % 2], b_Hbs[bi_ % 2]
                    fw.op(act, lambda e, hs_=hs_: e.copy(hs_[:, :], H[:, :]), reads=[b_H], writes=[bhs_])
                    fw.dma(sp, Hb_d[c], hs_[:, :], reads=[bhs_], writes=[d_Hb[c]])
                    fw.op(dve, lambda e, c=c: e.tensor_tensor(coef[:, :], dt_all[:, c, hb:hb + 8], dte[:, c, hb:hb + 8], ALU.mult), reads=[b_dt[c], b_dte[c]], writes=[b_coef])
                    fw.op(dve, lambda e, c=c: e.tensor_tensor(v3(Xe[:, :]), v3(xs[:, c, :]), bc8(coef[:, :]), ALU.mult), reads=[b_xs[c], b_coef], writes=[b_Xe])
                    ps, pbs = fw.next_psum()
                    fw.op(pe, lambda e, ps=ps, c=c: e.matmul(ps[:, :], Btok[:, c, :], Xe[:, :], start=True, stop=True), reads=[b_Btok[c], b_Xe], writes=[pbs])
                    fw.op(pool, lambda e, c=c: e.tensor_tensor(v3(H[:, :]), v3(H[:, :]), bc8(etot[:, c, hb:hb + 8]), ALU.mult), reads=[b_etot[c]], writes=[b_H])
                    fw.op(dve, lambda e, ps=ps: e.tensor_tensor(H[:, :], H[:, :], ps[:, :], ALU.add), reads=[pbs], writes=[b_H])

                fw.op(pool, lambda e: e.memset(H[:, :], 0.0), writes=[b_H])
                for c in range(NT):
                    cs_ = slice(c * 128, (c + 1) * 128)
                    hs_, bhs_ = Hbs[c % 2], b_Hbs[c % 2]
                    fw.dma(sp, hs_[:, :], Hb_d[c], reads=[d_Hb[c]], writes=[bhs_])
                    fw.op(act, lambda e: e.copy(Hbf[:, :], H[:, :]), reads=[b_H], writes=[b_Hbf])
                    fw.op(dve, lambda e, c=c: e.tensor_tensor(coef[:, :], dt_all[:, c, hf:hf + 8], dte[:, c, hf:hf + 8], ALU.mult), reads=[b_dt[c], b_dte[c]], writes=[b_coef])
                    fw.op(dve, lambda e, c=c: e.tensor_tensor(v3(Xe[:, :]), v3(xs[:, c, :]), bc8(coef[:, :]), ALU.mult), reads=[b_xs[c], b_coef], writes=[b_Xe])
                    fw.op(pool, lambda e, c=c: e.tensor_tensor(v3(Xtf[:, :]), v3(xs[:, c, :]), bc8(dt_all[:, c, hf:hf + 8]), ALU.mult), reads=[b_xs[c], b_dt[c]], writes=[b_Xtf])
                    fw.op(pool, lambda e, c=c: e.tensor_tensor(v3(Xtb[:, :]), v3(xs[:, c, :]), bc8(dt_all[:, c, hb:hb + 8]), ALU.mult), reads=[b_xs[c], b_dt[c]], writes=[b_Xtb])
                    pyf, pbyf = fw.next_psum()
                    fw.op(pe, lambda e, pyf=pyf, cs_=cs_: e.matmul(pyf[:, :], CT[:, cs_], Hbf[:, :], start=True, stop=True), reads=[b_CT, b_Hbf], writes=[pbyf])
                    pyb, pbyb = fw.next_psum()
                    fw.op(pe, lambda e, pyb=pyb, cs_=cs_, hs_=hs_: e.matmul(pyb[:, :], CT[:, cs_], hs_[:, :], start=True, stop=True), reads=[b_CT, bhs_], writes=[pbyb])
                    fw.op(dve, lambda e, pyf=pyf, c=c: e.tensor_tensor(v3(t1[:, :]), v3(pyf[:, :]), bc8(dfs[:, c, hf:hf + 8]), ALU.mult), reads=[pbyf, b_dfs[c]], writes=[b_t1])
                    fw.op(dve, lambda e, pyb=pyb, c=c: e.tensor_tensor(v3(t2[:, :]), v3(pyb[:, :]), bc8(dfs[:, c, hb:hb + 8]), ALU.mult), reads=[pbyb, b_dfs[c]], writes=[b_t2])
                    fw.op(pool, lambda e: e.tensor_tensor(t1[:, :], t1[:, :], t2[:, :], ALU.add), reads=[b_t2], writes=[b_t1])
                    fw.op(pool, lambda e, c=c: e.tensor_tensor(v3(t2[:, :]), v3(xs[:, c, :]), bc8(dsk[:, g * 8:(g + 1) * 8]), ALU.mult), reads=[b_xs[c], b_dsk], writes=[b_t2])
                    fw.op(pool, lambda e: e.tensor_tensor(t1[:, :], t1[:, :], t2[:, :], ALU.add), reads=[b_t2], writes=[b_t1])
                    ps, pbs = fw.next_psum()
                    fw.op(pe, lambda e, ps=ps, c=c: e.matmul(ps[:, :], Btok[:, c, :], Xe[:, :], start=True, stop=True), reads=[b_Btok[c], b_Xe], writes=[pbs])
                    fw.op(pool, lambda e, c=c: e.tensor_tensor(v3(H[:, :]), v3(H[:, :]), bc8(etot[:, c, hf:hf + 8]), ALU.mult), reads=[b_etot[c], b_Hbf], writes=[b_H])
                    fw.op(dve, lambda e, ps=ps: e.tensor_tensor(H[:, :], H[:, :], ps[:, :], ALU.add), reads=[pbs], writes=[b_H])
                    pcb, pbcb = fw.next_psum()
                    fw.op(pe, lambda e, pcb=pcb, cs_=cs_: e.matmul(pcb[:, 0:128], BT[:, cs_], CT[:, cs_], start=True, stop=True), reads=[b_BT, b_CT], writes=[pbcb])
                    fw.op(dve, lambda e, pcb=pcb: e.tensor_tensor(CBf[:, :], pcb[:, 0:128], ule[:, :], ALU.mult), reads=[pbcb, b_ule], writes=[b_CBf])
                    fw.op(dve, lambda e, pcb=pcb: e.tensor_tensor(CBb[:, :], pcb[:, 0:128], uge[:, :], ALU.mult), reads=[pbcb, b_uge], writes=[b_CBb])
                    pyd, pbyd = fw.next_psum()
                    for hq in range(4):
                        pr, pbr = fw.next_psum()
                        combos = [(h, d_) for h in (2 * hq, 2 * hq + 1) for d_ in (0, 1)]
                        for i, (h, d_) in enumerate(combos):
                            col = (hf if d_ == 0 else hb) + h
                            U = ule if d_ == 0 else uge
                            bU = b_ule if d_ == 0 else b_uge
                            fw.op(pe, lambda e, pr=pr, i=i, c=c, col=col, U=U: e.matmul(pr[:, i * 128:(i + 1) * 128], a_all[:, c, col:col + 1].to_broadcast([128, 128]), U[:, :], start=True, stop=True),
                                  reads=[b_a[c], bU], writes=[pbr])
                        for i, (h, d_) in enumerate(combos):
                            col = (hf if d_ == 0 else hb) + h
                            E_, bE = Es[lm % 3], b_Es[lm % 3]; L_, bL = Ls[lm % 3], b_Ls[lm % 3]; M_, bM = Ms[lm % 3], b_Ms[lm % 3]; lm += 1
                            CB_, bCB = (CBf, b_CBf) if d_ == 0 else (CBb, b_CBb)
                            Xt_, bXt = (Xtf, b_Xtf) if d_ == 0 else (Xtb, b_Xtb)
                            fw.op(dve, lambda e, pr=pr, i=i, c=c, col=col, E_=E_: e.tensor_scalar(E_[:, :], pr[:, i * 128:(i + 1) * 128], negcs[:, c, col:col + 1], zcol[:, 0:1], ALU.add, ALU.min),
                                  reads=[pbr, b_ncs[c], b_z], writes=[bE])
                            fw.op(act, lambda e, E_=E_, L_=L_: e.activation(L_[:, :], E_[:, :], AF.Exp), reads=[bE], writes=[bL])
                            fw.op(pool, lambda e, L_=L_, M_=M_, CB_=CB_: e.tensor_tensor(M_[:, :], L_[:, :], CB_[:, :], ALU.mult), reads=[bL, bCB], writes=[bM])
                            fw.op(pe, lambda e, pyd=pyd, h=h, M_=M_, Xt_=Xt_, d_=d_: e.matmul(pyd[:, h * 64:(h + 1) * 64], M_[:, :], Xt_[:, h * 64:(h + 1) * 64], start=(d_ == 0), stop=(d_ == 1)),
                                  reads=[bM, bXt], writes=[pbyd])
                    if dbg and c == NT - 1:
                        dd = {}
                        for nm, pp, pbb in (("pyd", pyd, pbyd),):
                            tt = fw.sb([128, 512], F32, "dbg_" + nm); bb = Buf()
                            fw.op(dve, lambda e, tt=tt, pp=pp: e.tensor_copy(tt[:, :], pp[:, :]), reads=[pbb], writes=[bb])
                            dd[nm] = (tt, bb)
                        fw.dbgd = dd
                    fw.op(dve, lambda e, pyd=pyd: e.tensor_tensor(t1[:, :], t1[:, :], pyd[:, :], ALU.add), reads=[pbyd], writes=[b_t1])
                    pz, pbz = fw.next_psum()
                    for kc in range(16):
                        fw.op(pe, lambda e, kc=kc, pz=pz, cs_=cs_: e.matmul(pz[:, :], aT[:, kc, cs_], wz[:, kc, :], start=(kc == 0), stop=(kc == 15)),
                              reads=[b_aT[c], b_wz], writes=[pbz])
                    fw.op(act, lambda e, pz=pz: e.activation(sz[:, :], pz[:, :], AF.Silu), reads=[pbz], writes=[b_sz])
                    fw.op(dve, lambda e: e.tensor_tensor(gy[:, :], t1[:, :], sz[:, :], ALU.mult), reads=[b_t1, b_sz], writes=[b_gy])
                    fw.op(act, lambda e: e.activation(t2[:, :], gy[:, :], AF.Square, accum_out=ssq[:, :]), reads=[b_gy], writes=[b_t2, b_ssq])
                    fw.op(act, lambda e: e.activation(ssq[:, :], ssq[:, :], AF.Sqrt, bias=EPS, scale=1.0 / 512), writes=[b_ssq])
                    fw.op(dve, lambda e: e.reciprocal(ssq[:, :], ssq[:, :]), writes=[b_ssq])
                    o_, bo = outs[c % 2], b_outs[c % 2]
                    fw.op(dve, lambda e, o_=o_: e.scalar_tensor_tensor(o_[:, :], gy[:, :], ssq[:, 0:1], snw[:, g * 512:(g + 1) * 512], ALU.mult, ALU.mult),
                          reads=[b_gy, b_ssq, b_snw], writes=[bo])
                    if fused:
                        d_mix.extend(out_fn(fw, c * 128, g * 512, 512, o_, bo))
                    else:
                        db = Buf(); d_mix.append(db)
                        fw.dma(sp, mix[c * 128:(c + 1) * 128, g * 512:(g + 1) * 512], o_[:, :], reads=[bo], writes=[db])
            if dbg:
                def dump(name, ap, shape, dt, bufs):
                    o = nc.dram_tensor("dbg_" + name, shape, dt, kind="ExternalOutput").ap()
                    db = Buf(); d_mix.append(db)
                    fw.dma(sp, o, ap, reads=bufs, writes=[db])
                dump("aT0", aT[:, 0, :], [128, NTOK], BF16, b_aT)
                dump("dt", dt_all[:, :, :], [128, NT, 32], F32, b_dt)
                dump("negcs", negcs[:, :, :], [128, NT, 32], F32, b_ncs)
                dump("dfs", dfs[:, :, :], [128, NT, 32], F32, b_dfs)
                dump("dte", dte[:, :, :], [128, NT, 32], F32, b_dte)
                dump("etot", etot[:, :, :], [128, NT, 32], F32, b_etot)
                dump("xs", xs[:, :, :], [128, NT, 512], BF16, b_xs)
                dump("Btok", Btok[:, :, :], [128, NT, 128], BF16, b_Btok)
                dump("BT", BT[:, :], [128, NTOK], BF16, [b_BT])
                dump("CT", CT[:, :], [128, NTOK], BF16, [b_CT])
                dump("H", H[:, :], [128, 512], F32, [b_H])
                dump("t1", t1[:, :], [128, 512], F32, [b_t1])
                dump("gy", gy[:, :], [128, 512], F32, [b_gy])
                for nm, (tt, bb) in fw.dbgd.items():
                    dump(nm, tt[:, :], [128, 512], F32, [bb])
                dump("CBf", CBf[:, :], [128, 128], F32, [b_CBf])
                dump("CBb", CBb[:, :], [128, 128], F32, [b_CBb])
                dump("Elast", Es[(lm - 1) % 3][:, :], [128, 128], F32, [b_Es[(lm - 1) % 3]])
                dump("Llast", Ls[(lm - 1) % 3][:, :], [128, 128], F32, [b_Ls[(lm - 1) % 3]])
                dump("Mlast", Ms[(lm - 1) % 3][:, :], [128, 128], BF16, [b_Ms[(lm - 1) % 3]])
                dump("Xtf", Xtf[:, :], [128, 512], BF16, [b_Xtf])
                dump("Xtb", Xtb[:, :], [128, 512], BF16, [b_Xtb])
            fw.barrier()
        if fused and ctx.get("mid_hook"):
            ctx["mid_hook"]()

        if stop_after != "ssd":
          with ExitStack() as st:
            fw.stack = st
            V1 = fw.sb([128, NT, 8, 129], BF16, "V1"); b_V1 = [Buf() for _ in range(NT)]
            fw.op(pool, lambda e: e.memset(V1[:, :, :, :].rearrange("p a b c -> p (a b c)"), 1.0), writes=b_V1)
            with ExitStack() as st2:
                fw.stack = st2
                wv = fw.sb([128, 16, 1024], BF16, "wv"); b_wv = Buf()
                fw.dma(pool, wv[:], w_in[:, V0:V0 + 1024].rearrange("(kc p) c -> p kc c", p=128), writes=[b_wv])
                for t in range(NT):
                    for hh_ in range(2):
                        pt, pb = fw.next_psum()
                        for kc in range(16):
                            fw.op(pe, lambda e, kc=kc, pt=pt, t=t, hh_=hh_: e.matmul(pt[:, :], aT[:, kc, t * 128:(t + 1) * 128], wv[:, kc, hh_ * 512:(hh_ + 1) * 512], start=(kc == 0), stop=(kc == 15)),
                                  reads=[b_aT[t], b_wv], writes=[pb])
                        dstv = V1[:, t, hh_ * 4:(hh_ + 1) * 4, 0:128]
                        src = pt[:, :].rearrange("p (a b) -> p a b", a=4)
                        if hh_ == 0:
                            fw.op(act, lambda e, dstv=dstv, src=src: e.copy(dstv, src), reads=[pb], writes=[b_V1[t]])
                        else:
                            fw.op(dve, lambda e, dstv=dstv, src=src: e.tensor_copy(dstv, src), reads=[pb], writes=[b_V1[t]])
                fw.barrier()
            fw.stack = st
            qw = fw.sb([128, 1], F32, "qw"); b_qw = Buf(); fw.dma(sp, qw[:], qnw, writes=[b_qw])
            fw.op(dve, lambda e: e.tensor_scalar(qw[:, :], qw[:, :], float(128 ** -0.5), None, ALU.mult), writes=[b_qw])
            kw = fw.sb([128, 1], F32, "kw"); b_kw = Buf(); fw.dma(sp, kw[:], knw, writes=[b_kw])
            qTs = [fw.sb([128, NTOK], BF16, "qT%d" % i) for i in range(2)]; b_qTs = [Buf() for _ in range(2)]
            kTs = [fw.sb([128, NTOK], BF16, "kT%d" % i) for i in range(2)]; b_kTs = [Buf() for _ in range(2)]
            wqs = [fw.sb([128, 16, 128], BF16, "wq%d" % i) for i in range(2)]; b_wqs = [Buf() for _ in range(2)]
            wks = [fw.sb([128, 16, 128], BF16, "wk%d" % i) for i in range(2)]; b_wks = [Buf() for _ in range(2)]
            bts = [fw.sb([128, 25, 128], BF16, "bt%d" % i) for i in range(2)]; b_bts = [Buf() for _ in range(2)]
            sqs = [fw.sb([128, 512], F32, "sq%d" % i) for i in range(2)]; b_sqs = [Buf() for _ in range(2)]
            rs = [fw.sb([128, 512], F32, "rs%d" % i) for i in range(2)]; b_rs = [Buf() for _ in range(2)]
            PTs = [fw.sb([128, 7, 128], BF16, "PT%d" % i) for i in range(3)]; b_PTs = [Buf() for _ in range(3)]
            rec = [fw.sb([128, 1], F32, "rec%d" % i) for i in range(2)]; b_rec = [Buf() for _ in range(2)]
            ons = [fw.sb([128, 128], F32, "on%d" % i) for i in range(3)]; b_ons = [Buf() for _ in range(3)]
            cnt = 0; pcnt = 0
            for hd in range(8):
                i2 = hd % 2
                qT, bqT = qTs[i2], b_qTs[i2]; kT, bkT = kTs[i2], b_kTs[i2]
                wq, bwq = wqs[i2], b_wqs[i2]; wk, bwk = wks[i2], b_wks[i2]
                bt, bbt = bts[i2], b_bts[i2]
                fw.dma(pool, wq[:], w_in[:, Q0 + hd * 128:Q0 + (hd + 1) * 128].rearrange("(kc p) c -> p kc c", p=128), writes=[bwq])
                fw.dma(pool, wk[:], w_in[:, K0 + hd * 128:K0 + (hd + 1) * 128].rearrange("(kc p) c -> p kc c", p=128), writes=[bwk])
                fw.dma(pool, bt[:], biasT[hd].rearrange("c k q -> k c q"), writes=[bbt])
                blks = []
                for (wt_, bwt_, dstT, bdst, nwc, bnw) in ((wq, bwq, qT, bqT, qw, b_qw), (wk, bwk, kT, bkT, kw, b_kw)):
                    for (s0, n) in TB:
                        blk = {}

                        def s1(blk=blk, wt_=wt_, bwt_=bwt_, s0=s0, n=n):
                            nonlocal cnt
                            pt, pb = fw.next_psum(); blk["pt"] = (pt, pb); blk["i"] = cnt % 2; cnt += 1
                            for kc in range(16):
                                fw.op(pe, lambda e: e.matmul(pt[:, 0:n], wt_[:, kc, :], aT[:, kc, s0:s0 + n], start=(kc == 0), stop=(kc == 15)),
                                      reads=[bwt_] + [b_aT[t] for t in tiles_of(s0, n)], writes=[pb])
                            sq_, bsq = sqs[blk["i"]], b_sqs[blk["i"]]
                            fw.op(act, lambda e: e.activation(sq_[:, 0:n], pt[:, 0:n], AF.Square), reads=[pb], writes=[bsq])

                        def s2(blk=blk, dstT=dstT, bdst=bdst, nwc=nwc, bnw=bnw, s0=s0, n=n):
                            pt, pb = blk["pt"]; i_ = blk["i"]
                            sq_, bsq = sqs[i_], b_sqs[i_]; r_, br = rs[i_], b_rs[i_]
                            p2, pb2 = fw.next_psum()
                            fw.op(pe, lambda e: e.matmul(p2[:, 0:n], ones[:, :], sq_[:, 0:n], start=True, stop=True), reads=[b_ones, bsq], writes=[pb2])
                            fw.op(act, lambda e: e.activation(r_[:, 0:n], p2[:, 0:n], AF.Sqrt, bias=EPS, scale=1.0 / 128), reads=[pb2], writes=[br])
                            fw.op(dve, lambda e: e.reciprocal(r_[:, 0:n], r_[:, 0:n]), writes=[br])
                            fw.op(dve, lambda e: e.tensor_tensor(r_[:, 0:n], pt[:, 0:n], r_[:, 0:n], ALU.mult), reads=[pb], writes=[br])
                            fw.op(pool, lambda e: e.tensor_scalar(dstT[:, s0:s0 + n], r_[:, 0:n], nwc[:, 0:1], None, ALU.mult), reads=[br, bnw], writes=[bdst])
                        blks.append((s1, s2))
                nb = len(blks)
                blks[0][0](); blks[1][0]()
                for i in range(nb):
                    blks[i][1]()
                    if i + 2 < nb:
                        blks[i + 2][0]()

                def keys_of(tq):
                    if tq < 2:
                        return [(0, None), (1, None)]
                    j = tq - 2
                    a = min(max(2 * j - 4, 0), 22)
                    cls = 0 if j == 0 else 1 if j == 1 else 3 if j == 14 else 4 if j == 15 else 2
                    return [(2 + a // 2 + i, cls * 5 + i) for i in range(5)] + [(0, None), (1, None)]

                def att1(tq):
                    nonlocal pcnt
                    keys = keys_of(tq)
                    PT, bPT = PTs[pcnt % 3], b_PTs[pcnt % 3]
                    on, bon = ons[pcnt % 3], b_ons[pcnt % 3]
                    rc, brc = rec[pcnt % 2], b_rec[pcnt % 2]; pcnt += 1
                    nk = len(keys)
                    for b0 in range(0, nk, 4):
                        pS, pbS = fw.next_psum()
                        grp = keys[b0:b0 + 4]
                        for i, (kt, bi) in enumerate(grp):
                            fw.op(pe, lambda e: e.matmul(pS[:, i * 128:(i + 1) * 128], kT[:, kt * 128:(kt + 1) * 128], qT[:, tq * 128:(tq + 1) * 128], start=True, stop=(bi is None)),
                                  reads=[bkT, bqT], writes=[pbS])
                            if bi is not None:
                                fw.op(pe, lambda e: e.matmul(pS[:, i * 128:(i + 1) * 128], identb[:, :], bt[:, bi, :], start=False, stop=True),
                                      reads=[b_identb, bbt], writes=[pbS])
                        ng = len(grp)
                        fw.op(act, lambda e: e.activation(PT[:, b0:b0 + ng, :].rearrange("p a b -> p (a b)"), pS[:, 0:ng * 128], AF.Exp),
                              reads=[pbS], writes=[bPT])
                    return (tq, keys, PT, bPT, on, bon, rc, brc)

                def att2(st_):
                    tq, keys, PT, bPT, on, bon, rc, brc = st_
                    nk = len(keys)
                    pO, pbO = fw.next_psum()
                    for i, (kt, bi) in enumerate(keys):
                        fw.op(pe, lambda e: e.matmul(pO[:, 0:129], PT[:, i, :], V1[:, kt, hd, :], start=(i == 0), stop=(i == nk - 1)),
                              reads=[bPT, b_V1[kt]], writes=[pbO])
                    fw.op(dve, lambda e: e.reciprocal(rc[:, :], pO[:, 128:129]), reads=[pbO], writes=[brc])
                    fw.op(dve, lambda e: e.tensor_scalar(on[:, :], pO[:, 0:128], rc[:, 0:1], None, ALU.mult), reads=[pbO, brc], writes=[bon])
                    if fused:
                        d_mix.extend(out_fn(fw, tq * 128, 1024 + hd * 128, 128, on, bon))
                    else:
                        db = Buf(); d_mix.append(db)
                        fw.dma(sp, mix[tq * 128:(tq + 1) * 128, 1024 + hd * 128:1024 + (hd + 1) * 128], on[:, :], reads=[bon], writes=[db])

                cur = att1(0)
                for tq in range(NT):
                    nxt = att1(tq + 1) if tq + 1 < NT else None
                    att2(cur)
                    cur = nxt
            fw.barrier()
        if not fused:
            fw.finish(d_mix)
        print("phaseA0 ops", fw.n_ops, "waits", fw.n_waits)
    return d_mix if fused else nc

import math
from contextlib import ExitStack

NT = 18
NTOK = NT * 128
NLAT = 2048
GQ0, DQ0, GK0, DK0, GV0, DV0, NCOL1 = 0, 1024, 2048, 2304, 3328, 3584, 4608
TB = [(0, 256)] + [(256 + i * 512, 512) for i in range(4)]
LAMBDA_INIT = 0.8 - 0.6 * math.exp(-0.3 * 1)


def build_phaseA1(ctx=None):
    fused = ctx is not None
    nc = ctx["nc"] if fused else bass.Bass("TRN2", target_bir_lowering=False)
    pre = ctx["pre"] if fused else ""
    out_fn = ctx["out_fn"] if fused else None
    I = lambda name, shape, dt=F32: nc.dram_tensor(pre + name, shape, dt, kind="ExternalInput").ap()
    h_src = ctx.get("h_src") if fused else None
    h_all = I("h_all", [NTOK, D]) if h_src is None else h_src[0]
    h_bufs = () if h_src is None else h_src[1]
    row_of = None if h_src is None else h_src[2]
    cvec = I("cvec", [2, D])
    ada_w = I("ada_w", [D, 4096]); ada_b = I("ada_b", [1, 4096])
    norm_w1 = I("norm_w1", [1, D])
    w_in = I("w_in", [D, NCOL1])
    nws = I("nws", [128, 4])
    lamv = I("lamv", [1, 512])
    subln = I("subln", [1, 256])
    cosT_d = I("cosT", [128, NLAT]); sinT_d = I("sinT", [128, NLAT]); rmT_d = I("rmT", [128, 128])
    ident_d = I("ident", [128, 128])
    mix = None if fused else nc.dram_tensor("mix_part", [NLAT, 2048], F32, kind="ExternalOutput").ap()
    modrows = nc.dram_tensor(pre + "modrows", [2, 4096], F32).ap()
    d_mod = Buf("modrows"); d_mix = []

    with ExitStack() as st0:
        if fused:
            fw = ctx["fw"]; fw.stack = st0; fw.ps_pool = list(range(8))
        else:
            fw = FW(nc, st0)
        pe, dve, act, pool, sp = fw.pe, fw.dve, fw.act, fw.pool, fw.sp
        ident, b_ident = emit_consts(fw, ident_d)
        ones = fw.sb([128, 128], F32, "ones"); b_ones = Buf(); fw.op(pool, lambda e: e.memset(ones[:, :], 1.0), writes=[b_ones])
        aT = fw.sb([128, 16, NTOK], BF16, "aT"); b_aT = [Buf() for _ in range(NT)]
        emit_mods(fw, cvec, ada_w, ada_b, 4096, modrows, d_mod, ident, b_ident)
        emit_aT(fw, h_all, NT, 2, norm_w1, modrows, d_mod, aT, b_aT, ident, b_ident, row_of=row_of, h_bufs=h_bufs)

        def tiles_of(s, n):
            return list(range(s // 128, (s + n) // 128))

        with ExitStack() as st:
            fw.stack = st
            V1g = fw.sb([128, NT, 2, 129], BF16, "V1g"); V1d = fw.sb([128, NT, 4, 257], BF16, "V1d"); b_V = [Buf() for _ in range(NT)]
            fw.op(pool, lambda e: e.memset(V1g[:, :, :, :].rearrange("p a b c -> p (a b c)"), 1.0), writes=b_V)
            fw.op(pool, lambda e: e.memset(V1d[:, :, :, :].rearrange("p a b c -> p (a b c)"), 1.0), writes=b_V)
            with ExitStack() as st2:
                fw.stack = st2
                wv = fw.sb([128, 16, 1280], BF16, "wv"); b_wv = Buf()
                fw.dma(pool, wv[:], w_in[:, GV0:GV0 + 1280].rearrange("(kc p) c -> p kc c", p=128), writes=[b_wv])
                for t in range(NT):
                    for part in range(3):
                        c0 = part * 512; n = 512 if part < 2 else 256
                        pt, pb = fw.next_psum()
                        for kc in range(16):
                            fw.op(pe, lambda e, kc=kc, pt=pt, t=t, c0=c0, n=n: e.matmul(pt[:, 0:n], aT[:, kc, t * 128:(t + 1) * 128], wv[:, kc, c0:c0 + n], start=(kc == 0), stop=(kc == 15)),
                                  reads=[b_aT[t], b_wv], writes=[pb])
                        if part == 0:
                            fw.op(act, lambda e, pt=pt, t=t: e.copy(V1g[:, t, :, 0:128], pt[:, 0:256].rearrange("p (a b) -> p a b", a=2)), reads=[pb], writes=[b_V[t]])
                            fw.op(dve, lambda e, pt=pt, t=t: e.tensor_copy(V1d[:, t, 0, 0:256], pt[:, 256:512]), reads=[pb], writes=[b_V[t]])
                        elif part == 1:
                            fw.op(act, lambda e, pt=pt, t=t: e.copy(V1d[:, t, 1:3, 0:256], pt[:, 0:512].rearrange("p (a b) -> p a b", a=2)), reads=[pb], writes=[b_V[t]])
                        else:
                            fw.op(dve, lambda e, pt=pt, t=t: e.tensor_copy(V1d[:, t, 3, 0:256], pt[:, 0:256]), reads=[pb], writes=[b_V[t]])
                fw.barrier()
            fw.stack = st
            fw.ps_pool = [0, 1, 2, 3]
            cosT = fw.sb([128, NLAT], F32, "cosT"); b_cos = Buf(); fw.dma(sp, cosT[:], cosT_d, writes=[b_cos])
            sinT = fw.sb([128, NLAT], F32, "sinT"); b_sin = Buf(); fw.dma(sp, sinT[:], sinT_d, writes=[b_sin])
            rmT = fw.sb([128, 128], F32, "rmT"); b_rm = Buf(); fw.dma(sp, rmT[:], rmT_d, writes=[b_rm])
            nw4 = fw.sb([128, 4], F32, "nw4"); b_nw4 = Buf(); fw.dma(sp, nw4[:], nws, writes=[b_nw4])
            nwq = fw.sb([128, 4], F32, "nwq"); b_nwq = Buf()
            fw.op(dve, lambda e: e.tensor_scalar(nwq[:, :], nw4[:, :], float(128 ** -0.5), None, ALU.mult), reads=[b_nw4], writes=[b_nwq])
            sub = fw.sb([128, 256], F32, "sub"); b_sub = Buf(); fw.dma(sp, sub[:], bview(subln), writes=[b_sub])
            fw.op(dve, lambda e: e.tensor_scalar(sub[:, :], sub[:, :], float(1.0 - LAMBDA_INIT), None, ALU.mult), writes=[b_sub])
            lv = fw.sb([128, 512], F32, "lv"); b_lv = Buf(); fw.dma(sp, lv[:], bview(lamv), writes=[b_lv])
            lt = fw.sb([128, 256], F32, "lt"); b_lt = Buf()
            fw.op(dve, lambda e: e.tensor_tensor(lt[:, :].rearrange("p (a b) -> p a b", a=2), lv[:, :].rearrange("p (a c b) -> p a c b", a=2, c=2)[:, :, 0, :],
                                                 lv[:, :].rearrange("p (a c b) -> p a c b", a=2, c=2)[:, :, 1, :], ALU.mult), reads=[b_lv], writes=[b_lt])
            ld = fw.sb([128, 2], F32, "ld"); b_ld = Buf()
            fw.op(dve, lambda e: e.reduce_sum(ld[:, :], lt[:, :].rearrange("p (a b) -> p a b", a=2), axis=AX.X), reads=[b_lt], writes=[b_ld])
            fw.op(act, lambda e: e.activation(ld[:, :], ld[:, :], AF.Exp), writes=[b_ld])
            nlam = fw.sb([128, 1], F32, "nlam"); b_nlam = Buf()
            fw.op(dve, lambda e: e.tensor_tensor(nlam[:, :], ld[:, 1:2], ld[:, 0:1], ALU.subtract), reads=[b_ld], writes=[b_nlam])
            fw.op(dve, lambda e: e.tensor_scalar(nlam[:, :], nlam[:, :], float(-LAMBDA_INIT), None, ALU.add), writes=[b_nlam])

            sqs = [fw.sb([128, 512], F32, "sq%d" % i) for i in range(2)]; b_sqs = [Buf() for _ in range(2)]
            rs = [fw.sb([128, 512], F32, "rs%d" % i) for i in range(2)]; b_rs = [Buf() for _ in range(2)]
            xws = [fw.sb([128, 512], F32, "xw%d" % i) for i in range(2)]; b_xws = [Buf() for _ in range(2)]
            us = [fw.sb([128, 512], F32, "u%d" % i) for i in range(2)]; b_us = [Buf() for _ in range(2)]
            wus = [fw.sb([128, 16, 128], BF16, "wu%d" % i) for i in range(3)]; b_wus = [Buf() for _ in range(3)]
            st_ = {"cnt": 0, "w": 0}

            pend = []

            def qk_unit(col0, nwcol, b_nwcol, dstT, bdst, with_ctx):
                wu, bwu = wus[st_["w"] % 3], b_wus[st_["w"] % 3]; st_["w"] += 1
                first = [True]
                for (s0, n) in (TB if with_ctx else TB[1:]):
                    blk = {}

                    def s1(blk=blk, s0=s0, n=n, is_first=first[0]):
                        if is_first:
                            fw.dma(pool, wu[:], w_in[:, col0:col0 + 128].rearrange("(kc p) c -> p kc c", p=128), writes=[bwu])
                        i2 = st_["cnt"] % 2; st_["cnt"] += 1
                        blk["i2"] = i2
                        sq_, bsq = sqs[i2], b_sqs[i2]
                        pt, pb = fw.next_psum(); blk["pt"] = (pt, pb)
                        for kc in range(16):
                            fw.op(pe, lambda e: e.matmul(pt[:, 0:n], wu[:, kc, :], aT[:, kc, s0:s0 + n], start=(kc == 0), stop=(kc == 15)),
                                  reads=[bwu] + [b_aT[t] for t in tiles_of(s0, n)], writes=[pb])
                        fw.op(act, lambda e: e.activation(sq_[:, 0:n], pt[:, 0:n], AF.Square), reads=[pb], writes=[bsq])

                    def s2(blk=blk, s0=s0, n=n):
                        i2 = blk["i2"]; pt, pb = blk["pt"]
                        sq_, bsq = sqs[i2], b_sqs[i2]; r_, br = rs[i2], b_rs[i2]; xw, bxw = xws[i2], b_xws[i2]
                        p2, pb2 = fw.next_psum()
                        fw.op(pe, lambda e: e.matmul(p2[:, 0:n], ones[:, :], sq_[:, 0:n], start=True, stop=True), reads=[b_ones, bsq], writes=[pb2])
                        fw.op(act, lambda e: e.activation(r_[:, 0:n], p2[:, 0:n], AF.Sqrt, bias=EPS, scale=1.0 / 128), reads=[pb2], writes=[br])
                        fw.op(dve, lambda e: e.reciprocal(r_[:, 0:n], r_[:, 0:n]), writes=[br])
                        fw.op(dve, lambda e: e.tensor_tensor(r_[:, 0:n], pt[:, 0:n], r_[:, 0:n], ALU.mult), reads=[pb], writes=[br])
                        d0 = s0 if with_ctx else s0 - 256
                        if s0 == 0:
                            fw.op(pool, lambda e: e.tensor_scalar(dstT[:, d0:d0 + n], r_[:, 0:n], nwcol, None, ALU.mult), reads=[br, b_nwcol], writes=[bdst])
                        else:
                            fw.op(pool, lambda e: e.tensor_scalar(xw[:, 0:n], r_[:, 0:n], nwcol, None, ALU.mult), reads=[br, b_nwcol], writes=[bxw])

                    def s3(blk=blk, s0=s0, n=n):
                        if s0 == 0:
                            return
                        i2 = blk["i2"]
                        xw, bxw = xws[i2], b_xws[i2]; u_, bu = us[i2], b_us[i2]
                        d0 = s0 if with_ctx else s0 - 256
                        l0 = s0 - 256
                        p3, pb3 = fw.next_psum()
                        fw.op(pe, lambda e: e.matmul(p3[:, 0:n], rmT[:, :], xw[:, 0:n], start=True, stop=True), reads=[b_rm, bxw], writes=[pb3])
                        fw.op(dve, lambda e: e.tensor_tensor(u_[:, 0:n], p3[:, 0:n], sinT[:, l0:l0 + n], ALU.mult), reads=[pb3, b_sin], writes=[bu])
                        fw.op(pool, lambda e: e.tensor_tensor(xw[:, 0:n], xw[:, 0:n], cosT[:, l0:l0 + n], ALU.mult), reads=[b_cos], writes=[bxw])
                        fw.op(pool, lambda e: e.tensor_tensor(dstT[:, d0:d0 + n], xw[:, 0:n], u_[:, 0:n], ALU.add), reads=[bxw, bu], writes=[bdst])

                    pend.append((s1, s2, s3))
                    first[0] = False

            def flush_qk():
                fw.ps_pool = list(range(8))
                blks = list(pend); pend.clear()
                nb = len(blks)
                for i in range(min(2, nb)):
                    blks[i][0]()
                for i in range(nb):
                    blks[i][1]()
                    if i + 2 < nb:
                        blks[i + 2][0]()
                    blks[i][2]()
                fw.ps_pool = [0, 1, 2, 3]

            kTs = [fw.sb([128, NTOK], BF16, "kT%d" % i) for i in range(2)]; b_kTs = [Buf() for _ in range(2)]
            qTs = [fw.sb([128, NLAT], BF16, "qT%d" % i) for i in range(2)]; b_qTs = [Buf() for _ in range(2)]
            PTs = [fw.sb([128, 512], BF16, "PT%d" % i) for i in range(3)]; b_PTs = [Buf() for _ in range(3)]
            ogs = [fw.sb([128, 128], F32, "og%d" % i) for i in range(3)]; b_ogs = [Buf() for _ in range(3)]
            rcs = [fw.sb([128, 1], F32, "rc%d" % i) for i in range(3)]; b_rcs = [Buf() for _ in range(3)]
            o0n = fw.sb([128, 4, 256], F32, "o0n"); b_o0n = [Buf() for _ in range(4)]
            ods = [fw.sb([128, 256], F32, "od%d" % i) for i in range(2)]; b_ods = [Buf() for _ in range(2)]
            o1s = [fw.sb([128, 256], F32, "o1_%d" % i) for i in range(2)]; b_o1s = [Buf() for _ in range(2)]
            sq2 = fw.sb([128, 256], F32, "sq2"); b_sq2 = Buf()
            ss2 = [fw.sb([128, 1], F32, "ss2_%d" % i) for i in range(2)]; b_ss2 = [Buf() for _ in range(2)]
            cn = {"pt": 0, "o": 0, "k": 0, "q": 0, "d": 0}

            def attend(qT, bqT, kT, bkT, vfn, vw, banks, qb, finish):
                stride = 512
                per_bank = 1
                def score(kt):
                    pS, pbS = fw.next_psum()
                    fw.op(pe, lambda e: e.matmul(pS[:, :], kT[:, kt * 128:(kt + 1) * 128], qT[:, qb * 512:(qb + 1) * 512], start=True, stop=True),
                          reads=[bkT, bqT], writes=[pbS])
                    return pS, pbS
                cur = score(0)
                for kt in range(NT):
                    nxt = score(kt + 1) if kt + 1 < NT else None
                    pS, pbS = cur
                    PT, bPT = PTs[cn["pt"] % 3], b_PTs[cn["pt"] % 3]; cn["pt"] += 1
                    fw.op(act, lambda e: e.activation(PT[:, :], pS[:, :], AF.Exp), reads=[pbS], writes=[bPT])
                    for qs in range(4):
                        bank = banks[qs // per_bank]; off = (qs % per_bank) * stride
                        pO, pbO = fw.psum[bank]
                        fw.op(pe, lambda e: e.matmul(pO[:, off:off + vw], PT[:, qs * 128:(qs + 1) * 128], vfn(kt), start=(kt == 0), stop=(kt == NT - 1)),
                              reads=[bPT, b_V[kt]], writes=[pbO])
                    cur = nxt
                for qs in range(4):
                    bank = banks[qs // per_bank]; off = (qs % per_bank) * stride
                    pO, pbO = fw.psum[bank]
                    finish(qs, pO, pbO, off)

            for kv in range(2):
                kT, bkT = kTs[cn["k"] % 2], b_kTs[cn["k"] % 2]; cn["k"] += 1
                qk_unit(GK0 + kv * 128, nw4[:, 1:2], b_nw4, kT, bkT, True)
                for hq in range(4):
                    hd = kv * 4 + hq
                    qT, bqT = qTs[cn["q"] % 2], b_qTs[cn["q"] % 2]; cn["q"] += 1
                    qk_unit(GQ0 + hd * 128, nwq[:, 0:1], b_nwq, qT, bqT, False)
                    flush_qk()
                    for qb in range(4):
                        def fin(qs, pO, pbO, off, hd=hd, qb=qb):
                            i3 = cn["o"] % 3; cn["o"] += 1
                            og, bog = ogs[i3], b_ogs[i3]; rc, brc = rcs[i3], b_rcs[i3]
                            fw.op(dve, lambda e: e.reciprocal(rc[:, :], pO[:, off + 128:off + 129]), reads=[pbO], writes=[brc])
                            fw.op(dve, lambda e: e.tensor_scalar(og[:, :], pO[:, off:off + 128], rc[:, 0:1], None, ALU.mult), reads=[pbO, brc], writes=[bog])
                            r0 = qb * 512 + qs * 128
                            if fused:
                                d_mix.extend(out_fn(fw, r0, hd * 128, 128, og, bog))
                            else:
                                db = Buf(); d_mix.append(db)
                                fw.dma(sp, mix[r0:r0 + 128, hd * 128:(hd + 1) * 128], og[:, :], reads=[bog], writes=[db])
                        attend(qT, bqT, kT, bkT, lambda kt, kv=kv: V1g[:, kt, kv, :], 129, [4, 5, 6, 7], qb, fin)
            if fused and ctx.get("mid_hook"):
                ctx["mid_hook"]()
            for h in range(4):
                kq = []
                for c in range(2):
                    kT, bkT = kTs[cn["k"] % 2], b_kTs[cn["k"] % 2]; cn["k"] += 1
                    qk_unit(DK0 + (h * 2 + c) * 128, nw4[:, 3:4], b_nw4, kT, bkT, True)
                    qT, bqT = qTs[cn["q"] % 2], b_qTs[cn["q"] % 2]; cn["q"] += 1
                    qk_unit(DQ0 + (h * 2 + c) * 128, nwq[:, 2:3], b_nwq, qT, bqT, False)
                    kq.append((kT, bkT, qT, bqT))
                flush_qk()
                for qb in range(4):
                    for c in range(2):
                        kT, bkT, qT, bqT = kq[c]
                        if c == 0:
                            def fin(qs, pO, pbO, off):
                                i3 = cn["o"] % 3; cn["o"] += 1
                                rc, brc = rcs[i3], b_rcs[i3]
                                fw.op(dve, lambda e: e.reciprocal(rc[:, :], pO[:, off + 256:off + 257]), reads=[pbO], writes=[brc])
                                fw.op(dve, lambda e: e.tensor_scalar(o0n[:, qs, :], pO[:, off:off + 256], rc[:, 0:1], None, ALU.mult), reads=[pbO, brc], writes=[b_o0n[qs]])
                        else:
                            def fin(qs, pO, pbO, off, h=h, qb=qb):
                                i3 = cn["o"] % 3; cn["o"] += 1
                                i2 = cn["d"] % 2; cn["d"] += 1
                                rc, brc = rcs[i3], b_rcs[i3]
                                o1, bo1 = o1s[i2], b_o1s[i2]; od, bod = ods[i2], b_ods[i2]; s2, bs2 = ss2[i2], b_ss2[i2]
                                fw.op(dve, lambda e: e.reciprocal(rc[:, :], pO[:, off + 256:off + 257]), reads=[pbO], writes=[brc])
                                fw.op(dve, lambda e: e.tensor_scalar(rc[:, :], rc[:, :], nlam[:, 0:1], None, ALU.mult), reads=[b_nlam], writes=[brc])
                                fw.op(dve, lambda e: e.scalar_tensor_tensor(o1[:, :], pO[:, off:off + 256], rc[:, 0:1], o0n[:, qs, :], ALU.mult, ALU.add),
                                      reads=[pbO, brc, b_o0n[qs]], writes=[bo1])
                                fw.op(act, lambda e: e.activation(sq2[:, :], o1[:, :], AF.Square, accum_out=s2[:, :]), reads=[bo1], writes=[b_sq2, bs2])
                                fw.op(act, lambda e: e.activation(s2[:, :], s2[:, :], AF.Sqrt, bias=EPS, scale=1.0 / 256), writes=[bs2])
                                fw.op(dve, lambda e: e.reciprocal(s2[:, :], s2[:, :]), writes=[bs2])
                                fw.op(dve, lambda e: e.scalar_tensor_tensor(od[:, :], o1[:, :], s2[:, 0:1], sub[:, :], ALU.mult, ALU.mult), reads=[bo1, bs2, b_sub], writes=[bod])
                                r0 = qb * 512 + qs * 128
                                if fused:
                                    d_mix.extend(out_fn(fw, r0, 1024 + h * 256, 256, od, bod))
                                else:
                                    db = Buf(); d_mix.append(db)
                                    fw.dma(sp, mix[r0:r0 + 128, 1024 + h * 256:1024 + (h + 1) * 256], od[:, :], reads=[bod], writes=[db])
                        attend(qT, bqT, kT, bkT, lambda kt, h=h: V1d[:, kt, h, :], 257, [4, 5, 6, 7], qb, fin)
            fw.barrier()
        if not fused:
            fw.finish(d_mix)
        fw.ps_pool = list(range(8))
        print("phaseA1 ops", fw.n_ops, "waits", fw.n_waits)
    return d_mix if fused else nc

from contextlib import ExitStack

PAIRS = [[0, 1], [2, 3], [4, 5], [6, 7]]
CC_BYTES = 4 * 1024 * 1024


def build_fused():
    nc = bass.Bass("TRN2", target_bir_lowering=False)
    msk_d = nc.dram_tensor("msk", [128, 2], F32, kind="ExternalInput").ap()
    x1s = [nc.dram_tensor("x1s%d" % i, [2 * 2304, 1024], BF16).ap() for i in range(2)]
    x1d = [nc.dram_tensor("x1d%d" % i, [2 * 2304, 1024], BF16).ap() for i in range(2)]
    x2s = nc.dram_tensor("x2s", [2 * 1152, 2048], F32).ap(); x2d = nc.dram_tensor("x2d", [2 * 1152, 2048], F32).ap()
    x3s = [nc.dram_tensor("x3s%d" % i, [2 * 2048, 1024], BF16).ap() for i in range(2)]
    x3d = [nc.dram_tensor("x3d%d" % i, [2 * 2048, 1024], BF16).ap() for i in range(2)]
    h1own = nc.dram_tensor("h1own", [1024, 2048], F32).ap()
    with ExitStack() as st0:
        fw = FW(nc, st0)
        cc = Src("cc", fw._sem("cc"), 1)
        msk = fw.sb([128, 2], F32, "msk"); b_msk = Buf()
        fw.dma(fw.sp, msk[:], msk_d, writes=[b_msk])

        def staging(shape, dt, n):
            stk = fw.stack
            key = "_stg_%s_%d" % (str(dt), shape[1])
            if not hasattr(stk, key):
                setattr(stk, key, {"t": [fw.sb(shape, dt, "stg%d" % i) for i in range(n)], "b": [Buf() for _ in range(n)], "i": 0})
            d = getattr(stk, key)
            k = d["i"] % n; d["i"] += 1
            return d["t"][k], d["b"][k]

        def mk_mix_out(dsts, R, wlists):
            def out_fn(fw_, r0, c0, n, tile, btile):
                res = []
                part = 0 if c0 < 1024 else 1
                cc0 = c0 - part * 1024
                for s in range(2):
                    u, bu = staging([128, 512], BF16, 4)
                    fw.op(fw.pool, lambda e: e.tensor_scalar(u[:, 0:n], tile[:, :], msk[:, s:s + 1], None, ALU.mult), reads=[btile, b_msk], writes=[bu])
                    db = Buf(); res.append(db); wlists[part].append(db)
                    fw.dma(fw.sp, dsts[part][s * R + r0:s * R + r0 + 128, cc0:cc0 + n], u[:, 0:n], reads=[bu], writes=[db])
                return res
            return out_fn

        b_h1own = []

        def b0_out(fw_, t, h1, bh1):
            res = []
            for s in range(2):
                u, bu = staging([128, 2048], F32, 4)
                fw.op(fw.pool, lambda e: e.tensor_scalar(u[:, :], h1[:, :], msk[:, s:s + 1], None, ALU.mult), reads=[bh1, b_msk], writes=[bu])
                db = Buf(); res.append(db)
                fw.dma(fw.sp, x2s[s * 1152 + t * 128:s * 1152 + (t + 1) * 128, :], u[:, :], reads=[bu], writes=[db])
            if t >= 1:
                db = Buf(); b_h1own.append(db); res.append(db)
                fw.dma(fw.sp, h1own[(t - 1) * 128:t * 128, :], h1[:, :], reads=[bh1], writes=[db])
            return res

        def exchange(src, dst, writers):
            pool = fw.pool
            for b in writers:
                pool.wait(b.w)
            nrows = src.shape[0]
            elt = 2 if src.dtype == BF16 else 4
            rows_per = CC_BYTES // (src.shape[1] * elt)
            for r0 in range(0, nrows, rows_per):
                r1 = min(nrows, r0 + rows_per)
                ins = pool.raw.collective_compute("AllReduce", ALU.add, replica_groups=PAIRS,
                                                  ins=[src[r0:r1, :].opt()], outs=[dst[r0:r1, :].opt()])
                ins.then_inc(cc.sem)
                cc.n += 1
            eb = Buf()
            eb.w = Ev(cc, cc.n, dict(pool.clock))
            return eb

        shared = {}
        wl1 = [[], []]; ev1 = [None, None]

        def hook1():
            ev1[0] = exchange(x1s[0], x1d[0], wl1[0])
        build_phaseA0(ctx=dict(nc=nc, fw=fw, pre="a0_", out_fn=mk_mix_out(x1s, 2304, wl1), mid_hook=hook1))
        ev1[1] = exchange(x1s[1], x1d[1], wl1[1])
        w2_ = build_phaseB(9, True, ctx=dict(nc=nc, fw=fw, pre="b0_", mix_src=[(x1d[0], [ev1[0]]), (x1d[1], [ev1[1]])], out_fn=b0_out, shared=shared))
        e2 = exchange(x2s, x2d, w2_)

        def row_of(t):
            return 0 if t == 0 else 1152 if t == 1 else 128 + (t - 2) * 128 if t < 10 else 1280 + (t - 10) * 128

        wl3 = [[], []]; ev3 = [None, None]

        def hook3():
            ev3[0] = exchange(x3s[0], x3d[0], wl3[0])
        build_phaseA1(ctx=dict(nc=nc, fw=fw, pre="a1_", h_src=(x2d, [e2], row_of), out_fn=mk_mix_out(x3s, 2048, wl3), mid_hook=hook3))
        ev3[1] = exchange(x3s[1], x3d[1], wl3[1])
        outs = build_phaseB(8, False, ctx=dict(nc=nc, fw=fw, pre="b1_", h_src=(h1own, list(b_h1own)), mix_src=[(x3d[0], [ev3[0]]), (x3d[1], [ev3[1]])], out_fn=None, shared=shared))
        fw.finish(outs)
        print("fused ops", fw.n_ops, "waits", fw.n_waits)
    return nc

import numpy as np

PERM = np.concatenate([np.arange(0, 1024), np.arange(2048, 3072), np.arange(1024, 2048), np.arange(3072, 4096)])
_CONST = {}

def consts():
    if "ident" not in _CONST:
        s = np.arange(128)
        _CONST["ident"] = np.eye(128, dtype=np.float32)
        _CONST["trilt"] = (s[:, None] < s[None, :]).astype(np.float32)
        _CONST["ule"] = (s[:, None] <= s[None, :]).astype(np.float32)
        _CONST["uge"] = (s[:, None] >= s[None, :]).astype(np.float32)
        _CONST["iota64"] = np.arange(64, dtype=np.float32).reshape(1, 64)
    return _CONST

def f32c(a):
    return np.ascontiguousarray(a, dtype=np.float32)

def na_bias_tables(rpb, heads):
    classes = [(0, 0), (1, 0), (2, 0), (14, 22), (15, 22)]
    out = np.full((len(heads), 25, 128, 128), -30000.0, np.float32)
    k = np.arange(128); q = np.arange(128)
    for ci, (j, a) in enumerate(classes):
        r = 2 * j + q // 64; c = q % 64
        r0 = np.clip(r - 4, 0, 24); cs = np.clip(c - 8, 0, 48)
        for i in range(5):
            kr = a + 2 * i + k // 64; kc = k % 64
            vis = ((kr[:, None] >= r0[None, :]) & (kr[:, None] < r0[None, :] + 8) &
                   (kc[:, None] >= cs[None, :]) & (kc[:, None] < cs[None, :] + 16))
            ri = np.clip(kr[:, None] - r[None, :] + 7, 0, 14)
            cj = np.clip(kc[:, None] - c[None, :] + 15, 0, 30)
            for hi, h in enumerate(heads):
                vals = rpb[h][ri, cj]
                out[hi, ci * 5 + i] = np.where(vis, vals, np.float32(-30000.0))
    return out

def pack_A0(P, b, hh, h_all):
    in_w = P["ev_in_w"][0]
    gs = [2 * hh, 2 * hh + 1]
    heads16 = np.arange(16 * hh, 16 * hh + 16)
    nah = np.arange(8 * hh, 8 * hh + 8)
    cols = np.concatenate(
        [np.arange(g * 512, (g + 1) * 512) for g in gs] +
        [2048 + np.arange(g * 512, (g + 1) * 512) for g in gs] +
        [4096 + np.arange(g * 128, (g + 1) * 128) for g in gs] +
        [4608 + np.arange(g * 128, (g + 1) * 128) for g in gs] +
        [5120 + heads16, 5152 + heads16] +
        [5184 + np.arange(h * 128, (h + 1) * 128) for h in nah] +
        [5184 + 2048 + np.arange(h * 128, (h + 1) * 128) for h in nah] +
        [5184 + 4096 + np.arange(h * 128, (h + 1) * 128) for h in nah])
    chans = np.concatenate([np.arange(g * 512, (g + 1) * 512) for g in gs] +
                           [2048 + np.arange(g * 128, (g + 1) * 128) for g in gs] +
                           [2560 + np.arange(g * 128, (g + 1) * 128) for g in gs])
    d = {k: consts()[k] for k in ("ident", "ule", "uge")}
    d.update({
        "h_all": h_all, "cvec": np.stack([P["c"][b], P["c_ctx"]], 0),
        "ada_w": P["ada_w"][0][:, 0:4096], "ada_b": P["ada_b"][0][None, 0:4096],
        "norm_w1": P["norm_w"][0, 0][None, :], "w_in": in_w[:, cols],
        "convw": P["ev_conv_w"][0][:, chans].T, "convb": P["ev_conv_b"][0][chans].reshape(12, 128).T,
        "dt_bias": np.concatenate([P["ev_dt_bias"][0][0, heads16], P["ev_dt_bias"][0][1, heads16]])[None, :],
        "a_log": np.concatenate([P["ev_a_log"][0][0, heads16], P["ev_a_log"][0][1, heads16]])[None, :],
        "d_skip": P["ev_d_skip"][0][heads16][None, :],
        "ssd_nw": np.concatenate([P["ev_ssd_norm_w"][0][g * 512:(g + 1) * 512] for g in gs])[None, :],
        "qnw": P["ev_na_q_norm"][0][:, None], "knw": P["ev_na_k_norm"][0][:, None],
        "biasT": na_bias_tables(P["ev_na_rpb"][0], list(nah)),
    })
    return {k: f32c(v) for k, v in d.items()}

def rope_tables():
    if "cosT" not in _CONST:
        t = np.arange(2048)
        row = (t // 64).astype(np.float32); col = (t % 64).astype(np.float32)
        inv = (1.0 / (np.float32(10000.0) ** (np.arange(0, 64, 2, dtype=np.float32) / np.float32(64)))).astype(np.float32)
        ang = np.concatenate([row[:, None] * inv[None], col[:, None] * inv[None]], -1).astype(np.float32)
        c = np.cos(ang).astype(np.float32); s = np.sin(ang).astype(np.float32)
        _CONST["cosT"] = np.ascontiguousarray(np.repeat(c, 2, axis=1).T)
        _CONST["sinT"] = np.ascontiguousarray(np.repeat(s, 2, axis=1).T)
        rm = np.zeros((128, 128), np.float32)
        for i in range(64):
            rm[2 * i + 1, 2 * i] = -1.0
            rm[2 * i, 2 * i + 1] = 1.0
        _CONST["rmT"] = rm
    return _CONST["cosT"], _CONST["sinT"], _CONST["rmT"]

def pack_A1(P, b, hh, h_all):
    in_w = P["od_in_w"][0]
    cols = np.concatenate(
        [np.arange((8 * hh + i) * 128, (8 * hh + i + 1) * 128) for i in range(8)] +
        [2048 + np.arange((8 * hh + i) * 128, (8 * hh + i + 1) * 128) for i in range(8)] +
        [4096 + np.arange((2 * hh + i) * 128, (2 * hh + i + 1) * 128) for i in range(2)] +
        [5120 + np.arange((8 * hh + i) * 128, (8 * hh + i + 1) * 128) for i in range(8)] +
        [4608 + np.arange((2 * hh + i) * 128, (2 * hh + i + 1) * 128) for i in range(2)] +
        [7168 + np.arange((4 * hh + i) * 256, (4 * hh + i + 1) * 256) for i in range(4)])
    cosT, sinT, rmT = rope_tables()
    d = {
        "ident": consts()["ident"],
        "h_all": h_all, "cvec": np.stack([P["c"][b], P["c_ctx"]], 0),
        "ada_w": P["ada_w"][1][:, 0:4096], "ada_b": P["ada_b"][1][None, 0:4096],
        "norm_w1": P["norm_w"][1, 0][None, :], "w_in": in_w[:, cols],
        "nws": np.stack([P["od_gqa_q_norm"][0], P["od_gqa_k_norm"][0], P["od_diff_q_norm"][0], P["od_diff_k_norm"][0]], 1),
        "lamv": P["od_lambda"][0].reshape(1, 512), "subln": P["od_diff_subln"][0][None, :],
        "cosT": cosT, "sinT": sinT, "rmT": rmT,
    }
    return {k: f32c(v) for k, v in d.items()}


_PROG = {}


def _shared_B(P, layer):
    ow = P["ev_out_w"][0] if layer == 0 else P["od_out_w"][0]
    c = consts()
    sh = {"ident": c["ident"], "trilt": c["trilt"], "iota64": c["iota64"],
          "ada_w": P["ada_w"][layer][:, 4096:], "ada_b": P["ada_b"][layer][None, 4096:],
          "norm_w2": P["norm_w"][layer, 1][None, :], "out_w": ow[PERM],
          "gwew": np.concatenate([P["moe_group_w"][layer], P["moe_expert_w"][layer]], 1),
          "w1": P["moe_w1"][layer], "w3": P["moe_w3"][layer], "w2": P["moe_w2"][layer]}
    return {k: f32c(v) for k, v in sh.items()}


def kernel(**inputs):
    P = {k: np.asarray(v) for k, v in inputs.items()}
    B = 4
    cores = [(b, hh) for b in range(B) for hh in range(2)]
    if "nc" not in _PROG:
        _PROG["nc"] = build_fused()
    shB = [_shared_B(P, 0), _shared_B(P, 1)]
    a0 = {}; a1 = {}
    maps = []
    for (b, hh) in cores:
        h_all = np.concatenate([P["ctx"][b], P["x"][b]], 0)
        if hh not in a0:
            a0[hh] = pack_A0(P, b, hh, h_all)
            a1[hh] = pack_A1(P, b, hh, h_all)
            a1[hh].pop("h_all")
        cvec = f32c(np.stack([P["c"][b], P["c_ctx"]], 0))
        d = {}
        for k, v in a0[hh].items():
            d["a0_" + k] = v
        d["a0_h_all"] = f32c(h_all); d["a0_cvec"] = cvec
        for k, v in a1[hh].items():
            d["a1_" + k] = v
        d["a1_cvec"] = cvec
        for l, pre in ((0, "b0_"), (1, "b1_")):
            for k, v in shB[l].items():
                d[pre + k] = v
            d[pre + "cvec"] = cvec
        rows0 = np.concatenate([np.arange(hh * 128, (hh + 1) * 128), 256 + np.arange(hh * 1024, (hh + 1) * 1024)])
        d["b0_h_in"] = f32c(h_all[rows0])
        g0 = rows0.reshape(9, 128).T
        d["b0_gidx"] = np.ascontiguousarray(np.stack([g0, 2304 + g0], -1).astype(np.int32))
        g1 = (hh * 1024 + np.arange(1024)).reshape(8, 128).T
        d["b1_gidx"] = np.ascontiguousarray(np.stack([g1, 2048 + g1], -1).astype(np.int32))
        m = np.zeros((128, 2), np.float32); m[:, hh] = 1.0
        d["msk"] = m
        maps.append(d)
    res = run_bass_kernel_spmd(_PROG["nc"], maps, core_ids=list(range(8)))
    out = np.zeros((B, 2048, 2048), np.float32)
    for i, (b, hh) in enumerate(cores):
        out[b, hh * 1024:(hh + 1) * 1024] = res.results[i]["h_out"]
    return out
```

```python
import numpy as np
import ml_dtypes
import concourse.bass as bass
import concourse.mybir as mybir
from concourse.bass_utils import run_bass_kernel_spmd

F32 = mybir.dt.float32
BF16 = mybir.dt.bfloat16
I32 = mybir.dt.int32
U32 = mybir.dt.uint32
AF = mybir.ActivationFunctionType
ALU = mybir.AluOpType
AX = mybir.AxisListType


class Ev:
    __slots__ = ("src", "count", "clock")

    def __init__(self, src, count, clock):
        self.src = src
        self.count = count
        self.clock = clock


class Src:
    def __init__(self, name, sem, mult):
        self.name = name
        self.sem = sem
        self.mult = mult
        self.n = 0


class Buf:
    __slots__ = ("name", "w", "r")

    def __init__(self, name=""):
        self.name = name
        self.w = None
        self.r = {}


class Eng:
    def __init__(self, fw, raw, name):
        self.fw = fw
        self.raw = raw
        self.name = name
        self.src = Src(name, fw._sem("s_" + name), 1)
        self.clock = {}
        self.slots = []
        self.slot_i = 0

    def wait(self, ev):
        if ev is None:
            return
        if self.name == "pe" and ev.src.name.startswith("pe"):
            return
        if self.clock.get(ev.src.name, 0) >= ev.count:
            return
        self.raw.wait_ge(ev.src.sem, ev.count * ev.src.mult)
        self.fw.n_waits += 1
        for k, v in ev.clock.items():
            if self.clock.get(k, 0) < v:
                self.clock[k] = v
        self.clock[ev.src.name] = ev.count


class FW:
    def __init__(self, nc, stack, n_dma_slots=12):
        self.nc = nc
        self.stack = stack
        self.sem_stack = stack
        self.n_waits = 0
        self.n_ops = 0
        self.pe = Eng(self, nc.tensor, "pe")
        self.dve = Eng(self, nc.vector, "dve")
        self.act = Eng(self, nc.scalar, "act")
        self.pool = Eng(self, nc.gpsimd, "pool")
        self.sp = Eng(self, nc.sync, "sp")
        for e in (self.sp, self.pool, self.act):
            for i in range(n_dma_slots):
                nm = "d_%s%d" % (e.name, i)
                e.slots.append(Src(nm, self._sem(nm), 16))
        self.uid = 0
        self.psum = []
        for i in range(8):
            t = stack.enter_context(nc.psum_tensor("ps%d" % i, [128, 512], F32))
            self.psum.append((t, Buf("ps%d" % i)))
        self.ps_i = 0
        self.ps_pool = list(range(8))

    def _sem(self, name):
        return self.sem_stack.enter_context(self.nc.semaphore(name))

    def sb(self, shape, dtype, name=None):
        self.uid += 1
        name = "sb%d_%s" % (self.uid, name or "t")
        t = self.stack.enter_context(self.nc.sbuf_tensor(name, list(shape), dtype))
        return t

    def next_psum(self):
        self.ps_i = (self.ps_i + 1) % len(self.ps_pool)
        return self.psum[self.ps_pool[self.ps_i]]

    def _deps(self, eng, reads, writes):
        for b in reads:
            eng.wait(b.w)
        for b in writes:
            eng.wait(b.w)
            for ev in list(b.r.values()):
                eng.wait(ev)

    def _mark(self, ev, reads, writes):
        for b in reads:
            b.r[ev.src.name] = ev
        for b in writes:
            b.w = ev
            b.r = {}

    def op(self, eng, fn, reads=(), writes=()):
        self._deps(eng, reads, writes)
        ins = fn(eng.raw)
        if eng.src.n >= 30000:
            eng.gen = getattr(eng, "gen", 0) + 1
            nm = "%s_g%d" % (eng.name, eng.gen)
            eng.src = Src(nm, self._sem("s_" + nm), 1)
        eng.src.n += 1
        ins.then_inc(eng.src.sem, 1)
        clock = dict(eng.clock)
        ev = Ev(eng.src, eng.src.n, clock)
        self._mark(ev, reads, writes)
        self.n_ops += 1
        return ev

    def dma(self, eng, out, in_, reads=(), writes=(), fn=None):
        slot = eng.slots[eng.slot_i]
        eng.slot_i = (eng.slot_i + 1) % len(eng.slots)
        if slot.n > 0:
            eng.wait(Ev(slot, slot.n, {}))
        self._deps(eng, reads, writes)
        if fn is None:
            ins = eng.raw.dma_start(out=out, in_=in_)
        else:
            ins = fn(eng.raw)
        ins.then_inc(slot.sem, 16)
        slot.n += 1
        ev = Ev(slot, slot.n, dict(eng.clock))
        self._mark(ev, reads, writes)
        self.n_ops += 1
        return ev

    def bound_reg(self, val):
        if not hasattr(self, "_bregs"):
            self._bregs = {}
        if val not in self._bregs:
            self._bregs[val] = self.nc.gpsimd.to_reg(val)
        return self._bregs[val]

    def engines(self):
        return (self.pe, self.dve, self.act, self.pool, self.sp)

    def barrier(self):
        evs = []
        for e in self.engines():
            if e.src.n > 0:
                evs.append(Ev(e.src, e.src.n, dict(e.clock)))
            for s in e.slots:
                if s.n > 0:
                    evs.append(Ev(s, s.n, {}))
        for e in self.engines():
            for ev in evs:
                e.wait(ev)

    def finish(self, bufs):
        for b in bufs:
            self.sp.wait(b.w)

from contextlib import ExitStack

D = 2048
EPS = 1e-6


def bview(ap_row, n=128):
    return ap_row.partition_broadcast(n)


def emit_consts(fw, ident_d):
    ident = fw.sb([128, 128], F32, "ident"); b_ident = Buf("ident")
    fw.dma(fw.sp, ident[:], ident_d, writes=[b_ident])
    return ident, b_ident


def emit_mods(fw, cvec, ada_w, ada_b, ncols, modrows, d_mod, ident, b_ident):
    pe, dve, act, pool, sp = fw.pe, fw.dve, fw.act, fw.pool, fw.sp
    outer = fw.stack
    with ExitStack() as st:
        fw.stack = st
        cv = fw.sb([2, D], F32, "cv"); b_cv = Buf()
        fw.dma(sp, cv[:], cvec, writes=[b_cv])
        siluT = fw.sb([128, 16, 2], BF16, "siluT"); b_sT = Buf()
        pt, pb = fw.next_psum()
        for kc in range(16):
            fw.op(pe, lambda e, kc=kc: e.transpose(pt[:, 2 * kc:2 * kc + 2], cv[0:2, kc * 128:(kc + 1) * 128], ident[0:2, 0:2]),
                  reads=[b_cv, b_ident], writes=[pb])
        fw.op(act, lambda e: e.activation(siluT[:, :, :].rearrange("p a b -> p (a b)"), pt[:, 0:32], AF.Silu), reads=[pb], writes=[b_sT])
        adab = fw.sb([2, ncols], F32, "adab"); b_adab = Buf()
        fw.dma(sp, adab[:], bview(ada_b, 2), writes=[b_adab])
        wts = [fw.sb([128, 16, 512], BF16, "adaw%d" % i) for i in range(2)]
        b_wts = [Buf() for _ in range(2)]
        modsb = fw.sb([2, ncols], F32, "modsb"); b_modsb = Buf()
        for j in range(ncols // 512):
            wt, bw = wts[j % 2], b_wts[j % 2]
            fw.dma(pool, wt[:], ada_w[:, j * 512:(j + 1) * 512].rearrange("(kc p) c -> p kc c", p=128), writes=[bw])
            pt, pb = fw.next_psum()
            for kc in range(16):
                fw.op(pe, lambda e, kc=kc, wt=wt, pt=pt: e.matmul(pt[0:2, :], siluT[:, kc, :], wt[:, kc, :], start=(kc == 0), stop=(kc == 15)),
                      reads=[b_sT, bw], writes=[pb])
            fw.op(dve, lambda e, pt=pt, j=j: e.tensor_tensor(modsb[:, j * 512:(j + 1) * 512], pt[0:2, :], adab[:, j * 512:(j + 1) * 512], ALU.add),
                  reads=[pb, b_adab], writes=[b_modsb])
        fw.dma(sp, modrows, modsb[:, :], reads=[b_modsb], writes=[d_mod])
        fw.barrier()
    fw.stack = outer


def emit_aT(fw, h_all, n_tiles, n_ctx_tiles, norm_w1, modrows, d_mod, aT, b_aT, ident, b_ident, row_of=None, h_bufs=()):
    pe, dve, act, pool, sp = fw.pe, fw.dve, fw.act, fw.pool, fw.sp
    outer = fw.stack
    with ExitStack() as st:
        fw.stack = st
        nw = fw.sb([128, D], F32, "nw"); b_nw = Buf()
        fw.dma(sp, nw[:], bview(norm_w1), writes=[b_nw])
        nsc = []; sh = []; b_nsc = []; b_sh = []
        for v in range(2):
            s = fw.sb([128, D], F32, "sh1_%d" % v); bs = Buf()
            fw.dma(sp, s[:], bview(modrows[v:v + 1, 0:2048]), reads=[d_mod], writes=[bs])
            sh.append(s); b_sh.append(bs)
            c = fw.sb([128, D], F32, "nsc1_%d" % v); bc = Buf()
            fw.dma(sp, c[:], bview(modrows[v:v + 1, 2048:4096]), reads=[d_mod], writes=[bc])
            fw.op(dve, lambda e, c=c: e.scalar_tensor_tensor(c[:, :], c[:, :], 1.0, nw[:, :], ALU.add, ALU.mult), reads=[b_nw], writes=[bc])
            nsc.append(c); b_nsc.append(bc)
        hts = [fw.sb([128, D], F32, "ht%d" % i) for i in range(2)]; b_hts = [Buf() for _ in range(2)]
        ats = [fw.sb([128, D], F32, "at%d" % i) for i in range(2)]; b_ats = [Buf() for _ in range(2)]
        sq = fw.sb([128, D], F32, "sq"); b_sq = Buf()
        ssqs = [fw.sb([128, 1], F32, "ssq%d" % i) for i in range(2)]; b_ssqs = [Buf() for _ in range(2)]
        for t in range(n_tiles):
            v = 1 if t < n_ctx_tiles else 0
            ht, bh = hts[t % 2], b_hts[t % 2]
            at, ba = ats[t % 2], b_ats[t % 2]
            ssq, b_ssq = ssqs[t % 2], b_ssqs[t % 2]
            r0 = t * 128 if row_of is None else row_of(t)
            fw.dma(sp, ht[:], h_all[r0:r0 + 128, :], reads=list(h_bufs), writes=[bh])
            fw.op(act, lambda e, ht=ht, ssq=ssq: e.activation(sq[:, :], ht[:, :], AF.Square, accum_out=ssq[:, :]), reads=[bh], writes=[b_sq, b_ssq])
            fw.op(act, lambda e, ssq=ssq: e.activation(ssq[:, :], ssq[:, :], AF.Sqrt, bias=EPS, scale=1.0 / D), writes=[b_ssq])
            fw.op(dve, lambda e, ssq=ssq: e.reciprocal(ssq[:, :], ssq[:, :]), writes=[b_ssq])
            fw.op(dve, lambda e, ht=ht, at=at, ssq=ssq, v=v: e.scalar_tensor_tensor(at[:, :], ht[:, :], ssq[:, 0:1], nsc[v][:, :], ALU.mult, ALU.mult),
                  reads=[bh, b_ssq, b_nsc[v]], writes=[ba])
            fw.op(pool, lambda e, at=at, v=v: e.tensor_tensor(at[:, :], at[:, :], sh[v][:, :], ALU.add), reads=[b_sh[v]], writes=[ba])
            for q in range(4):
                pt, pb = fw.next_psum()
                for i in range(4):
                    kc = q * 4 + i
                    fw.op(pe, lambda e, pt=pt, i=i, kc=kc, at=at: e.transpose(pt[:, i * 128:(i + 1) * 128], at[:, kc * 128:(kc + 1) * 128], ident[:, :]),
                          reads=[ba, b_ident], writes=[pb])
                dst = aT[:, q * 4:(q + 1) * 4, t * 128:(t + 1) * 128]
                src = pt[:, :].rearrange("p (a b) -> p a b", a=4)
                if q % 2 == 0:
                    fw.op(act, lambda e, dst=dst, src=src: e.copy(dst, src), reads=[pb], writes=[b_aT[t]])
                else:
                    fw.op(dve, lambda e, dst=dst, src=src: e.tensor_copy(dst, src), reads=[pb], writes=[b_aT[t]])
        fw.barrier()
    fw.stack = outer

from contextlib import ExitStack

D = 2048
CAP = 128
NE = 64
EPS = 1e-6


def bview(ap_row, n=128):
    return ap_row.partition_broadcast(n)


def build_phaseB(n_tiles, has_ctx, ctx=None):
    fused = ctx is not None
    nc = ctx["nc"] if fused else bass.Bass("TRN2", target_bir_lowering=False)
    pre = ctx["pre"] if fused else ""
    T = n_tiles * 128
    I = lambda name, shape, dt=F32: nc.dram_tensor(pre + name, shape, dt, kind="ExternalInput").ap()
    h_src = ctx.get("h_src") if fused else None
    h_in = I("h_in", [T, D]) if h_src is None else h_src[0]
    h_bufs = [] if h_src is None else h_src[1]
    mix_in = None if fused else I("mix_in", [T, 4096])
    gidx_d = I("gidx", [128, n_tiles, 2], I32) if fused else None
    cvec = I("cvec", [2, D])
    ada_w = I("ada_w", [D, 8192])
    ada_b = I("ada_b", [1, 8192])
    norm_w2 = I("norm_w2", [1, D])
    out_w = I("out_w", [4096, D])
    gwew = I("gwew", [D, 72])
    w1 = I("w1", [NE, D, 512])
    w3 = I("w3", [NE, D, 512])
    w2 = I("w2", [NE, 512, D])
    ident_d = I("ident", [128, 128])
    trilt_d = I("trilt", [128, 128])
    iota_d = I("iota64", [1, 64])
    out_fn = ctx.get("out_fn") if fused else None
    h_out = nc.dram_tensor("h_out", [T, D], F32, kind="ExternalOutput").ap() if out_fn is None else None
    modrows = nc.dram_tensor(pre + "modrows", [2, 8192], F32).ap()
    m_all = nc.dram_tensor(pre + "m_all", [T, D], F32).ap()
    h1_all = nc.dram_tensor(pre + "h1_all", [T, D], F32).ap()
    reuse_x = fused and ("xdisp" in ctx.get("shared", {}))
    if reuse_x:
        xdisp = ctx["shared"]["xdisp"]
    else:
        xdisp = nc.dram_tensor(pre + "xdisp", [NE * CAP, D], F32).ap()
        if fused and "shared" in ctx:
            ctx["shared"]["xdisp"] = xdisp
    ybuf = nc.dram_tensor(pre + "ybuf", [NE * CAP, D], BF16).ap()
    d_mod = Buf("modrows"); d_m = [Buf() for _ in range(n_tiles)]; d_h1 = [Buf() for _ in range(n_tiles)]
    d_x = Buf("xdisp"); d_y = [Buf() for _ in range(NE)]; d_out = [Buf() for _ in range(n_tiles)]
    d_xz = [] if reuse_x else [Buf() for _ in range(NE * CAP // 512)]

    with ExitStack() as st0:
        if fused:
            fw = ctx["fw"]; fw.stack = st0; fw.ps_pool = list(range(8))
        else:
            fw = FW(nc, st0)
        pe, dve, act, pool, sp = fw.pe, fw.dve, fw.act, fw.pool, fw.sp
        ident = fw.sb([128, 128], F32, "ident"); b_ident = Buf()
        fw.dma(sp, ident[:], ident_d, writes=[b_ident])
        trilt = fw.sb([128, 128], BF16, "trilt"); b_tri = Buf()
        fw.dma(pool, trilt[:], trilt_d, writes=[b_tri])
        ones_bf = fw.sb([128, 128], BF16, "ones_bf"); b_ones = Buf()
        fw.op(pool, lambda e: e.memset(ones_bf[:, :], 1.0), writes=[b_ones])
        iota = fw.sb([128, 64], F32, "iota"); b_iota = Buf()
        fw.dma(sp, iota[:], bview(iota_d), writes=[b_iota])
        zcol = fw.sb([128, 1], F32, "zcol"); b_z = Buf()
        fw.op(pool, lambda e: e.memset(zcol[:, :], 0.0), writes=[b_z])
        dest_all = fw.sb([128, n_tiles, 2], I32, "dest_all"); b_dest = [Buf() for _ in range(n_tiles)]
        gate_all = fw.sb([128, n_tiles, 2], F32, "gate_all"); b_gate = [Buf() for _ in range(n_tiles)]
        with ExitStack() as st:
            fw.stack = st
            if not reuse_x:
                zt = fw.sb([128, 8192], F32, "zt"); b_zt = Buf()
                fw.op(pool, lambda e: e.memset(zt[:, :], 0.0), writes=[b_zt])
                xv = xdisp.rearrange("(a p r) d -> a p (r d)", p=128, r=4)
                for a in range(NE * CAP // 512):
                    fw.dma(sp, xv[a], zt[:, :], reads=[b_zt], writes=[d_xz[a]])
            cv = fw.sb([2, D], F32, "cv"); b_cv = Buf()
            fw.dma(sp, cv[:], cvec, writes=[b_cv])
            siluT = fw.sb([128, 16, 2], BF16, "siluT"); b_sT = Buf()
            pt, pb = fw.next_psum()
            for kc in range(16):
                fw.op(pe, lambda e, kc=kc: e.transpose(pt[:, 2 * kc:2 * kc + 2], cv[0:2, kc * 128:(kc + 1) * 128], ident[0:2, 0:2]),
                      reads=[b_cv, b_ident], writes=[pb])
            fw.op(act, lambda e: e.activation(siluT[:, :, :].rearrange("p a b -> p (a b)"), pt[:, 0:32], AF.Silu), reads=[pb], writes=[b_sT])
            adab = fw.sb([2, 8192], F32, "adab"); b_adab = Buf()
            fw.dma(sp, adab[:], bview(ada_b, 2), writes=[b_adab])
            wts = [fw.sb([128, 16, 512], BF16, "adaw%d" % i) for i in range(2)]
            b_wts = [Buf() for _ in range(2)]
            modsb = fw.sb([2, 8192], F32, "modsb"); b_modsb = Buf()
            for j in range(16):
                wt, bw = wts[j % 2], b_wts[j % 2]
                fw.dma(pool, wt[:], ada_w[:, j * 512:(j + 1) * 512].rearrange("(kc p) c -> p kc c", p=128), writes=[bw])
                pt, pb = fw.next_psum()
                for kc in range(16):
                    fw.op(pe, lambda e, kc=kc, wt=wt, pt=pt: e.matmul(pt[0:2, :], siluT[:, kc, :], wt[:, kc, :], start=(kc == 0), stop=(kc == 15)),
                          reads=[b_sT, bw], writes=[pb])
                fw.op(dve, lambda e, pt=pt, j=j: e.tensor_tensor(modsb[:, j * 512:(j + 1) * 512], pt[0:2, :], adab[:, j * 512:(j + 1) * 512], ALU.add),
                      reads=[pb, b_adab], writes=[b_modsb])
            fw.dma(sp, modrows, modsb[:, :], reads=[b_modsb], writes=[d_mod])
            fw.barrier()
        with ExitStack() as st:
            fw.stack = st
            mixT = fw.sb([128, n_tiles, 32, 128], BF16, "mixT"); b_mixT = [Buf() for _ in range(n_tiles)]
            mts = [fw.sb([128, 4096], F32, "mixt%d" % i) for i in range(2)]; b_mts = [Buf() for _ in range(2)]
            if fused:
                parts = ctx["mix_src"]
                gix = fw.sb([128, n_tiles, 2], I32, "gix"); b_gix = Buf()
                fw.dma(sp, gix[:], gidx_d, writes=[b_gix])
                m16 = [fw.sb([128, 1024], BF16, "m16_%d" % i) for i in range(8)]; b_m16 = [Buf() for _ in range(8)]
                mcnt = 0
            for t in range(n_tiles):
                mt, bm = mts[t % 2], b_mts[t % 2]
                if not fused:
                    fw.dma(sp, mt[:], mix_in[t * 128:(t + 1) * 128, :], writes=[bm])
                else:
                    for k in range(2):
                        for a_, (gsrc, gbufs, pc0) in enumerate(parts):
                            pw = gsrc.shape[1]
                            mm, bmm = m16[mcnt % 8], b_m16[mcnt % 8]; mcnt += 1
                            fw.dma(pool, None, None, reads=gbufs + [b_gix], writes=[bmm],
                                   fn=lambda e: e.indirect_dma_start(
                                       out=mm[:, 0:pw], out_offset=None, in_=gsrc[:, :],
                                       in_offset=bass.IndirectOffsetOnAxis(ap=gix[:, t, k:k + 1], axis=0),
                                       bounds_check=fw.bound_reg(gsrc.shape[0] - 1), oob_is_err=False))
                            c0_ = k * 2048 + pc0
                            if a_ % 2 == 0:
                                fw.op(act, lambda e: e.copy(mt[:, c0_:c0_ + pw], mm[:, 0:pw]), reads=[bmm], writes=[bm])
                            else:
                                fw.op(dve, lambda e: e.tensor_copy(mt[:, c0_:c0_ + pw], mm[:, 0:pw]), reads=[bmm], writes=[bm])
                for q in range(8):
                    pt, pb = fw.next_psum()
                    for i in range(4):
                        kc = q * 4 + i
                        fw.op(pe, lambda e, pt=pt, i=i, kc=kc, mt=mt: e.transpose(pt[:, i * 128:(i + 1) * 128], mt[:, kc * 128:(kc + 1) * 128], ident[:, :]),
                              reads=[bm, b_ident], writes=[pb])
                    eng = act if q % 2 == 0 else dve
                    if eng is act:
                        fw.op(act, lambda e, pt=pt, t=t, q=q: e.copy(mixT[:, t, q * 4:(q + 1) * 4, :].rearrange("p a b -> p (a b)"), pt[:, :]),
                              reads=[pb], writes=[b_mixT[t]])
                    else:
                        fw.op(dve, lambda e, pt=pt, t=t, q=q: e.tensor_copy(mixT[:, t, q * 4:(q + 1) * 4, :].rearrange("p a b -> p (a b)"), pt[:, :]),
                              reads=[pb], writes=[b_mixT[t]])
            ows = [fw.sb([128, 32, 256], BF16, "ow%d" % i) for i in range(2)]; b_ows = [Buf() for _ in range(2)]
            mbs = [fw.sb([128, 256], F32, "mb%d" % i) for i in range(3)]; b_mbs = [Buf() for _ in range(3)]
            cnt = 0
            for j in range(8):
                ow, bo = ows[j % 2], b_ows[j % 2]
                fw.dma(pool, ow[:], out_w[:, j * 256:(j + 1) * 256].rearrange("(kc p) c -> p kc c", p=128), writes=[bo])
                for t in range(n_tiles):
                    pt, pb = fw.next_psum()
                    for kc in range(32):
                        fw.op(pe, lambda e, pt=pt, kc=kc, t=t, ow=ow: e.matmul(pt[:, 0:256], mixT[:, t, kc, :], ow[:, kc, :], start=(kc == 0), stop=(kc == 31)),
                              reads=[b_mixT[t], bo], writes=[pb])
                    mb, bmb = mbs[cnt % 3], b_mbs[cnt % 3]; cnt += 1
                    if cnt % 2:
                        fw.op(act, lambda e, pt=pt, mb=mb: e.copy(mb[:, :], pt[:, 0:256]), reads=[pb], writes=[bmb])
                    else:
                        fw.op(dve, lambda e, pt=pt, mb=mb: e.tensor_copy(mb[:, :], pt[:, 0:256]), reads=[pb], writes=[bmb])
                    fw.dma(sp, m_all[t * 128:(t + 1) * 128, j * 256:(j + 1) * 256], mb[:, :], reads=[bmb], writes=[d_m[t]])
            fw.barrier()
        with ExitStack() as st:
            fw.stack = st
            nvar = 2 if has_ctx else 1
            g1 = []; nsc = []; sh2 = []
            b_g1 = []; b_nsc = []; b_sh2 = []
            nw = fw.sb([128, D], F32, "nw"); b_nw = Buf()
            fw.dma(sp, nw[:], bview(norm_w2), writes=[b_nw])
            for v in range(nvar):
                a = fw.sb([128, D], F32, "g1_%d" % v); ba = Buf()
                fw.dma(sp, a[:], bview(modrows[v:v + 1, 0:2048]), reads=[d_mod], writes=[ba])
                g1.append(a); b_g1.append(ba)
                s = fw.sb([128, D], F32, "sh2_%d" % v); bs = Buf()
                fw.dma(sp, s[:], bview(modrows[v:v + 1, 2048:4096]), reads=[d_mod], writes=[bs])
                sh2.append(s); b_sh2.append(bs)
                c = fw.sb([128, D], F32, "nsc_%d" % v); bc = Buf()
                fw.dma(sp, c[:], bview(modrows[v:v + 1, 4096:6144]), reads=[d_mod], writes=[bc])
                fw.op(dve, lambda e, c=c: e.scalar_tensor_tensor(c[:, :], c[:, :], 1.0, nw[:, :], ALU.add, ALU.mult), reads=[b_nw], writes=[bc])
                nsc.append(c); b_nsc.append(bc)
            gw = fw.sb([128, 16, 72], F32, "gw"); b_gw = Buf()
            fw.dma(sp, gw[:], gwew.rearrange("(kc p) c -> p kc c", p=128), writes=[b_gw])
            acum = fw.sb([128, 64], F32, "acum"); b_acum = Buf()
            fw.op(pool, lambda e: e.memset(acum[:, :], 0.0), writes=[b_acum])
            acum_bf = fw.sb([128, 64], BF16, "acum_bf"); b_acbf = Buf()
            fw.op(pool, lambda e: e.memset(acum_bf[:, :], 0.0), writes=[b_acbf])
            NB = 2
            hts = [fw.sb([128, D], F32, "ht%d" % i) for i in range(NB)]; b_hts = [Buf() for _ in range(NB)]
            mts = [fw.sb([128, D], F32, "mt%d" % i) for i in range(NB)]; b_mts = [Buf() for _ in range(NB)]
            fts = [fw.sb([128, D], F32, "ft%d" % i) for i in range(NB)]; b_fts = [Buf() for _ in range(NB)]
            fTs = [fw.sb([128, 16, 128], F32, "fT%d" % i) for i in range(NB)]; b_fTs = [Buf() for _ in range(NB)]
            sq = fw.sb([128, D], F32, "sq"); b_sq = Buf()
            sm = {}
            def S(name, shape, dt=F32):
                if name not in sm:
                    sm[name] = (fw.sb(shape, dt, "r_" + name), Buf(name))
                return sm[name]
            for t in range(n_tiles):
                v = 1 if (has_ctx and t == 0) else 0
                ht, bh = hts[t % NB], b_hts[t % NB]
                mt, bm = mts[t % NB], b_mts[t % NB]
                ft, bf = fts[t % NB], b_fts[t % NB]
                fT, bfT = fTs[t % NB], b_fTs[t % NB]
                fw.dma(sp, ht[:], h_in[t * 128:(t + 1) * 128, :], reads=h_bufs, writes=[bh])
                fw.dma(sp, mt[:], m_all[t * 128:(t + 1) * 128, :], reads=[d_m[t]], writes=[bm])
                fw.op(pool, lambda e, mt=mt, v=v: e.tensor_tensor(mt[:, :], mt[:, :], g1[v][:, :], ALU.mult), reads=[b_g1[v]], writes=[bm])
                fw.op(dve, lambda e, mt=mt, ht=ht: e.tensor_tensor(ht[:, :], mt[:, :], ht[:, :], ALU.add), reads=[bm], writes=[bh])
                fw.dma(sp, h1_all[t * 128:(t + 1) * 128, :], ht[:, :], reads=[bh], writes=[d_h1[t]])
                ssq, b_ssq = S("ssq", [128, 1])
                fw.op(act, lambda e, ht=ht: e.activation(sq[:, :], ht[:, :], AF.Square, accum_out=ssq[:, :]), reads=[bh], writes=[b_sq, b_ssq])
                rstd, b_rstd = S("rstd", [128, 1])
                fw.op(act, lambda e: e.activation(rstd[:, :], ssq[:, :], AF.Sqrt, bias=EPS, scale=1.0 / D), reads=[b_ssq], writes=[b_rstd])
                fw.op(dve, lambda e: e.reciprocal(rstd[:, :], rstd[:, :]), writes=[b_rstd])
                fw.op(dve, lambda e, ht=ht, ft=ft, v=v: e.scalar_tensor_tensor(ft[:, :], ht[:, :], rstd[:, 0:1], nsc[v][:, :], ALU.mult, ALU.mult),
                      reads=[bh, b_rstd, b_nsc[v]], writes=[bf])
                fw.op(pool, lambda e, ft=ft, v=v: e.tensor_tensor(ft[:, :], ft[:, :], sh2[v][:, :], ALU.add), reads=[b_sh2[v]], writes=[bf])
                for q in range(4):
                    pt, pb = fw.next_psum()
                    for i in range(4):
                        kc = q * 4 + i
                        fw.op(pe, lambda e, pt=pt, i=i, kc=kc, ft=ft: e.transpose(pt[:, i * 128:(i + 1) * 128], ft[:, kc * 128:(kc + 1) * 128], ident[:, :]),
                              reads=[bf, b_ident], writes=[pb])
                    if q % 2 == 0:
                        fw.op(act, lambda e, pt=pt, q=q, fT=fT: e.copy(fT[:, q * 4:(q + 1) * 4, :].rearrange("p a b -> p (a b)"), pt[:, :]), reads=[pb], writes=[bfT])
                    else:
                        fw.op(dve, lambda e, pt=pt, q=q, fT=fT: e.tensor_copy(fT[:, q * 4:(q + 1) * 4, :].rearrange("p a b -> p (a b)"), pt[:, :]), reads=[pb], writes=[bfT])
                pl, pbl = fw.next_psum()
                for kc in range(16):
                    fw.op(pe, lambda e, kc=kc, fT=fT, pl=pl: e.matmul(pl[:, 0:72], fT[:, kc, :], gw[:, kc, :], start=(kc == 0), stop=(kc == 15)),
                          reads=[bfT, b_gw], writes=[pbl])
                lg, b_lg = S("lg", [128, 72])
                fw.op(dve, lambda e, pl=pl: e.tensor_copy(lg[:, :], pl[:, 0:72]), reads=[pbl], writes=[b_lg])
                g8, b_g8 = S("g8", [128, 8])
                fw.op(dve, lambda e: e.max(g8[:, :], lg[:, 0:8]), reads=[b_lg], writes=[b_g8])
                ngm, b_ngm = S("ngm", [128, 1])
                fw.op(dve, lambda e: e.tensor_scalar(ngm[:, :], g8[:, 0:1], -1.0, None, ALU.mult), reads=[b_g8], writes=[b_ngm])
                gex, b_gex = S("gex", [128, 8]); gsum, b_gsum = S("gsum", [128, 1])
                fw.op(act, lambda e: e.activation(gex[:, :], lg[:, 0:8], AF.Exp, bias=ngm[:, 0:1], scale=1.0, accum_out=gsum[:, :]),
                      reads=[b_lg, b_ngm], writes=[b_gex, b_gsum])
                ggate, b_gg = S("ggate", [128, 1])
                fw.op(dve, lambda e: e.reciprocal(ggate[:, :], gsum[:, :]), reads=[b_gsum], writes=[b_gg])
                pen, b_pen = S("pen", [128, 8])
                fw.op(dve, lambda e: e.tensor_scalar(pen[:, :], lg[:, 0:8], g8[:, 0:1], zcol[:, 0:1], ALU.is_equal, ALU.add), reads=[b_lg, b_g8, b_z], writes=[b_pen])
                fw.op(dve, lambda e: e.tensor_scalar(pen[:, :], pen[:, :], -1.0, 1e9, ALU.add, ALU.mult), writes=[b_pen])
                lem, b_lem = S("lem", [128, 64])
                fw.op(dve, lambda e: e.tensor_tensor(lem[:, :].rearrange("p (g e) -> p g e", g=8), lg[:, 8:72].rearrange("p (g e) -> p g e", g=8),
                                                     pen[:, :].unsqueeze(2).to_broadcast([128, 8, 8]), ALU.add), reads=[b_lg, b_pen], writes=[b_lem])
                t8, b_t8 = S("t8", [128, 8]); i8, b_i8 = S("i8", [128, 8], U32)
                fw.op(dve, lambda e: e.max(t8[:, :], lem[:, :]), reads=[b_lem], writes=[b_t8])
                fw.op(dve, lambda e: e.max_index(i8[:, :], t8[:, :], lem[:, :]), reads=[b_lem, b_t8], writes=[b_i8])
                ef, b_ef = S("ef", [128, 2])
                fw.op(dve, lambda e: e.tensor_copy(ef[:, :], i8[:, 0:2]), reads=[b_i8], writes=[b_ef])
                dd, b_dd = S("dd", [128, 1])
                fw.op(dve, lambda e: e.tensor_tensor(dd[:, :], t8[:, 1:2], t8[:, 0:1], ALU.subtract), reads=[b_t8], writes=[b_dd])
                ex, b_ex = S("ex", [128, 1])
                fw.op(act, lambda e: e.activation(ex[:, :], dd[:, :], AF.Exp), reads=[b_dd], writes=[b_ex])
                den, b_den = S("den", [128, 1])
                fw.op(dve, lambda e: e.tensor_scalar(den[:, :], ex[:, :], 1.0, None, ALU.add), reads=[b_ex], writes=[b_den])
                fw.op(dve, lambda e: e.reciprocal(den[:, :], den[:, :]), writes=[b_den])
                fw.op(dve, lambda e, t=t: e.tensor_tensor(gate_all[:, t, 0:1], den[:, :], ggate[:, :], ALU.mult), reads=[b_den, b_gg], writes=[b_gate[t]])
                fw.op(dve, lambda e, t=t: e.tensor_tensor(gate_all[:, t, 1:2], gate_all[:, t, 0:1], ex[:, :], ALU.mult), reads=[b_ex], writes=[b_gate[t]])
                A0, b_A0 = S("A0", [128, 64]); A1, b_A1 = S("A1", [128, 64]); A, b_A = S("A", [128, 64]); Abf, b_Abf = S("Abf", [128, 64], BF16)
                fw.op(dve, lambda e: e.tensor_scalar(A0[:, :], iota[:, :], ef[:, 0:1], None, ALU.is_equal), reads=[b_iota, b_ef], writes=[b_A0])
                fw.op(dve, lambda e: e.tensor_scalar(A1[:, :], iota[:, :], ef[:, 1:2], None, ALU.is_equal), reads=[b_iota, b_ef], writes=[b_A1])
                fw.op(dve, lambda e: e.tensor_tensor(A[:, :], A0[:, :], A1[:, :], ALU.add), reads=[b_A0, b_A1], writes=[b_A])
                fw.op(dve, lambda e: e.tensor_copy(Abf[:, :], A[:, :]), reads=[b_A], writes=[b_Abf])
                pr, pbr = fw.next_psum()
                fw.op(pe, lambda e, pr=pr: e.matmul(pr[:, 0:64], trilt[:, :], Abf[:, :], start=True, stop=False), reads=[b_tri, b_Abf], writes=[pbr])
                fw.op(pe, lambda e, pr=pr: e.matmul(pr[:, 0:64], ones_bf[:, :], acum_bf[:, :], start=False, stop=True), reads=[b_ones, b_acbf], writes=[pbr])
                rk, b_rk = S("rk", [128, 2]); tmp, b_tmp = S("tmp", [128, 64])
                for k, (Ak, bAk) in enumerate(((A0, b_A0), (A1, b_A1))):
                    fw.op(dve, lambda e, Ak=Ak, pr=pr: e.tensor_tensor(tmp[:, :], Ak[:, :], pr[:, 0:64], ALU.mult), reads=[bAk, pbr], writes=[b_tmp])
                    fw.op(dve, lambda e, k=k: e.reduce_sum(rk[:, k:k + 1], tmp[:, :], axis=AX.X), reads=[b_tmp], writes=[b_rk])
                fw.op(dve, lambda e: e.tensor_tensor(acum[:, :], acum[:, :], A[:, :], ALU.add), reads=[b_A], writes=[b_acum])
                fw.op(dve, lambda e: e.tensor_copy(acum_bf[:, :], acum[:, :]), reads=[b_acum], writes=[b_acbf])
                df, b_df = S("df", [128, 2]); ov, b_ov = S("ov", [128, 2])
                fw.op(dve, lambda e: e.tensor_scalar(ov[:, :], rk[:, :], float(CAP), 1e6, ALU.is_ge, ALU.mult), reads=[b_rk], writes=[b_ov])
                fw.op(dve, lambda e: e.scalar_tensor_tensor(df[:, :], ef[:, :], float(CAP), rk[:, :], ALU.mult, ALU.add), reads=[b_ef, b_rk], writes=[b_df])
                fw.op(dve, lambda e: e.tensor_tensor(df[:, :], df[:, :], ov[:, :], ALU.add), reads=[b_ov], writes=[b_df])
                fw.op(dve, lambda e, t=t: e.tensor_copy(dest_all[:, t, :], df[:, :]), reads=[b_df], writes=[b_dest[t]])
                for k in range(2):
                    fw.dma(pool, None, None, reads=[bf, b_dest[t]] + d_xz, writes=[d_x],
                           fn=lambda e, t=t, k=k, ft=ft: e.indirect_dma_start(
                               out=xdisp[:, :], out_offset=bass.IndirectOffsetOnAxis(ap=dest_all[:, t, k:k + 1], axis=0),
                               in_=ft[:, :], in_offset=None, bounds_check=fw.bound_reg(NE * CAP - 1), oob_is_err=False))
            fw.barrier()
        with ExitStack() as st:
            fw.stack = st
            NW = 2
            w1s = [fw.sb([128, 16, 512], BF16, "w1s%d" % i) for i in range(NW)]; b_w1s = [Buf() for _ in range(NW)]
            w3s = [fw.sb([128, 16, 512], BF16, "w3s%d" % i) for i in range(NW)]; b_w3s = [Buf() for _ in range(NW)]
            w2s = [fw.sb([128, 4, D], BF16, "w2s%d" % i) for i in range(NW)]; b_w2s = [Buf() for _ in range(NW)]
            xes = [fw.sb([128, D], F32, "xe%d" % i) for i in range(2)]; b_xes = [Buf() for _ in range(2)]
            xTs = [fw.sb([128, 16, 128], BF16, "xT%d" % i) for i in range(2)]; b_xTs = [Buf() for _ in range(2)]
            sil = fw.sb([128, 512], F32, "sil"); b_sil = Buf()
            hact = fw.sb([128, 512], F32, "hact"); b_hact = Buf()
            hT = fw.sb([128, 4, 128], BF16, "hT"); b_hT = Buf()
            yes = [fw.sb([128, D], BF16, "ye%d" % i) for i in range(2)]; b_yes = [Buf() for _ in range(2)]
            for ex_ in range(NE):
                i2 = ex_ % 2
                w1t, bw1 = w1s[ex_ % NW], b_w1s[ex_ % NW]
                w3t, bw3 = w3s[ex_ % NW], b_w3s[ex_ % NW]
                w2t, bw2 = w2s[ex_ % NW], b_w2s[ex_ % NW]
                fw.dma(pool, w1t[:], w1[ex_].rearrange("(kc p) h -> p kc h", p=128), writes=[bw1])
                fw.dma(pool, w3t[:], w3[ex_].rearrange("(kc p) h -> p kc h", p=128), writes=[bw3])
                fw.dma(pool, w2t[:], w2[ex_].rearrange("(hc p) d -> p hc d", p=128), writes=[bw2])
                xe, bxe = xes[i2], b_xes[i2]
                xT, bxT = xTs[i2], b_xTs[i2]
                fw.dma(sp, xe[:], xdisp[ex_ * CAP:(ex_ + 1) * CAP, :], reads=[d_x] + ([d_xz[ex_ // 4]] if d_xz else []), writes=[bxe])
                for q in range(4):
                    pt, pb = fw.next_psum()
                    for i in range(4):
                        kc = q * 4 + i
                        fw.op(pe, lambda e, pt=pt, i=i, kc=kc, xe=xe: e.transpose(pt[:, i * 128:(i + 1) * 128], xe[:, kc * 128:(kc + 1) * 128], ident[:, :]),
                              reads=[bxe, b_ident], writes=[pb])
                    if q % 2 == 0:
                        fw.op(act, lambda e, pt=pt, q=q, xT=xT: e.copy(xT[:, q * 4:(q + 1) * 4, :].rearrange("p a b -> p (a b)"), pt[:, :]), reads=[pb], writes=[bxT])
                    else:
                        fw.op(dve, lambda e, pt=pt, q=q, xT=xT: e.tensor_copy(xT[:, q * 4:(q + 1) * 4, :].rearrange("p a b -> p (a b)"), pt[:, :]), reads=[pb], writes=[bxT])
                p1, pb1 = fw.next_psum()
                for kc in range(16):
                    fw.op(pe, lambda e, kc=kc, p1=p1, xT=xT, w1t=w1t: e.matmul(p1[:, :], xT[:, kc, :], w1t[:, kc, :], start=(kc == 0), stop=(kc == 15)),
                          reads=[bxT, bw1], writes=[pb1])
                p3, pb3 = fw.next_psum()
                for kc in range(16):
                    fw.op(pe, lambda e, kc=kc, p3=p3, xT=xT, w3t=w3t: e.matmul(p3[:, :], xT[:, kc, :], w3t[:, kc, :], start=(kc == 0), stop=(kc == 15)),
                          reads=[bxT, bw3], writes=[pb3])
                fw.op(act, lambda e, p1=p1: e.activation(sil[:, :], p1[:, :], AF.Silu), reads=[pb1], writes=[b_sil])
                fw.op(dve, lambda e, p3=p3: e.tensor_tensor(hact[:, :], sil[:, :], p3[:, :], ALU.mult), reads=[b_sil, pb3], writes=[b_hact])
                pt, pb = fw.next_psum()
                for hc in range(4):
                    fw.op(pe, lambda e, pt=pt, hc=hc: e.transpose(pt[:, hc * 128:(hc + 1) * 128], hact[:, hc * 128:(hc + 1) * 128], ident[:, :]),
                          reads=[b_hact, b_ident], writes=[pb])
                fw.op(act, lambda e, pt=pt: e.copy(hT[:, :, :].rearrange("p a b -> p (a b)"), pt[:, :]), reads=[pb], writes=[b_hT])
                ye, bye = yes[i2], b_yes[i2]
                for db in range(4):
                    py, pby = fw.next_psum()
                    for hc in range(4):
                        fw.op(pe, lambda e, py=py, hc=hc, db=db, w2t=w2t: e.matmul(py[:, :], hT[:, hc, :], w2t[:, hc, db * 512:(db + 1) * 512], start=(hc == 0), stop=(hc == 3)),
                              reads=[b_hT, bw2], writes=[pby])
                    if db % 2 == 0:
                        fw.op(dve, lambda e, py=py, db=db, ye=ye: e.tensor_copy(ye[:, db * 512:(db + 1) * 512], py[:, :]), reads=[pby], writes=[bye])
                    else:
                        fw.op(act, lambda e, py=py, db=db, ye=ye: e.copy(ye[:, db * 512:(db + 1) * 512], py[:, :]), reads=[pby], writes=[bye])
                fw.dma(sp, ybuf[ex_ * CAP:(ex_ + 1) * CAP, :], ye[:, :], reads=[bye], writes=[d_y[ex_]])
            fw.barrier()
        with ExitStack() as st:
            fw.stack = st
            nvar = 2 if has_ctx else 1
            g2 = []; b_g2 = []
            for v in range(nvar):
                a = fw.sb([128, D], F32, "g2_%d" % v); ba = Buf()
                fw.dma(sp, a[:], bview(modrows[v:v + 1, 6144:8192]), reads=[d_mod], writes=[ba])
                g2.append(a); b_g2.append(ba)
            r0s = [fw.sb([128, D], BF16, "r0_%d" % i) for i in range(2)]; b_r0s = [Buf() for _ in range(2)]
            r1s = [fw.sb([128, D], BF16, "r1_%d" % i) for i in range(2)]; b_r1s = [Buf() for _ in range(2)]
            ycs = [fw.sb([128, D], F32, "yc_%d" % i) for i in range(2)]; b_ycs = [Buf() for _ in range(2)]
            h1s = [fw.sb([128, D], F32, "h1_%d" % i) for i in range(2)]; b_h1s = [Buf() for _ in range(2)]
            for t in range(n_tiles):
                v = 1 if (has_ctx and t == 0) else 0
                r0, br0 = r0s[t % 2], b_r0s[t % 2]
                r1, br1 = r1s[t % 2], b_r1s[t % 2]
                h1, bh1 = h1s[t % 2], b_h1s[t % 2]
                fw.dma(sp, h1[:], h1_all[t * 128:(t + 1) * 128, :], reads=[d_h1[t]], writes=[bh1])
                for k, (r, br) in enumerate(((r0, br0), (r1, br1))):
                    fw.op(pool, lambda e, r=r: e.memset(r[:, :], 0.0), writes=[br])
                    fw.dma(pool, None, None, reads=d_y + [b_dest[t]], writes=[br],
                           fn=lambda e, r=r, t=t, k=k: e.indirect_dma_start(
                               out=r[:, :], out_offset=None, in_=ybuf[:, :],
                               in_offset=bass.IndirectOffsetOnAxis(ap=dest_all[:, t, k:k + 1], axis=0),
                               bounds_check=fw.bound_reg(NE * CAP - 1), oob_is_err=False))
                yc, byc = ycs[t % 2], b_ycs[t % 2]
                fw.op(dve, lambda e: e.tensor_scalar(yc[:, :], r0[:, :], gate_all[:, t, 0:1], None, ALU.mult), reads=[br0, b_gate[t]], writes=[byc])
                fw.op(dve, lambda e: e.scalar_tensor_tensor(yc[:, :], r1[:, :], gate_all[:, t, 1:2], yc[:, :], ALU.mult, ALU.add),
                      reads=[br1, b_gate[t]], writes=[byc])
                fw.op(pool, lambda e: e.tensor_tensor(yc[:, :], yc[:, :], g2[v][:, :], ALU.mult), reads=[b_g2[v]], writes=[byc])
                fw.op(dve, lambda e: e.tensor_tensor(h1[:, :], h1[:, :], yc[:, :], ALU.add), reads=[byc], writes=[bh1])
                if out_fn is None:
                    fw.dma(sp, h_out[t * 128:(t + 1) * 128, :], h1[:, :], reads=[bh1], writes=[d_out[t]])
                else:
                    d_out[t] = out_fn(fw, t, h1, bh1)
            if not fused:
                fw.finish(d_out)
            else:
                fw.barrier()
        print("phaseB ops", fw.n_ops, "waits", fw.n_waits)
    if fused:
        return [b for x in d_out for b in (x if isinstance(x, list) else [x])]
    return nc

from contextlib import ExitStack

NT = 18
NTOK = NT * 128
Z0, X0, B0, C0, DT0, Q0, K0, V0, NCOL = 0, 1024, 2048, 2304, 2560, 2592, 3616, 4640, 5664
TB = [(0, 256)] + [(256 + i * 512, 512) for i in range(4)]


def build_phaseA0(stop_after=None, dbg=False, ctx=None):
    fused = ctx is not None
    nc = ctx["nc"] if fused else bass.Bass("TRN2", target_bir_lowering=False)
    pre = ctx["pre"] if fused else ""
    out_fn = ctx["out_fn"] if fused else None
    I = lambda name, shape, dt=F32: nc.dram_tensor(pre + name, shape, dt, kind="ExternalInput").ap()
    h_all = I("h_all", [NTOK, D])
    cvec = I("cvec", [2, D])
    ada_w = I("ada_w", [D, 4096]); ada_b = I("ada_b", [1, 4096])
    norm_w1 = I("norm_w1", [1, D])
    w_in = I("w_in", [D, NCOL])
    convw = I("convw", [1536, 5]); convb = I("convb", [128, 12])
    dt_bias = I("dt_bias", [1, 32]); a_log = I("a_log", [1, 32]); d_skip = I("d_skip", [1, 16]); ssd_nw = I("ssd_nw", [1, 1024])
    qnw = I("qnw", [128, 1]); knw = I("knw", [128, 1])
    biasT = I("biasT", [8, 25, 128, 128])
    ident_d = I("ident", [128, 128]); ule_d = I("ule", [128, 128]); uge_d = I("uge", [128, 128])
    mix = None if fused else nc.dram_tensor("mix_part", [NTOK, 2048], F32, kind="ExternalOutput").ap()
    modrows = nc.dram_tensor(pre + "modrows", [2, 4096], F32).ap()
    Hb_d = nc.dram_tensor(pre + "Hb_d", [NT, 128, 512], BF16).ap()
    d_mod = Buf("modrows"); d_mix = []

    with ExitStack() as st0:
        if fused:
            fw = ctx["fw"]; fw.stack = st0; fw.ps_pool = list(range(8))
        else:
            fw = FW(nc, st0)
        pe, dve, act, pool, sp = fw.pe, fw.dve, fw.act, fw.pool, fw.sp
        ident, b_ident = emit_consts(fw, ident_d)
        ule = fw.sb([128, 128], F32, "ule"); b_ule = Buf(); fw.dma(sp, ule[:], ule_d, writes=[b_ule])
        uge = fw.sb([128, 128], F32, "uge"); b_uge = Buf(); fw.dma(sp, uge[:], uge_d, writes=[b_uge])
        ones = fw.sb([128, 128], F32, "ones"); b_ones = Buf(); fw.op(pool, lambda e: e.memset(ones[:, :], 1.0), writes=[b_ones])
        identb = fw.sb([128, 128], BF16, "identb"); b_identb = Buf(); fw.dma(pool, identb[:], ident_d, writes=[b_identb])
        zcol = fw.sb([128, 1], F32, "zcol"); b_z = Buf(); fw.op(pool, lambda e: e.memset(zcol[:, :], 0.0), writes=[b_z])
        aT = fw.sb([128, 16, NTOK], BF16, "aT"); b_aT = [Buf() for _ in range(NT)]
        emit_mods(fw, cvec, ada_w, ada_b, 4096, modrows, d_mod, ident, b_ident)
        emit_aT(fw, h_all, NT, 2, norm_w1, modrows, d_mod, aT, b_aT, ident, b_ident)

        def tiles_of(s, n):
            return list(range(s // 128, (s + n) // 128))

        with ExitStack() as st:
            fw.stack = st
            wdt = fw.sb([128, 16, 32], BF16, "wdt"); b_wdt = Buf()
            fw.dma(pool, wdt[:], w_in[:, DT0:DT0 + 32].rearrange("(kc p) c -> p kc c", p=128), writes=[b_wdt])
            dtb = fw.sb([128, 32], F32, "dtb"); b_dtb = Buf(); fw.dma(sp, dtb[:], bview(dt_bias), writes=[b_dtb])
            Aneg = fw.sb([128, 32], F32, "Aneg"); b_A = Buf(); fw.dma(sp, Aneg[:], bview(a_log), writes=[b_A])
            fw.op(act, lambda e: e.activation(Aneg[:, :], Aneg[:, :], AF.Exp), writes=[b_A])
            fw.op(dve, lambda e: e.tensor_scalar(Aneg[:, :], Aneg[:, :], -1.0, None, ALU.mult), writes=[b_A])
            dsk = fw.sb([128, 16], F32, "dsk"); b_dsk = Buf(); fw.dma(sp, dsk[:], bview(d_skip), writes=[b_dsk])
            snw = fw.sb([128, 1024], F32, "snw"); b_snw = Buf(); fw.dma(sp, snw[:], bview(ssd_nw), writes=[b_snw])
            cw = fw.sb([128, 12, 5], F32, "cw"); b_cw = Buf(); fw.dma(sp, cw[:], convw.rearrange("(ct p) k -> p ct k", p=128), writes=[b_cw])
            cb = fw.sb([128, 12], F32, "cb"); b_cb = Buf(); fw.dma(sp, cb[:], convb, writes=[b_cb])
            def T3(name):
                return fw.sb([128, NT, 32], F32, name), [Buf() for _ in range(NT)]
            dt_all, b_dt = T3("dt_all"); a_all, b_a = T3("a_all"); negcs, b_ncs = T3("negcs")
            dfs, b_dfs = T3("dfs"); dte, b_dte = T3("dte"); etot, b_etot = T3("etot")
            tmpA = fw.sb([128, 32], F32, "tmpA"); b_tA = Buf(); tmpB = fw.sb([128, 32], F32, "tmpB"); b_tB = Buf()
            tmpC = fw.sb([128, 32], F32, "tmpC"); b_tC = Buf()
            for t in range(NT):
                pt, pb = fw.next_psum()
                for kc in range(16):
                    fw.op(pe, lambda e, kc=kc, pt=pt, t=t: e.matmul(pt[:, 0:32], aT[:, kc, t * 128:(t + 1) * 128], wdt[:, kc, :], start=(kc == 0), stop=(kc == 15)),
                          reads=[b_aT[t], b_wdt], writes=[pb])
                fw.op(dve, lambda e, pt=pt: e.tensor_tensor(tmpA[:, :], pt[:, 0:32], dtb[:, :], ALU.add), reads=[pb, b_dtb], writes=[b_tA])
                fw.op(act, lambda e: e.activation(tmpB[:, :], tmpA[:, :], AF.Abs), reads=[b_tA], writes=[b_tB])
                fw.op(act, lambda e: e.activation(tmpB[:, :], tmpB[:, :], AF.Exp, scale=-1.0), writes=[b_tB])
                fw.op(act, lambda e: e.activation(tmpB[:, :], tmpB[:, :], AF.Ln, bias=1.0, scale=1.0), writes=[b_tB])
                fw.op(dve, lambda e: e.tensor_scalar(tmpA[:, :], tmpA[:, :], 0.0, None, ALU.max), writes=[b_tA])
                fw.op(dve, lambda e, t=t: e.tensor_tensor(dt_all[:, t, :], tmpA[:, :], tmpB[:, :], ALU.add), reads=[b_tA, b_tB], writes=[b_dt[t]])
                fw.op(dve, lambda e, t=t: e.tensor_tensor(a_all[:, t, :], dt_all[:, t, :], Aneg[:, :], ALU.mult), reads=[b_dt[t], b_A], writes=[b_a[t]])
                pc_, pbc = fw.next_psum()
                fw.op(pe, lambda e, pc_=pc_, t=t: e.matmul(pc_[:, 0:16], ule[:, :], a_all[:, t, 0:16], start=True, stop=True), reads=[b_ule, b_a[t]], writes=[pbc])
                fw.op(pe, lambda e, pc_=pc_, t=t: e.matmul(pc_[:, 16:32], uge[:, :], a_all[:, t, 16:32], start=True, stop=True), reads=[b_uge, b_a[t]], writes=[pbc])
                fw.op(pe, lambda e, pc_=pc_, t=t: e.matmul(pc_[:, 32:64], ones[:, :], a_all[:, t, :], start=True, stop=True), reads=[b_ones, b_a[t]], writes=[pbc])
                fw.op(dve, lambda e, pc_=pc_, t=t: e.tensor_scalar(negcs[:, t, :], pc_[:, 0:32], -1.0, None, ALU.mult), reads=[pbc], writes=[b_ncs[t]])
                fw.op(act, lambda e, pc_=pc_, t=t: e.activation(dfs[:, t, :], pc_[:, 0:32], AF.Exp), reads=[pbc], writes=[b_dfs[t]])
                fw.op(act, lambda e, pc_=pc_, t=t: e.activation(etot[:, t, :], pc_[:, 32:64], AF.Exp), reads=[pbc], writes=[b_etot[t]])
                fw.op(dve, lambda e, pc_=pc_, t=t: e.tensor_tensor(tmpC[:, :], pc_[:, 32:64], negcs[:, t, :], ALU.add), reads=[pbc, b_ncs[t]], writes=[b_tC])
                fw.op(act, lambda e, t=t: e.activation(dte[:, t, :], tmpC[:, :], AF.Exp), reads=[b_tC], writes=[b_dte[t]])

            xs = fw.sb([128, NT, 512], BF16, "xs"); b_xs = [Buf() for _ in range(NT)]
            Btok = fw.sb([128, NT, 128], BF16, "Btok"); b_Btok = [Buf() for _ in range(NT)]
            BT = fw.sb([128, NTOK], BF16, "BT"); b_BT = Buf()
            CT = fw.sb([128, NTOK], BF16, "CT"); b_CT = Buf()
            d_Hb = [Buf() for _ in range(NT)]
            Hbs = [fw.sb([128, 512], BF16, "Hbs%d" % i) for i in range(2)]; b_Hbs = [Buf() for _ in range(2)]
            pcs = [fw.sb([128, 2312], F32, "pc%d" % i) for i in range(1)]; b_pcs = [Buf() for _ in range(1)]
            for i in range(1):
                fw.op(pool, lambda e, i=i: e.memset(pcs[i][:, :], 0.0), writes=[b_pcs[i]])
            acc = fw.sb([128, 2308], F32, "acc"); b_acc = Buf()
            wcs = [fw.sb([128, 16, 128], BF16, "wc%d" % i) for i in range(2)]; b_wcs = [Buf() for _ in range(2)]
            wz = fw.sb([128, 16, 512], BF16, "wz"); b_wz = Buf()
            H = fw.sb([128, 512], F32, "H"); b_H = Buf()
            Hbf = fw.sb([128, 512], BF16, "Hbf"); b_Hbf = Buf()
            coef = fw.sb([128, 8], F32, "coef"); b_coef = Buf()
            Xe = fw.sb([128, 512], BF16, "Xe"); b_Xe = Buf()
            Xtf = fw.sb([128, 512], BF16, "Xtf"); b_Xtf = Buf()
            Xtb = fw.sb([128, 512], BF16, "Xtb"); b_Xtb = Buf()
            CBf = fw.sb([128, 128], F32, "CBf"); b_CBf = Buf()
            CBb = fw.sb([128, 128], F32, "CBb"); b_CBb = Buf()
            Es = [fw.sb([128, 128], F32, "E%d" % i) for i in range(3)]; b_Es = [Buf() for _ in range(3)]
            Ls = [fw.sb([128, 128], F32, "L%d" % i) for i in range(3)]; b_Ls = [Buf() for _ in range(3)]
            Ms = [fw.sb([128, 128], BF16, "M%d" % i) for i in range(3)]; b_Ms = [Buf() for _ in range(3)]
            t1 = fw.sb([128, 512], F32, "t1"); b_t1 = Buf()
            t2 = fw.sb([128, 512], F32, "t2"); b_t2 = Buf()
            sz = fw.sb([128, 512], F32, "sz"); b_sz = Buf()
            gy = fw.sb([128, 512], F32, "gy"); b_gy = Buf()
            ssq = fw.sb([128, 1], F32, "ssq_s"); b_ssq = Buf()
            outs = [fw.sb([128, 512], F32, "o%d" % i) for i in range(2)]; b_outs = [Buf() for _ in range(2)]
            wcnt = 0
            lm = 0

            def bc8(ap):
                return ap.unsqueeze(2).to_broadcast([128, 8, 64])

            def v3(ap):
                return ap.rearrange("p (h d) -> p h d", h=8)

            for g in range(1 if dbg else 2):
                hf = g * 8
                hb = 16 + g * 8
                fw.dma(pool, wz[:], w_in[:, Z0 + g * 512:Z0 + (g + 1) * 512].rearrange("(kc p) c -> p kc c", p=128), writes=[b_wz])
                for ci in range(6):
                    if ci < 4:
                        col0 = X0 + g * 512 + ci * 128; ct = g * 4 + ci
                    elif ci == 4:
                        col0 = B0 + g * 128; ct = 8 + g
                    else:
                        col0 = C0 + g * 128; ct = 10 + g
                    wc, bwc = wcs[wcnt % 2], b_wcs[wcnt % 2]
                    pc, bpc = pcs[0], b_pcs[0]
                    wcnt += 1
                    fw.dma(pool, wc[:], w_in[:, col0:col0 + 128].rearrange("(kc p) c -> p kc c", p=128), writes=[bwc])
                    for (s0, n) in TB:
                        pt, pb = fw.next_psum()
                        for kc in range(16):
                            fw.op(pe, lambda e, kc=kc, pt=pt, wc=wc, s0=s0, n=n: e.matmul(pt[:, 0:n], wc[:, kc, :], aT[:, kc, s0:s0 + n], start=(kc == 0), stop=(kc == 15)),
                                  reads=[bwc] + [b_aT[t] for t in tiles_of(s0, n)], writes=[pb])
                        off = 2 if s0 == 0 else 6
                        fw.op(act, lambda e, pt=pt, pc=pc, s0=s0, n=n, off=off: e.copy(pc[:, s0 + off:s0 + off + n], pt[:, 0:n]), reads=[pb], writes=[bpc])
                    fw.op(dve, lambda e, pc=pc, ct=ct: e.tensor_scalar(acc[:, :], pc[:, 0:2308], cw[:, ct, 0:1], cb[:, ct:ct + 1], ALU.mult, ALU.add),
                          reads=[bpc, b_cw, b_cb], writes=[b_acc])
                    for k in range(1, 5):
                        eng = dve
                        fw.op(eng, lambda e, pc=pc, ct=ct, k=k: e.scalar_tensor_tensor(acc[:, :], pc[:, k:k + 2308], cw[:, ct, k:k + 1], acc[:, :], ALU.mult, ALU.add),
                              reads=[bpc, b_cw], writes=[b_acc])
                    if ci == 5:
                        fw.op(act, lambda e: e.activation(CT[:, 0:256], acc[:, 0:256], AF.Silu), reads=[b_acc], writes=[b_CT])
                        fw.op(act, lambda e: e.activation(CT[:, 256:NTOK], acc[:, 260:2308], AF.Silu), reads=[b_acc], writes=[b_CT])
                        continue
                    fw.op(act, lambda e: e.activation(acc[:, 0:256], acc[:, 0:256], AF.Silu), writes=[b_acc])
                    fw.op(act, lambda e: e.activation(acc[:, 260:2308], acc[:, 260:2308], AF.Silu), writes=[b_acc])
                    if ci == 4:
                        fw.op(pool, lambda e: e.tensor_copy(BT[:, 0:256], acc[:, 0:256]), reads=[b_acc], writes=[b_BT])
                        fw.op(pool, lambda e: e.tensor_copy(BT[:, 256:NTOK], acc[:, 260:2308]), reads=[b_acc], writes=[b_BT])
                    for q in range(5):
                        ts = list(range(q * 4, min(NT, q * 4 + 4)))
                        pt, pb = fw.next_psum()
                        for i, t in enumerate(ts):
                            a0 = t * 128 if t < 2 else 260 + (t - 2) * 128
                            fw.op(pe, lambda e, pt=pt, i=i, a0=a0: e.transpose(pt[:, i * 128:(i + 1) * 128], acc[:, a0:a0 + 128], ident[:, :]),
                                  reads=[b_acc, b_ident], writes=[pb])
                        n = len(ts)
                        src = pt[:, 0:n * 128].rearrange("p (a b) -> p a b", a=n)
                        if ci < 4:
                            dstv = xs[:, ts[0]:ts[0] + n, ci * 128:(ci + 1) * 128]; bl = [b_xs[t] for t in ts]
                        else:
                            dstv = Btok[:, ts[0]:ts[0] + n, :]; bl = [b_Btok[t] for t in ts]
                        if q % 2 == 0:
                            fw.op(dve, lambda e, dstv=dstv, src=src: e.tensor_copy(dstv, src), reads=[pb], writes=bl)
                        else:
                            fw.op(act, lambda e, dstv=dstv, src=src: e.copy(dstv, src), reads=[pb], writes=bl)

                fw.op(pool, lambda e: e.memset(H[:, :], 0.0), writes=[b_H])
                border = [1, 0] + list(range(17, 1, -1))
                for bi_, c in enumerate(border):
                    hs_, bhs_ = Hbs[bi_ % 2], b_Hbs[bi_ % 2]
                    fw.op(act, lambda e, hs_=hs_: e.copy(hs_[:, :], H[:, :]), reads=[b_H], writes=[bhs_])
                    fw.dma(sp, Hb_d[c], hs_[:, :], reads=[bhs_], writes=[d_Hb[c]])
                    fw.op(dve, lambda e, c=c: e.tensor_tensor(coef[:, :], dt_all[:, c, hb:hb + 8], dte[:, c, hb:hb + 8], ALU.mult), reads=[b_dt[c], b_dte[c]], writes=[b_coef])
                    fw.op(dve, lambda e, c=c: e.tensor_tensor(v3(Xe[:, :]), v3(xs[:, c, :]), bc8(coef[:, :]), ALU.mult), reads=[b_xs[c], b_coef], writes=[b_Xe])
                    ps, pbs = fw.next_psum()
                    fw.op(pe, lambda e, ps=ps, c=c: e.matmul(ps[:, :], Btok[:, c, :], Xe[:, :], start=True, stop=True), reads=[b_Btok[c], b_Xe], writes=[pbs])
                    fw.op(pool, lambda e, c=c: e.tensor_tensor(v3(H[:, :]), v3(H[:, :]), bc8(etot[:, c, hb:hb + 8]), ALU.mult), reads=[b_etot[c]], writes=[b_H])
                    fw.op(dve, lambda e, ps=ps: e.tensor_tensor(H[:, :], H[:, :], ps[:, :], ALU.add), reads=[pbs], writes=[b_H])

                fw.op(pool, lambda e: e.memset(H[:, :], 0.0), writes=[b_H])
                for c in range(NT):
                    cs_ = slice(c * 128, (c + 1) * 128)
                    hs_, bhs_ = Hbs[c % 2], b_Hbs[c % 2]
                    fw.dma(sp, hs_[:, :], Hb_d[c], reads=[d_Hb[c]], writes=[bhs_])
                    fw.op(act, lambda e: e.copy(Hbf[:, :], H[:, :]), reads=[b_H], writes=[b_Hbf])
                    fw.op(dve, lambda e, c=c: e.tensor_tensor(coef[:, :], dt_all[:, c, hf:hf + 8], dte[:, c, hf:hf + 8], ALU.mult), reads=[b_dt[c], b_dte[c]], writes=[b_coef])
                    fw.op(dve, lambda e, c=c: e.tensor_tensor(v3(Xe[:, :]), v3(xs[:, c, :]), bc8(coef[:, :]), ALU.mult), reads=[b_xs[c], b_coef], writes=[b_Xe])
                    fw.op(pool, lambda e, c=c: e.tensor_tensor(v3(Xtf[:, :]), v3(xs[:, c, :]), bc8(dt_all[:, c, hf:hf + 8]), ALU.mult), reads=[b_xs[c], b_dt[c]], writes=[b_Xtf])
                    fw.op(pool, lambda e, c=c: e.tensor_tensor(v3(Xtb[:, :]), v3(xs[:, c, :]), bc8(dt_all[:, c, hb:hb + 8]), ALU.mult), reads=[b_xs[c], b_dt[c]], writes=[b_Xtb])
                    pyf, pbyf = fw.next_psum()
                    fw.op(pe, lambda e, pyf=pyf, cs_=cs_: e.matmul(pyf[:, :], CT[:, cs_], Hbf[:, :], start=True, stop=True), reads=[b_CT, b_Hbf], writes=[pbyf])
                    pyb, pbyb = fw.next_psum()
                    fw.op(pe, lambda e, pyb=pyb, cs_=cs_, hs_=hs_: e.matmul(pyb[:, :], CT[:, cs_], hs_[:, :], start=True, stop=True), reads=[b_CT, bhs_], writes=[pbyb])
                    fw.op(dve, lambda e, pyf=pyf, c=c: e.tensor_tensor(v3(t1[:, :]), v3(pyf[:, :]), bc8(dfs[:, c, hf:hf + 8]), ALU.mult), reads=[pbyf, b_dfs[c]], writes=[b_t1])
                    fw.op(dve, lambda e, pyb=pyb, c=c: e.tensor_tensor(v3(t2[:, :]), v3(pyb[:, :]), bc8(dfs[:, c, hb:hb + 8]), ALU.mult), reads=[pbyb, b_dfs[c]], writes=[b_t2])
                    fw.op(pool, lambda e: e.tensor_tensor(t1[:, :], t1[:, :], t2[:, :], ALU.add), reads=[b_t2], writes=[b_t1])
                    fw.op(pool, lambda e, c=c: e.tensor_tensor(v3(t2[:, :]), v3(xs[:, c, :]), bc8(dsk[:, g * 8:(g + 1) * 8]), ALU.mult), reads=[b_xs[c], b_dsk], writes=[b_t2])
                    fw.op(pool, lambda e: e.tensor_tensor(t1[:, :], t1[:, :], t2[:, :], ALU.add), reads=[b_t2], writes=[b_t1])
                    ps, pbs = fw.next_psum()
                    fw.op(pe, lambda e, ps=ps, c=c: e.matmul(ps[:, :], Btok[:, c, :], Xe[:, :], start=True, stop=True), reads=[b_Btok[c], b_Xe], writes=[pbs])
                    fw.op(pool, lambda e, c=c: e.tensor_tensor(v3(H[:, :]), v3(H[:, :]), bc8(etot[:, c, hf:hf + 8]), ALU.mult), reads=[b_etot[c], b_Hbf], writes=[b_H])
                    fw.op(dve, lambda e, ps=ps: e.tensor_tensor(H[:, :], H[:, :], ps[:, :], ALU.add), reads=[pbs], writes=[b_H])
                    pcb, pbcb = fw.next_psum()
                    fw.op(pe, lambda e, pcb=pcb, cs_=cs_: e.matmul(pcb[:, 0:128], BT[:, cs_], CT[:, cs_], start=True, stop=True), reads=[b_BT, b_CT], writes=[pbcb])
                    fw.op(dve, lambda e, pcb=pcb: e.tensor_tensor(CBf[:, :], pcb[:, 0:128], ule[:, :], ALU.mult), reads=[pbcb, b_ule], writes=[b_CBf])
                    fw.op(dve, lambda e, pcb=pcb: e.tensor_tensor(CBb[:, :], pcb[:, 0:128], uge[:, :], ALU.mult), reads=[pbcb, b_uge], writes=[b_CBb])
                    pyd, pbyd = fw.next_psum()
                    prs = {}

                    def emitR(hq):
                        pr, pbr = fw.next_psum()
                        combos = [(h, d_) for h in (2 * hq, 2 * hq + 1) for d_ in (0, 1)]
                        for i, (h, d_) in enumerate(combos):
                            col = (hf if d_ == 0 else hb) + h
                            U = ule if d_ == 0 else uge
                            bU = b_ule if d_ == 0 else b_uge
                            fw.op(pe, lambda e: e.matmul(pr[:, i * 128:(i + 1) * 128], a_all[:, c, col:col + 1].to_broadcast([128, 128]), U[:, :], start=True, stop=True),
                                  reads=[b_a[c], bU], writes=[pbr])
                        prs[hq] = (pr, pbr, combos)
                    emitR(0)
                    pz, pbz = fw.next_psum()
                    for kc in range(16):
                        fw.op(pe, lambda e: e.matmul(pz[:, :], aT[:, kc, cs_], wz[:, kc, :], start=(kc == 0), stop=(kc == 15)),
                              reads=[b_aT[c], b_wz], writes=[pbz])
                    fw.op(act, lambda e: e.activation(sz[:, :], pz[:, :], AF.Silu), reads=[pbz], writes=[b_sz])
                    for hq in range(4):
                        if hq + 1 < 4:
                            emitR(hq + 1)
                        pr, pbr, combos = prs[hq]
                        for i, (h, d_) in enumerate(combos):
                            col = (hf if d_ == 0 else hb) + h
                            E_, bE = Es[lm % 3], b_Es[lm % 3]; L_, bL = Ls[lm % 3], b_Ls[lm % 3]; M_, bM = Ms[lm % 3], b_Ms[lm % 3]; lm += 1
                            CB_, bCB = (CBf, b_CBf) if d_ == 0 else (CBb, b_CBb)
                            Xt_, bXt = (Xtf, b_Xtf) if d_ == 0 else (Xtb, b_Xtb)
                            fw.op(dve, lambda e: e.tensor_scalar(E_[:, :], pr[:, i * 128:(i + 1) * 128], negcs[:, c, col:col + 1], zcol[:, 0:1], ALU.add, ALU.min),
                                  reads=[pbr, b_ncs[c], b_z], writes=[bE])
                            fw.op(act, lambda e: e.activation(L_[:, :], E_[:, :], AF.Exp), reads=[bE], writes=[bL])
                            fw.op(pool, lambda e: e.tensor_tensor(M_[:, :], L_[:, :], CB_[:, :], ALU.mult), reads=[bL, bCB], writes=[bM])
                            fw.op(pe, lambda e: e.matmul(pyd[:, h * 64:(h + 1) * 64], M_[:, :], Xt_[:, h * 64:(h + 1) * 64], start=(d_ == 0), stop=(d_ == 1)),
                                  reads=[bM, bXt], writes=[pbyd])
                    if dbg and c == NT - 1:
                        dd = {}
                        for nm, pp, pbb in (("pyd", pyd, pbyd),):
                            tt = fw.sb([128, 512], F32, "dbg_" + nm); bb = Buf()
                            fw.op(dve, lambda e, tt=tt, pp=pp: e.tensor_copy(tt[:, :], pp[:, :]), reads=[pbb], writes=[bb])
                            dd[nm] = (tt, bb)
                        fw.dbgd = dd
                    fw.op(dve, lambda e, pyd=pyd: e.tensor_tensor(t1[:, :], t1[:, :], pyd[:, :], ALU.add), reads=[pbyd], writes=[b_t1])
                    fw.op(dve, lambda e: e.tensor_tensor(gy[:, :], t1[:, :], sz[:, :], ALU.mult), reads=[b_t1, b_sz], writes=[b_gy])
                    fw.op(act, lambda e: e.activation(t2[:, :], gy[:, :], AF.Square, accum_out=ssq[:, :]), reads=[b_gy], writes=[b_t2, b_ssq])
                    fw.op(act, lambda e: e.activation(ssq[:, :], ssq[:, :], AF.Sqrt, bias=EPS, scale=1.0 / 512), writes=[b_ssq])
                    fw.op(dve, lambda e: e.reciprocal(ssq[:, :], ssq[:, :]), writes=[b_ssq])
                    o_, bo = outs[c % 2], b_outs[c % 2]
                    fw.op(dve, lambda e, o_=o_: e.scalar_tensor_tensor(o_[:, :], gy[:, :], ssq[:, 0:1], snw[:, g * 512:(g + 1) * 512], ALU.mult, ALU.mult),
                          reads=[b_gy, b_ssq, b_snw], writes=[bo])
                    if fused:
                        d_mix.extend(out_fn(fw, c * 128, g * 512, 512, o_, bo))
                    else:
                        db = Buf(); d_mix.append(db)
                        fw.dma(sp, mix[c * 128:(c + 1) * 128, g * 512:(g + 1) * 512], o_[:, :], reads=[bo], writes=[db])
            if dbg:
                def dump(name, ap, shape, dt, bufs):
                    o = nc.dram_tensor("dbg_" + name, shape, dt, kind="ExternalOutput").ap()
                    db = Buf(); d_mix.append(db)
                    fw.dma(sp, o, ap, reads=bufs, writes=[db])
                dump("aT0", aT[:, 0, :], [128, NTOK], BF16, b_aT)
                dump("dt", dt_all[:, :, :], [128, NT, 32], F32, b_dt)
                dump("negcs", negcs[:, :, :], [128, NT, 32], F32, b_ncs)
                dump("dfs", dfs[:, :, :], [128, NT, 32], F32, b_dfs)
                dump("dte", dte[:, :, :], [128, NT, 32], F32, b_dte)
                dump("etot", etot[:, :, :], [128, NT, 32], F32, b_etot)
                dump("xs", xs[:, :, :], [128, NT, 512], BF16, b_xs)
                dump("Btok", Btok[:, :, :], [128, NT, 128], BF16, b_Btok)
                dump("BT", BT[:, :], [128, NTOK], BF16, [b_BT])
                dump("CT", CT[:, :], [128, NTOK], BF16, [b_CT])
                dump("H", H[:, :], [128, 512], F32, [b_H])
                dump("t1", t1[:, :], [128, 512], F32, [b_t1])
                dump("gy", gy[:, :], [128, 512], F32, [b_gy])
                for nm, (tt, bb) in fw.dbgd.items():
                    dump(nm, tt[:, :], [128, 512], F32, [bb])
                dump("CBf", CBf[:, :], [128, 128], F32, [b_CBf])
                dump("CBb", CBb[:, :], [128, 128], F32, [b_CBb])
                dump("Elast", Es[(lm - 1) % 3][:, :], [128, 128], F32, [b_Es[(lm - 1) % 3]])
                dump("Llast", Ls[(lm - 1) % 3][:, :], [128, 128], F32, [b_Ls[(lm - 1) % 3]])
                dump("Mlast", Ms[(lm - 1) % 3][:, :], [128, 128], BF16, [b_Ms[(lm - 1) % 3]])
                dump("Xtf", Xtf[:, :], [128, 512], BF16, [b_Xtf])
                dump("Xtb", Xtb[:, :], [128, 512], BF16, [b_Xtb])
            fw.barrier()
        if fused and ctx.get("mid_hook"):
            ctx["mid_hook"](0)

        if stop_after != "ssd":
          with ExitStack() as st:
            fw.stack = st
            V1 = fw.sb([128, NT, 8, 129], BF16, "V1"); b_V1 = [Buf() for _ in range(NT)]
            fw.op(pool, lambda e: e.memset(V1[:, :, :, :].rearrange("p a b c -> p (a b c)"), 1.0), writes=b_V1)
            with ExitStack() as st2:
                fw.stack = st2
                wv = fw.sb([128, 16, 1024], BF16, "wv"); b_wv = Buf()
                fw.dma(pool, wv[:], w_in[:, V0:V0 + 1024].rearrange("(kc p) c -> p kc c", p=128), writes=[b_wv])
                for t in range(NT):
                    for hh_ in range(2):
                        pt, pb = fw.next_psum()
                        for kc in range(16):
                            fw.op(pe, lambda e, kc=kc, pt=pt, t=t, hh_=hh_: e.matmul(pt[:, :], aT[:, kc, t * 128:(t + 1) * 128], wv[:, kc, hh_ * 512:(hh_ + 1) * 512], start=(kc == 0), stop=(kc == 15)),
                                  reads=[b_aT[t], b_wv], writes=[pb])
                        dstv = V1[:, t, hh_ * 4:(hh_ + 1) * 4, 0:128]
                        src = pt[:, :].rearrange("p (a b) -> p a b", a=4)
                        if hh_ == 0:
                            fw.op(act, lambda e, dstv=dstv, src=src: e.copy(dstv, src), reads=[pb], writes=[b_V1[t]])
                        else:
                            fw.op(dve, lambda e, dstv=dstv, src=src: e.tensor_copy(dstv, src), reads=[pb], writes=[b_V1[t]])
                fw.barrier()
            fw.stack = st
            qw = fw.sb([128, 1], F32, "qw"); b_qw = Buf(); fw.dma(sp, qw[:], qnw, writes=[b_qw])
            fw.op(dve, lambda e: e.tensor_scalar(qw[:, :], qw[:, :], float(128 ** -0.5), None, ALU.mult), writes=[b_qw])
            kw = fw.sb([128, 1], F32, "kw"); b_kw = Buf(); fw.dma(sp, kw[:], knw, writes=[b_kw])
            qTs = [fw.sb([128, NTOK], BF16, "qT%d" % i) for i in range(2)]; b_qTs = [Buf() for _ in range(2)]
            kTs = [fw.sb([128, NTOK], BF16, "kT%d" % i) for i in range(2)]; b_kTs = [Buf() for _ in range(2)]
            wqs = [fw.sb([128, 16, 128], BF16, "wq%d" % i) for i in range(2)]; b_wqs = [Buf() for _ in range(2)]
            wks = [fw.sb([128, 16, 128], BF16, "wk%d" % i) for i in range(2)]; b_wks = [Buf() for _ in range(2)]
            bts = [fw.sb([128, 25, 128], BF16, "bt%d" % i) for i in range(2)]; b_bts = [Buf() for _ in range(2)]
            sqs = [fw.sb([128, 512], F32, "sq%d" % i) for i in range(2)]; b_sqs = [Buf() for _ in range(2)]
            rs = [fw.sb([128, 512], F32, "rs%d" % i) for i in range(2)]; b_rs = [Buf() for _ in range(2)]
            PTs = [fw.sb([128, 7, 128], BF16, "PT%d" % i) for i in range(3)]; b_PTs = [Buf() for _ in range(3)]
            rec = [fw.sb([128, 1], F32, "rec%d" % i) for i in range(2)]; b_rec = [Buf() for _ in range(2)]
            ons = [fw.sb([128, 128], F32, "on%d" % i) for i in range(3)]; b_ons = [Buf() for _ in range(3)]
            cnt = 0; pcnt = 0
            for hd in range(8):
                i2 = hd % 2
                qT, bqT = qTs[i2], b_qTs[i2]; kT, bkT = kTs[i2], b_kTs[i2]
                wq, bwq = wqs[i2], b_wqs[i2]; wk, bwk = wks[i2], b_wks[i2]
                bt, bbt = bts[i2], b_bts[i2]
                fw.dma(pool, wq[:], w_in[:, Q0 + hd * 128:Q0 + (hd + 1) * 128].rearrange("(kc p) c -> p kc c", p=128), writes=[bwq])
                fw.dma(pool, wk[:], w_in[:, K0 + hd * 128:K0 + (hd + 1) * 128].rearrange("(kc p) c -> p kc c", p=128), writes=[bwk])
                fw.dma(pool, bt[:], biasT[hd].rearrange("c k q -> k c q"), writes=[bbt])
                blks = []
                for (wt_, bwt_, dstT, bdst, nwc, bnw) in ((wq, bwq, qT, bqT, qw, b_qw), (wk, bwk, kT, bkT, kw, b_kw)):
                    for (s0, n) in TB:
                        blk = {}

                        def s1(blk=blk, wt_=wt_, bwt_=bwt_, s0=s0, n=n):
                            nonlocal cnt
                            pt, pb = fw.next_psum(); blk["pt"] = (pt, pb); blk["i"] = cnt % 2; cnt += 1
                            for kc in range(16):
                                fw.op(pe, lambda e: e.matmul(pt[:, 0:n], wt_[:, kc, :], aT[:, kc, s0:s0 + n], start=(kc == 0), stop=(kc == 15)),
                                      reads=[bwt_] + [b_aT[t] for t in tiles_of(s0, n)], writes=[pb])
                            sq_, bsq = sqs[blk["i"]], b_sqs[blk["i"]]
                            fw.op(act, lambda e: e.activation(sq_[:, 0:n], pt[:, 0:n], AF.Square), reads=[pb], writes=[bsq])

                        def s2(blk=blk, dstT=dstT, bdst=bdst, nwc=nwc, bnw=bnw, s0=s0, n=n):
                            pt, pb = blk["pt"]; i_ = blk["i"]
                            sq_, bsq = sqs[i_], b_sqs[i_]; r_, br = rs[i_], b_rs[i_]
                            p2, pb2 = fw.next_psum()
                            fw.op(pe, lambda e: e.matmul(p2[:, 0:n], ones[:, :], sq_[:, 0:n], start=True, stop=True), reads=[b_ones, bsq], writes=[pb2])
                            fw.op(act, lambda e: e.activation(r_[:, 0:n], p2[:, 0:n], AF.Sqrt, bias=EPS, scale=1.0 / 128), reads=[pb2], writes=[br])
                            fw.op(dve, lambda e: e.reciprocal(r_[:, 0:n], r_[:, 0:n]), writes=[br])
                            fw.op(dve, lambda e: e.tensor_tensor(r_[:, 0:n], pt[:, 0:n], r_[:, 0:n], ALU.mult), reads=[pb], writes=[br])
                            fw.op(pool, lambda e: e.tensor_scalar(dstT[:, s0:s0 + n], r_[:, 0:n], nwc[:, 0:1], None, ALU.mult), reads=[br, bnw], writes=[bdst])
                        blks.append((s1, s2))
                nb = len(blks)
                blks[0][0](); blks[1][0]()
                for i in range(nb):
                    blks[i][1]()
                    if i + 2 < nb:
                        blks[i + 2][0]()

                def keys_of(tq):
                    if tq < 2:
                        return [(0, None), (1, None)]
                    j = tq - 2
                    a = min(max(2 * j - 4, 0), 22)
                    cls = 0 if j == 0 else 1 if j == 1 else 3 if j == 14 else 4 if j == 15 else 2
                    return [(2 + a // 2 + i, cls * 5 + i) for i in range(5)] + [(0, None), (1, None)]

                def att1(tq):
                    nonlocal pcnt
                    keys = keys_of(tq)
                    PT, bPT = PTs[pcnt % 3], b_PTs[pcnt % 3]
                    on, bon = ons[pcnt % 3], b_ons[pcnt % 3]
                    rc, brc = rec[pcnt % 2], b_rec[pcnt % 2]; pcnt += 1
                    nk = len(keys)
                    for b0 in range(0, nk, 4):
                        pS, pbS = fw.next_psum()
                        grp = keys[b0:b0 + 4]
                        for i, (kt, bi) in enumerate(grp):
                            fw.op(pe, lambda e: e.matmul(pS[:, i * 128:(i + 1) * 128], kT[:, kt * 128:(kt + 1) * 128], qT[:, tq * 128:(tq + 1) * 128], start=True, stop=(bi is None)),
                                  reads=[bkT, bqT], writes=[pbS])
                            if bi is not None:
                                fw.op(pe, lambda e: e.matmul(pS[:, i * 128:(i + 1) * 128], identb[:, :], bt[:, bi, :], start=False, stop=True),
                                      reads=[b_identb, bbt], writes=[pbS])
                        ng = len(grp)
                        fw.op(act, lambda e: e.activation(PT[:, b0:b0 + ng, :].rearrange("p a b -> p (a b)"), pS[:, 0:ng * 128], AF.Exp),
                              reads=[pbS], writes=[bPT])
                    return (tq, keys, PT, bPT, on, bon, rc, brc)

                def att2(st_):
                    tq, keys, PT, bPT, on, bon, rc, brc = st_
                    nk = len(keys)
                    pO, pbO = fw.next_psum()
                    for i, (kt, bi) in enumerate(keys):
                        fw.op(pe, lambda e: e.matmul(pO[:, 0:129], PT[:, i, :], V1[:, kt, hd, :], start=(i == 0), stop=(i == nk - 1)),
                              reads=[bPT, b_V1[kt]], writes=[pbO])
                    fw.op(dve, lambda e: e.reciprocal(rc[:, :], pO[:, 128:129]), reads=[pbO], writes=[brc])
                    fw.op(dve, lambda e: e.tensor_scalar(on[:, :], pO[:, 0:128], rc[:, 0:1], None, ALU.mult), reads=[pbO, brc], writes=[bon])
                    if fused:
                        d_mix.extend(out_fn(fw, tq * 128, 1024 + hd * 128, 128, on, bon))
                    else:
                        db = Buf(); d_mix.append(db)
                        fw.dma(sp, mix[tq * 128:(tq + 1) * 128, 1024 + hd * 128:1024 + (hd + 1) * 128], on[:, :], reads=[bon], writes=[db])

                cur = att1(0)
                for tq in range(NT):
                    nxt = att1(tq + 1) if tq + 1 < NT else None
                    att2(cur)
                    cur = nxt
                if fused and ctx.get("mid_hook") and hd == 3:
                    ctx["mid_hook"](1)
            fw.barrier()
        if not fused:
            fw.finish(d_mix)
        print("phaseA0 ops", fw.n_ops, "waits", fw.n_waits)
    return d_mix if fused else nc

import math
from contextlib import ExitStack

NT = 18
NTOK = NT * 128
NLAT = 2048
GQ0, DQ0, GK0, DK0, GV0, DV0, NCOL1 = 0, 1024, 2048, 2304, 3328, 3584, 4608
TB = [(0, 256)] + [(256 + i * 512, 512) for i in range(4)]
LAMBDA_INIT = 0.8 - 0.6 * math.exp(-0.3 * 1)


def build_phaseA1(ctx=None):
    fused = ctx is not None
    nc = ctx["nc"] if fused else bass.Bass("TRN2", target_bir_lowering=False)
    pre = ctx["pre"] if fused else ""
    out_fn = ctx["out_fn"] if fused else None
    I = lambda name, shape, dt=F32: nc.dram_tensor(pre + name, shape, dt, kind="ExternalInput").ap()
    h_src = ctx.get("h_src") if fused else None
    h_all = I("h_all", [NTOK, D]) if h_src is None else h_src[0]
    h_bufs = () if h_src is None else h_src[1]
    row_of = None if h_src is None else h_src[2]
    cvec = I("cvec", [2, D])
    ada_w = I("ada_w", [D, 4096]); ada_b = I("ada_b", [1, 4096])
    norm_w1 = I("norm_w1", [1, D])
    w_in = I("w_in", [D, NCOL1])
    nws = I("nws", [128, 4])
    lamv = I("lamv", [1, 512])
    subln = I("subln", [1, 256])
    cosT_d = I("cosT", [128, NLAT]); sinT_d = I("sinT", [128, NLAT]); rmT_d = I("rmT", [128, 128])
    ident_d = I("ident", [128, 128])
    mix = None if fused else nc.dram_tensor("mix_part", [NLAT, 2048], F32, kind="ExternalOutput").ap()
    modrows = nc.dram_tensor(pre + "modrows", [2, 4096], F32).ap()
    d_mod = Buf("modrows"); d_mix = []

    with ExitStack() as st0:
        if fused:
            fw = ctx["fw"]; fw.stack = st0; fw.ps_pool = list(range(8))
        else:
            fw = FW(nc, st0)
        pe, dve, act, pool, sp = fw.pe, fw.dve, fw.act, fw.pool, fw.sp
        ident, b_ident = emit_consts(fw, ident_d)
        ones = fw.sb([128, 128], F32, "ones"); b_ones = Buf(); fw.op(pool, lambda e: e.memset(ones[:, :], 1.0), writes=[b_ones])
        aT = fw.sb([128, 16, NTOK], BF16, "aT"); b_aT = [Buf() for _ in range(NT)]
        emit_mods(fw, cvec, ada_w, ada_b, 4096, modrows, d_mod, ident, b_ident)
        emit_aT(fw, h_all, NT, 2, norm_w1, modrows, d_mod, aT, b_aT, ident, b_ident, row_of=row_of, h_bufs=h_bufs)

        def tiles_of(s, n):
            return list(range(s // 128, (s + n) // 128))

        with ExitStack() as st:
            fw.stack = st
            V1g = fw.sb([128, NT, 2, 129], BF16, "V1g"); V1d = fw.sb([128, NT, 4, 257], BF16, "V1d"); b_V = [Buf() for _ in range(NT)]
            fw.op(pool, lambda e: e.memset(V1g[:, :, :, :].rearrange("p a b c -> p (a b c)"), 1.0), writes=b_V)
            fw.op(pool, lambda e: e.memset(V1d[:, :, :, :].rearrange("p a b c -> p (a b c)"), 1.0), writes=b_V)
            with ExitStack() as st2:
                fw.stack = st2
                wv = fw.sb([128, 16, 1280], BF16, "wv"); b_wv = Buf()
                fw.dma(pool, wv[:], w_in[:, GV0:GV0 + 1280].rearrange("(kc p) c -> p kc c", p=128), writes=[b_wv])
                for t in range(NT):
                    for part in range(3):
                        c0 = part * 512; n = 512 if part < 2 else 256
                        pt, pb = fw.next_psum()
                        for kc in range(16):
                            fw.op(pe, lambda e, kc=kc, pt=pt, t=t, c0=c0, n=n: e.matmul(pt[:, 0:n], aT[:, kc, t * 128:(t + 1) * 128], wv[:, kc, c0:c0 + n], start=(kc == 0), stop=(kc == 15)),
                                  reads=[b_aT[t], b_wv], writes=[pb])
                        if part == 0:
                            fw.op(act, lambda e, pt=pt, t=t: e.copy(V1g[:, t, :, 0:128], pt[:, 0:256].rearrange("p (a b) -> p a b", a=2)), reads=[pb], writes=[b_V[t]])
                            fw.op(dve, lambda e, pt=pt, t=t: e.tensor_copy(V1d[:, t, 0, 0:256], pt[:, 256:512]), reads=[pb], writes=[b_V[t]])
                        elif part == 1:
                            fw.op(act, lambda e, pt=pt, t=t: e.copy(V1d[:, t, 1:3, 0:256], pt[:, 0:512].rearrange("p (a b) -> p a b", a=2)), reads=[pb], writes=[b_V[t]])
                        else:
                            fw.op(dve, lambda e, pt=pt, t=t: e.tensor_copy(V1d[:, t, 3, 0:256], pt[:, 0:256]), reads=[pb], writes=[b_V[t]])
                fw.barrier()
            fw.stack = st
            fw.ps_pool = [0, 1, 2, 3]
            cosT = fw.sb([128, NLAT], F32, "cosT"); b_cos = Buf(); fw.dma(sp, cosT[:], cosT_d, writes=[b_cos])
            sinT = fw.sb([128, NLAT], F32, "sinT"); b_sin = Buf(); fw.dma(sp, sinT[:], sinT_d, writes=[b_sin])
            rmT = fw.sb([128, 128], F32, "rmT"); b_rm = Buf(); fw.dma(sp, rmT[:], rmT_d, writes=[b_rm])
            nw4 = fw.sb([128, 4], F32, "nw4"); b_nw4 = Buf(); fw.dma(sp, nw4[:], nws, writes=[b_nw4])
            nwq = fw.sb([128, 4], F32, "nwq"); b_nwq = Buf()
            fw.op(dve, lambda e: e.tensor_scalar(nwq[:, :], nw4[:, :], float(128 ** -0.5), None, ALU.mult), reads=[b_nw4], writes=[b_nwq])
            sub = fw.sb([128, 256], F32, "sub"); b_sub = Buf(); fw.dma(sp, sub[:], bview(subln), writes=[b_sub])
            fw.op(dve, lambda e: e.tensor_scalar(sub[:, :], sub[:, :], float(1.0 - LAMBDA_INIT), None, ALU.mult), writes=[b_sub])
            lv = fw.sb([128, 512], F32, "lv"); b_lv = Buf(); fw.dma(sp, lv[:], bview(lamv), writes=[b_lv])
            lt = fw.sb([128, 256], F32, "lt"); b_lt = Buf()
            fw.op(dve, lambda e: e.tensor_tensor(lt[:, :].rearrange("p (a b) -> p a b", a=2), lv[:, :].rearrange("p (a c b) -> p a c b", a=2, c=2)[:, :, 0, :],
                                                 lv[:, :].rearrange("p (a c b) -> p a c b", a=2, c=2)[:, :, 1, :], ALU.mult), reads=[b_lv], writes=[b_lt])
            ld = fw.sb([128, 2], F32, "ld"); b_ld = Buf()
            fw.op(dve, lambda e: e.reduce_sum(ld[:, :], lt[:, :].rearrange("p (a b) -> p a b", a=2), axis=AX.X), reads=[b_lt], writes=[b_ld])
            fw.op(act, lambda e: e.activation(ld[:, :], ld[:, :], AF.Exp), writes=[b_ld])
            nlam = fw.sb([128, 1], F32, "nlam"); b_nlam = Buf()
            fw.op(dve, lambda e: e.tensor_tensor(nlam[:, :], ld[:, 1:2], ld[:, 0:1], ALU.subtract), reads=[b_ld], writes=[b_nlam])
            fw.op(dve, lambda e: e.tensor_scalar(nlam[:, :], nlam[:, :], float(-LAMBDA_INIT), None, ALU.add), writes=[b_nlam])

            sqs = [fw.sb([128, 512], F32, "sq%d" % i) for i in range(2)]; b_sqs = [Buf() for _ in range(2)]
            rs = [fw.sb([128, 512], F32, "rs%d" % i) for i in range(2)]; b_rs = [Buf() for _ in range(2)]
            xws = [fw.sb([128, 512], F32, "xw%d" % i) for i in range(2)]; b_xws = [Buf() for _ in range(2)]
            us = [fw.sb([128, 512], F32, "u%d" % i) for i in range(2)]; b_us = [Buf() for _ in range(2)]
            wus = [fw.sb([128, 16, 128], BF16, "wu%d" % i) for i in range(3)]; b_wus = [Buf() for _ in range(3)]
            st_ = {"cnt": 0, "w": 0}

            pend = []

            def qk_unit(col0, nwcol, b_nwcol, dstT, bdst, with_ctx):
                wu, bwu = wus[st_["w"] % 3], b_wus[st_["w"] % 3]; st_["w"] += 1
                first = [True]
                for (s0, n) in (TB if with_ctx else TB[1:]):
                    blk = {}

                    def s1(blk=blk, s0=s0, n=n, is_first=first[0]):
                        if is_first:
                            fw.dma(pool, wu[:], w_in[:, col0:col0 + 128].rearrange("(kc p) c -> p kc c", p=128), writes=[bwu])
                        i2 = st_["cnt"] % 2; st_["cnt"] += 1
                        blk["i2"] = i2
                        sq_, bsq = sqs[i2], b_sqs[i2]
                        pt, pb = fw.next_psum(); blk["pt"] = (pt, pb)
                        for kc in range(16):
                            fw.op(pe, lambda e: e.matmul(pt[:, 0:n], wu[:, kc, :], aT[:, kc, s0:s0 + n], start=(kc == 0), stop=(kc == 15)),
                                  reads=[bwu] + [b_aT[t] for t in tiles_of(s0, n)], writes=[pb])
                        fw.op(act, lambda e: e.activation(sq_[:, 0:n], pt[:, 0:n], AF.Square), reads=[pb], writes=[bsq])

                    def s2(blk=blk, s0=s0, n=n):
                        i2 = blk["i2"]; pt, pb = blk["pt"]
                        sq_, bsq = sqs[i2], b_sqs[i2]; r_, br = rs[i2], b_rs[i2]; xw, bxw = xws[i2], b_xws[i2]
                        p2, pb2 = fw.next_psum()
                        fw.op(pe, lambda e: e.matmul(p2[:, 0:n], ones[:, :], sq_[:, 0:n], start=True, stop=True), reads=[b_ones, bsq], writes=[pb2])
                        fw.op(act, lambda e: e.activation(r_[:, 0:n], p2[:, 0:n], AF.Sqrt, bias=EPS, scale=1.0 / 128), reads=[pb2], writes=[br])
                        fw.op(dve, lambda e: e.reciprocal(r_[:, 0:n], r_[:, 0:n]), writes=[br])
                        fw.op(dve, lambda e: e.tensor_tensor(r_[:, 0:n], pt[:, 0:n], r_[:, 0:n], ALU.mult), reads=[pb], writes=[br])
                        d0 = s0 if with_ctx else s0 - 256
                        if s0 == 0:
                            fw.op(pool, lambda e: e.tensor_scalar(dstT[:, d0:d0 + n], r_[:, 0:n], nwcol, None, ALU.mult), reads=[br, b_nwcol], writes=[bdst])
                        else:
                            fw.op(pool, lambda e: e.tensor_scalar(xw[:, 0:n], r_[:, 0:n], nwcol, None, ALU.mult), reads=[br, b_nwcol], writes=[bxw])

                    def s3(blk=blk, s0=s0, n=n):
                        if s0 == 0:
                            return
                        i2 = blk["i2"]
                        xw, bxw = xws[i2], b_xws[i2]; u_, bu = us[i2], b_us[i2]
                        d0 = s0 if with_ctx else s0 - 256
                        l0 = s0 - 256
                        p3, pb3 = fw.next_psum()
                        fw.op(pe, lambda e: e.matmul(p3[:, 0:n], rmT[:, :], xw[:, 0:n], start=True, stop=True), reads=[b_rm, bxw], writes=[pb3])
                        fw.op(dve, lambda e: e.tensor_tensor(u_[:, 0:n], p3[:, 0:n], sinT[:, l0:l0 + n], ALU.mult), reads=[pb3, b_sin], writes=[bu])
                        fw.op(pool, lambda e: e.tensor_tensor(xw[:, 0:n], xw[:, 0:n], cosT[:, l0:l0 + n], ALU.mult), reads=[b_cos], writes=[bxw])
                        fw.op(pool, lambda e: e.tensor_tensor(dstT[:, d0:d0 + n], xw[:, 0:n], u_[:, 0:n], ALU.add), reads=[bxw, bu], writes=[bdst])

                    pend.append((s1, s2, s3))
                    first[0] = False

            def flush_qk():
                fw.ps_pool = list(range(8))
                blks = list(pend); pend.clear()
                nb = len(blks)
                for i in range(min(2, nb)):
                    blks[i][0]()
                for i in range(nb):
                    blks[i][1]()
                    if i + 2 < nb:
                        blks[i + 2][0]()
                    blks[i][2]()
                fw.ps_pool = [0, 1, 2, 3]

            kTs = [fw.sb([128, NTOK], BF16, "kT%d" % i) for i in range(2)]; b_kTs = [Buf() for _ in range(2)]
            qTs = [fw.sb([128, NLAT], BF16, "qT%d" % i) for i in range(2)]; b_qTs = [Buf() for _ in range(2)]
            PTs = [fw.sb([128, 512], BF16, "PT%d" % i) for i in range(3)]; b_PTs = [Buf() for _ in range(3)]
            ogs = [fw.sb([128, 128], F32, "og%d" % i) for i in range(3)]; b_ogs = [Buf() for _ in range(3)]
            rcs = [fw.sb([128, 1], F32, "rc%d" % i) for i in range(3)]; b_rcs = [Buf() for _ in range(3)]
            o0n = fw.sb([128, 4, 256], F32, "o0n"); b_o0n = [Buf() for _ in range(4)]
            ods = [fw.sb([128, 256], F32, "od%d" % i) for i in range(2)]; b_ods = [Buf() for _ in range(2)]
            o1s = [fw.sb([128, 256], F32, "o1_%d" % i) for i in range(2)]; b_o1s = [Buf() for _ in range(2)]
            sq2 = fw.sb([128, 256], F32, "sq2"); b_sq2 = Buf()
            ss2 = [fw.sb([128, 1], F32, "ss2_%d" % i) for i in range(2)]; b_ss2 = [Buf() for _ in range(2)]
            cn = {"pt": 0, "o": 0, "k": 0, "q": 0, "d": 0}

            def attend(qT, bqT, kT, bkT, vfn, vw, banks, qb, finish):
                stride = 512
                per_bank = 1
                def score(kt):
                    pS, pbS = fw.next_psum()
                    fw.op(pe, lambda e: e.matmul(pS[:, :], kT[:, kt * 128:(kt + 1) * 128], qT[:, qb * 512:(qb + 1) * 512], start=True, stop=True),
                          reads=[bkT, bqT], writes=[pbS])
                    return pS, pbS
                cur = score(0)
                for kt in range(NT):
                    nxt = score(kt + 1) if kt + 1 < NT else None
                    pS, pbS = cur
                    PT, bPT = PTs[cn["pt"] % 3], b_PTs[cn["pt"] % 3]; cn["pt"] += 1
                    fw.op(act, lambda e: e.activation(PT[:, :], pS[:, :], AF.Exp), reads=[pbS], writes=[bPT])
                    for qs in range(4):
                        bank = banks[qs // per_bank]; off = (qs % per_bank) * stride
                        pO, pbO = fw.psum[bank]
                        fw.op(pe, lambda e: e.matmul(pO[:, off:off + vw], PT[:, qs * 128:(qs + 1) * 128], vfn(kt), start=(kt == 0), stop=(kt == NT - 1)),
                              reads=[bPT, b_V[kt]], writes=[pbO])
                    cur = nxt
                for qs in range(4):
                    bank = banks[qs // per_bank]; off = (qs % per_bank) * stride
                    pO, pbO = fw.psum[bank]
                    finish(qs, pO, pbO, off)

            for kv in range(2):
                kT, bkT = kTs[cn["k"] % 2], b_kTs[cn["k"] % 2]; cn["k"] += 1
                qk_unit(GK0 + kv * 128, nw4[:, 1:2], b_nw4, kT, bkT, True)
                for hq in range(4):
                    hd = kv * 4 + hq
                    qT, bqT = qTs[cn["q"] % 2], b_qTs[cn["q"] % 2]; cn["q"] += 1
                    qk_unit(GQ0 + hd * 128, nwq[:, 0:1], b_nwq, qT, bqT, False)
                    flush_qk()
                    for qb in range(4):
                        def fin(qs, pO, pbO, off, hd=hd, qb=qb):
                            i3 = cn["o"] % 3; cn["o"] += 1
                            og, bog = ogs[i3], b_ogs[i3]; rc, brc = rcs[i3], b_rcs[i3]
                            fw.op(dve, lambda e: e.reciprocal(rc[:, :], pO[:, off + 128:off + 129]), reads=[pbO], writes=[brc])
                            fw.op(dve, lambda e: e.tensor_scalar(og[:, :], pO[:, off:off + 128], rc[:, 0:1], None, ALU.mult), reads=[pbO, brc], writes=[bog])
                            r0 = qb * 512 + qs * 128
                            if fused:
                                d_mix.extend(out_fn(fw, r0, hd * 128, 128, og, bog))
                            else:
                                db = Buf(); d_mix.append(db)
                                fw.dma(sp, mix[r0:r0 + 128, hd * 128:(hd + 1) * 128], og[:, :], reads=[bog], writes=[db])
                        attend(qT, bqT, kT, bkT, lambda kt, kv=kv: V1g[:, kt, kv, :], 129, [4, 5, 6, 7], qb, fin)
            if fused and ctx.get("mid_hook"):
                ctx["mid_hook"](0)
            for h in range(4):
                kq = []
                for c in range(2):
                    kT, bkT = kTs[cn["k"] % 2], b_kTs[cn["k"] % 2]; cn["k"] += 1
                    qk_unit(DK0 + (h * 2 + c) * 128, nw4[:, 3:4], b_nw4, kT, bkT, True)
                    qT, bqT = qTs[cn["q"] % 2], b_qTs[cn["q"] % 2]; cn["q"] += 1
                    qk_unit(DQ0 + (h * 2 + c) * 128, nwq[:, 2:3], b_nwq, qT, bqT, False)
                    kq.append((kT, bkT, qT, bqT))
                flush_qk()
                for qb in range(4):
                    for c in range(2):
                        kT, bkT, qT, bqT = kq[c]
                        if c == 0:
                            def fin(qs, pO, pbO, off):
                                i3 = cn["o"] % 3; cn["o"] += 1
                                rc, brc = rcs[i3], b_rcs[i3]
                                fw.op(dve, lambda e: e.reciprocal(rc[:, :], pO[:, off + 256:off + 257]), reads=[pbO], writes=[brc])
                                fw.op(dve, lambda e: e.tensor_scalar(o0n[:, qs, :], pO[:, off:off + 256], rc[:, 0:1], None, ALU.mult), reads=[pbO, brc], writes=[b_o0n[qs]])
                        else:
                            def fin(qs, pO, pbO, off, h=h, qb=qb):
                                i3 = cn["o"] % 3; cn["o"] += 1
                                i2 = cn["d"] % 2; cn["d"] += 1
                                rc, brc = rcs[i3], b_rcs[i3]
                                o1, bo1 = o1s[i2], b_o1s[i2]; od, bod = ods[i2], b_ods[i2]; s2, bs2 = ss2[i2], b_ss2[i2]
                                fw.op(dve, lambda e: e.reciprocal(rc[:, :], pO[:, off + 256:off + 257]), reads=[pbO], writes=[brc])
                                fw.op(dve, lambda e: e.tensor_scalar(rc[:, :], rc[:, :], nlam[:, 0:1], None, ALU.mult), reads=[b_nlam], writes=[brc])
                                fw.op(dve, lambda e: e.scalar_tensor_tensor(o1[:, :], pO[:, off:off + 256], rc[:, 0:1], o0n[:, qs, :], ALU.mult, ALU.add),
                                      reads=[pbO, brc, b_o0n[qs]], writes=[bo1])
                                fw.op(act, lambda e: e.activation(sq2[:, :], o1[:, :], AF.Square, accum_out=s2[:, :]), reads=[bo1], writes=[b_sq2, bs2])
                                fw.op(act, lambda e: e.activation(s2[:, :], s2[:, :], AF.Sqrt, bias=EPS, scale=1.0 / 256), writes=[bs2])
                                fw.op(dve, lambda e: e.reciprocal(s2[:, :], s2[:, :]), writes=[bs2])
                                fw.op(dve, lambda e: e.scalar_tensor_tensor(od[:, :], o1[:, :], s2[:, 0:1], sub[:, :], ALU.mult, ALU.mult), reads=[bo1, bs2, b_sub], writes=[bod])
                                r0 = qb * 512 + qs * 128
                                if fused:
                                    d_mix.extend(out_fn(fw, r0, 1024 + h * 256, 256, od, bod))
                                else:
                                    db = Buf(); d_mix.append(db)
                                    fw.dma(sp, mix[r0:r0 + 128, 1024 + h * 256:1024 + (h + 1) * 256], od[:, :], reads=[bod], writes=[db])
                        attend(qT, bqT, kT, bkT, lambda kt, h=h: V1d[:, kt, h, :], 257, [4, 5, 6, 7], qb, fin)
                if fused and ctx.get("mid_hook") and h == 1:
                    ctx["mid_hook"](1)
            fw.barrier()
        if not fused:
            fw.finish(d_mix)
        fw.ps_pool = list(range(8))
        print("phaseA1 ops", fw.n_ops, "waits", fw.n_waits)
    return d_mix if fused else nc

from contextlib import ExitStack

PAIRS = [[0, 1], [2, 3], [4, 5], [6, 7]]
CC_BYTES = 4 * 1024 * 1024


def build_fused():
    nc = bass.Bass("TRN2", target_bir_lowering=False)
    msk_d = nc.dram_tensor("msk", [128, 2], F32, kind="ExternalInput").ap()
    PW = [1024, 512, 512]; PC = [0, 1024, 1536]
    x1s = [nc.dram_tensor("x1s%d" % i, [2 * 2304, PW[i]], BF16).ap() for i in range(3)]
    x1d = [nc.dram_tensor("x1d%d" % i, [2 * 2304, PW[i]], BF16).ap() for i in range(3)]
    x2s = nc.dram_tensor("x2s", [2 * 1152, 2048], F32).ap(); x2d = nc.dram_tensor("x2d", [2 * 1152, 2048], F32).ap()
    x3s = [nc.dram_tensor("x3s%d" % i, [2 * 2048, PW[i]], BF16).ap() for i in range(3)]
    x3d = [nc.dram_tensor("x3d%d" % i, [2 * 2048, PW[i]], BF16).ap() for i in range(3)]
    h1own = nc.dram_tensor("h1own", [1024, 2048], F32).ap()
    with ExitStack() as st0:
        fw = FW(nc, st0)
        cc = Src("cc", fw._sem("cc"), 1)
        msk = fw.sb([128, 2], F32, "msk"); b_msk = Buf()
        fw.dma(fw.sp, msk[:], msk_d, writes=[b_msk])

        def staging(shape, dt, n):
            stk = fw.stack
            key = "_stg_%s_%d" % (str(dt), shape[1])
            if not hasattr(stk, key):
                setattr(stk, key, {"t": [fw.sb(shape, dt, "stg%d" % i) for i in range(n)], "b": [Buf() for _ in range(n)], "i": 0})
            d = getattr(stk, key)
            k = d["i"] % n; d["i"] += 1
            return d["t"][k], d["b"][k]

        def mk_mix_out(dsts, R, wlists):
            def out_fn(fw_, r0, c0, n, tile, btile):
                res = []
                part = 0 if c0 < 1024 else 1 if c0 < 1536 else 2
                cc0 = c0 - PC[part]
                for s in range(2):
                    u, bu = staging([128, 512], BF16, 4)
                    if s == 0:
                        fw.op(fw.act, lambda e: e.mul(u[:, 0:n], tile[:, :], msk[:, s:s + 1]), reads=[btile, b_msk], writes=[bu])
                    else:
                        fw.op(fw.dve, lambda e: e.tensor_scalar(u[:, 0:n], tile[:, :], msk[:, s:s + 1], None, ALU.mult), reads=[btile, b_msk], writes=[bu])
                    db = Buf(); res.append(db); wlists[part].append(db)
                    fw.dma(fw.sp, dsts[part][s * R + r0:s * R + r0 + 128, cc0:cc0 + n], u[:, 0:n], reads=[bu], writes=[db])
                return res
            return out_fn

        b_h1own = []

        def b0_out(fw_, t, h1, bh1):
            res = []
            for s in range(2):
                u, bu = staging([128, 2048], F32, 4)
                if s == 0:
                    fw.op(fw.act, lambda e: e.mul(u[:, :], h1[:, :], msk[:, s:s + 1]), reads=[bh1, b_msk], writes=[bu])
                else:
                    fw.op(fw.pool, lambda e: e.tensor_scalar(u[:, :], h1[:, :], msk[:, s:s + 1], None, ALU.mult), reads=[bh1, b_msk], writes=[bu])
                db = Buf(); res.append(db)
                fw.dma(fw.sp, x2s[s * 1152 + t * 128:s * 1152 + (t + 1) * 128, :], u[:, :], reads=[bu], writes=[db])
            if t >= 1:
                db = Buf(); b_h1own.append(db); res.append(db)
                fw.dma(fw.sp, h1own[(t - 1) * 128:t * 128, :], h1[:, :], reads=[bh1], writes=[db])
            return res

        def exchange(src, dst, writers):
            pool = fw.pool
            for b in writers:
                pool.wait(b.w)
            nrows = src.shape[0]
            elt = 2 if src.dtype == BF16 else 4
            rows_per = CC_BYTES // (src.shape[1] * elt)
            for r0 in range(0, nrows, rows_per):
                r1 = min(nrows, r0 + rows_per)
                ins = pool.raw.collective_compute("AllReduce", ALU.add, replica_groups=PAIRS,
                                                  ins=[src[r0:r1, :].opt()], outs=[dst[r0:r1, :].opt()])
                ins.then_inc(cc.sem)
                cc.n += 1
            eb = Buf()
            eb.w = Ev(cc, cc.n, dict(pool.clock))
            return eb

        shared = {}
        wl1 = [[], [], []]; ev1 = [None, None, None]

        def hook1(i):
            ev1[i] = exchange(x1s[i], x1d[i], wl1[i])
        build_phaseA0(ctx=dict(nc=nc, fw=fw, pre="a0_", out_fn=mk_mix_out(x1s, 2304, wl1), mid_hook=hook1))
        hook1(2)
        w2_ = build_phaseB(9, True, ctx=dict(nc=nc, fw=fw, pre="b0_", mix_src=[(x1d[i], [ev1[i]], PC[i]) for i in range(3)], out_fn=b0_out, shared=shared))
        e2 = exchange(x2s, x2d, w2_)

        def row_of(t):
            return 0 if t == 0 else 1152 if t == 1 else 128 + (t - 2) * 128 if t < 10 else 1280 + (t - 10) * 128

        wl3 = [[], [], []]; ev3 = [None, None, None]

        def hook3(i):
            ev3[i] = exchange(x3s[i], x3d[i], wl3[i])
        build_phaseA1(ctx=dict(nc=nc, fw=fw, pre="a1_", h_src=(x2d, [e2], row_of), out_fn=mk_mix_out(x3s, 2048, wl3), mid_hook=hook3))
        hook3(2)
        outs = build_phaseB(8, False, ctx=dict(nc=nc, fw=fw, pre="b1_", h_src=(h1own, list(b_h1own)), mix_src=[(x3d[i], [ev3[i]], PC[i]) for i in range(3)], out_fn=None, shared=shared))
        fw.finish(outs)
        print("fused ops", fw.n_ops, "waits", fw.n_waits)
    return nc

import numpy as np

PERM = np.concatenate([np.arange(0, 1024), np.arange(2048, 3072), np.arange(1024, 2048), np.arange(3072, 4096)])
_CONST = {}

def consts():
    if "ident" not in _CONST:
        s = np.arange(128)
        _CONST["ident"] = np.eye(128, dtype=np.float32)
        _CONST["trilt"] = (s[:, None] < s[None, :]).astype(np.float32)
        _CONST["ule"] = (s[:, None] <= s[None, :]).astype(np.float32)
        _CONST["uge"] = (s[:, None] >= s[None, :]).astype(np.float32)
        _CONST["iota64"] = np.arange(64, dtype=np.float32).reshape(1, 64)
    return _CONST

def f32c(a):
    return np.ascontiguousarray(a, dtype=np.float32)

def na_bias_tables(rpb, heads):
    classes = [(0, 0), (1, 0), (2, 0), (14, 22), (15, 22)]
    out = np.full((len(heads), 25, 128, 128), -30000.0, np.float32)
    k = np.arange(128); q = np.arange(128)
    for ci, (j, a) in enumerate(classes):
        r = 2 * j + q // 64; c = q % 64
        r0 = np.clip(r - 4, 0, 24); cs = np.clip(c - 8, 0, 48)
        for i in range(5):
            kr = a + 2 * i + k // 64; kc = k % 64
            vis = ((kr[:, None] >= r0[None, :]) & (kr[:, None] < r0[None, :] + 8) &
                   (kc[:, None] >= cs[None, :]) & (kc[:, None] < cs[None, :] + 16))
            ri = np.clip(kr[:, None] - r[None, :] + 7, 0, 14)
            cj = np.clip(kc[:, None] - c[None, :] + 15, 0, 30)
            for hi, h in enumerate(heads):
                vals = rpb[h][ri, cj]
                out[hi, ci * 5 + i] = np.where(vis, vals, np.float32(-30000.0))
    return out

def pack_A0(P, b, hh, h_all):
    in_w = P["ev_in_w"][0]
    gs = [2 * hh, 2 * hh + 1]
    heads16 = np.arange(16 * hh, 16 * hh + 16)
    nah = np.arange(8 * hh, 8 * hh + 8)
    cols = np.concatenate(
        [np.arange(g * 512, (g + 1) * 512) for g in gs] +
        [2048 + np.arange(g * 512, (g + 1) * 512) for g in gs] +
        [4096 + np.arange(g * 128, (g + 1) * 128) for g in gs] +
        [4608 + np.arange(g * 128, (g + 1) * 128) for g in gs] +
        [5120 + heads16, 5152 + heads16] +
        [5184 + np.arange(h * 128, (h + 1) * 128) for h in nah] +
        [5184 + 2048 + np.arange(h * 128, (h + 1) * 128) for h in nah] +
        [5184 + 4096 + np.arange(h * 128, (h + 1) * 128) for h in nah])
    chans = np.concatenate([np.arange(g * 512, (g + 1) * 512) for g in gs] +
                           [2048 + np.arange(g * 128, (g + 1) * 128) for g in gs] +
                           [2560 + np.arange(g * 128, (g + 1) * 128) for g in gs])
    d = {k: consts()[k] for k in ("ident", "ule", "uge")}
    d.update({
        "h_all": h_all, "cvec": np.stack([P["c"][b], P["c_ctx"]], 0),
        "ada_w": P["ada_w"][0][:, 0:4096], "ada_b": P["ada_b"][0][None, 0:4096],
        "norm_w1": P["norm_w"][0, 0][None, :], "w_in": in_w[:, cols],
        "convw": P["ev_conv_w"][0][:, chans].T, "convb": P["ev_conv_b"][0][chans].reshape(12, 128).T,
        "dt_bias": np.concatenate([P["ev_dt_bias"][0][0, heads16], P["ev_dt_bias"][0][1, heads16]])[None, :],
        "a_log": np.concatenate([P["ev_a_log"][0][0, heads16], P["ev_a_log"][0][1, heads16]])[None, :],
        "d_skip": P["ev_d_skip"][0][heads16][None, :],
        "ssd_nw": np.concatenate([P["ev_ssd_norm_w"][0][g * 512:(g + 1) * 512] for g in gs])[None, :],
        "qnw": P["ev_na_q_norm"][0][:, None], "knw": P["ev_na_k_norm"][0][:, None],
        "biasT": na_bias_tables(P["ev_na_rpb"][0], list(nah)),
    })
    return {k: f32c(v) for k, v in d.items()}

def rope_tables():
    if "cosT" not in _CONST:
        t = np.arange(2048)
        row = (t // 64).astype(np.float32); col = (t % 64).astype(np.float32)
        inv = (1.0 / (np.float32(10000.0) ** (np.arange(0, 64, 2, dtype=np.float32) / np.float32(64)))).astype(np.float32)
        ang = np.concatenate([row[:, None] * inv[None], col[:, None] * inv[None]], -1).astype(np.float32)
        c = np.cos(ang).astype(np.float32); s = np.sin(ang).astype(np.float32)
        _CONST["cosT"] = np.ascontiguousarray(np.repeat(c, 2, axis=1).T)
        _CONST["sinT"] = np.ascontiguousarray(np.repeat(s, 2, axis=1).T)
        rm = np.zeros((128, 128), np.float32)
        for i in range(64):
            rm[2 * i + 1, 2 * i] = -1.0
            rm[2 * i, 2 * i + 1] = 1.0
        _CONST["rmT"] = rm
    return _CONST["cosT"], _CONST["sinT"], _CONST["rmT"]

def pack_A1(P, b, hh, h_all):
    in_w = P["od_in_w"][0]
    cols = np.concatenate(
        [np.arange((8 * hh + i) * 128, (8 * hh + i + 1) * 128) for i in range(8)] +
        [2048 + np.arange((8 * hh + i) * 128, (8 * hh + i + 1) * 128) for i in range(8)] +
        [4096 + np.arange((2 * hh + i) * 128, (2 * hh + i + 1) * 128) for i in range(2)] +
        [5120 + np.arange((8 * hh + i) * 128, (8 * hh + i + 1) * 128) for i in range(8)] +
        [4608 + np.arange((2 * hh + i) * 128, (2 * hh + i + 1) * 128) for i in range(2)] +
        [7168 + np.arange((4 * hh + i) * 256, (4 * hh + i + 1) * 256) for i in range(4)])
    cosT, sinT, rmT = rope_tables()
    d = {
        "ident": consts()["ident"],
        "h_all": h_all, "cvec": np.stack([P["c"][b], P["c_ctx"]], 0),
        "ada_w": P["ada_w"][1][:, 0:4096], "ada_b": P["ada_b"][1][None, 0:4096],
        "norm_w1": P["norm_w"][1, 0][None, :], "w_in": in_w[:, cols],
        "nws": np.stack([P["od_gqa_q_norm"][0], P["od_gqa_k_norm"][0], P["od_diff_q_norm"][0], P["od_diff_k_norm"][0]], 1),
        "lamv": P["od_lambda"][0].reshape(1, 512), "subln": P["od_diff_subln"][0][None, :],
        "cosT": cosT, "sinT": sinT, "rmT": rmT,
    }
    return {k: f32c(v) for k, v in d.items()}


_PROG = {}


def _shared_B(P, layer):
    ow = P["ev_out_w"][0] if layer == 0 else P["od_out_w"][0]
    c = consts()
    sh = {"ident": c["ident"], "trilt": c["trilt"], "iota64": c["iota64"],
          "ada_w": P["ada_w"][layer][:, 4096:], "ada_b": P["ada_b"][layer][None, 4096:],
          "norm_w2": P["norm_w"][layer, 1][None, :], "out_w": ow[PERM],
          "gwew": np.concatenate([P["moe_group_w"][layer], P["moe_expert_w"][layer]], 1),
          "w1": P["moe_w1"][layer], "w3": P["moe_w3"][layer], "w2": P["moe_w2"][layer]}
    return {k: f32c(v) for k, v in sh.items()}


def kernel(**inputs):
    P = {k: np.asarray(v) for k, v in inputs.items()}
    B = 4
    cores = [(b, hh) for b in range(B) for hh in range(2)]
    if "nc" not in _PROG:
        _PROG["nc"] = build_fused()
    shB = [_shared_B(P, 0), _shared_B(P, 1)]
    a0 = {}; a1 = {}
    maps = []
    for (b, hh) in cores:
        h_all = np.concatenate([P["ctx"][b], P["x"][b]], 0)
        if hh not in a0:
            a0[hh] = pack_A0(P, b, hh, h_all)
            a1[hh] = pack_A1(P, b, hh, h_all)
            a1[hh].pop("h_all")
        cvec = f32c(np.stack([P["c"][b], P["c_ctx"]], 0))
        d = {}
        for k, v in a0[hh].items():
            d["a0_" + k] = v
        d["a0_h_all"] = f32c(h_all); d["a0_cvec"] = cvec
        for k, v in a1[hh].items():
            d["a1_" + k] = v
        d["a1_cvec"] = cvec
        for l, pre in ((0, "b0_"), (1, "b1_")):
            for k, v in shB[l].items():
                d[pre + k] = v
            d[pre + "cvec"] = cvec
        rows0 = np.concatenate([np.arange(hh * 128, (hh + 1) * 128), 256 + np.arange(hh * 1024, (hh + 1) * 1024)])
        d["b0_h_in"] = f32c(h_all[rows0])
        g0 = rows0.reshape(9, 128).T
        d["b0_gidx"] = np.ascontiguousarray(np.stack([g0, 2304 + g0], -1).astype(np.int32))
        g1 = (hh * 1024 + np.arange(1024)).reshape(8, 128).T
        d["b1_gidx"] = np.ascontiguousarray(np.stack([g1, 2048 + g1], -1).astype(np.int32))
        m = np.zeros((128, 2), np.float32); m[:, hh] = 1.0
        d["msk"] = m
        maps.append(d)
    res = run_bass_kernel_spmd(_PROG["nc"], maps, core_ids=list(range(8)))
    out = np.zeros((B, 2048, 2048), np.float32)
    for i, (b, hh) in enumerate(cores):
        out[b, hh * 1024:(hh + 1) * 1024] = res.results[i]["h_out"]
    return out
```

```python
import numpy as np
import ml_dtypes
import concourse.bass as bass
import concourse.mybir as mybir
from concourse.bass_utils import run_bass_kernel_spmd

F32 = mybir.dt.float32
BF16 = mybir.dt.bfloat16
I32 = mybir.dt.int32
U32 = mybir.dt.uint32
AF = mybir.ActivationFunctionType
ALU = mybir.AluOpType
AX = mybir.AxisListType


class Ev:
    __slots__ = ("src", "count", "clock")

    def __init__(self, src, count, clock):
        self.src = src
        self.count = count
        self.clock = clock


class Src:
    def __init__(self, name, sem, mult):
        self.name = name
        self.sem = sem
        self.mult = mult
        self.n = 0


class Buf:
    __slots__ = ("name", "w", "r")

    def __init__(self, name=""):
        self.name = name
        self.w = None
        self.r = {}


class Eng:
    def __init__(self, fw, raw, name):
        self.fw = fw
        self.raw = raw
        self.name = name
        self.src = Src(name, fw._sem("s_" + name), 1)
        self.clock = {}
        self.slots = []
        self.slot_i = 0

    def wait(self, ev):
        if ev is None:
            return
        if self.name == "pe" and ev.src.name.startswith("pe"):
            return
        if self.clock.get(ev.src.name, 0) >= ev.count:
            return
        self.raw.wait_ge(ev.src.sem, ev.count * ev.src.mult)
        self.fw.n_waits += 1
        for k, v in ev.clock.items():
            if self.clock.get(k, 0) < v:
                self.clock[k] = v
        self.clock[ev.src.name] = ev.count


class FW:
    def __init__(self, nc, stack, n_dma_slots=12):
        self.nc = nc
        self.stack = stack
        self.sem_stack = stack
        self.n_waits = 0
        self.n_ops = 0
        self.pe = Eng(self, nc.tensor, "pe")
        self.dve = Eng(self, nc.vector, "dve")
        self.act = Eng(self, nc.scalar, "act")
        self.pool = Eng(self, nc.gpsimd, "pool")
        self.sp = Eng(self, nc.sync, "sp")
        for e in (self.sp, self.pool, self.act):
            for i in range(n_dma_slots):
                nm = "d_%s%d" % (e.name, i)
                e.slots.append(Src(nm, self._sem(nm), 16))
        self.uid = 0
        self.psum = []
        for i in range(8):
            t = stack.enter_context(nc.psum_tensor("ps%d" % i, [128, 512], F32))
            self.psum.append((t, Buf("ps%d" % i)))
        self.ps_i = 0
        self.ps_pool = list(range(8))

    def _sem(self, name):
        return self.sem_stack.enter_context(self.nc.semaphore(name))

    def sb(self, shape, dtype, name=None):
        self.uid += 1
        name = "sb%d_%s" % (self.uid, name or "t")
        t = self.stack.enter_context(self.nc.sbuf_tensor(name, list(shape), dtype))
        return t

    def next_psum(self):
        self.ps_i = (self.ps_i + 1) % len(self.ps_pool)
        return self.psum[self.ps_pool[self.ps_i]]

    def _deps(self, eng, reads, writes):
        for b in reads:
            eng.wait(b.w)
        for b in writes:
            eng.wait(b.w)
            for ev in list(b.r.values()):
                eng.wait(ev)

    def _mark(self, ev, reads, writes):
        for b in reads:
            b.r[ev.src.name] = ev
        for b in writes:
            b.w = ev
            b.r = {}

    def op(self, eng, fn, reads=(), writes=()):
        self._deps(eng, reads, writes)
        ins = fn(eng.raw)
        if eng.src.n >= 30000:
            eng.gen = getattr(eng, "gen", 0) + 1
            nm = "%s_g%d" % (eng.name, eng.gen)
            eng.src = Src(nm, self._sem("s_" + nm), 1)
        eng.src.n += 1
        ins.then_inc(eng.src.sem, 1)
        clock = dict(eng.clock)
        ev = Ev(eng.src, eng.src.n, clock)
        self._mark(ev, reads, writes)
        self.n_ops += 1
        return ev

    def dma(self, eng, out, in_, reads=(), writes=(), fn=None):
        slot = eng.slots[eng.slot_i]
        eng.slot_i = (eng.slot_i + 1) % len(eng.slots)
        if slot.n > 0:
            eng.wait(Ev(slot, slot.n, {}))
        self._deps(eng, reads, writes)
        if fn is None:
            ins = eng.raw.dma_start(out=out, in_=in_)
        else:
            ins = fn(eng.raw)
        ins.then_inc(slot.sem, 16)
        slot.n += 1
        ev = Ev(slot, slot.n, dict(eng.clock))
        self._mark(ev, reads, writes)
        self.n_ops += 1
        return ev

    def bound_reg(self, val):
        if not hasattr(self, "_bregs"):
            self._bregs = {}
        if val not in self._bregs:
            self._bregs[val] = self.nc.gpsimd.to_reg(val)
        return self._bregs[val]

    def engines(self):
        return (self.pe, self.dve, self.act, self.pool, self.sp)

    def barrier(self):
        evs = []
        for e in self.engines():
            if e.src.n > 0:
                evs.append(Ev(e.src, e.src.n, dict(e.clock)))
            for s in e.slots:
                if s.n > 0:
                    evs.append(Ev(s, s.n, {}))
        for e in self.engines():
            for ev in evs:
                e.wait(ev)

    def finish(self, bufs):
        for b in bufs:
            self.sp.wait(b.w)

from contextlib import ExitStack

D = 2048
EPS = 1e-6


def bview(ap_row, n=128):
    return ap_row.partition_broadcast(n)


def emit_consts(fw, ident_d):
    ident = fw.sb([128, 128], F32, "ident"); b_ident = Buf("ident")
    fw.dma(fw.sp, ident[:], ident_d, writes=[b_ident])
    return ident, b_ident


def emit_mods(fw, cvec, ada_w, ada_b, ncols, modrows, d_mod, ident, b_ident):
    pe, dve, act, pool, sp = fw.pe, fw.dve, fw.act, fw.pool, fw.sp
    outer = fw.stack
    with ExitStack() as st:
        fw.stack = st
        cv = fw.sb([2, D], F32, "cv"); b_cv = Buf()
        fw.dma(sp, cv[:], cvec, writes=[b_cv])
        siluT = fw.sb([128, 16, 2], BF16, "siluT"); b_sT = Buf()
        pt, pb = fw.next_psum()
        for kc in range(16):
            fw.op(pe, lambda e, kc=kc: e.transpose(pt[:, 2 * kc:2 * kc + 2], cv[0:2, kc * 128:(kc + 1) * 128], ident[0:2, 0:2]),
                  reads=[b_cv, b_ident], writes=[pb])
        fw.op(act, lambda e: e.activation(siluT[:, :, :].rearrange("p a b -> p (a b)"), pt[:, 0:32], AF.Silu), reads=[pb], writes=[b_sT])
        adab = fw.sb([2, ncols], F32, "adab"); b_adab = Buf()
        fw.dma(sp, adab[:], bview(ada_b, 2), writes=[b_adab])
        wts = [fw.sb([128, 16, 512], BF16, "adaw%d" % i) for i in range(2)]
        b_wts = [Buf() for _ in range(2)]
        modsb = fw.sb([2, ncols], F32, "modsb"); b_modsb = Buf()
        for j in range(ncols // 512):
            wt, bw = wts[j % 2], b_wts[j % 2]
            fw.dma(pool, wt[:], ada_w[:, j * 512:(j + 1) * 512].rearrange("(kc p) c -> p kc c", p=128), writes=[bw])
            pt, pb = fw.next_psum()
            for kc in range(16):
                fw.op(pe, lambda e, kc=kc, wt=wt, pt=pt: e.matmul(pt[0:2, :], siluT[:, kc, :], wt[:, kc, :], start=(kc == 0), stop=(kc == 15)),
                      reads=[b_sT, bw], writes=[pb])
            fw.op(dve, lambda e, pt=pt, j=j: e.tensor_tensor(modsb[:, j * 512:(j + 1) * 512], pt[0:2, :], adab[:, j * 512:(j + 1) * 512], ALU.add),
                  reads=[pb, b_adab], writes=[b_modsb])
        fw.dma(sp, modrows, modsb[:, :], reads=[b_modsb], writes=[d_mod])
        fw.barrier()
    fw.stack = outer


def emit_aT(fw, h_all, n_tiles, n_ctx_tiles, norm_w1, modrows, d_mod, aT, b_aT, ident, b_ident, row_of=None, h_bufs=()):
    pe, dve, act, pool, sp = fw.pe, fw.dve, fw.act, fw.pool, fw.sp
    outer = fw.stack
    with ExitStack() as st:
        fw.stack = st
        nw = fw.sb([128, D], F32, "nw"); b_nw = Buf()
        fw.dma(sp, nw[:], bview(norm_w1), writes=[b_nw])
        nsc = []; sh = []; b_nsc = []; b_sh = []
        for v in range(2):
            s = fw.sb([128, D], F32, "sh1_%d" % v); bs = Buf()
            fw.dma(sp, s[:], bview(modrows[v:v + 1, 0:2048]), reads=[d_mod], writes=[bs])
            sh.append(s); b_sh.append(bs)
            c = fw.sb([128, D], F32, "nsc1_%d" % v); bc = Buf()
            fw.dma(sp, c[:], bview(modrows[v:v + 1, 2048:4096]), reads=[d_mod], writes=[bc])
            fw.op(dve, lambda e, c=c: e.scalar_tensor_tensor(c[:, :], c[:, :], 1.0, nw[:, :], ALU.add, ALU.mult), reads=[b_nw], writes=[bc])
            nsc.append(c); b_nsc.append(bc)
        hts = [fw.sb([128, D], F32, "ht%d" % i) for i in range(2)]; b_hts = [Buf() for _ in range(2)]
        ats = [fw.sb([128, D], F32, "at%d" % i) for i in range(2)]; b_ats = [Buf() for _ in range(2)]
        sq = fw.sb([128, D], F32, "sq"); b_sq = Buf()
        ssqs = [fw.sb([128, 1], F32, "ssq%d" % i) for i in range(2)]; b_ssqs = [Buf() for _ in range(2)]
        for t in range(n_tiles):
            v = 1 if t < n_ctx_tiles else 0
            ht, bh = hts[t % 2], b_hts[t % 2]
            at, ba = ats[t % 2], b_ats[t % 2]
            ssq, b_ssq = ssqs[t % 2], b_ssqs[t % 2]
            r0 = t * 128 if row_of is None else row_of(t)
            fw.dma(sp, ht[:], h_all[r0:r0 + 128, :], reads=list(h_bufs(t)) if callable(h_bufs) else list(h_bufs), writes=[bh])
            fw.op(act, lambda e, ht=ht, ssq=ssq: e.activation(sq[:, :], ht[:, :], AF.Square, accum_out=ssq[:, :]), reads=[bh], writes=[b_sq, b_ssq])
            fw.op(act, lambda e, ssq=ssq: e.activation(ssq[:, :], ssq[:, :], AF.Sqrt, bias=EPS, scale=1.0 / D), writes=[b_ssq])
            fw.op(dve, lambda e, ssq=ssq: e.reciprocal(ssq[:, :], ssq[:, :]), writes=[b_ssq])
            fw.op(dve, lambda e, ht=ht, at=at, ssq=ssq, v=v: e.scalar_tensor_tensor(at[:, :], ht[:, :], ssq[:, 0:1], nsc[v][:, :], ALU.mult, ALU.mult),
                  reads=[bh, b_ssq, b_nsc[v]], writes=[ba])
            fw.op(pool, lambda e, at=at, v=v: e.tensor_tensor(at[:, :], at[:, :], sh[v][:, :], ALU.add), reads=[b_sh[v]], writes=[ba])
            for q in range(4):
                pt, pb = fw.next_psum()
                for i in range(4):
                    kc = q * 4 + i
                    fw.op(pe, lambda e, pt=pt, i=i, kc=kc, at=at: e.transpose(pt[:, i * 128:(i + 1) * 128], at[:, kc * 128:(kc + 1) * 128], ident[:, :]),
                          reads=[ba, b_ident], writes=[pb])
                dst = aT[:, q * 4:(q + 1) * 4, t * 128:(t + 1) * 128]
                src = pt[:, :].rearrange("p (a b) -> p a b", a=4)
                if q % 2 == 0:
                    fw.op(act, lambda e, dst=dst, src=src: e.copy(dst, src), reads=[pb], writes=[b_aT[t]])
                else:
                    fw.op(dve, lambda e, dst=dst, src=src: e.tensor_copy(dst, src), reads=[pb], writes=[b_aT[t]])
        fw.barrier()
    fw.stack = outer

from contextlib import ExitStack

D = 2048
CAP = 128
NE = 64
EPS = 1e-6


def bview(ap_row, n=128):
    return ap_row.partition_broadcast(n)


def build_phaseB(n_tiles, has_ctx, ctx=None):
    fused = ctx is not None
    nc = ctx["nc"] if fused else bass.Bass("TRN2", target_bir_lowering=False)
    pre = ctx["pre"] if fused else ""
    T = n_tiles * 128
    I = lambda name, shape, dt=F32: nc.dram_tensor(pre + name, shape, dt, kind="ExternalInput").ap()
    h_src = ctx.get("h_src") if fused else None
    h_in = I("h_in", [T, D]) if h_src is None else h_src[0]
    h_bufs = [] if h_src is None else h_src[1]
    mix_in = None if fused else I("mix_in", [T, 4096])
    gidx_d = I("gidx", [128, n_tiles, 2], I32) if fused else None
    cvec = I("cvec", [2, D])
    ada_w = I("ada_w", [D, 8192])
    ada_b = I("ada_b", [1, 8192])
    norm_w2 = I("norm_w2", [1, D])
    out_w = I("out_w", [4096, D])
    gwew = I("gwew", [D, 72])
    w1 = I("w1", [NE, D, 512])
    w3 = I("w3", [NE, D, 512])
    w2 = I("w2", [NE, 512, D])
    ident_d = I("ident", [128, 128])
    trilt_d = I("trilt", [128, 128])
    iota_d = I("iota64", [1, 64])
    out_fn = ctx.get("out_fn") if fused else None
    h_out = nc.dram_tensor("h_out", [T, D], F32, kind="ExternalOutput").ap() if out_fn is None else None
    modrows = nc.dram_tensor(pre + "modrows", [2, 8192], F32).ap()
    m_all = nc.dram_tensor(pre + "m_all", [T, D], F32).ap()
    h1_all = nc.dram_tensor(pre + "h1_all", [T, D], F32).ap()
    reuse_x = fused and ("xdisp" in ctx.get("shared", {}))
    if reuse_x:
        xdisp = ctx["shared"]["xdisp"]
    else:
        xdisp = nc.dram_tensor(pre + "xdisp", [NE * CAP, D], F32).ap()
        if fused and "shared" in ctx:
            ctx["shared"]["xdisp"] = xdisp
    ybuf = nc.dram_tensor(pre + "ybuf", [NE * CAP, D], BF16).ap()
    d_mod = Buf("modrows"); d_m = [Buf() for _ in range(n_tiles)]; d_h1 = [Buf() for _ in range(n_tiles)]
    d_x = Buf("xdisp"); d_y = [Buf() for _ in range(NE)]; d_out = [Buf() for _ in range(n_tiles)]
    d_xz = [] if reuse_x else [Buf() for _ in range(NE * CAP // 512)]

    with ExitStack() as st0:
        if fused:
            fw = ctx["fw"]; fw.stack = st0; fw.ps_pool = list(range(8))
        else:
            fw = FW(nc, st0)
        pe, dve, act, pool, sp = fw.pe, fw.dve, fw.act, fw.pool, fw.sp
        ident = fw.sb([128, 128], F32, "ident"); b_ident = Buf()
        fw.dma(sp, ident[:], ident_d, writes=[b_ident])
        trilt = fw.sb([128, 128], BF16, "trilt"); b_tri = Buf()
        fw.dma(pool, trilt[:], trilt_d, writes=[b_tri])
        ones_bf = fw.sb([128, 128], BF16, "ones_bf"); b_ones = Buf()
        fw.op(pool, lambda e: e.memset(ones_bf[:, :], 1.0), writes=[b_ones])
        iota = fw.sb([128, 64], F32, "iota"); b_iota = Buf()
        fw.dma(sp, iota[:], bview(iota_d), writes=[b_iota])
        zcol = fw.sb([128, 1], F32, "zcol"); b_z = Buf()
        fw.op(pool, lambda e: e.memset(zcol[:, :], 0.0), writes=[b_z])
        dest_all = fw.sb([128, n_tiles, 2], I32, "dest_all"); b_dest = [Buf() for _ in range(n_tiles)]
        gate_all = fw.sb([128, n_tiles, 2], F32, "gate_all"); b_gate = [Buf() for _ in range(n_tiles)]
        with ExitStack() as st:
            fw.stack = st
            if not reuse_x:
                zt = fw.sb([128, 8192], F32, "zt"); b_zt = Buf()
                fw.op(pool, lambda e: e.memset(zt[:, :], 0.0), writes=[b_zt])
                xv = xdisp.rearrange("(a p r) d -> a p (r d)", p=128, r=4)
                for a in range(NE * CAP // 512):
                    fw.dma(sp, xv[a], zt[:, :], reads=[b_zt], writes=[d_xz[a]])
            cv = fw.sb([2, D], F32, "cv"); b_cv = Buf()
            fw.dma(sp, cv[:], cvec, writes=[b_cv])
            siluT = fw.sb([128, 16, 2], BF16, "siluT"); b_sT = Buf()
            pt, pb = fw.next_psum()
            for kc in range(16):
                fw.op(pe, lambda e, kc=kc: e.transpose(pt[:, 2 * kc:2 * kc + 2], cv[0:2, kc * 128:(kc + 1) * 128], ident[0:2, 0:2]),
                      reads=[b_cv, b_ident], writes=[pb])
            fw.op(act, lambda e: e.activation(siluT[:, :, :].rearrange("p a b -> p (a b)"), pt[:, 0:32], AF.Silu), reads=[pb], writes=[b_sT])
            adab = fw.sb([2, 8192], F32, "adab"); b_adab = Buf()
            fw.dma(sp, adab[:], bview(ada_b, 2), writes=[b_adab])
            wts = [fw.sb([128, 16, 512], BF16, "adaw%d" % i) for i in range(2)]
            b_wts = [Buf() for _ in range(2)]
            modsb = fw.sb([2, 8192], F32, "modsb"); b_modsb = Buf()
            for j in range(16):
                wt, bw = wts[j % 2], b_wts[j % 2]
                fw.dma(pool, wt[:], ada_w[:, j * 512:(j + 1) * 512].rearrange("(kc p) c -> p kc c", p=128), writes=[bw])
                pt, pb = fw.next_psum()
                for kc in range(16):
                    fw.op(pe, lambda e, kc=kc, wt=wt, pt=pt: e.matmul(pt[0:2, :], siluT[:, kc, :], wt[:, kc, :], start=(kc == 0), stop=(kc == 15)),
                          reads=[b_sT, bw], writes=[pb])
                fw.op(dve, lambda e, pt=pt, j=j: e.tensor_tensor(modsb[:, j * 512:(j + 1) * 512], pt[0:2, :], adab[:, j * 512:(j + 1) * 512], ALU.add),
                      reads=[pb, b_adab], writes=[b_modsb])
            fw.dma(sp, modrows, modsb[:, :], reads=[b_modsb], writes=[d_mod])
            fw.barrier()
        with ExitStack() as st:
            fw.stack = st
            mixT = fw.sb([128, n_tiles, 32, 128], BF16, "mixT"); b_mixT = [Buf() for _ in range(n_tiles)]
            mts = [fw.sb([128, 4096], F32, "mixt%d" % i) for i in range(2)]; b_mts = [Buf() for _ in range(2)]
            if fused:
                parts = ctx["mix_src"]
                gix = fw.sb([128, n_tiles, 2], I32, "gix"); b_gix = Buf()
                fw.dma(sp, gix[:], gidx_d, writes=[b_gix])
                m16 = [fw.sb([128, 1024], BF16, "m16_%d" % i) for i in range(8)]; b_m16 = [Buf() for _ in range(8)]
                mcnt = 0
            for t in range(n_tiles):
                mt, bm = mts[t % 2], b_mts[t % 2]
                if not fused:
                    fw.dma(sp, mt[:], mix_in[t * 128:(t + 1) * 128, :], writes=[bm])
                else:
                    for k in range(2):
                        for a_, (gsrc, gbufs, pc0) in enumerate(parts):
                            pw = gsrc.shape[1]
                            mm, bmm = m16[mcnt % 8], b_m16[mcnt % 8]; mcnt += 1
                            fw.dma(pool, None, None, reads=gbufs + [b_gix], writes=[bmm],
                                   fn=lambda e: e.indirect_dma_start(
                                       out=mm[:, 0:pw], out_offset=None, in_=gsrc[:, :],
                                       in_offset=bass.IndirectOffsetOnAxis(ap=gix[:, t, k:k + 1], axis=0),
                                       bounds_check=fw.bound_reg(gsrc.shape[0] - 1), oob_is_err=False))
                            c0_ = k * 2048 + pc0
                            if a_ % 2 == 0:
                                fw.op(act, lambda e: e.copy(mt[:, c0_:c0_ + pw], mm[:, 0:pw]), reads=[bmm], writes=[bm])
                            else:
                                fw.op(dve, lambda e: e.tensor_copy(mt[:, c0_:c0_ + pw], mm[:, 0:pw]), reads=[bmm], writes=[bm])
                for q in range(8):
                    pt, pb = fw.next_psum()
                    for i in range(4):
                        kc = q * 4 + i
                        fw.op(pe, lambda e, pt=pt, i=i, kc=kc, mt=mt: e.transpose(pt[:, i * 128:(i + 1) * 128], mt[:, kc * 128:(kc + 1) * 128], ident[:, :]),
                              reads=[bm, b_ident], writes=[pb])
                    eng = act if q % 2 == 0 else dve
                    if eng is act:
                        fw.op(act, lambda e, pt=pt, t=t, q=q: e.copy(mixT[:, t, q * 4:(q + 1) * 4, :].rearrange("p a b -> p (a b)"), pt[:, :]),
                              reads=[pb], writes=[b_mixT[t]])
                    else:
                        fw.op(dve, lambda e, pt=pt, t=t, q=q: e.tensor_copy(mixT[:, t, q * 4:(q + 1) * 4, :].rearrange("p a b -> p (a b)"), pt[:, :]),
                              reads=[pb], writes=[b_mixT[t]])
            ows = [fw.sb([128, 32, 256], BF16, "ow%d" % i) for i in range(2)]; b_ows = [Buf() for _ in range(2)]
            mbs = [fw.sb([128, 256], F32, "mb%d" % i) for i in range(3)]; b_mbs = [Buf() for _ in range(3)]
            cnt = 0
            for j in range(8):
                ow, bo = ows[j % 2], b_ows[j % 2]
                fw.dma(pool, ow[:], out_w[:, j * 256:(j + 1) * 256].rearrange("(kc p) c -> p kc c", p=128), writes=[bo])
                for t in range(n_tiles):
                    pt, pb = fw.next_psum()
                    for kc in range(32):
                        fw.op(pe, lambda e, pt=pt, kc=kc, t=t, ow=ow: e.matmul(pt[:, 0:256], mixT[:, t, kc, :], ow[:, kc, :], start=(kc == 0), stop=(kc == 31)),
                              reads=[b_mixT[t], bo], writes=[pb])
                    mb, bmb = mbs[cnt % 3], b_mbs[cnt % 3]; cnt += 1
                    if cnt % 2:
                        fw.op(act, lambda e, pt=pt, mb=mb: e.copy(mb[:, :], pt[:, 0:256]), reads=[pb], writes=[bmb])
                    else:
                        fw.op(dve, lambda e, pt=pt, mb=mb: e.tensor_copy(mb[:, :], pt[:, 0:256]), reads=[pb], writes=[bmb])
                    fw.dma(sp, m_all[t * 128:(t + 1) * 128, j * 256:(j + 1) * 256], mb[:, :], reads=[bmb], writes=[d_m[t]])
            fw.barrier()
        with ExitStack() as st:
            fw.stack = st
            nvar = 2 if has_ctx else 1
            g1 = []; nsc = []; sh2 = []
            b_g1 = []; b_nsc = []; b_sh2 = []
            nw = fw.sb([128, D], F32, "nw"); b_nw = Buf()
            fw.dma(sp, nw[:], bview(norm_w2), writes=[b_nw])
            for v in range(nvar):
                a = fw.sb([128, D], F32, "g1_%d" % v); ba = Buf()
                fw.dma(sp, a[:], bview(modrows[v:v + 1, 0:2048]), reads=[d_mod], writes=[ba])
                g1.append(a); b_g1.append(ba)
                s = fw.sb([128, D], F32, "sh2_%d" % v); bs = Buf()
                fw.dma(sp, s[:], bview(modrows[v:v + 1, 2048:4096]), reads=[d_mod], writes=[bs])
                sh2.append(s); b_sh2.append(bs)
                c = fw.sb([128, D], F32, "nsc_%d" % v); bc = Buf()
                fw.dma(sp, c[:], bview(modrows[v:v + 1, 4096:6144]), reads=[d_mod], writes=[bc])
                fw.op(dve, lambda e, c=c: e.scalar_tensor_tensor(c[:, :], c[:, :], 1.0, nw[:, :], ALU.add, ALU.mult), reads=[b_nw], writes=[bc])
                nsc.append(c); b_nsc.append(bc)
            gw = fw.sb([128, 16, 72], F32, "gw"); b_gw = Buf()
            fw.dma(sp, gw[:], gwew.rearrange("(kc p) c -> p kc c", p=128), writes=[b_gw])
            acum = fw.sb([128, 64], F32, "acum"); b_acum = Buf()
            fw.op(pool, lambda e: e.memset(acum[:, :], 0.0), writes=[b_acum])
            acum_bf = fw.sb([128, 64], BF16, "acum_bf"); b_acbf = Buf()
            fw.op(pool, lambda e: e.memset(acum_bf[:, :], 0.0), writes=[b_acbf])
            NB = 2
            hts = [fw.sb([128, D], F32, "ht%d" % i) for i in range(NB)]; b_hts = [Buf() for _ in range(NB)]
            mts = [fw.sb([128, D], F32, "mt%d" % i) for i in range(NB)]; b_mts = [Buf() for _ in range(NB)]
            fts = [fw.sb([128, D], F32, "ft%d" % i) for i in range(NB)]; b_fts = [Buf() for _ in range(NB)]
            fTs = [fw.sb([128, 16, 128], F32, "fT%d" % i) for i in range(NB)]; b_fTs = [Buf() for _ in range(NB)]
            sq = fw.sb([128, D], F32, "sq"); b_sq = Buf()
            sm = {}
            def S(name, shape, dt=F32):
                if name not in sm:
                    sm[name] = (fw.sb(shape, dt, "r_" + name), Buf(name))
                return sm[name]
            for t in range(n_tiles):
                v = 1 if (has_ctx and t == 0) else 0
                ht, bh = hts[t % NB], b_hts[t % NB]
                mt, bm = mts[t % NB], b_mts[t % NB]
                ft, bf = fts[t % NB], b_fts[t % NB]
                fT, bfT = fTs[t % NB], b_fTs[t % NB]
                fw.dma(sp, ht[:], h_in[t * 128:(t + 1) * 128, :], reads=h_bufs, writes=[bh])
                fw.dma(sp, mt[:], m_all[t * 128:(t + 1) * 128, :], reads=[d_m[t]], writes=[bm])
                fw.op(pool, lambda e, mt=mt, v=v: e.tensor_tensor(mt[:, :], mt[:, :], g1[v][:, :], ALU.mult), reads=[b_g1[v]], writes=[bm])
                fw.op(dve, lambda e, mt=mt, ht=ht: e.tensor_tensor(ht[:, :], mt[:, :], ht[:, :], ALU.add), reads=[bm], writes=[bh])
                fw.dma(sp, h1_all[t * 128:(t + 1) * 128, :], ht[:, :], reads=[bh], writes=[d_h1[t]])
                ssq, b_ssq = S("ssq", [128, 1])
                fw.op(act, lambda e, ht=ht: e.activation(sq[:, :], ht[:, :], AF.Square, accum_out=ssq[:, :]), reads=[bh], writes=[b_sq, b_ssq])
                rstd, b_rstd = S("rstd", [128, 1])
                fw.op(act, lambda e: e.activation(rstd[:, :], ssq[:, :], AF.Sqrt, bias=EPS, scale=1.0 / D), reads=[b_ssq], writes=[b_rstd])
                fw.op(dve, lambda e: e.reciprocal(rstd[:, :], rstd[:, :]), writes=[b_rstd])
                fw.op(dve, lambda e, ht=ht, ft=ft, v=v: e.scalar_tensor_tensor(ft[:, :], ht[:, :], rstd[:, 0:1], nsc[v][:, :], ALU.mult, ALU.mult),
                      reads=[bh, b_rstd, b_nsc[v]], writes=[bf])
                fw.op(pool, lambda e, ft=ft, v=v: e.tensor_tensor(ft[:, :], ft[:, :], sh2[v][:, :], ALU.add), reads=[b_sh2[v]], writes=[bf])
                for q in range(4):
                    pt, pb = fw.next_psum()
                    for i in range(4):
                        kc = q * 4 + i
                        fw.op(pe, lambda e, pt=pt, i=i, kc=kc, ft=ft: e.transpose(pt[:, i * 128:(i + 1) * 128], ft[:, kc * 128:(kc + 1) * 128], ident[:, :]),
                              reads=[bf, b_ident], writes=[pb])
                    if q % 2 == 0:
                        fw.op(act, lambda e, pt=pt, q=q, fT=fT: e.copy(fT[:, q * 4:(q + 1) * 4, :].rearrange("p a b -> p (a b)"), pt[:, :]), reads=[pb], writes=[bfT])
                    else:
                        fw.op(dve, lambda e, pt=pt, q=q, fT=fT: e.tensor_copy(fT[:, q * 4:(q + 1) * 4, :].rearrange("p a b -> p (a b)"), pt[:, :]), reads=[pb], writes=[bfT])
                pl, pbl = fw.next_psum()
                for kc in range(16):
                    fw.op(pe, lambda e, kc=kc, fT=fT, pl=pl: e.matmul(pl[:, 0:72], fT[:, kc, :], gw[:, kc, :], start=(kc == 0), stop=(kc == 15)),
                          reads=[bfT, b_gw], writes=[pbl])
                lg, b_lg = S("lg", [128, 72])
                fw.op(dve, lambda e, pl=pl: e.tensor_copy(lg[:, :], pl[:, 0:72]), reads=[pbl], writes=[b_lg])
                g8, b_g8 = S("g8", [128, 8])
                fw.op(dve, lambda e: e.max(g8[:, :], lg[:, 0:8]), reads=[b_lg], writes=[b_g8])
                ngm, b_ngm = S("ngm", [128, 1])
                fw.op(dve, lambda e: e.tensor_scalar(ngm[:, :], g8[:, 0:1], -1.0, None, ALU.mult), reads=[b_g8], writes=[b_ngm])
                gex, b_gex = S("gex", [128, 8]); gsum, b_gsum = S("gsum", [128, 1])
                fw.op(act, lambda e: e.activation(gex[:, :], lg[:, 0:8], AF.Exp, bias=ngm[:, 0:1], scale=1.0, accum_out=gsum[:, :]),
                      reads=[b_lg, b_ngm], writes=[b_gex, b_gsum])
                ggate, b_gg = S("ggate", [128, 1])
                fw.op(dve, lambda e: e.reciprocal(ggate[:, :], gsum[:, :]), reads=[b_gsum], writes=[b_gg])
                pen, b_pen = S("pen", [128, 8])
                fw.op(dve, lambda e: e.tensor_scalar(pen[:, :], lg[:, 0:8], g8[:, 0:1], zcol[:, 0:1], ALU.is_equal, ALU.add), reads=[b_lg, b_g8, b_z], writes=[b_pen])
                fw.op(dve, lambda e: e.tensor_scalar(pen[:, :], pen[:, :], -1.0, 1e9, ALU.add, ALU.mult), writes=[b_pen])
                lem, b_lem = S("lem", [128, 64])
                fw.op(dve, lambda e: e.tensor_tensor(lem[:, :].rearrange("p (g e) -> p g e", g=8), lg[:, 8:72].rearrange("p (g e) -> p g e", g=8),
                                                     pen[:, :].unsqueeze(2).to_broadcast([128, 8, 8]), ALU.add), reads=[b_lg, b_pen], writes=[b_lem])
                t8, b_t8 = S("t8", [128, 8]); i8, b_i8 = S("i8", [128, 8], U32)
                fw.op(dve, lambda e: e.max(t8[:, :], lem[:, :]), reads=[b_lem], writes=[b_t8])
                fw.op(dve, lambda e: e.max_index(i8[:, :], t8[:, :], lem[:, :]), reads=[b_lem, b_t8], writes=[b_i8])
                ef, b_ef = S("ef", [128, 2])
                fw.op(dve, lambda e: e.tensor_copy(ef[:, :], i8[:, 0:2]), reads=[b_i8], writes=[b_ef])
                dd, b_dd = S("dd", [128, 1])
                fw.op(dve, lambda e: e.tensor_tensor(dd[:, :], t8[:, 1:2], t8[:, 0:1], ALU.subtract), reads=[b_t8], writes=[b_dd])
                ex, b_ex = S("ex", [128, 1])
                fw.op(act, lambda e: e.activation(ex[:, :], dd[:, :], AF.Exp), reads=[b_dd], writes=[b_ex])
                den, b_den = S("den", [128, 1])
                fw.op(dve, lambda e: e.tensor_scalar(den[:, :], ex[:, :], 1.0, None, ALU.add), reads=[b_ex], writes=[b_den])
                fw.op(dve, lambda e: e.reciprocal(den[:, :], den[:, :]), writes=[b_den])
                fw.op(dve, lambda e, t=t: e.tensor_tensor(gate_all[:, t, 0:1], den[:, :], ggate[:, :], ALU.mult), reads=[b_den, b_gg], writes=[b_gate[t]])
                fw.op(dve, lambda e, t=t: e.tensor_tensor(gate_all[:, t, 1:2], gate_all[:, t, 0:1], ex[:, :], ALU.mult), reads=[b_ex], writes=[b_gate[t]])
                A0, b_A0 = S("A0", [128, 64]); A1, b_A1 = S("A1", [128, 64]); A, b_A = S("A", [128, 64]); Abf, b_Abf = S("Abf", [128, 64], BF16)
                fw.op(dve, lambda e: e.tensor_scalar(A0[:, :], iota[:, :], ef[:, 0:1], None, ALU.is_equal), reads=[b_iota, b_ef], writes=[b_A0])
                fw.op(dve, lambda e: e.tensor_scalar(A1[:, :], iota[:, :], ef[:, 1:2], None, ALU.is_equal), reads=[b_iota, b_ef], writes=[b_A1])
                fw.op(dve, lambda e: e.tensor_tensor(A[:, :], A0[:, :], A1[:, :], ALU.add), reads=[b_A0, b_A1], writes=[b_A])
                fw.op(dve, lambda e: e.tensor_copy(Abf[:, :], A[:, :]), reads=[b_A], writes=[b_Abf])
                pr, pbr = fw.next_psum()
                fw.op(pe, lambda e, pr=pr: e.matmul(pr[:, 0:64], trilt[:, :], Abf[:, :], start=True, stop=False), reads=[b_tri, b_Abf], writes=[pbr])
                fw.op(pe, lambda e, pr=pr: e.matmul(pr[:, 0:64], ones_bf[:, :], acum_bf[:, :], start=False, stop=True), reads=[b_ones, b_acbf], writes=[pbr])
                rk, b_rk = S("rk", [128, 2]); tmp, b_tmp = S("tmp", [128, 64])
                for k, (Ak, bAk) in enumerate(((A0, b_A0), (A1, b_A1))):
                    fw.op(dve, lambda e, Ak=Ak, pr=pr: e.tensor_tensor(tmp[:, :], Ak[:, :], pr[:, 0:64], ALU.mult), reads=[bAk, pbr], writes=[b_tmp])
                    fw.op(dve, lambda e, k=k: e.reduce_sum(rk[:, k:k + 1], tmp[:, :], axis=AX.X), reads=[b_tmp], writes=[b_rk])
                fw.op(dve, lambda e: e.tensor_tensor(acum[:, :], acum[:, :], A[:, :], ALU.add), reads=[b_A], writes=[b_acum])
                fw.op(dve, lambda e: e.tensor_copy(acum_bf[:, :], acum[:, :]), reads=[b_acum], writes=[b_acbf])
                df, b_df = S("df", [128, 2]); ov, b_ov = S("ov", [128, 2])
                fw.op(dve, lambda e: e.tensor_scalar(ov[:, :], rk[:, :], float(CAP), 1e6, ALU.is_ge, ALU.mult), reads=[b_rk], writes=[b_ov])
                fw.op(dve, lambda e: e.scalar_tensor_tensor(df[:, :], ef[:, :], float(CAP), rk[:, :], ALU.mult, ALU.add), reads=[b_ef, b_rk], writes=[b_df])
                fw.op(dve, lambda e: e.tensor_tensor(df[:, :], df[:, :], ov[:, :], ALU.add), reads=[b_ov], writes=[b_df])
                fw.op(dve, lambda e, t=t: e.tensor_copy(dest_all[:, t, :], df[:, :]), reads=[b_df], writes=[b_dest[t]])
                for k in range(2):
                    fw.dma(pool, None, None, reads=[bf, b_dest[t]] + d_xz, writes=[d_x],
                           fn=lambda e, t=t, k=k, ft=ft: e.indirect_dma_start(
                               out=xdisp[:, :], out_offset=bass.IndirectOffsetOnAxis(ap=dest_all[:, t, k:k + 1], axis=0),
                               in_=ft[:, :], in_offset=None, bounds_check=fw.bound_reg(NE * CAP - 1), oob_is_err=False))
            fw.barrier()
        with ExitStack() as st:
            fw.stack = st
            NW = 2
            w1s = [fw.sb([128, 16, 512], BF16, "w1s%d" % i) for i in range(NW)]; b_w1s = [Buf() for _ in range(NW)]
            w3s = [fw.sb([128, 16, 512], BF16, "w3s%d" % i) for i in range(NW)]; b_w3s = [Buf() for _ in range(NW)]
            w2s = [fw.sb([128, 4, D], BF16, "w2s%d" % i) for i in range(NW)]; b_w2s = [Buf() for _ in range(NW)]
            xes = [fw.sb([128, D], F32, "xe%d" % i) for i in range(2)]; b_xes = [Buf() for _ in range(2)]
            xTs = [fw.sb([128, 16, 128], BF16, "xT%d" % i) for i in range(2)]; b_xTs = [Buf() for _ in range(2)]
            sil = fw.sb([128, 512], F32, "sil"); b_sil = Buf()
            hact = fw.sb([128, 512], F32, "hact"); b_hact = Buf()
            hT = fw.sb([128, 4, 128], BF16, "hT"); b_hT = Buf()
            yes = [fw.sb([128, D], BF16, "ye%d" % i) for i in range(2)]; b_yes = [Buf() for _ in range(2)]
            for ex_ in range(NE):
                i2 = ex_ % 2
                w1t, bw1 = w1s[ex_ % NW], b_w1s[ex_ % NW]
                w3t, bw3 = w3s[ex_ % NW], b_w3s[ex_ % NW]
                w2t, bw2 = w2s[ex_ % NW], b_w2s[ex_ % NW]
                fw.dma(pool, w1t[:], w1[ex_].rearrange("(kc p) h -> p kc h", p=128), writes=[bw1])
                fw.dma(pool, w3t[:], w3[ex_].rearrange("(kc p) h -> p kc h", p=128), writes=[bw3])
                fw.dma(pool, w2t[:], w2[ex_].rearrange("(hc p) d -> p hc d", p=128), writes=[bw2])
                xe, bxe = xes[i2], b_xes[i2]
                xT, bxT = xTs[i2], b_xTs[i2]
                fw.dma(sp, xe[:], xdisp[ex_ * CAP:(ex_ + 1) * CAP, :], reads=[d_x] + ([d_xz[ex_ // 4]] if d_xz else []), writes=[bxe])
                for q in range(4):
                    pt, pb = fw.next_psum()
                    for i in range(4):
                        kc = q * 4 + i
                        fw.op(pe, lambda e, pt=pt, i=i, kc=kc, xe=xe: e.transpose(pt[:, i * 128:(i + 1) * 128], xe[:, kc * 128:(kc + 1) * 128], ident[:, :]),
                              reads=[bxe, b_ident], writes=[pb])
                    if q % 2 == 0:
                        fw.op(act, lambda e, pt=pt, q=q, xT=xT: e.copy(xT[:, q * 4:(q + 1) * 4, :].rearrange("p a b -> p (a b)"), pt[:, :]), reads=[pb], writes=[bxT])
                    else:
                        fw.op(dve, lambda e, pt=pt, q=q, xT=xT: e.tensor_copy(xT[:, q * 4:(q + 1) * 4, :].rearrange("p a b -> p (a b)"), pt[:, :]), reads=[pb], writes=[bxT])
                p1, pb1 = fw.next_psum()
                for kc in range(16):
                    fw.op(pe, lambda e, kc=kc, p1=p1, xT=xT, w1t=w1t: e.matmul(p1[:, :], xT[:, kc, :], w1t[:, kc, :], start=(kc == 0), stop=(kc == 15)),
                          reads=[bxT, bw1], writes=[pb1])
                p3, pb3 = fw.next_psum()
                for kc in range(16):
                    fw.op(pe, lambda e, kc=kc, p3=p3, xT=xT, w3t=w3t: e.matmul(p3[:, :], xT[:, kc, :], w3t[:, kc, :], start=(kc == 0), stop=(kc == 15)),
                          reads=[bxT, bw3], writes=[pb3])
                fw.op(act, lambda e, p1=p1: e.activation(sil[:, :], p1[:, :], AF.Silu), reads=[pb1], writes=[b_sil])
                fw.op(dve, lambda e, p3=p3: e.tensor_tensor(hact[:, :], sil[:, :], p3[:, :], ALU.mult), reads=[b_sil, pb3], writes=[b_hact])
                pt, pb = fw.next_psum()
                for hc in range(4):
                    fw.op(pe, lambda e, pt=pt, hc=hc: e.transpose(pt[:, hc * 128:(hc + 1) * 128], hact[:, hc * 128:(hc + 1) * 128], ident[:, :]),
                          reads=[b_hact, b_ident], writes=[pb])
                fw.op(act, lambda e, pt=pt: e.copy(hT[:, :, :].rearrange("p a b -> p (a b)"), pt[:, :]), reads=[pb], writes=[b_hT])
                ye, bye = yes[i2], b_yes[i2]
                for db in range(4):
                    py, pby = fw.next_psum()
                    for hc in range(4):
                        fw.op(pe, lambda e, py=py, hc=hc, db=db, w2t=w2t: e.matmul(py[:, :], hT[:, hc, :], w2t[:, hc, db * 512:(db + 1) * 512], start=(hc == 0), stop=(hc == 3)),
                              reads=[b_hT, bw2], writes=[pby])
                    if db % 2 == 0:
                        fw.op(dve, lambda e, py=py, db=db, ye=ye: e.tensor_copy(ye[:, db * 512:(db + 1) * 512], py[:, :]), reads=[pby], writes=[bye])
                    else:
                        fw.op(act, lambda e, py=py, db=db, ye=ye: e.copy(ye[:, db * 512:(db + 1) * 512], py[:, :]), reads=[pby], writes=[bye])
                fw.dma(sp, ybuf[ex_ * CAP:(ex_ + 1) * CAP, :], ye[:, :], reads=[bye], writes=[d_y[ex_]])
            fw.barrier()
        with ExitStack() as st:
            fw.stack = st
            nvar = 2 if has_ctx else 1
            g2 = []; b_g2 = []
            for v in range(nvar):
                a = fw.sb([128, D], F32, "g2_%d" % v); ba = Buf()
                fw.dma(sp, a[:], bview(modrows[v:v + 1, 6144:8192]), reads=[d_mod], writes=[ba])
                g2.append(a); b_g2.append(ba)
            r0s = [fw.sb([128, D], BF16, "r0_%d" % i) for i in range(2)]; b_r0s = [Buf() for _ in range(2)]
            r1s = [fw.sb([128, D], BF16, "r1_%d" % i) for i in range(2)]; b_r1s = [Buf() for _ in range(2)]
            ycs = [fw.sb([128, D], F32, "yc_%d" % i) for i in range(2)]; b_ycs = [Buf() for _ in range(2)]
            h1s = [fw.sb([128, D], F32, "h1_%d" % i) for i in range(2)]; b_h1s = [Buf() for _ in range(2)]
            for t in range(n_tiles):
                v = 1 if (has_ctx and t == 0) else 0
                r0, br0 = r0s[t % 2], b_r0s[t % 2]
                r1, br1 = r1s[t % 2], b_r1s[t % 2]
                h1, bh1 = h1s[t % 2], b_h1s[t % 2]
                fw.dma(sp, h1[:], h1_all[t * 128:(t + 1) * 128, :], reads=[d_h1[t]], writes=[bh1])
                for k, (r, br) in enumerate(((r0, br0), (r1, br1))):
                    fw.op(pool, lambda e, r=r: e.memset(r[:, :], 0.0), writes=[br])
                    fw.dma(pool, None, None, reads=d_y + [b_dest[t]], writes=[br],
                           fn=lambda e, r=r, t=t, k=k: e.indirect_dma_start(
                               out=r[:, :], out_offset=None, in_=ybuf[:, :],
                               in_offset=bass.IndirectOffsetOnAxis(ap=dest_all[:, t, k:k + 1], axis=0),
                               bounds_check=fw.bound_reg(NE * CAP - 1), oob_is_err=False))
                yc, byc = ycs[t % 2], b_ycs[t % 2]
                fw.op(dve, lambda e: e.tensor_scalar(yc[:, :], r0[:, :], gate_all[:, t, 0:1], None, ALU.mult), reads=[br0, b_gate[t]], writes=[byc])
                fw.op(dve, lambda e: e.scalar_tensor_tensor(yc[:, :], r1[:, :], gate_all[:, t, 1:2], yc[:, :], ALU.mult, ALU.add),
                      reads=[br1, b_gate[t]], writes=[byc])
                fw.op(pool, lambda e: e.tensor_tensor(yc[:, :], yc[:, :], g2[v][:, :], ALU.mult), reads=[b_g2[v]], writes=[byc])
                fw.op(dve, lambda e: e.tensor_tensor(h1[:, :], h1[:, :], yc[:, :], ALU.add), reads=[byc], writes=[bh1])
                if out_fn is None:
                    fw.dma(sp, h_out[t * 128:(t + 1) * 128, :], h1[:, :], reads=[bh1], writes=[d_out[t]])
                else:
                    d_out[t] = out_fn(fw, t, h1, bh1)
            if not fused:
                fw.finish(d_out)
            else:
                fw.barrier()
        print("phaseB ops", fw.n_ops, "waits", fw.n_waits)
    if fused:
        return [b for x in d_out for b in (x if isinstance(x, list) else [x])]
    return nc

from contextlib import ExitStack

NT = 18
NTOK = NT * 128
Z0, X0, B0, C0, DT0, Q0, K0, V0, NCOL = 0, 1024, 2048, 2304, 2560, 2592, 3616, 4640, 5664
TB = [(0, 256)] + [(256 + i * 512, 512) for i in range(4)]


def build_phaseA0(stop_after=None, dbg=False, ctx=None):
    fused = ctx is not None
    nc = ctx["nc"] if fused else bass.Bass("TRN2", target_bir_lowering=False)
    pre = ctx["pre"] if fused else ""
    out_fn = ctx["out_fn"] if fused else None
    I = lambda name, shape, dt=F32: nc.dram_tensor(pre + name, shape, dt, kind="ExternalInput").ap()
    h_all = I("h_all", [NTOK, D])
    cvec = I("cvec", [2, D])
    ada_w = I("ada_w", [D, 4096]); ada_b = I("ada_b", [1, 4096])
    norm_w1 = I("norm_w1", [1, D])
    w_in = I("w_in", [D, NCOL])
    convw = I("convw", [1536, 5]); convb = I("convb", [128, 12])
    dt_bias = I("dt_bias", [1, 32]); a_log = I("a_log", [1, 32]); d_skip = I("d_skip", [1, 16]); ssd_nw = I("ssd_nw", [1, 1024])
    qnw = I("qnw", [128, 1]); knw = I("knw", [128, 1])
    biasT = I("biasT", [8, 25, 128, 128])
    ident_d = I("ident", [128, 128]); ule_d = I("ule", [128, 128]); uge_d = I("uge", [128, 128])
    mix = None if fused else nc.dram_tensor("mix_part", [NTOK, 2048], F32, kind="ExternalOutput").ap()
    modrows = nc.dram_tensor(pre + "modrows", [2, 4096], F32).ap()
    Hb_d = nc.dram_tensor(pre + "Hb_d", [NT, 128, 512], BF16).ap()
    d_mod = Buf("modrows"); d_mix = []

    with ExitStack() as st0:
        if fused:
            fw = ctx["fw"]; fw.stack = st0; fw.ps_pool = list(range(8))
        else:
            fw = FW(nc, st0)
        pe, dve, act, pool, sp = fw.pe, fw.dve, fw.act, fw.pool, fw.sp
        ident, b_ident = emit_consts(fw, ident_d)
        ule = fw.sb([128, 128], F32, "ule"); b_ule = Buf(); fw.dma(sp, ule[:], ule_d, writes=[b_ule])
        uge = fw.sb([128, 128], F32, "uge"); b_uge = Buf(); fw.dma(sp, uge[:], uge_d, writes=[b_uge])
        ones = fw.sb([128, 128], F32, "ones"); b_ones = Buf(); fw.op(pool, lambda e: e.memset(ones[:, :], 1.0), writes=[b_ones])
        identb = fw.sb([128, 128], BF16, "identb"); b_identb = Buf(); fw.dma(pool, identb[:], ident_d, writes=[b_identb])
        zcol = fw.sb([128, 1], F32, "zcol"); b_z = Buf(); fw.op(pool, lambda e: e.memset(zcol[:, :], 0.0), writes=[b_z])
        aT = fw.sb([128, 16, NTOK], BF16, "aT"); b_aT = [Buf() for _ in range(NT)]
        emit_mods(fw, cvec, ada_w, ada_b, 4096, modrows, d_mod, ident, b_ident)
        emit_aT(fw, h_all, NT, 2, norm_w1, modrows, d_mod, aT, b_aT, ident, b_ident)

        def tiles_of(s, n):
            return list(range(s // 128, (s + n) // 128))

        with ExitStack() as st:
            fw.stack = st
            wdt = fw.sb([128, 16, 32], BF16, "wdt"); b_wdt = Buf()
            fw.dma(pool, wdt[:], w_in[:, DT0:DT0 + 32].rearrange("(kc p) c -> p kc c", p=128), writes=[b_wdt])
            dtb = fw.sb([128, 32], F32, "dtb"); b_dtb = Buf(); fw.dma(sp, dtb[:], bview(dt_bias), writes=[b_dtb])
            Aneg = fw.sb([128, 32], F32, "Aneg"); b_A = Buf(); fw.dma(sp, Aneg[:], bview(a_log), writes=[b_A])
            fw.op(act, lambda e: e.activation(Aneg[:, :], Aneg[:, :], AF.Exp), writes=[b_A])
            fw.op(dve, lambda e: e.tensor_scalar(Aneg[:, :], Aneg[:, :], -1.0, None, ALU.mult), writes=[b_A])
            dsk = fw.sb([128, 16], F32, "dsk"); b_dsk = Buf(); fw.dma(sp, dsk[:], bview(d_skip), writes=[b_dsk])
            snw = fw.sb([128, 1024], F32, "snw"); b_snw = Buf(); fw.dma(sp, snw[:], bview(ssd_nw), writes=[b_snw])
            cw = fw.sb([128, 12, 5], F32, "cw"); b_cw = Buf(); fw.dma(sp, cw[:], convw.rearrange("(ct p) k -> p ct k", p=128), writes=[b_cw])
            cb = fw.sb([128, 12], F32, "cb"); b_cb = Buf(); fw.dma(sp, cb[:], convb, writes=[b_cb])
            def T3(name):
                return fw.sb([128, NT, 32], F32, name), [Buf() for _ in range(NT)]
            dt_all, b_dt = T3("dt_all"); a_all, b_a = T3("a_all"); negcs, b_ncs = T3("negcs")
            dfs, b_dfs = T3("dfs"); dte, b_dte = T3("dte"); etot, b_etot = T3("etot")
            tmpA = fw.sb([128, 32], F32, "tmpA"); b_tA = Buf(); tmpB = fw.sb([128, 32], F32, "tmpB"); b_tB = Buf()
            tmpC = fw.sb([128, 32], F32, "tmpC"); b_tC = Buf()
            for t in range(NT):
                pt, pb = fw.next_psum()
                for kc in range(16):
                    fw.op(pe, lambda e, kc=kc, pt=pt, t=t: e.matmul(pt[:, 0:32], aT[:, kc, t * 128:(t + 1) * 128], wdt[:, kc, :], start=(kc == 0), stop=(kc == 15)),
                          reads=[b_aT[t], b_wdt], writes=[pb])
                fw.op(dve, lambda e, pt=pt: e.tensor_tensor(tmpA[:, :], pt[:, 0:32], dtb[:, :], ALU.add), reads=[pb, b_dtb], writes=[b_tA])
                fw.op(act, lambda e: e.activation(tmpB[:, :], tmpA[:, :], AF.Abs), reads=[b_tA], writes=[b_tB])
                fw.op(act, lambda e: e.activation(tmpB[:, :], tmpB[:, :], AF.Exp, scale=-1.0), writes=[b_tB])
                fw.op(act, lambda e: e.activation(tmpB[:, :], tmpB[:, :], AF.Ln, bias=1.0, scale=1.0), writes=[b_tB])
                fw.op(dve, lambda e: e.tensor_scalar(tmpA[:, :], tmpA[:, :], 0.0, None, ALU.max), writes=[b_tA])
                fw.op(dve, lambda e, t=t: e.tensor_tensor(dt_all[:, t, :], tmpA[:, :], tmpB[:, :], ALU.add), reads=[b_tA, b_tB], writes=[b_dt[t]])
                fw.op(dve, lambda e, t=t: e.tensor_tensor(a_all[:, t, :], dt_all[:, t, :], Aneg[:, :], ALU.mult), reads=[b_dt[t], b_A], writes=[b_a[t]])
                pc_, pbc = fw.next_psum()
                fw.op(pe, lambda e, pc_=pc_, t=t: e.matmul(pc_[:, 0:16], ule[:, :], a_all[:, t, 0:16], start=True, stop=True), reads=[b_ule, b_a[t]], writes=[pbc])
                fw.op(pe, lambda e, pc_=pc_, t=t: e.matmul(pc_[:, 16:32], uge[:, :], a_all[:, t, 16:32], start=True, stop=True), reads=[b_uge, b_a[t]], writes=[pbc])
                fw.op(pe, lambda e, pc_=pc_, t=t: e.matmul(pc_[:, 32:64], ones[:, :], a_all[:, t, :], start=True, stop=True), reads=[b_ones, b_a[t]], writes=[pbc])
                fw.op(dve, lambda e, pc_=pc_, t=t: e.tensor_scalar(negcs[:, t, :], pc_[:, 0:32], -1.0, None, ALU.mult), reads=[pbc], writes=[b_ncs[t]])
                fw.op(act, lambda e, pc_=pc_, t=t: e.activation(dfs[:, t, :], pc_[:, 0:32], AF.Exp), reads=[pbc], writes=[b_dfs[t]])
                fw.op(act, lambda e, pc_=pc_, t=t: e.activation(etot[:, t, :], pc_[:, 32:64], AF.Exp), reads=[pbc], writes=[b_etot[t]])
                fw.op(dve, lambda e, pc_=pc_, t=t: e.tensor_tensor(tmpC[:, :], pc_[:, 32:64], negcs[:, t, :], ALU.add), reads=[pbc, b_ncs[t]], writes=[b_tC])
                fw.op(act, lambda e, t=t: e.activation(dte[:, t, :], tmpC[:, :], AF.Exp), reads=[b_tC], writes=[b_dte[t]])

            xs = fw.sb([128, NT, 512], BF16, "xs"); b_xs = [Buf() for _ in range(NT)]
            Btok = fw.sb([128, NT, 128], BF16, "Btok"); b_Btok = [Buf() for _ in range(NT)]
            BT = fw.sb([128, NTOK], BF16, "BT"); b_BT = Buf()
            CT = fw.sb([128, NTOK], BF16, "CT"); b_CT = Buf()
            d_Hb = [Buf() for _ in range(NT)]
            Hbs = [fw.sb([128, 512], BF16, "Hbs%d" % i) for i in range(2)]; b_Hbs = [Buf() for _ in range(2)]
            pcs = [fw.sb([128, 2312], F32, "pc%d" % i) for i in range(1)]; b_pcs = [Buf() for _ in range(1)]
            for i in range(1):
                fw.op(pool, lambda e, i=i: e.memset(pcs[i][:, :], 0.0), writes=[b_pcs[i]])
            acc = fw.sb([128, 2308], F32, "acc"); b_acc = Buf()
            wcs = [fw.sb([128, 16, 128], BF16, "wc%d" % i) for i in range(2)]; b_wcs = [Buf() for _ in range(2)]
            wz = fw.sb([128, 16, 512], BF16, "wz"); b_wz = Buf()
            H = fw.sb([128, 512], F32, "H"); b_H = Buf()
            Hbf = fw.sb([128, 512], BF16, "Hbf"); b_Hbf = Buf()
            coef = fw.sb([128, 8], F32, "coef"); b_coef = Buf()
            Xe = fw.sb([128, 512], BF16, "Xe"); b_Xe = Buf()
            Xtf = fw.sb([128, 512], BF16, "Xtf"); b_Xtf = Buf()
            Xtb = fw.sb([128, 512], BF16, "Xtb"); b_Xtb = Buf()
            CBf = fw.sb([128, 128], F32, "CBf"); b_CBf = Buf()
            CBb = fw.sb([128, 128], F32, "CBb"); b_CBb = Buf()
            Es = [fw.sb([128, 128], F32, "E%d" % i) for i in range(3)]; b_Es = [Buf() for _ in range(3)]
            Ls = [fw.sb([128, 128], F32, "L%d" % i) for i in range(3)]; b_Ls = [Buf() for _ in range(3)]
            Ms = [fw.sb([128, 128], BF16, "M%d" % i) for i in range(3)]; b_Ms = [Buf() for _ in range(3)]
            t1 = fw.sb([128, 512], F32, "t1"); b_t1 = Buf()
            t2 = fw.sb([128, 512], F32, "t2"); b_t2 = Buf()
            sz = fw.sb([128, 512], F32, "sz"); b_sz = Buf()
            gy = fw.sb([128, 512], F32, "gy"); b_gy = Buf()
            ssq = fw.sb([128, 1], F32, "ssq_s"); b_ssq = Buf()
            outs = [fw.sb([128, 512], F32, "o%d" % i) for i in range(2)]; b_outs = [Buf() for _ in range(2)]
            wcnt = 0
            lm = 0

            def bc8(ap):
                return ap.unsqueeze(2).to_broadcast([128, 8, 64])

            def v3(ap):
                return ap.rearrange("p (h d) -> p h d", h=8)

            for g in range(1 if dbg else 2):
                hf = g * 8
                hb = 16 + g * 8
                fw.dma(pool, wz[:], w_in[:, Z0 + g * 512:Z0 + (g + 1) * 512].rearrange("(kc p) c -> p kc c", p=128), writes=[b_wz])
                for ci in range(6):
                    if ci < 4:
                        col0 = X0 + g * 512 + ci * 128; ct = g * 4 + ci
                    elif ci == 4:
                        col0 = B0 + g * 128; ct = 8 + g
                    else:
                        col0 = C0 + g * 128; ct = 10 + g
                    wc, bwc = wcs[wcnt % 2], b_wcs[wcnt % 2]
                    pc, bpc = pcs[0], b_pcs[0]
                    wcnt += 1
                    fw.dma(pool, wc[:], w_in[:, col0:col0 + 128].rearrange("(kc p) c -> p kc c", p=128), writes=[bwc])
                    for (s0, n) in TB:
                        pt, pb = fw.next_psum()
                        for kc in range(16):
                            fw.op(pe, lambda e, kc=kc, pt=pt, wc=wc, s0=s0, n=n: e.matmul(pt[:, 0:n], wc[:, kc, :], aT[:, kc, s0:s0 + n], start=(kc == 0), stop=(kc == 15)),
                                  reads=[bwc] + [b_aT[t] for t in tiles_of(s0, n)], writes=[pb])
                        off = 2 if s0 == 0 else 6
                        fw.op(act, lambda e, pt=pt, pc=pc, s0=s0, n=n, off=off: e.copy(pc[:, s0 + off:s0 + off + n], pt[:, 0:n]), reads=[pb], writes=[bpc])
                    fw.op(dve, lambda e, pc=pc, ct=ct: e.tensor_scalar(acc[:, :], pc[:, 0:2308], cw[:, ct, 0:1], cb[:, ct:ct + 1], ALU.mult, ALU.add),
                          reads=[bpc, b_cw, b_cb], writes=[b_acc])
                    for k in range(1, 5):
                        eng = dve
                        fw.op(eng, lambda e, pc=pc, ct=ct, k=k: e.scalar_tensor_tensor(acc[:, :], pc[:, k:k + 2308], cw[:, ct, k:k + 1], acc[:, :], ALU.mult, ALU.add),
                              reads=[bpc, b_cw], writes=[b_acc])
                    if ci == 5:
                        fw.op(act, lambda e: e.activation(CT[:, 0:256], acc[:, 0:256], AF.Silu), reads=[b_acc], writes=[b_CT])
                        fw.op(act, lambda e: e.activation(CT[:, 256:NTOK], acc[:, 260:2308], AF.Silu), reads=[b_acc], writes=[b_CT])
                        continue
                    fw.op(act, lambda e: e.activation(acc[:, 0:256], acc[:, 0:256], AF.Silu), writes=[b_acc])
                    fw.op(act, lambda e: e.activation(acc[:, 260:2308], acc[:, 260:2308], AF.Silu), writes=[b_acc])
                    if ci == 4:
                        fw.op(pool, lambda e: e.tensor_copy(BT[:, 0:256], acc[:, 0:256]), reads=[b_acc], writes=[b_BT])
                        fw.op(pool, lambda e: e.tensor_copy(BT[:, 256:NTOK], acc[:, 260:2308]), reads=[b_acc], writes=[b_BT])
                    for q in range(5):
                        ts = list(range(q * 4, min(NT, q * 4 + 4)))
                        pt, pb = fw.next_psum()
                        for i, t in enumerate(ts):
                            a0 = t * 128 if t < 2 else 260 + (t - 2) * 128
                            fw.op(pe, lambda e, pt=pt, i=i, a0=a0: e.transpose(pt[:, i * 128:(i + 1) * 128], acc[:, a0:a0 + 128], ident[:, :]),
                                  reads=[b_acc, b_ident], writes=[pb])
                        n = len(ts)
                        src = pt[:, 0:n * 128].rearrange("p (a b) -> p a b", a=n)
                        if ci < 4:
                            dstv = xs[:, ts[0]:ts[0] + n, ci * 128:(ci + 1) * 128]; bl = [b_xs[t] for t in ts]
                        else:
                            dstv = Btok[:, ts[0]:ts[0] + n, :]; bl = [b_Btok[t] for t in ts]
                        if q % 2 == 0:
                            fw.op(dve, lambda e, dstv=dstv, src=src: e.tensor_copy(dstv, src), reads=[pb], writes=bl)
                        else:
                            fw.op(act, lambda e, dstv=dstv, src=src: e.copy(dstv, src), reads=[pb], writes=bl)

                fw.op(pool, lambda e: e.memset(H[:, :], 0.0), writes=[b_H])
                border = [1, 0] + list(range(17, 1, -1))
                for bi_, c in enumerate(border):
                    hs_, bhs_ = Hbs[bi_ % 2], b_Hbs[bi_ % 2]
                    fw.op(act, lambda e, hs_=hs_: e.copy(hs_[:, :], H[:, :]), reads=[b_H], writes=[bhs_])
                    fw.dma(sp, Hb_d[c], hs_[:, :], reads=[bhs_], writes=[d_Hb[c]])
                    fw.op(dve, lambda e, c=c: e.tensor_tensor(coef[:, :], dt_all[:, c, hb:hb + 8], dte[:, c, hb:hb + 8], ALU.mult), reads=[b_dt[c], b_dte[c]], writes=[b_coef])
                    fw.op(dve, lambda e, c=c: e.tensor_tensor(v3(Xe[:, :]), v3(xs[:, c, :]), bc8(coef[:, :]), ALU.mult), reads=[b_xs[c], b_coef], writes=[b_Xe])
                    ps, pbs = fw.next_psum()
                    fw.op(pe, lambda e, ps=ps, c=c: e.matmul(ps[:, :], Btok[:, c, :], Xe[:, :], start=True, stop=True), reads=[b_Btok[c], b_Xe], writes=[pbs])
                    fw.op(pool, lambda e, c=c: e.tensor_tensor(v3(H[:, :]), v3(H[:, :]), bc8(etot[:, c, hb:hb + 8]), ALU.mult), reads=[b_etot[c]], writes=[b_H])
                    fw.op(dve, lambda e, ps=ps: e.tensor_tensor(H[:, :], H[:, :], ps[:, :], ALU.add), reads=[pbs], writes=[b_H])

                fw.op(pool, lambda e: e.memset(H[:, :], 0.0), writes=[b_H])
                for c in range(NT):
                    cs_ = slice(c * 128, (c + 1) * 128)
                    hs_, bhs_ = Hbs[c % 2], b_Hbs[c % 2]
                    fw.dma(sp, hs_[:, :], Hb_d[c], reads=[d_Hb[c]], writes=[bhs_])
                    fw.op(act, lambda e: e.copy(Hbf[:, :], H[:, :]), reads=[b_H], writes=[b_Hbf])
                    fw.op(dve, lambda e, c=c: e.tensor_tensor(coef[:, :], dt_all[:, c, hf:hf + 8], dte[:, c, hf:hf + 8], ALU.mult), reads=[b_dt[c], b_dte[c]], writes=[b_coef])
                    fw.op(dve, lambda e, c=c: e.tensor_tensor(v3(Xe[:, :]), v3(xs[:, c, :]), bc8(coef[:, :]), ALU.mult), reads=[b_xs[c], b_coef], writes=[b_Xe])
                    fw.op(pool, lambda e, c=c: e.tensor_tensor(v3(Xtf[:, :]), v3(xs[:, c, :]), bc8(dt_all[:, c, hf:hf + 8]), ALU.mult), reads=[b_xs[c], b_dt[c]], writes=[b_Xtf])
                    fw.op(pool, lambda e, c=c: e.tensor_tensor(v3(Xtb[:, :]), v3(xs[:, c, :]), bc8(dt_all[:, c, hb:hb + 8]), ALU.mult), reads=[b_xs[c], b_dt[c]], writes=[b_Xtb])
                    pyf, pbyf = fw.next_psum()
                    fw.op(pe, lambda e, pyf=pyf, cs_=cs_: e.matmul(pyf[:, :], CT[:, cs_], Hbf[:, :], start=True, stop=True), reads=[b_CT, b_Hbf], writes=[pbyf])
                    pyb, pbyb = fw.next_psum()
                    fw.op(pe, lambda e, pyb=pyb, cs_=cs_, hs_=hs_: e.matmul(pyb[:, :], CT[:, cs_], hs_[:, :], start=True, stop=True), reads=[b_CT, bhs_], writes=[pbyb])
                    fw.op(dve, lambda e, pyf=pyf, c=c: e.tensor_tensor(v3(t1[:, :]), v3(pyf[:, :]), bc8(dfs[:, c, hf:hf + 8]), ALU.mult), reads=[pbyf, b_dfs[c]], writes=[b_t1])
                    fw.op(dve, lambda e, pyb=pyb, c=c: e.tensor_tensor(v3(t2[:, :]), v3(pyb[:, :]), bc8(dfs[:, c, hb:hb + 8]), ALU.mult), reads=[pbyb, b_dfs[c]], writes=[b_t2])
                    fw.op(pool, lambda e: e.tensor_tensor(t1[:, :], t1[:, :], t2[:, :], ALU.add), reads=[b_t2], writes=[b_t1])
                    fw.op(pool, lambda e, c=c: e.tensor_tensor(v3(t2[:, :]), v3(xs[:, c, :]), bc8(dsk[:, g * 8:(g + 1) * 8]), ALU.mult), reads=[b_xs[c], b_dsk], writes=[b_t2])
                    fw.op(pool, lambda e: e.tensor_tensor(t1[:, :], t1[:, :], t2[:, :], ALU.add), reads=[b_t2], writes=[b_t1])
                    ps, pbs = fw.next_psum()
                    fw.op(pe, lambda e, ps=ps, c=c: e.matmul(ps[:, :], Btok[:, c, :], Xe[:, :], start=True, stop=True), reads=[b_Btok[c], b_Xe], writes=[pbs])
                    fw.op(pool, lambda e, c=c: e.tensor_tensor(v3(H[:, :]), v3(H[:, :]), bc8(etot[:, c, hf:hf + 8]), ALU.mult), reads=[b_etot[c], b_Hbf], writes=[b_H])
                    fw.op(dve, lambda e, ps=ps: e.tensor_tensor(H[:, :], H[:, :], ps[:, :], ALU.add), reads=[pbs], writes=[b_H])
                    pcb, pbcb = fw.next_psum()
                    fw.op(pe, lambda e, pcb=pcb, cs_=cs_: e.matmul(pcb[:, 0:128], BT[:, cs_], CT[:, cs_], start=True, stop=True), reads=[b_BT, b_CT], writes=[pbcb])
                    fw.op(dve, lambda e, pcb=pcb: e.tensor_tensor(CBf[:, :], pcb[:, 0:128], ule[:, :], ALU.mult), reads=[pbcb, b_ule], writes=[b_CBf])
                    fw.op(dve, lambda e, pcb=pcb: e.tensor_tensor(CBb[:, :], pcb[:, 0:128], uge[:, :], ALU.mult), reads=[pbcb, b_uge], writes=[b_CBb])
                    pyd, pbyd = fw.next_psum()
                    prs = {}

                    def emitR(hq):
                        pr, pbr = fw.next_psum()
                        combos = [(h, d_) for h in (2 * hq, 2 * hq + 1) for d_ in (0, 1)]
                        for i, (h, d_) in enumerate(combos):
                            col = (hf if d_ == 0 else hb) + h
                            U = ule if d_ == 0 else uge
                            bU = b_ule if d_ == 0 else b_uge
                            fw.op(pe, lambda e: e.matmul(pr[:, i * 128:(i + 1) * 128], a_all[:, c, col:col + 1].to_broadcast([128, 128]), U[:, :], start=True, stop=True),
                                  reads=[b_a[c], bU], writes=[pbr])
                        prs[hq] = (pr, pbr, combos)
                    emitR(0)
                    pz, pbz = fw.next_psum()
                    for kc in range(16):
                        fw.op(pe, lambda e: e.matmul(pz[:, :], aT[:, kc, cs_], wz[:, kc, :], start=(kc == 0), stop=(kc == 15)),
                              reads=[b_aT[c], b_wz], writes=[pbz])
                    fw.op(act, lambda e: e.activation(sz[:, :], pz[:, :], AF.Silu), reads=[pbz], writes=[b_sz])
                    for hq in range(4):
                        if hq + 1 < 4:
                            emitR(hq + 1)
                        pr, pbr, combos = prs[hq]
                        for i, (h, d_) in enumerate(combos):
                            col = (hf if d_ == 0 else hb) + h
                            E_, bE = Es[lm % 3], b_Es[lm % 3]; L_, bL = Ls[lm % 3], b_Ls[lm % 3]; M_, bM = Ms[lm % 3], b_Ms[lm % 3]; lm += 1
                            CB_, bCB = (CBf, b_CBf) if d_ == 0 else (CBb, b_CBb)
                            Xt_, bXt = (Xtf, b_Xtf) if d_ == 0 else (Xtb, b_Xtb)
                            fw.op(dve, lambda e: e.tensor_scalar(E_[:, :], pr[:, i * 128:(i + 1) * 128], negcs[:, c, col:col + 1], zcol[:, 0:1], ALU.add, ALU.min),
                                  reads=[pbr, b_ncs[c], b_z], writes=[bE])
                            fw.op(act, lambda e: e.activation(L_[:, :], E_[:, :], AF.Exp), reads=[bE], writes=[bL])
                            fw.op(pool, lambda e: e.tensor_tensor(M_[:, :], L_[:, :], CB_[:, :], ALU.mult), reads=[bL, bCB], writes=[bM])
                            fw.op(pe, lambda e: e.matmul(pyd[:, h * 64:(h + 1) * 64], M_[:, :], Xt_[:, h * 64:(h + 1) * 64], start=(d_ == 0), stop=(d_ == 1)),
                                  reads=[bM, bXt], writes=[pbyd])
                    if dbg and c == NT - 1:
                        dd = {}
                        for nm, pp, pbb in (("pyd", pyd, pbyd),):
                            tt = fw.sb([128, 512], F32, "dbg_" + nm); bb = Buf()
                            fw.op(dve, lambda e, tt=tt, pp=pp: e.tensor_copy(tt[:, :], pp[:, :]), reads=[pbb], writes=[bb])
                            dd[nm] = (tt, bb)
                        fw.dbgd = dd
                    fw.op(dve, lambda e, pyd=pyd: e.tensor_tensor(t1[:, :], t1[:, :], pyd[:, :], ALU.add), reads=[pbyd], writes=[b_t1])
                    fw.op(dve, lambda e: e.tensor_tensor(gy[:, :], t1[:, :], sz[:, :], ALU.mult), reads=[b_t1, b_sz], writes=[b_gy])
                    fw.op(act, lambda e: e.activation(t2[:, :], gy[:, :], AF.Square, accum_out=ssq[:, :]), reads=[b_gy], writes=[b_t2, b_ssq])
                    fw.op(act, lambda e: e.activation(ssq[:, :], ssq[:, :], AF.Sqrt, bias=EPS, scale=1.0 / 512), writes=[b_ssq])
                    fw.op(dve, lambda e: e.reciprocal(ssq[:, :], ssq[:, :]), writes=[b_ssq])
                    o_, bo = outs[c % 2], b_outs[c % 2]
                    fw.op(dve, lambda e, o_=o_: e.scalar_tensor_tensor(o_[:, :], gy[:, :], ssq[:, 0:1], snw[:, g * 512:(g + 1) * 512], ALU.mult, ALU.mult),
                          reads=[b_gy, b_ssq, b_snw], writes=[bo])
                    if fused:
                        d_mix.extend(out_fn(fw, c * 128, g * 512, 512, o_, bo))
                    else:
                        db = Buf(); d_mix.append(db)
                        fw.dma(sp, mix[c * 128:(c + 1) * 128, g * 512:(g + 1) * 512], o_[:, :], reads=[bo], writes=[db])
            if dbg:
                def dump(name, ap, shape, dt, bufs):
                    o = nc.dram_tensor("dbg_" + name, shape, dt, kind="ExternalOutput").ap()
                    db = Buf(); d_mix.append(db)
                    fw.dma(sp, o, ap, reads=bufs, writes=[db])
                dump("aT0", aT[:, 0, :], [128, NTOK], BF16, b_aT)
                dump("dt", dt_all[:, :, :], [128, NT, 32], F32, b_dt)
                dump("negcs", negcs[:, :, :], [128, NT, 32], F32, b_ncs)
                dump("dfs", dfs[:, :, :], [128, NT, 32], F32, b_dfs)
                dump("dte", dte[:, :, :], [128, NT, 32], F32, b_dte)
                dump("etot", etot[:, :, :], [128, NT, 32], F32, b_etot)
                dump("xs", xs[:, :, :], [128, NT, 512], BF16, b_xs)
                dump("Btok", Btok[:, :, :], [128, NT, 128], BF16, b_Btok)
                dump("BT", BT[:, :], [128, NTOK], BF16, [b_BT])
                dump("CT", CT[:, :], [128, NTOK], BF16, [b_CT])
                dump("H", H[:, :], [128, 512], F32, [b_H])
                dump("t1", t1[:, :], [128, 512], F32, [b_t1])
                dump("gy", gy[:, :], [128, 512], F32, [b_gy])
                for nm, (tt, bb) in fw.dbgd.items():
                    dump(nm, tt[:, :], [128, 512], F32, [bb])
                dump("CBf", CBf[:, :], [128, 128], F32, [b_CBf])
                dump("CBb", CBb[:, :], [128, 128], F32, [b_CBb])
                dump("Elast", Es[(lm - 1) % 3][:, :], [128, 128], F32, [b_Es[(lm - 1) % 3]])
                dump("Llast", Ls[(lm - 1) % 3][:, :], [128, 128], F32, [b_Ls[(lm - 1) % 3]])
                dump("Mlast", Ms[(lm - 1) % 3][:, :], [128, 128], BF16, [b_Ms[(lm - 1) % 3]])
                dump("Xtf", Xtf[:, :], [128, 512], BF16, [b_Xtf])
                dump("Xtb", Xtb[:, :], [128, 512], BF16, [b_Xtb])
            fw.barrier()
        if fused and ctx.get("mid_hook"):
            ctx["mid_hook"](0)

        if stop_after != "ssd":
          with ExitStack() as st:
            fw.stack = st
            V1 = fw.sb([128, NT, 8, 129], BF16, "V1"); b_V1 = [Buf() for _ in range(NT)]
            fw.op(pool, lambda e: e.memset(V1[:, :, :, :].rearrange("p a b c -> p (a b c)"), 1.0), writes=b_V1)
            with ExitStack() as st2:
                fw.stack = st2
                wv = fw.sb([128, 16, 1024], BF16, "wv"); b_wv = Buf()
                fw.dma(pool, wv[:], w_in[:, V0:V0 + 1024].rearrange("(kc p) c -> p kc c", p=128), writes=[b_wv])
                for t in range(NT):
                    for hh_ in range(2):
                        pt, pb = fw.next_psum()
                        for kc in range(16):
                            fw.op(pe, lambda e, kc=kc, pt=pt, t=t, hh_=hh_: e.matmul(pt[:, :], aT[:, kc, t * 128:(t + 1) * 128], wv[:, kc, hh_ * 512:(hh_ + 1) * 512], start=(kc == 0), stop=(kc == 15)),
                                  reads=[b_aT[t], b_wv], writes=[pb])
                        dstv = V1[:, t, hh_ * 4:(hh_ + 1) * 4, 0:128]
                        src = pt[:, :].rearrange("p (a b) -> p a b", a=4)
                        if hh_ == 0:
                            fw.op(act, lambda e, dstv=dstv, src=src: e.copy(dstv, src), reads=[pb], writes=[b_V1[t]])
                        else:
                            fw.op(dve, lambda e, dstv=dstv, src=src: e.tensor_copy(dstv, src), reads=[pb], writes=[b_V1[t]])
                fw.barrier()
            fw.stack = st
            qw = fw.sb([128, 1], F32, "qw"); b_qw = Buf(); fw.dma(sp, qw[:], qnw, writes=[b_qw])
            fw.op(dve, lambda e: e.tensor_scalar(qw[:, :], qw[:, :], float(128 ** -0.5), None, ALU.mult), writes=[b_qw])
            kw = fw.sb([128, 1], F32, "kw"); b_kw = Buf(); fw.dma(sp, kw[:], knw, writes=[b_kw])
            qTs = [fw.sb([128, NTOK], BF16, "qT%d" % i) for i in range(2)]; b_qTs = [Buf() for _ in range(2)]
            kTs = [fw.sb([128, NTOK], BF16, "kT%d" % i) for i in range(2)]; b_kTs = [Buf() for _ in range(2)]
            wqs = [fw.sb([128, 16, 128], BF16, "wq%d" % i) for i in range(2)]; b_wqs = [Buf() for _ in range(2)]
            wks = [fw.sb([128, 16, 128], BF16, "wk%d" % i) for i in range(2)]; b_wks = [Buf() for _ in range(2)]
            bts = [fw.sb([128, 25, 128], BF16, "bt%d" % i) for i in range(2)]; b_bts = [Buf() for _ in range(2)]
            sqs = [fw.sb([128, 512], F32, "sq%d" % i) for i in range(2)]; b_sqs = [Buf() for _ in range(2)]
            rs = [fw.sb([128, 512], F32, "rs%d" % i) for i in range(2)]; b_rs = [Buf() for _ in range(2)]
            PTs = [fw.sb([128, 7, 128], BF16, "PT%d" % i) for i in range(3)]; b_PTs = [Buf() for _ in range(3)]
            rec = [fw.sb([128, 1], F32, "rec%d" % i) for i in range(2)]; b_rec = [Buf() for _ in range(2)]
            ons = [fw.sb([128, 128], F32, "on%d" % i) for i in range(3)]; b_ons = [Buf() for _ in range(3)]
            cnt = 0; pcnt = 0
            for hd in range(8):
                i2 = hd % 2
                qT, bqT = qTs[i2], b_qTs[i2]; kT, bkT = kTs[i2], b_kTs[i2]
                wq, bwq = wqs[i2], b_wqs[i2]; wk, bwk = wks[i2], b_wks[i2]
                bt, bbt = bts[i2], b_bts[i2]
                fw.dma(pool, wq[:], w_in[:, Q0 + hd * 128:Q0 + (hd + 1) * 128].rearrange("(kc p) c -> p kc c", p=128), writes=[bwq])
                fw.dma(pool, wk[:], w_in[:, K0 + hd * 128:K0 + (hd + 1) * 128].rearrange("(kc p) c -> p kc c", p=128), writes=[bwk])
                fw.dma(pool, bt[:], biasT[hd].rearrange("c k q -> k c q"), writes=[bbt])
                blks = []
                for (wt_, bwt_, dstT, bdst, nwc, bnw) in ((wq, bwq, qT, bqT, qw, b_qw), (wk, bwk, kT, bkT, kw, b_kw)):
                    for (s0, n) in TB:
                        blk = {}

                        def s1(blk=blk, wt_=wt_, bwt_=bwt_, s0=s0, n=n):
                            nonlocal cnt
                            pt, pb = fw.next_psum(); blk["pt"] = (pt, pb); blk["i"] = cnt % 2; cnt += 1
                            for kc in range(16):
                                fw.op(pe, lambda e: e.matmul(pt[:, 0:n], wt_[:, kc, :], aT[:, kc, s0:s0 + n], start=(kc == 0), stop=(kc == 15)),
                                      reads=[bwt_] + [b_aT[t] for t in tiles_of(s0, n)], writes=[pb])
                            sq_, bsq = sqs[blk["i"]], b_sqs[blk["i"]]
                            fw.op(act, lambda e: e.activation(sq_[:, 0:n], pt[:, 0:n], AF.Square), reads=[pb], writes=[bsq])

                        def s2(blk=blk, dstT=dstT, bdst=bdst, nwc=nwc, bnw=bnw, s0=s0, n=n):
                            pt, pb = blk["pt"]; i_ = blk["i"]
                            sq_, bsq = sqs[i_], b_sqs[i_]; r_, br = rs[i_], b_rs[i_]
                            p2, pb2 = fw.next_psum()
                            fw.op(pe, lambda e: e.matmul(p2[:, 0:n], ones[:, :], sq_[:, 0:n], start=True, stop=True), reads=[b_ones, bsq], writes=[pb2])
                            fw.op(act, lambda e: e.activation(r_[:, 0:n], p2[:, 0:n], AF.Sqrt, bias=EPS, scale=1.0 / 128), reads=[pb2], writes=[br])
                            fw.op(dve, lambda e: e.reciprocal(r_[:, 0:n], r_[:, 0:n]), writes=[br])
                            fw.op(dve, lambda e: e.tensor_tensor(r_[:, 0:n], pt[:, 0:n], r_[:, 0:n], ALU.mult), reads=[pb], writes=[br])
                            fw.op(pool, lambda e: e.tensor_scalar(dstT[:, s0:s0 + n], r_[:, 0:n], nwc[:, 0:1], None, ALU.mult), reads=[br, bnw], writes=[bdst])
                        blks.append((s1, s2))
                nb = len(blks)
                blks[0][0](); blks[1][0]()
                for i in range(nb):
                    blks[i][1]()
                    if i + 2 < nb:
                        blks[i + 2][0]()

                def keys_of(tq):
                    if tq < 2:
                        return [(0, None), (1, None)]
                    j = tq - 2
                    a = min(max(2 * j - 4, 0), 22)
                    cls = 0 if j == 0 else 1 if j == 1 else 3 if j == 14 else 4 if j == 15 else 2
                    return [(2 + a // 2 + i, cls * 5 + i) for i in range(5)] + [(0, None), (1, None)]

                def att1(tq):
                    nonlocal pcnt
                    keys = keys_of(tq)
                    PT, bPT = PTs[pcnt % 3], b_PTs[pcnt % 3]
                    on, bon = ons[pcnt % 3], b_ons[pcnt % 3]
                    rc, brc = rec[pcnt % 2], b_rec[pcnt % 2]; pcnt += 1
                    nk = len(keys)
                    for b0 in range(0, nk, 4):
                        pS, pbS = fw.next_psum()
                        grp = keys[b0:b0 + 4]
                        for i, (kt, bi) in enumerate(grp):
                            fw.op(pe, lambda e: e.matmul(pS[:, i * 128:(i + 1) * 128], kT[:, kt * 128:(kt + 1) * 128], qT[:, tq * 128:(tq + 1) * 128], start=True, stop=(bi is None)),
                                  reads=[bkT, bqT], writes=[pbS])
                            if bi is not None:
                                fw.op(pe, lambda e: e.matmul(pS[:, i * 128:(i + 1) * 128], identb[:, :], bt[:, bi, :], start=False, stop=True),
                                      reads=[b_identb, bbt], writes=[pbS])
                        ng = len(grp)
                        fw.op(act, lambda e: e.activation(PT[:, b0:b0 + ng, :].rearrange("p a b -> p (a b)"), pS[:, 0:ng * 128], AF.Exp),
                              reads=[pbS], writes=[bPT])
                    return (tq, keys, PT, bPT, on, bon, rc, brc)

                def att2(st_):
                    tq, keys, PT, bPT, on, bon, rc, brc = st_
                    nk = len(keys)
                    pO, pbO = fw.next_psum()
                    for i, (kt, bi) in enumerate(keys):
                        fw.op(pe, lambda e: e.matmul(pO[:, 0:129], PT[:, i, :], V1[:, kt, hd, :], start=(i == 0), stop=(i == nk - 1)),
                              reads=[bPT, b_V1[kt]], writes=[pbO])
                    fw.op(dve, lambda e: e.reciprocal(rc[:, :], pO[:, 128:129]), reads=[pbO], writes=[brc])
                    fw.op(dve, lambda e: e.tensor_scalar(on[:, :], pO[:, 0:128], rc[:, 0:1], None, ALU.mult), reads=[pbO, brc], writes=[bon])
                    if fused:
                        d_mix.extend(out_fn(fw, tq * 128, 1024 + hd * 128, 128, on, bon))
                    else:
                        db = Buf(); d_mix.append(db)
                        fw.dma(sp, mix[tq * 128:(tq + 1) * 128, 1024 + hd * 128:1024 + (hd + 1) * 128], on[:, :], reads=[bon], writes=[db])

                cur = att1(0)
                for tq in range(NT):
                    nxt = att1(tq + 1) if tq + 1 < NT else None
                    att2(cur)
                    cur = nxt
                if fused and ctx.get("mid_hook") and hd == 3:
                    ctx["mid_hook"](1)
            fw.barrier()
        if not fused:
            fw.finish(d_mix)
        print("phaseA0 ops", fw.n_ops, "waits", fw.n_waits)
    return d_mix if fused else nc

import math
from contextlib import ExitStack

NT = 18
NTOK = NT * 128
NLAT = 2048
GQ0, DQ0, GK0, DK0, GV0, DV0, NCOL1 = 0, 1024, 2048, 2304, 3328, 3584, 4608
TB = [(0, 256)] + [(256 + i * 512, 512) for i in range(4)]
LAMBDA_INIT = 0.8 - 0.6 * math.exp(-0.3 * 1)


def build_phaseA1(ctx=None):
    fused = ctx is not None
    nc = ctx["nc"] if fused else bass.Bass("TRN2", target_bir_lowering=False)
    pre = ctx["pre"] if fused else ""
    out_fn = ctx["out_fn"] if fused else None
    I = lambda name, shape, dt=F32: nc.dram_tensor(pre + name, shape, dt, kind="ExternalInput").ap()
    h_src = ctx.get("h_src") if fused else None
    h_all = I("h_all", [NTOK, D]) if h_src is None else h_src[0]
    h_bufs = () if h_src is None else h_src[1]
    row_of = None if h_src is None else h_src[2]
    cvec = I("cvec", [2, D])
    ada_w = I("ada_w", [D, 4096]); ada_b = I("ada_b", [1, 4096])
    norm_w1 = I("norm_w1", [1, D])
    w_in = I("w_in", [D, NCOL1])
    nws = I("nws", [128, 4])
    lamv = I("lamv", [1, 512])
    subln = I("subln", [1, 256])
    cosT_d = I("cosT", [128, NLAT]); sinT_d = I("sinT", [128, NLAT]); rmT_d = I("rmT", [128, 128])
    ident_d = I("ident", [128, 128])
    mix = None if fused else nc.dram_tensor("mix_part", [NLAT, 2048], F32, kind="ExternalOutput").ap()
    modrows = nc.dram_tensor(pre + "modrows", [2, 4096], F32).ap()
    d_mod = Buf("modrows"); d_mix = []

    with ExitStack() as st0:
        if fused:
            fw = ctx["fw"]; fw.stack = st0; fw.ps_pool = list(range(8))
        else:
            fw = FW(nc, st0)
        pe, dve, act, pool, sp = fw.pe, fw.dve, fw.act, fw.pool, fw.sp
        ident, b_ident = emit_consts(fw, ident_d)
        ones = fw.sb([128, 128], F32, "ones"); b_ones = Buf(); fw.op(pool, lambda e: e.memset(ones[:, :], 1.0), writes=[b_ones])
        aT = fw.sb([128, 16, NTOK], BF16, "aT"); b_aT = [Buf() for _ in range(NT)]
        emit_mods(fw, cvec, ada_w, ada_b, 4096, modrows, d_mod, ident, b_ident)
        emit_aT(fw, h_all, NT, 2, norm_w1, modrows, d_mod, aT, b_aT, ident, b_ident, row_of=row_of, h_bufs=h_bufs)

        def tiles_of(s, n):
            return list(range(s // 128, (s + n) // 128))

        with ExitStack() as st:
            fw.stack = st
            V1g = fw.sb([128, NT, 2, 129], BF16, "V1g"); V1d = fw.sb([128, NT, 4, 257], BF16, "V1d"); b_V = [Buf() for _ in range(NT)]
            fw.op(pool, lambda e: e.memset(V1g[:, :, :, :].rearrange("p a b c -> p (a b c)"), 1.0), writes=b_V)
            fw.op(pool, lambda e: e.memset(V1d[:, :, :, :].rearrange("p a b c -> p (a b c)"), 1.0), writes=b_V)
            with ExitStack() as st2:
                fw.stack = st2
                wv = fw.sb([128, 16, 1280], BF16, "wv"); b_wv = Buf()
                fw.dma(pool, wv[:], w_in[:, GV0:GV0 + 1280].rearrange("(kc p) c -> p kc c", p=128), writes=[b_wv])
                for t in range(NT):
                    for part in range(3):
                        c0 = part * 512; n = 512 if part < 2 else 256
                        pt, pb = fw.next_psum()
                        for kc in range(16):
                            fw.op(pe, lambda e, kc=kc, pt=pt, t=t, c0=c0, n=n: e.matmul(pt[:, 0:n], aT[:, kc, t * 128:(t + 1) * 128], wv[:, kc, c0:c0 + n], start=(kc == 0), stop=(kc == 15)),
                                  reads=[b_aT[t], b_wv], writes=[pb])
                        if part == 0:
                            fw.op(act, lambda e, pt=pt, t=t: e.copy(V1g[:, t, :, 0:128], pt[:, 0:256].rearrange("p (a b) -> p a b", a=2)), reads=[pb], writes=[b_V[t]])
                            fw.op(dve, lambda e, pt=pt, t=t: e.tensor_copy(V1d[:, t, 0, 0:256], pt[:, 256:512]), reads=[pb], writes=[b_V[t]])
                        elif part == 1:
                            fw.op(act, lambda e, pt=pt, t=t: e.copy(V1d[:, t, 1:3, 0:256], pt[:, 0:512].rearrange("p (a b) -> p a b", a=2)), reads=[pb], writes=[b_V[t]])
                        else:
                            fw.op(dve, lambda e, pt=pt, t=t: e.tensor_copy(V1d[:, t, 3, 0:256], pt[:, 0:256]), reads=[pb], writes=[b_V[t]])
                fw.barrier()
            fw.stack = st
            fw.ps_pool = [0, 1, 2, 3]
            cosT = fw.sb([128, NLAT], F32, "cosT"); b_cos = Buf(); fw.dma(sp, cosT[:], cosT_d, writes=[b_cos])
            sinT = fw.sb([128, NLAT], F32, "sinT"); b_sin = Buf(); fw.dma(sp, sinT[:], sinT_d, writes=[b_sin])
            rmT = fw.sb([128, 128], F32, "rmT"); b_rm = Buf(); fw.dma(sp, rmT[:], rmT_d, writes=[b_rm])
            nw4 = fw.sb([128, 4], F32, "nw4"); b_nw4 = Buf(); fw.dma(sp, nw4[:], nws, writes=[b_nw4])
            nwq = fw.sb([128, 4], F32, "nwq"); b_nwq = Buf()
            fw.op(dve, lambda e: e.tensor_scalar(nwq[:, :], nw4[:, :], float(128 ** -0.5), None, ALU.mult), reads=[b_nw4], writes=[b_nwq])
            sub = fw.sb([128, 256], F32, "sub"); b_sub = Buf(); fw.dma(sp, sub[:], bview(subln), writes=[b_sub])
            fw.op(dve, lambda e: e.tensor_scalar(sub[:, :], sub[:, :], float(1.0 - LAMBDA_INIT), None, ALU.mult), writes=[b_sub])
            lv = fw.sb([128, 512], F32, "lv"); b_lv = Buf(); fw.dma(sp, lv[:], bview(lamv), writes=[b_lv])
            lt = fw.sb([128, 256], F32, "lt"); b_lt = Buf()
            fw.op(dve, lambda e: e.tensor_tensor(lt[:, :].rearrange("p (a b) -> p a b", a=2), lv[:, :].rearrange("p (a c b) -> p a c b", a=2, c=2)[:, :, 0, :],
                                                 lv[:, :].rearrange("p (a c b) -> p a c b", a=2, c=2)[:, :, 1, :], ALU.mult), reads=[b_lv], writes=[b_lt])
            ld = fw.sb([128, 2], F32, "ld"); b_ld = Buf()
            fw.op(dve, lambda e: e.reduce_sum(ld[:, :], lt[:, :].rearrange("p (a b) -> p a b", a=2), axis=AX.X), reads=[b_lt], writes=[b_ld])
            fw.op(act, lambda e: e.activation(ld[:, :], ld[:, :], AF.Exp), writes=[b_ld])
            nlam = fw.sb([128, 1], F32, "nlam"); b_nlam = Buf()
            fw.op(dve, lambda e: e.tensor_tensor(nlam[:, :], ld[:, 1:2], ld[:, 0:1], ALU.subtract), reads=[b_ld], writes=[b_nlam])
            fw.op(dve, lambda e: e.tensor_scalar(nlam[:, :], nlam[:, :], float(-LAMBDA_INIT), None, ALU.add), writes=[b_nlam])

            sqs = [fw.sb([128, 512], F32, "sq%d" % i) for i in range(2)]; b_sqs = [Buf() for _ in range(2)]
            rs = [fw.sb([128, 512], F32, "rs%d" % i) for i in range(2)]; b_rs = [Buf() for _ in range(2)]
            xws = [fw.sb([128, 512], F32, "xw%d" % i) for i in range(2)]; b_xws = [Buf() for _ in range(2)]
            us = [fw.sb([128, 512], F32, "u%d" % i) for i in range(2)]; b_us = [Buf() for _ in range(2)]
            wus = [fw.sb([128, 16, 128], BF16, "wu%d" % i) for i in range(3)]; b_wus = [Buf() for _ in range(3)]
            st_ = {"cnt": 0, "w": 0}

            pend = []

            def qk_unit(col0, nwcol, b_nwcol, dstT, bdst, with_ctx):
                wu, bwu = wus[st_["w"] % 3], b_wus[st_["w"] % 3]; st_["w"] += 1
                first = [True]
                for (s0, n) in (TB if with_ctx else TB[1:]):
                    blk = {}

                    def s1(blk=blk, s0=s0, n=n, is_first=first[0]):
                        if is_first:
                            fw.dma(pool, wu[:], w_in[:, col0:col0 + 128].rearrange("(kc p) c -> p kc c", p=128), writes=[bwu])
                        i2 = st_["cnt"] % 2; st_["cnt"] += 1
                        blk["i2"] = i2
                        sq_, bsq = sqs[i2], b_sqs[i2]
                        pt, pb = fw.next_psum(); blk["pt"] = (pt, pb)
                        for kc in range(16):
                            fw.op(pe, lambda e: e.matmul(pt[:, 0:n], wu[:, kc, :], aT[:, kc, s0:s0 + n], start=(kc == 0), stop=(kc == 15)),
                                  reads=[bwu] + [b_aT[t] for t in tiles_of(s0, n)], writes=[pb])
                        fw.op(act, lambda e: e.activation(sq_[:, 0:n], pt[:, 0:n], AF.Square), reads=[pb], writes=[bsq])

                    def s2(blk=blk, s0=s0, n=n):
                        i2 = blk["i2"]; pt, pb = blk["pt"]
                        sq_, bsq = sqs[i2], b_sqs[i2]; r_, br = rs[i2], b_rs[i2]; xw, bxw = xws[i2], b_xws[i2]
                        p2, pb2 = fw.next_psum()
                        fw.op(pe, lambda e: e.matmul(p2[:, 0:n], ones[:, :], sq_[:, 0:n], start=True, stop=True), reads=[b_ones, bsq], writes=[pb2])
                        fw.op(act, lambda e: e.activation(r_[:, 0:n], p2[:, 0:n], AF.Sqrt, bias=EPS, scale=1.0 / 128), reads=[pb2], writes=[br])
                        fw.op(dve, lambda e: e.reciprocal(r_[:, 0:n], r_[:, 0:n]), writes=[br])
                        fw.op(dve, lambda e: e.tensor_tensor(r_[:, 0:n], pt[:, 0:n], r_[:, 0:n], ALU.mult), reads=[pb], writes=[br])
                        d0 = s0 if with_ctx else s0 - 256
                        if s0 == 0:
                            fw.op(pool, lambda e: e.tensor_scalar(dstT[:, d0:d0 + n], r_[:, 0:n], nwcol, None, ALU.mult), reads=[br, b_nwcol], writes=[bdst])
                        else:
                            fw.op(pool, lambda e: e.tensor_scalar(xw[:, 0:n], r_[:, 0:n], nwcol, None, ALU.mult), reads=[br, b_nwcol], writes=[bxw])

                    def s3(blk=blk, s0=s0, n=n):
                        if s0 == 0:
                            return
                        i2 = blk["i2"]
                        xw, bxw = xws[i2], b_xws[i2]; u_, bu = us[i2], b_us[i2]
                        d0 = s0 if with_ctx else s0 - 256
                        l0 = s0 - 256
                        p3, pb3 = fw.next_psum()
                        fw.op(pe, lambda e: e.matmul(p3[:, 0:n], rmT[:, :], xw[:, 0:n], start=True, stop=True), reads=[b_rm, bxw], writes=[pb3])
                        fw.op(dve, lambda e: e.tensor_tensor(u_[:, 0:n], p3[:, 0:n], sinT[:, l0:l0 + n], ALU.mult), reads=[pb3, b_sin], writes=[bu])
                        fw.op(pool, lambda e: e.tensor_tensor(xw[:, 0:n], xw[:, 0:n], cosT[:, l0:l0 + n], ALU.mult), reads=[b_cos], writes=[bxw])
                        fw.op(pool, lambda e: e.tensor_tensor(dstT[:, d0:d0 + n], xw[:, 0:n], u_[:, 0:n], ALU.add), reads=[bxw, bu], writes=[bdst])

                    pend.append((s1, s2, s3))
                    first[0] = False

            def flush_qk():
                fw.ps_pool = list(range(8))
                blks = list(pend); pend.clear()
                nb = len(blks)
                for i in range(min(2, nb)):
                    blks[i][0]()
                for i in range(nb):
                    blks[i][1]()
                    if i + 2 < nb:
                        blks[i + 2][0]()
                    blks[i][2]()
                fw.ps_pool = [0, 1, 2, 3]

            kTs = [fw.sb([128, NTOK], BF16, "kT%d" % i) for i in range(2)]; b_kTs = [Buf() for _ in range(2)]
            qTs = [fw.sb([128, NLAT], BF16, "qT%d" % i) for i in range(2)]; b_qTs = [Buf() for _ in range(2)]
            PTs = [fw.sb([128, 512], BF16, "PT%d" % i) for i in range(3)]; b_PTs = [Buf() for _ in range(3)]
            ogs = [fw.sb([128, 128], F32, "og%d" % i) for i in range(3)]; b_ogs = [Buf() for _ in range(3)]
            rcs = [fw.sb([128, 1], F32, "rc%d" % i) for i in range(3)]; b_rcs = [Buf() for _ in range(3)]
            o0n = fw.sb([128, 4, 256], F32, "o0n"); b_o0n = [Buf() for _ in range(4)]
            ods = [fw.sb([128, 256], F32, "od%d" % i) for i in range(2)]; b_ods = [Buf() for _ in range(2)]
            o1s = [fw.sb([128, 256], F32, "o1_%d" % i) for i in range(2)]; b_o1s = [Buf() for _ in range(2)]
            sq2 = fw.sb([128, 256], F32, "sq2"); b_sq2 = Buf()
            ss2 = [fw.sb([128, 1], F32, "ss2_%d" % i) for i in range(2)]; b_ss2 = [Buf() for _ in range(2)]
            cn = {"pt": 0, "o": 0, "k": 0, "q": 0, "d": 0}

            def attend(qT, bqT, kT, bkT, vfn, vw, banks, qb, finish):
                stride = 512
                per_bank = 1
                def score(kt):
                    pS, pbS = fw.next_psum()
                    fw.op(pe, lambda e: e.matmul(pS[:, :], kT[:, kt * 128:(kt + 1) * 128], qT[:, qb * 512:(qb + 1) * 512], start=True, stop=True),
                          reads=[bkT, bqT], writes=[pbS])
                    return pS, pbS
                cur = score(0)
                for kt in range(NT):
                    nxt = score(kt + 1) if kt + 1 < NT else None
                    pS, pbS = cur
                    PT, bPT = PTs[cn["pt"] % 3], b_PTs[cn["pt"] % 3]; cn["pt"] += 1
                    fw.op(act, lambda e: e.activation(PT[:, :], pS[:, :], AF.Exp), reads=[pbS], writes=[bPT])
                    for qs in range(4):
                        bank = banks[qs // per_bank]; off = (qs % per_bank) * stride
                        pO, pbO = fw.psum[bank]
                        fw.op(pe, lambda e: e.matmul(pO[:, off:off + vw], PT[:, qs * 128:(qs + 1) * 128], vfn(kt), start=(kt == 0), stop=(kt == NT - 1)),
                              reads=[bPT, b_V[kt]], writes=[pbO])
                    cur = nxt
                for qs in range(4):
                    bank = banks[qs // per_bank]; off = (qs % per_bank) * stride
                    pO, pbO = fw.psum[bank]
                    finish(qs, pO, pbO, off)

            for kv in range(2):
                kT, bkT = kTs[cn["k"] % 2], b_kTs[cn["k"] % 2]; cn["k"] += 1
                qk_unit(GK0 + kv * 128, nw4[:, 1:2], b_nw4, kT, bkT, True)
                for hq in range(4):
                    hd = kv * 4 + hq
                    qT, bqT = qTs[cn["q"] % 2], b_qTs[cn["q"] % 2]; cn["q"] += 1
                    qk_unit(GQ0 + hd * 128, nwq[:, 0:1], b_nwq, qT, bqT, False)
                    flush_qk()
                    for qb in range(4):
                        def fin(qs, pO, pbO, off, hd=hd, qb=qb):
                            i3 = cn["o"] % 3; cn["o"] += 1
                            og, bog = ogs[i3], b_ogs[i3]; rc, brc = rcs[i3], b_rcs[i3]
                            fw.op(dve, lambda e: e.reciprocal(rc[:, :], pO[:, off + 128:off + 129]), reads=[pbO], writes=[brc])
                            fw.op(dve, lambda e: e.tensor_scalar(og[:, :], pO[:, off:off + 128], rc[:, 0:1], None, ALU.mult), reads=[pbO, brc], writes=[bog])
                            r0 = qb * 512 + qs * 128
                            if fused:
                                d_mix.extend(out_fn(fw, r0, hd * 128, 128, og, bog))
                            else:
                                db = Buf(); d_mix.append(db)
                                fw.dma(sp, mix[r0:r0 + 128, hd * 128:(hd + 1) * 128], og[:, :], reads=[bog], writes=[db])
                        attend(qT, bqT, kT, bkT, lambda kt, kv=kv: V1g[:, kt, kv, :], 129, [4, 5, 6, 7], qb, fin)
            if fused and ctx.get("mid_hook"):
                ctx["mid_hook"](0)
            for h in range(4):
                kq = []
                for c in range(2):
                    kT, bkT = kTs[cn["k"] % 2], b_kTs[cn["k"] % 2]; cn["k"] += 1
                    qk_unit(DK0 + (h * 2 + c) * 128, nw4[:, 3:4], b_nw4, kT, bkT, True)
                    qT, bqT = qTs[cn["q"] % 2], b_qTs[cn["q"] % 2]; cn["q"] += 1
                    qk_unit(DQ0 + (h * 2 + c) * 128, nwq[:, 2:3], b_nwq, qT, bqT, False)
                    kq.append((kT, bkT, qT, bqT))
                flush_qk()
                for qb in range(4):
                    for c in range(2):
                        kT, bkT, qT, bqT = kq[c]
                        if c == 0:
                            def fin(qs, pO, pbO, off):
                                i3 = cn["o"] % 3; cn["o"] += 1
                                rc, brc = rcs[i3], b_rcs[i3]
                                fw.op(dve, lambda e: e.reciprocal(rc[:, :], pO[:, off + 256:off + 257]), reads=[pbO], writes=[brc])
                                fw.op(dve, lambda e: e.tensor_scalar(o0n[:, qs, :], pO[:, off:off + 256], rc[:, 0:1], None, ALU.mult), reads=[pbO, brc], writes=[b_o0n[qs]])
                        else:
                            def fin(qs, pO, pbO, off, h=h, qb=qb):
                                i3 = cn["o"] % 3; cn["o"] += 1
                                i2 = cn["d"] % 2; cn["d"] += 1
                                rc, brc = rcs[i3], b_rcs[i3]
                                o1, bo1 = o1s[i2], b_o1s[i2]; od, bod = ods[i2], b_ods[i2]; s2, bs2 = ss2[i2], b_ss2[i2]
                                fw.op(dve, lambda e: e.reciprocal(rc[:, :], pO[:, off + 256:off + 257]), reads=[pbO], writes=[brc])
                                fw.op(dve, lambda e: e.tensor_scalar(rc[:, :], rc[:, :], nlam[:, 0:1], None, ALU.mult), reads=[b_nlam], writes=[brc])
                                fw.op(dve, lambda e: e.scalar_tensor_tensor(o1[:, :], pO[:, off:off + 256], rc[:, 0:1], o0n[:, qs, :], ALU.mult, ALU.add),
                                      reads=[pbO, brc, b_o0n[qs]], writes=[bo1])
                                fw.op(act, lambda e: e.activation(sq2[:, :], o1[:, :], AF.Square, accum_out=s2[:, :]), reads=[bo1], writes=[b_sq2, bs2])
                                fw.op(act, lambda e: e.activation(s2[:, :], s2[:, :], AF.Sqrt, bias=EPS, scale=1.0 / 256), writes=[bs2])
                                fw.op(dve, lambda e: e.reciprocal(s2[:, :], s2[:, :]), writes=[bs2])
                                fw.op(dve, lambda e: e.scalar_tensor_tensor(od[:, :], o1[:, :], s2[:, 0:1], sub[:, :], ALU.mult, ALU.mult), reads=[bo1, bs2, b_sub], writes=[bod])
                                r0 = qb * 512 + qs * 128
                                if fused:
                                    d_mix.extend(out_fn(fw, r0, 1024 + h * 256, 256, od, bod))
                                else:
                                    db = Buf(); d_mix.append(db)
                                    fw.dma(sp, mix[r0:r0 + 128, 1024 + h * 256:1024 + (h + 1) * 256], od[:, :], reads=[bod], writes=[db])
                        attend(qT, bqT, kT, bkT, lambda kt, h=h: V1d[:, kt, h, :], 257, [4, 5, 6, 7], qb, fin)
                if fused and ctx.get("mid_hook") and h == 1:
                    ctx["mid_hook"](1)
            fw.barrier()
        if not fused:
            fw.finish(d_mix)
        fw.ps_pool = list(range(8))
        print("phaseA1 ops", fw.n_ops, "waits", fw.n_waits)
    return d_mix if fused else nc

from contextlib import ExitStack

PAIRS = [[0, 1], [2, 3], [4, 5], [6, 7]]
CC_BYTES = 4 * 1024 * 1024


def build_fused():
    nc = bass.Bass("TRN2", target_bir_lowering=False)
    msk_d = nc.dram_tensor("msk", [128, 2], F32, kind="ExternalInput").ap()
    PW = [1024, 512, 512]; PC = [0, 1024, 1536]
    x1s = [nc.dram_tensor("x1s%d" % i, [2 * 2304, PW[i]], BF16).ap() for i in range(3)]
    x1d = [nc.dram_tensor("x1d%d" % i, [2 * 2304, PW[i]], BF16).ap() for i in range(3)]
    x2s = nc.dram_tensor("x2s", [2 * 1152, 2048], F32).ap(); x2d = nc.dram_tensor("x2d", [2 * 1152, 2048], F32).ap()
    x3s = [nc.dram_tensor("x3s%d" % i, [2 * 2048, PW[i]], BF16).ap() for i in range(3)]
    x3d = [nc.dram_tensor("x3d%d" % i, [2 * 2048, PW[i]], BF16).ap() for i in range(3)]
    h1own = nc.dram_tensor("h1own", [1024, 2048], F32).ap()
    with ExitStack() as st0:
        fw = FW(nc, st0)
        cc = Src("cc", fw._sem("cc"), 1)
        msk = fw.sb([128, 2], F32, "msk"); b_msk = Buf()
        fw.dma(fw.sp, msk[:], msk_d, writes=[b_msk])

        def staging(shape, dt, n):
            stk = fw.stack
            key = "_stg_%s_%d" % (str(dt), shape[1])
            if not hasattr(stk, key):
                setattr(stk, key, {"t": [fw.sb(shape, dt, "stg%d" % i) for i in range(n)], "b": [Buf() for _ in range(n)], "i": 0})
            d = getattr(stk, key)
            k = d["i"] % n; d["i"] += 1
            return d["t"][k], d["b"][k]

        def mk_mix_out(dsts, R, wlists):
            def out_fn(fw_, r0, c0, n, tile, btile):
                res = []
                part = 0 if c0 < 1024 else 1 if c0 < 1536 else 2
                cc0 = c0 - PC[part]
                for s in range(2):
                    u, bu = staging([128, 512], BF16, 4)
                    if s == 0:
                        fw.op(fw.act, lambda e: e.mul(u[:, 0:n], tile[:, :], msk[:, s:s + 1]), reads=[btile, b_msk], writes=[bu])
                    else:
                        fw.op(fw.dve, lambda e: e.tensor_scalar(u[:, 0:n], tile[:, :], msk[:, s:s + 1], None, ALU.mult), reads=[btile, b_msk], writes=[bu])
                    db = Buf(); res.append(db); wlists[part].append(db)
                    fw.dma(fw.sp, dsts[part][s * R + r0:s * R + r0 + 128, cc0:cc0 + n], u[:, 0:n], reads=[bu], writes=[db])
                return res
            return out_fn

        b_h1own = []
        x2w = {}
        x2ev = {}
        GROUPS = [(0, 4), (4, 8), (8, 9)]

        def x2_exchange(gi):
            t0, t1 = GROUPS[gi]
            for s in range(2):
                for t in range(t0, t1):
                    fw.pool.wait(x2w[(s, t)].w)
                r0 = s * 1152 + t0 * 128; r1 = s * 1152 + t1 * 128
                ins = fw.pool.raw.collective_compute("AllReduce", ALU.add, replica_groups=PAIRS,
                                                     ins=[x2s[r0:r1, :].opt()], outs=[x2d[r0:r1, :].opt()])
                ins.then_inc(cc.sem)
                cc.n += 1
                eb = Buf(); eb.w = Ev(cc, cc.n, dict(fw.pool.clock))
                x2ev[(s, gi)] = eb

        def b0_out(fw_, t, h1, bh1):
            if t == 5:
                x2_exchange(0)
            if t == 8:
                x2_exchange(1)
            res = []
            for s in range(2):
                u, bu = staging([128, 2048], F32, 4)
                if s == 0:
                    fw.op(fw.act, lambda e: e.mul(u[:, :], h1[:, :], msk[:, s:s + 1]), reads=[bh1, b_msk], writes=[bu])
                else:
                    fw.op(fw.pool, lambda e: e.tensor_scalar(u[:, :], h1[:, :], msk[:, s:s + 1], None, ALU.mult), reads=[bh1, b_msk], writes=[bu])
                db = Buf(); res.append(db); x2w[(s, t)] = db
                fw.dma(fw.sp, x2s[s * 1152 + t * 128:s * 1152 + (t + 1) * 128, :], u[:, :], reads=[bu], writes=[db])
            if t >= 1:
                db = Buf(); b_h1own.append(db); res.append(db)
                fw.dma(fw.sp, h1own[(t - 1) * 128:t * 128, :], h1[:, :], reads=[bh1], writes=[db])
            return res

        def exchange(src, dst, writers):
            pool = fw.pool
            for b in writers:
                pool.wait(b.w)
            nrows = src.shape[0]
            elt = 2 if src.dtype == BF16 else 4
            rows_per = CC_BYTES // (src.shape[1] * elt)
            for r0 in range(0, nrows, rows_per):
                r1 = min(nrows, r0 + rows_per)
                ins = pool.raw.collective_compute("AllReduce", ALU.add, replica_groups=PAIRS,
                                                  ins=[src[r0:r1, :].opt()], outs=[dst[r0:r1, :].opt()])
                ins.then_inc(cc.sem)
                cc.n += 1
            eb = Buf()
            eb.w = Ev(cc, cc.n, dict(pool.clock))
            return eb

        shared = {}
        wl1 = [[], [], []]; ev1 = [None, None, None]

        def hook1(i):
            ev1[i] = exchange(x1s[i], x1d[i], wl1[i])
        build_phaseA0(ctx=dict(nc=nc, fw=fw, pre="a0_", out_fn=mk_mix_out(x1s, 2304, wl1), mid_hook=hook1))
        hook1(2)
        w2_ = build_phaseB(9, True, ctx=dict(nc=nc, fw=fw, pre="b0_", mix_src=[(x1d[i], [ev1[i]], PC[i]) for i in range(3)], out_fn=b0_out, shared=shared))
        x2_exchange(2)

        def h_dep(t):
            s, lt = (0, 0) if t == 0 else (1, 0) if t == 1 else (0, t - 1) if t < 10 else (1, t - 9)
            gi = 0 if lt < 4 else 1 if lt < 8 else 2
            return [x2ev[(s, gi)]]

        def row_of(t):
            return 0 if t == 0 else 1152 if t == 1 else 128 + (t - 2) * 128 if t < 10 else 1280 + (t - 10) * 128

        wl3 = [[], [], []]; ev3 = [None, None, None]

        def hook3(i):
            ev3[i] = exchange(x3s[i], x3d[i], wl3[i])
        build_phaseA1(ctx=dict(nc=nc, fw=fw, pre="a1_", h_src=(x2d, h_dep, row_of), out_fn=mk_mix_out(x3s, 2048, wl3), mid_hook=hook3))
        hook3(2)
        outs = build_phaseB(8, False, ctx=dict(nc=nc, fw=fw, pre="b1_", h_src=(h1own, list(b_h1own)), mix_src=[(x3d[i], [ev3[i]], PC[i]) for i in range(3)], out_fn=None, shared=shared))
        fw.finish(outs)
        print("fused ops", fw.n_ops, "waits", fw.n_waits)
    return nc

import numpy as np

PERM = np.concatenate([np.arange(0, 1024), np.arange(2048, 3072), np.arange(1024, 2048), np.arange(3072, 4096)])
_CONST = {}

def consts():
    if "ident" not in _CONST:
        s = np.arange(128)
        _CONST["ident"] = np.eye(128, dtype=np.float32)
        _CONST["trilt"] = (s[:, None] < s[None, :]).astype(np.float32)
        _CONST["ule"] = (s[:, None] <= s[None, :]).astype(np.float32)
        _CONST["uge"] = (s[:, None] >= s[None, :]).astype(np.float32)
        _CONST["iota64"] = np.arange(64, dtype=np.float32).reshape(1, 64)
    return _CONST

def f32c(a):
    return np.ascontiguousarray(a, dtype=np.float32)

def na_bias_tables(rpb, heads):
    classes = [(0, 0), (1, 0), (2, 0), (14, 22), (15, 22)]
    out = np.full((len(heads), 25, 128, 128), -30000.0, np.float32)
    k = np.arange(128); q = np.arange(128)
    for ci, (j, a) in enumerate(classes):
        r = 2 * j + q // 64; c = q % 64
        r0 = np.clip(r - 4, 0, 24); cs = np.clip(c - 8, 0, 48)
        for i in range(5):
            kr = a + 2 * i + k // 64; kc = k % 64
            vis = ((kr[:, None] >= r0[None, :]) & (kr[:, None] < r0[None, :] + 8) &
                   (kc[:, None] >= cs[None, :]) & (kc[:, None] < cs[None, :] + 16))
            ri = np.clip(kr[:, None] - r[None, :] + 7, 0, 14)
            cj = np.clip(kc[:, None] - c[None, :] + 15, 0, 30)
            for hi, h in enumerate(heads):
                vals = rpb[h][ri, cj]
                out[hi, ci * 5 + i] = np.where(vis, vals, np.float32(-30000.0))
    return out

def pack_A0(P, b, hh, h_all):
    in_w = P["ev_in_w"][0]
    gs = [2 * hh, 2 * hh + 1]
    heads16 = np.arange(16 * hh, 16 * hh + 16)
    nah = np.arange(8 * hh, 8 * hh + 8)
    cols = np.concatenate(
        [np.arange(g * 512, (g + 1) * 512) for g in gs] +
        [2048 + np.arange(g * 512, (g + 1) * 512) for g in gs] +
        [4096 + np.arange(g * 128, (g + 1) * 128) for g in gs] +
        [4608 + np.arange(g * 128, (g + 1) * 128) for g in gs] +
        [5120 + heads16, 5152 + heads16] +
        [5184 + np.arange(h * 128, (h + 1) * 128) for h in nah] +
        [5184 + 2048 + np.arange(h * 128, (h + 1) * 128) for h in nah] +
        [5184 + 4096 + np.arange(h * 128, (h + 1) * 128) for h in nah])
    chans = np.concatenate([np.arange(g * 512, (g + 1) * 512) for g in gs] +
                           [2048 + np.arange(g * 128, (g + 1) * 128) for g in gs] +
                           [2560 + np.arange(g * 128, (g + 1) * 128) for g in gs])
    d = {k: consts()[k] for k in ("ident", "ule", "uge")}
    d.update({
        "h_all": h_all, "cvec": np.stack([P["c"][b], P["c_ctx"]], 0),
        "ada_w": P["ada_w"][0][:, 0:4096], "ada_b": P["ada_b"][0][None, 0:4096],
        "norm_w1": P["norm_w"][0, 0][None, :], "w_in": in_w[:, cols],
        "convw": P["ev_conv_w"][0][:, chans].T, "convb": P["ev_conv_b"][0][chans].reshape(12, 128).T,
        "dt_bias": np.concatenate([P["ev_dt_bias"][0][0, heads16], P["ev_dt_bias"][0][1, heads16]])[None, :],
        "a_log": np.concatenate([P["ev_a_log"][0][0, heads16], P["ev_a_log"][0][1, heads16]])[None, :],
        "d_skip": P["ev_d_skip"][0][heads16][None, :],
        "ssd_nw": np.concatenate([P["ev_ssd_norm_w"][0][g * 512:(g + 1) * 512] for g in gs])[None, :],
        "qnw": P["ev_na_q_norm"][0][:, None], "knw": P["ev_na_k_norm"][0][:, None],
        "biasT": na_bias_tables(P["ev_na_rpb"][0], list(nah)),
    })
    return {k: f32c(v) for k, v in d.items()}

def rope_tables():
    if "cosT" not in _CONST:
        t = np.arange(2048)
        row = (t // 64).astype(np.float32); col = (t % 64).astype(np.float32)
        inv = (1.0 / (np.float32(10000.0) ** (np.arange(0, 64, 2, dtype=np.float32) / np.float32(64)))).astype(np.float32)
        ang = np.concatenate([row[:, None] * inv[None], col[:, None] * inv[None]], -1).astype(np.float32)
        c = np.cos(ang).astype(np.float32); s = np.sin(ang).astype(np.float32)
        _CONST["cosT"] = np.ascontiguousarray(np.repeat(c, 2, axis=1).T)
        _CONST["sinT"] = np.ascontiguousarray(np.repeat(s, 2, axis=1).T)
        rm = np.zeros((128, 128), np.float32)
        for i in range(64):
            rm[2 * i + 1, 2 * i] = -1.0
            rm[2 * i, 2 * i + 1] = 1.0
        _CONST["rmT"] = rm
    return _CONST["cosT"], _CONST["sinT"], _CONST["rmT"]

def pack_A1(P, b, hh, h_all):
    in_w = P["od_in_w"][0]
    cols = np.concatenate(
        [np.arange((8 * hh + i) * 128, (8 * hh + i + 1) * 128) for i in range(8)] +
        [2048 + np.arange((8 * hh + i) * 128, (8 * hh + i + 1) * 128) for i in range(8)] +
        [4096 + np.arange((2 * hh + i) * 128, (2 * hh + i + 1) * 128) for i in range(2)] +
        [5120 + np.arange((8 * hh + i) * 128, (8 * hh + i + 1) * 128) for i in range(8)] +
        [4608 + np.arange((2 * hh + i) * 128, (2 * hh + i + 1) * 128) for i in range(2)] +
        [7168 + np.arange((4 * hh + i) * 256, (4 * hh + i + 1) * 256) for i in range(4)])
    cosT, sinT, rmT = rope_tables()
    d = {
        "ident": consts()["ident"],
        "h_all": h_all, "cvec": np.stack([P["c"][b], P["c_ctx"]], 0),
        "ada_w": P["ada_w"][1][:, 0:4096], "ada_b": P["ada_b"][1][None, 0:4096],
        "norm_w1": P["norm_w"][1, 0][None, :], "w_in": in_w[:, cols],
        "nws": np.stack([P["od_gqa_q_norm"][0], P["od_gqa_k_norm"][0], P["od_diff_q_norm"][0], P["od_diff_k_norm"][0]], 1),
        "lamv": P["od_lambda"][0].reshape(1, 512), "subln": P["od_diff_subln"][0][None, :],
        "cosT": cosT, "sinT": sinT, "rmT": rmT,
    }
    return {k: f32c(v) for k, v in d.items()}


_PROG = {}


def _shared_B(P, layer):
    ow = P["ev_out_w"][0] if layer == 0 else P["od_out_w"][0]
    c = consts()
    sh = {"ident": c["ident"], "trilt": c["trilt"], "iota64": c["iota64"],
          "ada_w": P["ada_w"][layer][:, 4096:], "ada_b": P["ada_b"][layer][None, 4096:],
          "norm_w2": P["norm_w"][layer, 1][None, :], "out_w": ow[PERM],
          "gwew": np.concatenate([P["moe_group_w"][layer], P["moe_expert_w"][layer]], 1),
          "w1": P["moe_w1"][layer], "w3": P["moe_w3"][layer], "w2": P["moe_w2"][layer]}
    return {k: f32c(v) for k, v in sh.items()}


def kernel(**inputs):
    P = {k: np.asarray(v) for k, v in inputs.items()}
    B = 4
    cores = [(b, hh) for b in range(B) for hh in range(2)]
    if "nc" not in _PROG:
        _PROG["nc"] = build_fused()
    shB = [_shared_B(P, 0), _shared_B(P, 1)]
    a0 = {}; a1 = {}
    maps = []
    for (b, hh) in cores:
        h_all = np.concatenate([P["ctx"][b], P["x"][b]], 0)
        if hh not in a0:
            a0[hh] = pack_A0(P, b, hh, h_all)
            a1[hh] = pack_A1(P, b, hh, h_all)
            a1[hh].pop("h_all")
        cvec = f32c(np.stack([P["c"][b], P["c_ctx"]], 0))
        d = {}
        for k, v in a0[hh].items():
            d["a0_" + k] = v
        d["a0_h_all"] = f32c(h_all); d["a0_cvec"] = cvec
        for k, v in a1[hh].items():
            d["a1_" + k] = v
        d["a1_cvec"] = cvec
        for l, pre in ((0, "b0_"), (1, "b1_")):
            for k, v in shB[l].items():
                d[pre + k] = v
            d[pre + "cvec"] = cvec
        rows0 = np.concatenate([np.arange(hh * 128, (hh + 1) * 128), 256 + np.arange(hh * 1024, (hh + 1) * 1024)])
        d["b0_h_in"] = f32c(h_all[rows0])
        g0 = rows0.reshape(9, 128).T
        d["b0_gidx"] = np.ascontiguousarray(np.stack([g0, 2304 + g0], -1).astype(np.int32))
        g1 = (hh * 1024 + np.arange(1024)).reshape(8, 128).T
        d["b1_gidx"] = np.ascontiguousarray(np.stack([g1, 2048 + g1], -1).astype(np.int32))
        m = np.zeros((128, 2), np.float32); m[:, hh] = 1.0
        d["msk"] = m
        maps.append(d)
    res = run_bass_kernel_spmd(_PROG["nc"], maps, core_ids=list(range(8)))
    out = np.zeros((B, 2048, 2048), np.float32)
    for i, (b, hh) in enumerate(cores):
        out[b, hh * 1024:(hh + 1) * 1024] = res.results[i]["h_out"]
    return out
```
